# Optimizing a Trainium2 kernel written in Bass

```python
import jax
import jax.numpy as jnp
from jax import lax
import numpy as np

D_MODEL = 1024
BATCH = 4
SEQ = 4096
DEPTH = 4

GRID_W = 64
CTX_LEN = 256
HEAD_DIM = 128
ROPE_THETA = 10000.0
EPS = 1e-6
NEG_INF = -1e30

RET_HEADS = 4
RET_DK = HEAD_DIM
RET_DV = HEAD_DIM
RET_WIDTH = RET_HEADS * RET_DV
RET_CHUNK = 128

ATT_Q_HEADS = 4
ATT_KV_HEADS = 2
ATT_GROUP = ATT_Q_HEADS // ATT_KV_HEADS
ATT_WIDTH = ATT_Q_HEADS * HEAD_DIM
ATT_KV_WIDTH = ATT_KV_HEADS * HEAD_DIM
WINDOW = 128
ATT_BLOCK = 128

EVEN_SPLITS = (RET_WIDTH, 2 * RET_WIDTH, 3 * RET_WIDTH, 4 * RET_WIDTH, 4 * RET_WIDTH + ATT_WIDTH, 4 * RET_WIDTH + ATT_WIDTH + ATT_KV_WIDTH)
EVEN_IN = 4 * RET_WIDTH + ATT_WIDTH + 2 * ATT_KV_WIDTH
EVEN_OUT = RET_WIDTH + ATT_WIDTH

HG_DK = 128
HG_HEADS = D_MODEL // HG_DK
HG_DV = D_MODEL // HG_HEADS
HG_FWIDTH = HG_HEADS * HG_DK
HG_VWIDTH = HG_HEADS * HG_DV
HG_CHUNK = 16
ODD_SPLITS = (HG_FWIDTH, 2 * HG_FWIDTH, 3 * HG_FWIDTH, 3 * HG_FWIDTH + HG_VWIDTH)
ODD_IN = 3 * HG_FWIDTH + 2 * HG_VWIDTH

MOE_GROUPS = 4
MOE_EXPERTS_PER_GROUP = 8
MOE_EXPERTS = MOE_GROUPS * MOE_EXPERTS_PER_GROUP
MOE_TOP_K = 2
MOE_FF = D_MODEL // 2
MOE_BLOCK = 128

kernel_name = 'hybrid_retention_swa_hgrn2_hmoe_dit'


def rms_norm(x, w):
    xf = x.astype(jnp.float32)
    y = xf * lax.rsqrt(jnp.mean(xf * xf, axis=-1, keepdims=True) + EPS)
    return (y * w.astype(jnp.float32)).astype(x.dtype)


def head_rms(o):
    return o * lax.rsqrt(jnp.mean(o * o, axis=-1, keepdims=True) + EPS)


def flip_seq(t):
    return jnp.flip(t, axis=1)


def axial_rope_tables(n_rows):
    row = jnp.repeat(jnp.arange(n_rows, dtype=jnp.float32), GRID_W)
    col = jnp.tile(jnp.arange(GRID_W, dtype=jnp.float32), n_rows)
    axis_dim = HEAD_DIM // 2
    inv_freq = ROPE_THETA ** (-jnp.arange(0, axis_dim, 2, dtype=jnp.float32) / axis_dim)
    ang_r = row[:, None] * inv_freq[None, :]
    ang_c = col[:, None] * inv_freq[None, :]
    return (jnp.cos(ang_r), jnp.sin(ang_r), jnp.cos(ang_c), jnp.sin(ang_c))


def _rotate(x, cos, sin):
    m = cos.shape[-1]
    x1, x2 = x[..., :m], x[..., m:]
    cs = cos[None, :, None, :]
    sn = sin[None, :, None, :]
    return jnp.concatenate([x1 * cs - x2 * sn, x2 * cs + x1 * sn], axis=-1)


def apply_axial_rope(x, tables):
    cos_r, sin_r, cos_c, sin_c = tables
    xf = x.astype(jnp.float32)
    half = HEAD_DIM // 2
    out = jnp.concatenate([_rotate(xf[..., :half], cos_r, sin_r), _rotate(xf[..., half:], cos_c, sin_c)], axis=-1)
    return out.astype(x.dtype)


def retention_scan(q, k, v, log_gamma, s0):
    B, T, H, dk = q.shape
    dv = v.shape[-1]
    C = RET_CHUNK
    N = T // C
    qc = q.astype(jnp.float32).reshape(B, N, C, H, dk)
    kc = k.astype(jnp.float32).reshape(B, N, C, H, dk)
    vc = v.astype(jnp.float32).reshape(B, N, C, H, dv)
    pos = jnp.arange(C, dtype=jnp.float32)
    rel = pos[:, None] - pos[None, :]
    decay = jnp.where(rel[None] >= 0, jnp.exp(jnp.maximum(rel, 0.0)[None] * log_gamma[:, None, None]), 0.0)
    scores = jnp.einsum('bnihd,bnjhd->bnhij', qc, kc) * decay
    intra = jnp.einsum('bnhij,bnjhe->bnihe', scores, vc)
    k_decay = jnp.exp((C - 1 - pos)[:, None] * log_gamma[None, :])
    u = jnp.einsum('bnjhd,jh,bnjhe->bnhde', kc, k_decay, vc)
    chunk_decay = jnp.exp(C * log_gamma)[None, :, None, None]

    def step(s, u_n):
        return chunk_decay * s + u_n, s

    s_fin, s_prev = lax.scan(step, s0.astype(jnp.float32), jnp.moveaxis(u, 1, 0))
    q_decay = jnp.exp((pos + 1.0)[:, None] * log_gamma[None, :])
    cross = jnp.einsum('bnihd,ih,nbhde->bnihe', qc, q_decay, s_prev)
    return (intra + cross).reshape(B, T, H, dv), s_fin


def bidirectional_retention(c_qkv, l_qkv, log_gamma):
    qc, kc, vc = c_qkv
    ql, kl, vl = l_qkv
    B, _, H, dk = qc.shape
    zero = jnp.zeros((B, H, dk, vc.shape[-1]), jnp.float32)
    o_cf, s_cf = retention_scan(qc, kc, vc, log_gamma[0], zero)
    o_cb, s_cb = retention_scan(flip_seq(qc), flip_seq(kc), flip_seq(vc), log_gamma[1], zero)
    o_lf, _ = retention_scan(ql, kl, vl, log_gamma[0], s_cf)
    o_lb, _ = retention_scan(flip_seq(ql), flip_seq(kl), flip_seq(vl), log_gamma[1], s_cb)
    return o_cf + flip_seq(o_cb), o_lf + flip_seq(o_lb)


def windowed_gqa_with_sink(c_qkv, l_qkv, sink, ctx_out):
    qc, kc, vc = c_qkv
    ql, kl, vl = l_qkv
    B, L = ql.shape[:2]
    Bs = ATT_BLOCK
    nb = L // Bs
    scale = HEAD_DIM ** -0.5
    sink = sink.astype(jnp.float32).reshape(ATT_KV_HEADS, ATT_GROUP)
    kc32 = kc.astype(jnp.float32)
    vc32 = vc.astype(jnp.float32)
    n_ctx = kc.shape[1]
    qg = ql.astype(jnp.float32).reshape(B, nb, Bs, ATT_KV_HEADS, ATT_GROUP, HEAD_DIM) * scale
    pad = ((0, 0), (Bs, Bs), (0, 0), (0, 0))
    kp = jnp.pad(kl.astype(jnp.float32), pad).reshape(B, nb + 2, Bs, ATT_KV_HEADS, HEAD_DIM)
    vp = jnp.pad(vl.astype(jnp.float32), pad).reshape(B, nb + 2, Bs, ATT_KV_HEADS, HEAD_DIM)
    kw = jnp.concatenate([kp[:, :-2], kp[:, 1:-1], kp[:, 2:]], axis=2)
    vw = jnp.concatenate([vp[:, :-2], vp[:, 1:-1], vp[:, 2:]], axis=2)
    s_loc = jnp.einsum('bnqhgd,bnkhd->bnhgqk', qg, kw)
    q_pos = jnp.arange(L).reshape(nb, Bs)
    k_pos = (jnp.arange(nb)[:, None] - 1) * Bs + jnp.arange(3 * Bs)[None, :]
    valid = (k_pos[:, None, :] >= 0) & (k_pos[:, None, :] < L) & (jnp.abs(q_pos[:, :, None] - k_pos[:, None, :]) <= WINDOW)
    s_loc = jnp.where(valid[None, :, None, None], s_loc, NEG_INF)
    s_ctx = jnp.einsum('bnqhgd,bkhd->bnhgqk', qg, kc32)
    s_sink = jnp.broadcast_to(sink[None, None, :, :, None, None], s_loc.shape[:-1] + (1,))
    p = jax.nn.softmax(jnp.concatenate([s_loc, s_ctx, s_sink], axis=-1), axis=-1)
    n_loc = 3 * Bs
    o_l = (jnp.einsum('bnhgqk,bnkhd->bnqhgd', p[..., :n_loc], vw)
           + jnp.einsum('bnhgqk,bkhd->bnqhgd', p[..., n_loc:n_loc + n_ctx], vc32))
    o_l = o_l.reshape(B, L, ATT_WIDTH)
    if not ctx_out:
        return None, o_l
    qgc = qc.astype(jnp.float32).reshape(B, n_ctx, ATT_KV_HEADS, ATT_GROUP, HEAD_DIM) * scale
    s_cc = jnp.einsum('bqhgd,bkhd->bhgqk', qgc, kc32)
    s_sink_c = jnp.broadcast_to(sink[None, :, :, None, None], s_cc.shape[:-1] + (1,))
    pc = jax.nn.softmax(jnp.concatenate([s_cc, s_sink_c], axis=-1), axis=-1)
    o_c = jnp.einsum('bhgqk,bkhd->bqhgd', pc[..., :n_ctx], vc32).reshape(B, n_ctx, ATT_WIDTH)
    return o_c, o_l


def even_mixer(h_c, h_l, w_in, w_out, ret_decay_raw, att_sink, rope, ctx_out):
    B = h_l.shape[0]

    def project(h, rotate):
        T = h.shape[1]
        rq, rk, rv, rg, aq, ak, av = jnp.split(h @ w_in, EVEN_SPLITS, axis=-1)
        rq = rq.reshape(B, T, RET_HEADS, RET_DK)
        rk = rk.reshape(B, T, RET_HEADS, RET_DK) * (RET_DK ** -0.5)
        rv = rv.reshape(B, T, RET_HEADS, RET_DV)
        aq = aq.reshape(B, T, ATT_Q_HEADS, HEAD_DIM)
        ak = ak.reshape(B, T, ATT_KV_HEADS, HEAD_DIM)
        av = av.reshape(B, T, ATT_KV_HEADS, HEAD_DIM)
        if rotate:
            rq, rk, aq, ak = (apply_axial_rope(t, rope) for t in (rq, rk, aq, ak))
        return (rq, rk, rv), rg, (aq, ak, av)

    c_ret, c_gate, c_att = project(h_c, False)
    l_ret, l_gate, l_att = project(h_l, True)
    log_gamma = -jnp.exp(ret_decay_raw.astype(jnp.float32))
    o_ret_c, o_ret_l = bidirectional_retention(c_ret, l_ret, log_gamma)
    o_att_c, o_att_l = windowed_gqa_with_sink(c_att, l_att, att_sink, ctx_out)

    def merge(o_ret, gate, o_att):
        T = o_ret.shape[1]
        ret = jax.nn.silu(gate.astype(jnp.float32)) * head_rms(o_ret).reshape(B, T, RET_WIDTH)
        y = jnp.concatenate([ret, o_att], axis=-1).astype(h_l.dtype) @ w_out
        return y.astype(h_l.dtype)

    y_l = merge(o_ret_l, l_gate, o_att_l)
    y_c = merge(o_ret_c, c_gate, o_att_c) if ctx_out else None
    return y_c, y_l


def hgrn2_scan(q, k, v, log_f, s0):
    B, T, H, dk = q.shape
    dv = v.shape[-1]
    C = HG_CHUNK
    N = T // C

    def chunks(t):
        return t.reshape(B, N, C, H, t.shape[-1]).transpose(1, 0, 3, 2, 4)

    lower = jnp.tril(jnp.ones((C, C), dtype=bool))[:, :, None]

    def step(s, inp):
        qn, kn, vn, fn = inp
        a = jnp.cumsum(fn, axis=2)
        o_inter = jnp.einsum('bhcd,bhde->bhce', qn * jnp.exp(a), s)
        diff = a[:, :, :, None, :] - a[:, :, None, :, :]
        dec = jnp.exp(jnp.where(lower, diff, NEG_INF))
        scores = jnp.einsum('bhid,bhjd,bhijd->bhij', qn, kn, dec)
        o_intra = jnp.einsum('bhij,bhje->bhie', scores, vn)
        a_last = a[:, :, -1:, :]
        s_new = jnp.exp(a_last[:, :, 0, :])[..., None] * s + jnp.einsum('bhjd,bhje->bhde', kn * jnp.exp(a_last - a), vn)
        return s_new, o_inter + o_intra

    s_fin, o = lax.scan(step, s0.astype(jnp.float32), (chunks(q), chunks(k), chunks(v), chunks(log_f)))
    return o.transpose(1, 0, 3, 2, 4).reshape(B, T, H, dv), s_fin


def odd_mixer(h_c, h_l, w_in, w_out, lower_bound, norm_w, ctx_out):
    B = h_l.shape[0]
    lb = lower_bound.astype(jnp.float32).reshape(HG_HEADS, HG_DK)

    def project(h):
        T = h.shape[1]
        q, zf, zb, i, g = jnp.split(h @ w_in, ODD_SPLITS, axis=-1)
        heads = lambda t, d: t.astype(jnp.float32).reshape(B, T, HG_HEADS, d)
        f_fwd = lb + (1.0 - lb) * jax.nn.sigmoid(heads(zf, HG_DK))
        f_bwd = lb + (1.0 - lb) * jax.nn.sigmoid(heads(zb, HG_DK))
        return heads(q, HG_DK), f_fwd, f_bwd, heads(i, HG_DV), g

    def scan_dir(q, f, i, s0, reverse):
        if reverse:
            q, f, i = flip_seq(q), flip_seq(f), flip_seq(i)
        o, s = hgrn2_scan(q, 1.0 - f, i, jnp.log(f), s0)
        return (flip_seq(o) if reverse else o), s

    qc, ffc, fbc, ic, gc = project(h_c)
    ql, ffl, fbl, il, gl = project(h_l)
    zero = jnp.zeros((B, HG_HEADS, HG_DK, HG_DV), jnp.float32)
    o_cf, s_cf = scan_dir(qc, ffc, ic, zero, False)
    o_cb, s_cb = scan_dir(qc, fbc, ic, zero, True)
    o_lf, _ = scan_dir(ql, ffl, il, s_cf, False)
    o_lb, _ = scan_dir(ql, fbl, il, s_cb, True)

    def readout(o, g):
        T = o.shape[1]
        y = rms_norm(o, norm_w) * jax.nn.silu(g.astype(jnp.float32).reshape(B, T, HG_HEADS, HG_DV))
        return (y.reshape(B, T, HG_VWIDTH).astype(h_l.dtype) @ w_out).astype(h_l.dtype)

    y_l = readout(o_lf + o_lb, gl)
    y_c = readout(o_cf + o_cb, gc) if ctx_out else None
    return y_c, y_l


def hierarchical_moe(h, wg, bg, we, be, w1, w3, w2):
    N, D = h.shape
    g_prob = jax.nn.softmax((h @ wg).astype(jnp.float32) + bg.astype(jnp.float32), axis=-1)
    g_p, g_idx = lax.top_k(g_prob, 1)
    e_all = jnp.einsum('nd,gde->nge', h, we).astype(jnp.float32) + be.astype(jnp.float32)
    sel = jnp.broadcast_to(g_idx[:, :, None], (N, 1, MOE_EXPERTS_PER_GROUP))
    e_logits = jnp.take_along_axis(e_all, sel, axis=1)[:, 0]
    e_top, e_idx = lax.top_k(e_logits, MOE_TOP_K)
    gate = g_p * jax.nn.softmax(e_top, axis=-1)
    expert = g_idx * MOE_EXPERTS_PER_GROUP + e_idx
    NK = N * MOE_TOP_K
    flat_e = expert.reshape(-1)
    flat_tok = jnp.arange(NK) // MOE_TOP_K
    flat_w = gate.reshape(-1)
    order = jnp.argsort(flat_e)
    s_e = flat_e[order]
    s_tok = flat_tok[order]
    s_w = flat_w[order]
    counts = jnp.bincount(flat_e, length=MOE_EXPERTS)
    padded = (counts + MOE_BLOCK - 1) // MOE_BLOCK * MOE_BLOCK
    pad_end = jnp.cumsum(padded)
    pad_start = pad_end - padded
    seg_start = jnp.cumsum(counts) - counts
    dest = pad_start[s_e] + jnp.arange(NK) - seg_start[s_e]
    n_blocks = -(-NK // MOE_BLOCK) + MOE_EXPERTS
    cap = n_blocks * MOE_BLOCK
    slot_tok = jnp.full((cap,), N, dtype=jnp.int32).at[dest].set(s_tok.astype(jnp.int32))
    block_expert = jnp.minimum(jnp.searchsorted(pad_end, jnp.arange(n_blocks) * MOE_BLOCK, side='right'), MOE_EXPERTS - 1)
    h_pad = jnp.concatenate([h, jnp.zeros((1, D), h.dtype)], axis=0)
    xb = h_pad[slot_tok].reshape(n_blocks, MOE_BLOCK, D)

    def expert_block(args):
        xblk, e = args
        return (jax.nn.silu(xblk @ w1[e]) * (xblk @ w3[e])) @ w2[e]

    yb = lax.map(expert_block, (xb, block_expert))
    y_sorted = yb.reshape(cap, D)[dest]
    return jnp.zeros((N, D), h.dtype).at[s_tok].add((y_sorted * s_w[:, None]).astype(h.dtype))


def setup_inputs(seed: int = 0) -> dict:
    key = jax.random.key(seed)
    ks = jax.random.split(key, 24)
    f32 = jnp.float32
    n_even = (DEPTH + 1) // 2
    n_odd = DEPTH // 2

    def nrm(k, shape, scale):
        return jax.random.normal(k, shape, f32) * scale

    base_decay = jnp.log(-jnp.log1p(-(2.0 ** (-5.0 - jnp.arange(RET_HEADS, dtype=f32)))))
    return {
        'x': nrm(ks[0], (BATCH, SEQ, D_MODEL), 1.0),
        'c': nrm(ks[1], (BATCH, D_MODEL), 1.0),
        'ctx': nrm(ks[2], (BATCH, CTX_LEN, D_MODEL), 1.0),
        'c_ctx': nrm(ks[3], (D_MODEL,), 1.0),
        'ada_w': nrm(ks[4], (DEPTH, D_MODEL, 6 * D_MODEL), 0.5 * D_MODEL ** -0.5),
        'ada_b': nrm(ks[5], (DEPTH, 6 * D_MODEL), 0.02),
        'norm_w': 1.0 + nrm(ks[6], (DEPTH, 2, D_MODEL), 0.02),
        'final_norm_w': 1.0 + nrm(ks[7], (D_MODEL,), 0.02),
        'ev_w_in': nrm(ks[8], (n_even, D_MODEL, EVEN_IN), D_MODEL ** -0.5),
        'ev_w_out': nrm(ks[9], (n_even, EVEN_OUT, D_MODEL), EVEN_OUT ** -0.5),
        'ret_decay_raw': base_decay[None, None, :] + nrm(ks[10], (n_even, 2, RET_HEADS), 0.05),
        'att_sink': nrm(ks[11], (n_even, ATT_Q_HEADS), 0.5),
        'od_w_in': nrm(ks[12], (n_odd, D_MODEL, ODD_IN), D_MODEL ** -0.5),
        'od_w_out': nrm(ks[13], (n_odd, HG_VWIDTH, D_MODEL), HG_VWIDTH ** -0.5),
        'hg_lb_logits': nrm(ks[14], (DEPTH, HG_FWIDTH), 0.1),
        'hg_norm_w': 1.0 + nrm(ks[15], (n_odd, HG_DV), 0.02),
        'moe_wg': nrm(ks[16], (DEPTH, D_MODEL, MOE_GROUPS), D_MODEL ** -0.5),
        'moe_bg': nrm(ks[17], (DEPTH, MOE_GROUPS), 0.01),
        'moe_we': nrm(ks[18], (DEPTH, MOE_GROUPS, D_MODEL, MOE_EXPERTS_PER_GROUP), D_MODEL ** -0.5),
        'moe_be': nrm(ks[19], (DEPTH, MOE_GROUPS, MOE_EXPERTS_PER_GROUP), 0.01),
        'moe_w1': nrm(ks[20], (DEPTH, MOE_EXPERTS, D_MODEL, MOE_FF), D_MODEL ** -0.5),
        'moe_w3': nrm(ks[21], (DEPTH, MOE_EXPERTS, D_MODEL, MOE_FF), D_MODEL ** -0.5),
        'moe_w2': nrm(ks[22], (DEPTH, MOE_EXPERTS, MOE_FF, D_MODEL), MOE_FF ** -0.5),
    }


def reference(x, c, ctx, c_ctx, ada_w, ada_b, norm_w, final_norm_w, ev_w_in, ev_w_out, ret_decay_raw, att_sink,
              od_w_in, od_w_out, hg_lb_logits, hg_norm_w, moe_wg, moe_bg, moe_we, moe_be, moe_w1, moe_w3, moe_w2):
    B, L, D = x.shape
    n_ctx = ctx.shape[1]
    n_rows = L // GRID_W
    rope = axial_rope_tables(n_rows)
    lb_cum = jnp.cumsum(jax.nn.softmax(hg_lb_logits.astype(jnp.float32), axis=0), axis=0)
    lower_bounds = lb_cum - lb_cum[:1]
    for l in range(DEPTH):
        last = l == DEPTH - 1
        mod_l = jnp.split(jax.nn.silu(c) @ ada_w[l] + ada_b[l], 6, axis=-1)
        mod_c = jnp.split(jax.nn.silu(c_ctx) @ ada_w[l] + ada_b[l], 6, axis=-1)
        h_l = rms_norm(x, norm_w[l, 0]) * (1.0 + mod_l[1][:, None]) + mod_l[0][:, None]
        h_c = rms_norm(ctx, norm_w[l, 0]) * (1.0 + mod_c[1]) + mod_c[0]
        p = l // 2
        if l % 2 == 0:
            y_c, y_l = even_mixer(h_c, h_l, ev_w_in[p], ev_w_out[p], ret_decay_raw[p], att_sink[p], rope, not last)
        else:
            y_c, y_l = odd_mixer(h_c, h_l, od_w_in[p], od_w_out[p], lower_bounds[l], hg_norm_w[p], not last)
        x = x + mod_l[2][:, None] * y_l
        h2_l = rms_norm(x, norm_w[l, 1]) * (1.0 + mod_l[4][:, None]) + mod_l[3][:, None]
        moe_params = (moe_wg[l], moe_bg[l], moe_we[l], moe_be[l], moe_w1[l], moe_w3[l], moe_w2[l])
        if last:
            y2 = hierarchical_moe(h2_l.reshape(B * L, D), *moe_params)
            x = x + mod_l[5][:, None] * y2.reshape(B, L, D)
        else:
            ctx = ctx + mod_c[2] * y_c
            h2_c = rms_norm(ctx, norm_w[l, 1]) * (1.0 + mod_c[4]) + mod_c[3]
            tokens = jnp.concatenate([h2_c.reshape(B * n_ctx, D), h2_l.reshape(B * L, D)], axis=0)
            y2 = hierarchical_moe(tokens, *moe_params)
            ctx = ctx + mod_c[5] * y2[:B * n_ctx].reshape(B, n_ctx, D)
            x = x + mod_l[5][:, None] * y2[B * n_ctx:].reshape(B, L, D)
    return rms_norm(x, final_norm_w)
```

```python
import contextlib
import numpy as np
import ml_dtypes
import concourse.bass as bass
import concourse.mybir as mybir
from concourse.bass_utils import run_bass_kernel_spmd

F32 = mybir.dt.float32
BF16 = mybir.dt.bfloat16
ALU = mybir.AluOpType
AF = mybir.ActivationFunctionType
AX = mybir.AxisListType
ENGINES = ("tensor", "vector", "scalar", "gpsimd", "sync")

D = 1024
L = 4096
NCTX = 256
B = 4
DEPTH = 4
EPS = 1e-6
NEG = -1e30


class Res:
    __slots__ = ("name", "writers", "readers")

    def __init__(self, name=""):
        self.name = name
        self.writers = []
        self.readers = []


class Op:
    __slots__ = ("eng", "fn", "deps", "sig", "sigval", "is_dma", "dsem", "dval", "cc", "ccval")

    def __init__(self, eng, fn, is_dma=False):
        self.eng = eng
        self.fn = fn
        self.deps = []
        self.sig = False
        self.sigval = 0
        self.is_dma = is_dma
        self.dsem = None
        self.dval = 0
        self.cc = False
        self.ccval = 0


class Sched:
    def __init__(self, nc, n_dma_sems=32):
        self.nc = nc
        self.ops = {e: [] for e in ENGINES}
        self.n_dma_sems = n_dma_sems
        self.dma_rr = 0
        self.dma_last = [None] * n_dma_sems
        self.dma_cnt = [0] * n_dma_sems
        self.nops = 0
        self.cc_count = 0

    def _add_dep(self, op, dep):
        if dep is None or dep is op:
            return
        if dep.eng == op.eng and not dep.is_dma and not op.is_dma and op.eng == "tensor":
            return
        if dep not in op.deps:
            op.deps.append(dep)
            dep.sig = True

    def op(self, eng, fn, reads=(), writes=(), updates=(), is_dma=False):
        o = Op(eng, fn, is_dma)
        for r in reads:
            for w in r.writers:
                self._add_dep(o, w)
        for r in list(writes) + list(updates):
            for w in r.writers:
                self._add_dep(o, w)
            for rd in r.readers:
                if rd.eng == eng and not rd.is_dma and not is_dma:
                    continue
                self._add_dep(o, rd)
        if is_dma:
            k = self.dma_rr
            self.dma_rr = (self.dma_rr + 1) % self.n_dma_sems
            self._add_dep(o, self.dma_last[k])
            self.dma_cnt[k] += 16
            o.dsem = k
            o.dval = self.dma_cnt[k]
            self.dma_last[k] = o
        for r in reads:
            r.readers = [x for x in r.readers if not (x.eng == eng and not x.is_dma and not is_dma)] + [o]
        for r in writes:
            r.writers = [o]
            r.readers = []
        for r in updates:
            r.writers = [x for x in r.writers if not (x.eng == eng and not x.is_dma and not is_dma)] + [o]
            r.readers = []
        self.ops[eng].append(o)
        self.nops += 1
        return o

    def barrier(self):
        lasts = []
        for e in ENGINES:
            for o in reversed(self.ops[e]):
                if not o.is_dma and not o.cc:
                    lasts.append(o)
                    break
        lasts += [o for o in self.dma_last if o is not None]
        for e in ENGINES:
            o = Op(e, lambda eng: eng.nop())
            for d in lasts:
                if d.eng == e and not d.is_dma:
                    continue
                if d not in o.deps:
                    o.deps.append(d)
                    d.sig = True
            self.ops[e].append(o)

    def collective(self, fn):
        self.barrier()
        fns = fn if isinstance(fn, (list, tuple)) else [fn]
        for f in fns:
            o = Op("gpsimd", f)
            o.cc = True
            self.cc_count += 1
            o.ccval = self.cc_count
            self.ops["gpsimd"].append(o)
        for e in ENGINES:
            w = Op(e, lambda eng: eng.nop())
            w.deps.append(o)
            self.ops[e].append(w)

    def cc_async(self, fn, reads=(), writes=()):
        o = self.op("gpsimd", fn, reads=reads, writes=writes)
        o.cc = True
        self.cc_count += 1
        o.ccval = self.cc_count
        return o

    def dma(self, out, in_, reads=(), writes=(), updates=(), eng="sync", **kw):
        return self.op(eng, lambda e: e.dma_start(out=out, in_=in_, **kw), reads, writes, updates, is_dma=True)

    def emit(self):
        nc = self.nc
        cnt = {e: 0 for e in ENGINES}
        for e in ENGINES:
            for o in self.ops[e]:
                if o.sig and not o.is_dma and not o.cc:
                    cnt[e] += 1
                    o.sigval = cnt[e]
        with contextlib.ExitStack() as st:
            esem = {e: st.enter_context(nc.semaphore("s_" + e)) for e in ENGINES}
            dsem = [st.enter_context(nc.semaphore("d_%d" % i)) for i in range(self.n_dma_sems)]
            ccsem = st.enter_context(nc.semaphore("s_cc"))
            block = st.enter_context(nc.Block())

            def make(ename):
                def body(eng):
                    waited = {}
                    for o in self.ops[ename]:
                        for d in o.deps:
                            if d.cc:
                                key, val, sem = ("c", 0), d.ccval, ccsem
                            elif d.is_dma:
                                key, val, sem = ("d", d.dsem), d.dval, dsem[d.dsem]
                            else:
                                key, val, sem = ("e", d.eng), d.sigval, esem[d.eng]
                            if waited.get(key, 0) < val:
                                eng.wait_ge(sem, val)
                                waited[key] = val
                        ins = o.fn(eng)
                        if o.cc:
                            ins.then_inc(ccsem, 1)
                        elif o.is_dma:
                            ins.then_inc(dsem[o.dsem], 16)
                        elif o.sig:
                            ins.then_inc(esem[ename], 1)
                    if ename == "sync":
                        for k in range(self.n_dma_sems):
                            if self.dma_cnt[k] > 0:
                                eng.wait_ge(dsem[k], self.dma_cnt[k])
                        for e2 in ENGINES:
                            if e2 != "sync" and cnt[e2] > 0:
                                eng.wait_ge(esem[e2], cnt[e2])
                return body

            for e in ENGINES:
                getattr(block, e)(make(e))


class Ctx:
    def __init__(self, name=""):
        self.nc = bass.Bass("TRN2", target_bir_lowering=False)
        self.S = Sched(self.nc)
        self.stack = contextlib.ExitStack()
        self.n = 0
        self.prefix = ""
        self.r_xf = [Res("xf%d" % k) for k in range(5)]
        self.r_yp = [Res("yp%d" % k) for k in range(5)]

    @contextlib.contextmanager
    def scope(self):
        saved = self.stack
        st = contextlib.ExitStack()
        self.stack = st
        with st:
            yield
        self.stack = saved
        self.S.barrier()

    def dram(self, name, shape, dt=F32):
        return self.nc.dram_tensor(name, list(shape), dt).ap()

    def sb(self, shape, dt, name=None):
        self.n += 1
        nm = self.prefix + (name or "sb") + "_%d" % self.n
        t = self.stack.enter_context(self.nc.sbuf_tensor(nm, list(shape), dt))
        return t, Res(nm)

    def ps(self, shape, dt, name=None):
        self.n += 1
        full = 512 if dt == F32 else 1024
        nm = self.prefix + (name or "ps") + "_%d" % self.n
        t = self.stack.enter_context(self.nc.psum_tensor(nm, [128, full], dt))
        n = int(np.prod(shape[1:]))
        v = t[0:shape[0], 0:n]
        if len(shape) == 3:
            v = v.rearrange("p (a b) -> p a b", a=shape[1])
        return v, Res(name or ("ps%d" % self.n))

    def din(self, name, shape, dt=F32):
        return self.nc.dram_tensor(name, list(shape), dt, kind="ExternalInput").ap()

    def dout(self, name, shape, dt=F32):
        return self.nc.dram_tensor(name, list(shape), dt, kind="ExternalOutput").ap()


def make_identity(C, dt=BF16):
    S = C.S
    idf, r_idf = C.sb([128, 128], F32)
    S.op("gpsimd", lambda e: e.memset(idf[:], 0.0), writes=[r_idf])
    S.op("gpsimd", lambda e: e.affine_select(out=idf[:], in_=idf[:], pattern=[[-1, 128]], compare_op=ALU.not_equal,
                                             fill=1.0, base=0, channel_multiplier=1), updates=[r_idf])
    if dt == F32:
        return idf, r_idf
    idb, r_idb = C.sb([128, 128], dt)
    S.op("vector", lambda e: e.tensor_copy(out=idb[:], in_=idf[:]), reads=[r_idf], writes=[r_idb])
    return idb, r_idb, idf, r_idf


def emit_moe(C, A, tiles, NEXP=16):
    nc, S = C.nc, C.S
    NT = len(tiles)
    NTOK = NT * 128
    skip_outproj = False
    xs, mgall, wo, modv, nw, wr, br, w1, w3, w2, ypart = (A[k] for k in "xs mgall wo modv nw wr br w1 w3 w2 ypart".split())
    with C.scope():
        idb, r_idb, idf, r_idf = make_identity(C)
        acc, _ = C.sb([128, NT, D], F32, "acc")
        r_acc = [[Res("acc%d_%d" % (t, h)) for h in range(2)] for t in range(NT)]
        h2T, _ = C.sb([128, 8, NTOK], BF16, "h2T")
        r_h2T = [Res("h2T%d" % t) for t in range(NT)]
        gates, _ = C.sb([128, NT, 32], F32, "gates")
        r_gates = [Res("g%d" % t) for t in range(NT)]
        gmlp = [C.sb([128, D], F32, "gmlp%d" % i) for i in range(2)]
        for i in range(2):
            S.dma(gmlp[i][0][:], modv[i, 5:6, :].partition_broadcast(128), writes=[gmlp[i][1]])
        wrt, r_wrt = C.sb([128, 8, 36], F32, "wrt")
        S.dma(wrt[:], wr.rearrange("(c p) j -> p c j", p=128), writes=[r_wrt])
        brt, r_brt = C.sb([128, 36], F32, "brt")
        S.dma(brt[:], br.partition_broadcast(128), writes=[r_brt])

        stA = contextlib.ExitStack()
        C_stack_saved = C.stack
        C.stack = stA
        with stA:
            wob, r_wob = C.sb([128, 8, D], BF16, "wob")
            if not skip_outproj:
                load_weight_bf16(C, wob, r_wob, wo, D)
            gmsa, r_gmsa = C.sb([128, D], F32, "gmsa")
            w2e, r_w2e = C.sb([128, D], F32, "w2e")
            sh2, r_sh2 = C.sb([128, D], F32, "sh2")
            nwt, r_nwt = C.sb([128, D], F32, "nwt")
            S.dma(nwt[:], nw.partition_broadcast(128), writes=[r_nwt])
            xt = [C.sb([128, D], F32, "xt%d" % i) for i in range(2)]
            mgt = [C.sb([128, D], BF16, "mgt%d" % i) for i in range(2)]
            mgT = [C.sb([128, 8, 128], BF16, "mgT%d" % i) for i in range(2)]
            junk, r_junk = C.sb([128, D], F32, "junk")
            tmpA_l = [C.sb([128, D], F32, "tmpA%d" % i) for i in range(2)]
            h2f_l = [C.sb([128, D], F32, "h2f%d" % i) for i in range(2)]
            h2b_l = [C.sb([128, D], BF16, "h2b%d" % i) for i in range(2)]
            h2Tf_l = [C.sb([128, 8, 128], F32, "h2Tf%d" % i) for i in range(2)]
            sm_l = [{k: C.sb([128, n], F32, "sm%d_" % i + k) for k, n in
                     dict(ss=1, rstd=1, lg=36, m4=1, ng=1, ex4=4, se=1, gp=1, oh=4, pen=4, msk=32, top8=8, sel=32, nt1=1, d21=1,
                          e21=1, coef=1, wf=32).items()} for i in range(2)]
            pT = [C.ps([128, 8, 128], BF16, "pT%d" % i) for i in range(2)]
            pTf = [C.ps([128, 4, 128], F32, "pTf%d" % i) for i in range(2)]
            pY = [C.ps([128, 512], F32, "pY%d" % i) for i in range(2)]
            pL, r_pL = C.ps([128, 36], F32, "pL")
            prev = [None]

            def do_tile(t):
                gt = tiles[t]
                tmpA, r_tmpA = tmpA_l[t % 2]
                h2f, r_h2f = h2f_l[t % 2]
                h2b, r_h2b = h2b_l[t % 2]
                h2Tf, r_h2Tf = h2Tf_l[t % 2]
                sm = sm_l[t % 2]
                si = 0 if gt < 2 else 1
                if si != prev[0]:
                    prev[0] = si
                    S.dma(gmsa[:], modv[si, 2:3, :].partition_broadcast(128), writes=[r_gmsa])
                    S.dma(sh2[:], modv[si, 3:4, :].partition_broadcast(128), writes=[r_sh2])
                    S.dma(w2e[:], modv[si, 4:5, :].partition_broadcast(128), writes=[r_w2e])
                    S.op("vector", lambda e: e.scalar_tensor_tensor(out=w2e[:], in0=w2e[:], scalar=1.0, in1=nwt[:], op0=ALU.add, op1=ALU.mult),
                         reads=[r_w2e, r_nwt], writes=[r_w2e])
                x_t, r_x = xt[t % 2]
                S.dma(x_t[:], xs[gt * 128:(gt + 1) * 128, :], reads=[C.r_xf[gt // 8]], writes=[r_x])
                a_t = acc[:, t, :]
                ra = r_acc[t]
                if not skip_outproj:
                    m_t, r_m = mgt[t % 2]
                    mT, r_mT = mgT[t % 2]
                    p_T, r_pT = pT[t % 2]
                    st_k = (gt // 16) * 2048
                    R_k = min(2048, 4352 - st_k)
                    r0 = 2 * st_k + gt * 128 - st_k
                    S.dma(m_t[:, 0:512], mgall[r0:r0 + 128, :], writes=[r_m], eng="gpsimd")
                    S.dma(m_t[:, 512:1024], mgall[r0 + R_k:r0 + R_k + 128, :], updates=[r_m], eng="gpsimd")
                    for c in range(8):
                        S.op("tensor", lambda e, c=c, p_T=p_T, m_t=m_t: e.transpose(out=p_T[:, c, :], in_=m_t[:, c * 128:(c + 1) * 128], identity=idb[:]),
                             reads=[r_m, r_idb], updates=[r_pT] if c else (), writes=() if c else [r_pT])
                    S.op("scalar", lambda e, mT=mT, p_T=p_T: e.copy(out=mT[:], in_=p_T[:]), reads=[r_pT], writes=[r_mT])
                    for hf in range(2):
                        p_Y, r_pY = pY[hf]
                        for c in range(8):
                            S.op("tensor", lambda e, c=c, hf=hf, p_Y=p_Y, mT=mT: e.matmul(out=p_Y[:], lhsT=mT[:, c, :], rhs=wob[:, c, hf * 512:(hf + 1) * 512],
                                                                                         start=(c == 0), stop=(c == 7)),
                                 reads=[r_mT, r_wob], updates=[r_pY] if c else (), writes=() if c else [r_pY])
                        sl = slice(hf * 512, (hf + 1) * 512)
                        S.op("vector", lambda e, p_Y=p_Y, sl=sl: e.tensor_tensor(out=tmpA[:, sl], in0=p_Y[:], in1=gmsa[:, sl], op=ALU.mult),
                             reads=[r_pY, r_gmsa], updates=[r_tmpA])
                        S.op("gpsimd", lambda e, sl=sl, a_t=a_t, x_t=x_t: e.tensor_tensor(out=a_t[:, sl], in0=tmpA[:, sl], in1=x_t[:, sl], op=ALU.add),
                             reads=[r_tmpA, r_x], writes=[ra[hf]])
                else:
                    for hf in range(2):
                        sl = slice(hf * 512, (hf + 1) * 512)
                        S.op("gpsimd", lambda e, sl=sl, a_t=a_t, x_t=x_t: e.tensor_copy(out=a_t[:, sl], in_=x_t[:, sl]), reads=[r_x], writes=[ra[hf]])
                ss, r_ss = sm["ss"]
                rstd, r_rstd = sm["rstd"]
                T_se, R_se = sm["se"]
                S.op("gpsimd", lambda e: e.memset(ss[:], 0.0), writes=[r_ss])
                S.op("gpsimd", lambda e: e.memset(T_se[:], 0.0), writes=[R_se])
                S.op("scalar", lambda e, a_t=a_t: e.activation(out=junk[:], in_=a_t, func=AF.Square, accum_out=ss[:]),
                     reads=ra, writes=[r_junk], updates=[r_ss])
                S.op("vector", lambda e: e.tensor_scalar(out=rstd[:], in0=ss[:], scalar1=1.0 / D, scalar2=EPS, op0=ALU.mult, op1=ALU.add),
                     reads=[r_ss], writes=[r_rstd])
                S.op("scalar", lambda e: e.activation(out=rstd[:], in_=rstd[:], func=AF.Sqrt), reads=[r_rstd], writes=[r_rstd])
                S.op("vector", lambda e: e.reciprocal(out=rstd[:], in_=rstd[:]), reads=[r_rstd], writes=[r_rstd])
                S.op("vector", lambda e, a_t=a_t: e.scalar_tensor_tensor(out=h2f[:], in0=a_t, scalar=rstd[:, 0:1], in1=w2e[:], op0=ALU.mult, op1=ALU.mult),
                     reads=ra + [r_rstd, r_w2e], writes=[r_h2f])
                S.op("gpsimd", lambda e: e.tensor_tensor(out=h2f[:], in0=h2f[:], in1=sh2[:], op=ALU.add), reads=[r_h2f, r_sh2], writes=[r_h2f])
                for hf in range(2):
                    sl = slice(hf * 512, (hf + 1) * 512)
                    S.op("scalar", lambda e, a_t=a_t, sl=sl: e.mul(out=a_t[:, sl], in_=a_t[:, sl], mul=0.5), reads=[], writes=[ra[hf]])
                S.op("vector", lambda e: e.tensor_copy(out=h2b[:], in_=h2f[:]), reads=[r_h2f], writes=[r_h2b])
                p_T, r_pT = pT[(t + 1) % 2]
                for c in range(8):
                    S.op("tensor", lambda e, c=c, p_T=p_T: e.transpose(out=p_T[:, c, :], in_=h2b[:, c * 128:(c + 1) * 128], identity=idb[:]),
                         reads=[r_h2b, r_idb], updates=[r_pT] if c else (), writes=() if c else [r_pT])
                S.op("scalar", lambda e, p_T=p_T, t=t: e.copy(out=h2T[:, :, t * 128:(t + 1) * 128], in_=p_T[:]), reads=[r_pT], writes=[r_h2T[t]])
                for g4 in range(2):
                    p_f, r_pf = pTf[g4]
                    for c in range(4):
                        cc = g4 * 4 + c
                        S.op("tensor", lambda e, c=c, cc=cc, p_f=p_f: e.transpose(out=p_f[:, c, :], in_=h2f[:, cc * 128:(cc + 1) * 128], identity=idf[:]),
                             reads=[r_h2f, r_idf], updates=[r_pf] if c else (), writes=() if c else [r_pf])
                    S.op("vector" if g4 else "scalar", (lambda e, p_f=p_f, g4=g4: e.tensor_copy(out=h2Tf[:, g4 * 4:(g4 + 1) * 4, :], in_=p_f[:])) if g4 else
                         (lambda e, p_f=p_f, g4=g4: e.copy(out=h2Tf[:, g4 * 4:(g4 + 1) * 4, :], in_=p_f[:])),
                         reads=[r_pf], updates=[r_h2Tf])
                for c in range(8):
                    S.op("tensor", lambda e, c=c: e.matmul(out=pL[:], lhsT=h2Tf[:, c, :], rhs=wrt[:, c, :], start=(c == 0), stop=(c == 7)),
                         reads=[r_h2Tf, r_wrt], updates=[r_pL] if c else (), writes=() if c else [r_pL])
                def T(k):
                    return sm[k][0]

                def Rr(k):
                    return sm[k][1]
                V = lambda fn, reads, writes: S.op("vector", fn, reads=reads, writes=writes)
                V(lambda e: e.tensor_tensor(out=T("lg")[:], in0=pL[:], in1=brt[:], op=ALU.add), [r_pL, r_brt], [Rr("lg")])
                V(lambda e: e.reduce_max(out=T("m4")[:], in_=T("lg")[:, 0:4], axis=AX.X), [Rr("lg")], [Rr("m4")])
                V(lambda e: e.tensor_scalar(out=T("ng")[:], in0=T("m4")[:], scalar1=-1.0, scalar2=None, op0=ALU.mult), [Rr("m4")], [Rr("ng")])
                S.op("scalar", lambda e: e.activation(out=T("ex4")[:], in_=T("lg")[:, 0:4], func=AF.Exp, bias=T("ng")[:, 0:1], scale=1.0, accum_out=T("se")[:]),
                     reads=[Rr("lg"), Rr("ng")], writes=[Rr("ex4")], updates=[Rr("se")])
                V(lambda e: e.reciprocal(out=T("gp")[:], in_=T("se")[:]), [Rr("se")], [Rr("gp")])
                V(lambda e: e.tensor_scalar(out=T("oh")[:], in0=T("lg")[:, 0:4], scalar1=T("m4")[:, 0:1], scalar2=None, op0=ALU.is_ge), [Rr("lg"), Rr("m4")], [Rr("oh")])
                V(lambda e: e.tensor_scalar(out=T("pen")[:], in0=T("oh")[:], scalar1=-1.0, scalar2=-NEG, op0=ALU.add, op1=ALU.mult), [Rr("oh")], [Rr("pen")])
                V(lambda e: e.tensor_tensor(out=T("msk")[:].rearrange("p (g x) -> p g x", g=4), in0=T("lg")[:, 4:36].rearrange("p (g x) -> p g x", g=4),
                                            in1=T("pen")[:].rearrange("p (g o) -> p g o", o=1).broadcast_to([128, 4, 8]), op=ALU.add),
                  [Rr("lg"), Rr("pen")], [Rr("msk")])
                V(lambda e: e.max(out=T("top8")[:], in_=T("msk")[:]), [Rr("msk")], [Rr("top8")])
                V(lambda e: e.tensor_scalar(out=T("sel")[:], in0=T("msk")[:], scalar1=T("top8")[:, 1:2], scalar2=None, op0=ALU.is_ge), [Rr("msk"), Rr("top8")], [Rr("sel")])
                V(lambda e: e.tensor_scalar(out=T("nt1")[:], in0=T("top8")[:, 0:1], scalar1=-1.0, scalar2=None, op0=ALU.mult), [Rr("top8")], [Rr("nt1")])
                V(lambda e: e.tensor_tensor(out=T("d21")[:], in0=T("top8")[:, 1:2], in1=T("top8")[:, 0:1], op=ALU.subtract), [Rr("top8")], [Rr("d21")])
                S.op("scalar", lambda e: e.activation(out=T("e21")[:], in_=T("d21")[:], func=AF.Exp), reads=[Rr("d21")], writes=[Rr("e21")])
                S.op("scalar", lambda e: e.activation(out=T("wf")[:], in_=T("msk")[:], func=AF.Exp, bias=T("nt1")[:, 0:1], scale=1.0),
                     reads=[Rr("msk"), Rr("nt1")], writes=[Rr("wf")])
                V(lambda e: e.tensor_scalar(out=T("coef")[:], in0=T("e21")[:], scalar1=1.0, scalar2=None, op0=ALU.add), [Rr("e21")], [Rr("coef")])
                V(lambda e: e.reciprocal(out=T("coef")[:], in_=T("coef")[:]), [Rr("coef")], [Rr("coef")])
                V(lambda e: e.tensor_tensor(out=T("coef")[:], in0=T("coef")[:], in1=T("gp")[:], op=ALU.mult), [Rr("coef"), Rr("gp")], [Rr("coef")])
                V(lambda e, t=t: e.scalar_tensor_tensor(out=gates[:, t, :], in0=T("wf")[:], scalar=T("coef")[:, 0:1], in1=T("sel")[:], op0=ALU.mult, op1=ALU.mult),
                  [Rr("wf"), Rr("coef"), Rr("sel")], [r_gates[t]])
            for t in range(NT):
                do_tile(t)
        C.stack = C_stack_saved
        S.barrier()

        stB = contextlib.ExitStack()
        C.stack = stB
        with stB:
            NST = 3
            stg = [C.sb([128, 4, 512], F32, "stg%d" % i) for i in range(NST)]
            w1b = [C.sb([128, 8, 512], BF16, "w1b%d" % i) for i in range(2)]
            w3b = [C.sb([128, 8, 512], BF16, "w3b%d" % i) for i in range(2)]
            w2b = [C.sb([128, 4, D], BF16, "w2b%d" % i) for i in range(2)]
            sg = [C.sb([128, 512], F32, "sg%d" % i) for i in range(2)]
            act = [C.sb([128, 4, 512], BF16, "act%d" % i) for i in range(2)]
            tmpB = [C.sb([128, 512], F32, "tmpB%d" % i) for i in range(2)]
            pU1 = [C.ps([128, 512], F32, "pU1_%d" % i) for i in range(2)]
            pU3 = [C.ps([128, 512], F32, "pU3_%d" % i) for i in range(2)]
            pYB = [C.ps([128, 512], F32, "pYB%d" % i) for i in range(3)]
            chunks = []
            t0 = 0
            while t0 < NT:
                chunks.append(list(range(t0, min(t0 + 4, NT))))
                t0 += 4
            si_ = 0
            ny = 0
            nact = 0
            for ex in range(NEXP):
                pb = ex % 2
                pieces = []
                for wsrc, dst in ((w1, w1b[pb]), (w3, w3b[pb])):
                    for half in range(2):
                        pieces.append((wsrc[ex, half * 512:(half + 1) * 512, :].rearrange("(c p) f -> p c f", p=128), dst[0][:, half * 4:(half + 1) * 4, :], dst[1]))
                for half in range(2):
                    pieces.append((w2[ex, half * 256:(half + 1) * 256, :].rearrange("(c p) f -> p c f", p=128),
                                   w2b[pb][0][:, half * 2:(half + 1) * 2, :], w2b[pb][1]))
                for pi, (src, dstap, r_dst) in enumerate(pieces):
                    s_t, r_s = stg[si_ % NST]
                    sv = s_t[:] if pi < 4 else s_t[:].rearrange("p (c a) f -> p c (a f)", a=2)
                    S.dma(sv, src, writes=[r_s], eng=("sync" if si_ % 2 == 0 else "gpsimd"))
                    S.op("scalar", lambda e, sv=sv, dstap=dstap: e.copy(out=dstap, in_=sv), reads=[r_s], updates=[r_dst])
                    si_ += 1
                W1, rW1 = w1b[pb]
                W3, rW3 = w3b[pb]
                W2, rW2 = w2b[pb]
                for ch in chunks:
                    n = len(ch) * 128
                    c0 = ch[0] * 128
                    a_t, r_a = act[nact % 2]
                    nact += 1
                    rh = [r_h2T[t] for t in ch]
                    for fc in range(4):
                        p1, r_p1 = pU1[fc % 2]
                        p3, r_p3 = pU3[fc % 2]
                        for c in range(8):
                            S.op("tensor", lambda e, c=c, fc=fc, p1=p1, W1=W1, c0=c0, n=n: e.matmul(out=p1[:, 0:n], lhsT=W1[:, c, fc * 128:(fc + 1) * 128], rhs=h2T[:, c, c0:c0 + n],
                                                                                                       start=(c == 0), stop=(c == 7)),
                                 reads=rh + [rW1], updates=[r_p1] if c else (), writes=() if c else [r_p1])
                        for c in range(8):
                            S.op("tensor", lambda e, c=c, fc=fc, p3=p3, W3=W3, c0=c0, n=n: e.matmul(out=p3[:, 0:n], lhsT=W3[:, c, fc * 128:(fc + 1) * 128], rhs=h2T[:, c, c0:c0 + n],
                                                                                                       start=(c == 0), stop=(c == 7)),
                                 reads=rh + [rW3], updates=[r_p3] if c else (), writes=() if c else [r_p3])
                        s_g, r_sg = sg[fc % 2]
                        S.op("scalar", lambda e, s_g=s_g, p1=p1, n=n: e.activation(out=s_g[:, 0:n], in_=p1[:, 0:n], func=AF.Silu), reads=[r_p1], writes=[r_sg])
                        S.op("vector", lambda e, s_g=s_g, p3=p3, a_t=a_t, fc=fc, n=n: e.tensor_tensor(out=a_t[:, fc, 0:n], in0=s_g[:, 0:n], in1=p3[:, 0:n], op=ALU.mult),
                             reads=[r_sg, r_p3], updates=[r_a] if fc else (), writes=() if fc else [r_a])
                    for ti, t in enumerate(ch):
                        gm = gmlp[0 if tiles[t] < 2 else 1]
                        for hf in range(2):
                            p_y, r_py = pYB[ny % 3]
                            tb, r_tb = tmpB[ny % 2]
                            ny += 1
                            for fc in range(4):
                                S.op("tensor", lambda e, fc=fc, p_y=p_y, a_t=a_t, ti=ti, W2=W2, hf=hf: e.matmul(out=p_y[:], lhsT=a_t[:, fc, ti * 128:(ti + 1) * 128],
                                                                                                                  rhs=W2[:, fc, hf * 512:(hf + 1) * 512], start=(fc == 0), stop=(fc == 3)),
                                     reads=[r_a, rW2], updates=[r_py] if fc else (), writes=() if fc else [r_py])
                            sl = slice(hf * 512, (hf + 1) * 512)
                            S.op("vector", lambda e, p_y=p_y, tb=tb, t=t, ex=ex, gm=gm, sl=sl: e.scalar_tensor_tensor(out=tb[:], in0=p_y[:], scalar=gates[:, t, ex:ex + 1], in1=gm[0][:, sl],
                                                                                                                        op0=ALU.mult, op1=ALU.mult),
                                 reads=[r_py, r_gates[t], gm[1]], writes=[r_tb])
                            S.op("gpsimd", lambda e, tb=tb, t=t, sl=sl: e.tensor_tensor(out=acc[:, t, sl], in0=acc[:, t, sl], in1=tb[:], op=ALU.add),
                                 reads=[r_tb], writes=[r_acc[t][hf]])
        C.stack = C_stack_saved
        for t in range(NT):
            gt = tiles[t]
            S.dma(ypart[gt * 128:(gt + 1) * 128, :], acc[:, t, :], reads=r_acc[t], updates=[C.r_yp[gt // 8]])


class NormIn:
    def __init__(self, C, xs, modv, nw, idb, r_idb, nps=2):
        self.C, self.xs, self.modv = C, xs, modv
        S = C.S
        self.idb, self.r_idb = idb, r_idb
        self.w1e, self.r_w1e = C.sb([128, D], F32, "w1e")
        self.sh1, self.r_sh1 = C.sb([128, D], F32, "sh1")
        self.nwt, self.r_nwt = C.sb([128, D], F32, "nwt1")
        S.dma(self.nwt[:], nw.partition_broadcast(128), writes=[self.r_nwt])
        self.xt = [C.sb([128, D], F32, "nxt%d" % i) for i in range(2)]
        self.tmp_l = [C.sb([128, D], F32, "ntmp%d" % i) for i in range(2)]
        self.hb_l = [C.sb([128, D], BF16, "nhb%d" % i) for i in range(2)]
        self.ss_l = [C.sb([128, 1], F32, "nss%d" % i) for i in range(2)]
        self.rstd_l = [C.sb([128, 1], F32, "nrstd%d" % i) for i in range(2)]
        self.pT = [C.ps([128, 8, 128], BF16, "npT%d" % i) for i in range(nps)]
        if nps == 1:
            self.pT = self.pT * 2
        self.hT = [C.sb([128, 8, 128], BF16, "nhT%d" % i) for i in range(2)]
        self.cur = None
        self.k = 0

    def load_mod(self, si):
        S, modv = self.C.S, self.modv
        S.dma(self.sh1[:], modv[si, 0:1, :].partition_broadcast(128), writes=[self.r_sh1])
        S.dma(self.w1e[:], modv[si, 1:2, :].partition_broadcast(128), writes=[self.r_w1e])
        S.op("vector", lambda e: e.scalar_tensor_tensor(out=self.w1e[:], in0=self.w1e[:], scalar=1.0, in1=self.nwt[:], op0=ALU.add, op1=ALU.mult),
             reads=[self.r_w1e, self.r_nwt], writes=[self.r_w1e])

    def tile(self, t):
        S = self.C.S
        need = 0 if t < 2 else 1
        if need != self.cur:
            self.load_mod(need)
            self.cur = need
        k = self.k
        self.k += 1
        x_t, r_x = self.xt[k % 2]
        S.dma(x_t[:], self.xs[t * 128:(t + 1) * 128, :], reads=[self.C.r_xf[t // 8]], writes=[r_x])
        ss, r_ss = self.ss_l[k % 2]
        rstd, r_rstd = self.rstd_l[k % 2]
        tmp, r_tmp = self.tmp_l[k % 2]
        hb, r_hb = self.hb_l[k % 2]
        S.op("gpsimd", lambda e: e.memset(ss[:], 0.0), writes=[r_ss])
        S.op("scalar", lambda e: e.activation(out=tmp[:], in_=x_t[:], func=AF.Square, accum_out=ss[:]), reads=[r_x], writes=[r_tmp], updates=[r_ss])
        S.op("vector", lambda e: e.tensor_scalar(out=rstd[:], in0=ss[:], scalar1=1.0 / D, scalar2=EPS, op0=ALU.mult, op1=ALU.add), reads=[r_ss], writes=[r_rstd])
        S.op("scalar", lambda e: e.activation(out=rstd[:], in_=rstd[:], func=AF.Sqrt), reads=[r_rstd], writes=[r_rstd])
        S.op("vector", lambda e: e.reciprocal(out=rstd[:], in_=rstd[:]), reads=[r_rstd], writes=[r_rstd])
        S.op("vector", lambda e: e.scalar_tensor_tensor(out=tmp[:], in0=x_t[:], scalar=rstd[:, 0:1], in1=self.w1e[:], op0=ALU.mult, op1=ALU.mult),
             reads=[r_x, r_rstd, self.r_w1e], writes=[r_tmp])
        S.op("gpsimd", lambda e: e.tensor_tensor(out=hb[:], in0=tmp[:], in1=self.sh1[:], op=ALU.add), reads=[r_tmp, self.r_sh1], writes=[r_hb])
        p_T, r_pT = self.pT[k % 2]
        h_T, r_hT = self.hT[k % 2]
        for c in range(8):
            S.op("tensor", lambda e, c=c: e.transpose(out=p_T[:, c, :], in_=hb[:, c * 128:(c + 1) * 128], identity=self.idb[:]),
                 reads=[r_hb, self.r_idb], updates=[r_pT] if c else (), writes=() if c else [r_pT])
        S.op("scalar", lambda e: e.copy(out=h_T[:], in_=p_T[:]), reads=[r_pT], writes=[r_hT])
        return h_T, r_hT


def load_weight_bf16(C, dst, r_dst, src, ncols, col0=0, eng_cast="gpsimd"):
    S = C.S
    saved = C.stack
    tmpst = contextlib.ExitStack()
    C.stack = tmpst
    with tmpst:
        stgs = [C.sb([128, max(2048, ncols)], F32, "lwst%d_%d" % (C.n, i)) for i in range(2)]
        step = max(1, 2048 // ncols)
        k = 0
        for c0 in range(0, 8, step):
            nck = min(step, 8 - c0)
            s_t, r_s = stgs[k % 2]
            sv = s_t[:, 0:nck * ncols].rearrange("p (c f) -> p c f", c=nck)
            S.dma(sv, src[c0 * 128:(c0 + nck) * 128, :].rearrange("(c p) f -> p c f", p=128), writes=[r_s], eng=("sync" if k % 2 == 0 else "gpsimd"))
            S.op(eng_cast, lambda e, sv=sv, c0=c0, nck=nck: e.tensor_copy(out=dst[:, c0:c0 + nck, col0:col0 + ncols], in_=sv), reads=[r_s], updates=[r_dst])
            k += 1
    C.stack = saved
    S.barrier()


def emit_even(C, A):
    nc, S = C.nc, C.S
    NT = 34
    NTOK = NT * 128
    xs, modv, nw, wc, ropec, ropes, draw, sink, cmat, crow, ccol, mgo = (A[k] for k in "xs modv nw wc ropec ropes draw sink cmat crow ccol mgo".split())
    SC = 128 ** -0.5
    with C.scope():
        idb, r_idb, idf, r_idf = make_identity(C)
        qkT, _ = C.sb([128, 7, NTOK], BF16, "qkT")
        r_qkT = [Res("qkT%d" % t) for t in range(NT)]
        tm, _ = C.sb([128, NT, 896], BF16, "tm")
        r_tm = [Res("tm%d" % t) for t in range(NT)]
        cm, r_cm = C.sb([128, 6, 128], F32, "cm")
        S.dma(cm[:], cmat, writes=[r_cm])
        cr, r_cr = C.sb([128, 256], F32, "cr")
        S.dma(cr[:], crow.partition_broadcast(128), writes=[r_cr])
        cc, r_cc = C.sb([128, 2], F32, "cc")
        S.dma(cc[:], ccol, writes=[r_cc])
        lg, r_lg = C.sb([128, 4], F32, "lg")
        S.dma(lg[:], draw.partition_broadcast(128), writes=[r_lg])
        S.op("scalar", lambda e: e.activation(out=lg[:], in_=lg[:], func=AF.Exp), reads=[r_lg], writes=[r_lg])
        S.op("vector", lambda e: e.tensor_scalar(out=lg[:], in0=lg[:], scalar1=-1.0, scalar2=None, op0=ALU.mult), reads=[r_lg], writes=[r_lg])
        skt, r_skt = C.sb([128, 2], F32, "skt")
        S.dma(skt[:], sink.partition_broadcast(128), writes=[r_skt])
        dmask, r_dmask = C.sb([128, 4, 128], F32, "dmask")
        qdec, r_qdec = C.sb([128, 4, 128], F32, "qdec")
        kdec, r_kdec = C.sb([128, 4], F32, "kdec")
        cdec, r_cdec = C.sb([128, 4], F32, "cdec")
        c128, r_c128 = C.sb([128, 1], F32, "c128")
        S.op("gpsimd", lambda e: e.memset(c128[:], 128.0), writes=[r_c128])
        for dr in range(2):
            for h in range(2):
                ix = dr * 2 + h
                lgc = lg[:, ix:ix + 1]
                S.op("vector", lambda e, ix=ix, dr=dr, lgc=lgc: e.tensor_scalar(out=dmask[:, ix, :], in0=cm[:, dr, :], scalar1=lgc, scalar2=None, op0=ALU.mult),
                     reads=[r_cm, r_lg], updates=[r_dmask])
                S.op("scalar", lambda e, ix=ix: e.activation(out=dmask[:, ix, :], in_=dmask[:, ix, :], func=AF.Exp), reads=[], updates=[r_dmask])
                S.op("vector", lambda e, ix=ix, dr=dr: e.tensor_tensor(out=dmask[:, ix, :], in0=dmask[:, ix, :], in1=cm[:, 2 + dr, :], op=ALU.mult),
                     reads=[r_cm], updates=[r_dmask])
                S.op("vector", lambda e, ix=ix, dr=dr, lgc=lgc: e.tensor_scalar(out=qdec[:, ix, :], in0=cr[:, dr * 128:(dr + 1) * 128], scalar1=lgc, scalar2=None, op0=ALU.mult),
                     reads=[r_cr, r_lg], updates=[r_qdec])
                S.op("scalar", lambda e, ix=ix: e.activation(out=qdec[:, ix, :], in_=qdec[:, ix, :], func=AF.Exp), reads=[], updates=[r_qdec])
                S.op("vector", lambda e, ix=ix, dr=dr, lgc=lgc: e.tensor_scalar(out=kdec[:, ix:ix + 1], in0=cc[:, dr:dr + 1], scalar1=lgc, scalar2=None, op0=ALU.mult),
                     reads=[r_cc, r_lg], updates=[r_kdec])
                S.op("vector", lambda e, ix=ix, lgc=lgc: e.tensor_scalar(out=cdec[:, ix:ix + 1], in0=c128[:], scalar1=lgc, scalar2=None, op0=ALU.mult),
                     reads=[r_c128, r_lg], updates=[r_cdec])
        S.op("scalar", lambda e: e.activation(out=kdec[:], in_=kdec[:], func=AF.Exp), reads=[], updates=[r_kdec])
        S.op("scalar", lambda e: e.activation(out=cdec[:], in_=cdec[:], func=AF.Exp), reads=[], updates=[r_cdec])
        S.op("vector", lambda e: e.tensor_scalar(out=kdec[:], in0=kdec[:], scalar1=SC, scalar2=None, op0=ALU.mult), reads=[], updates=[r_kdec])

        stP = contextlib.ExitStack()
        saved = C.stack
        C.stack = stP
        with stP:
            wcb, r_wcb = C.sb([128, 8, 1536], BF16, "wcb")
            load_weight_bf16(C, wcb, r_wcb, wc, 1536)
            NI = NormIn(C, xs, modv, nw, idb, r_idb)
            cosT = [C.sb([128, 128], F32, "cos%d" % i) for i in range(2)]
            sinT = [C.sb([128, 128], F32, "sin%d" % i) for i in range(2)]
            pP = [C.ps([128, 512], F32, "pP%d" % i) for i in range(3)]
            pT7, r_pT7 = C.ps([128, 7, 128], BF16, "pT7")
            pf, r_pf = C.sb([128, 896], F32, "pf")
            t1, r_t1 = C.sb([128, 896], F32, "t1")
            t2, r_t2 = C.sb([128, 896], F32, "t2")
            Rb, r_Rb = C.sb([128, 896], BF16, "Rb")
            for t in range(NT):
                h_T, r_hT = NI.tile(t)
                c_t, r_c = cosT[t % 2]
                s_t, r_s = sinT[t % 2]
                S.dma(c_t[:], ropec[t * 128:(t + 1) * 128, :], writes=[r_c], eng="gpsimd")
                S.dma(s_t[:], ropes[t * 128:(t + 1) * 128, :], writes=[r_s], eng="gpsimd")
                for nb in range(3):
                    p, r_p = pP[nb]
                    for c in range(8):
                        S.op("tensor", lambda e, c=c, nb=nb, p=p, h_T=h_T: e.matmul(out=p[:], lhsT=h_T[:, c, :], rhs=wcb[:, c, nb * 512:(nb + 1) * 512], start=(c == 0), stop=(c == 7)),
                             reads=[r_hT, r_wcb], updates=[pP[nb][1]] if c else (), writes=() if c else [pP[nb][1]])
                S.op("scalar", lambda e: e.copy(out=pf[:, 0:512], in_=pP[0][0][:]), reads=[pP[0][1]], writes=[r_pf])
                S.op("scalar", lambda e: e.copy(out=pf[:, 512:896], in_=pP[1][0][:, 0:384]), reads=[pP[1][1]], updates=[r_pf])
                S.op("scalar", lambda e, t=t: e.copy(out=tm[:, t, 256:384], in_=pP[1][0][:, 384:512]), reads=[pP[1][1]], updates=[r_tm[t]])
                S.op("scalar", lambda e, t=t: e.copy(out=tm[:, t, 384:896], in_=pP[2][0][:]), reads=[pP[2][1]], updates=[r_tm[t]])
                S.op("vector", lambda e, c_t=c_t: e.tensor_tensor(out=t1[:].rearrange("p (h d) -> p h d", h=7), in0=pf[:].rearrange("p (h d) -> p h d", h=7),
                                                                  in1=c_t[:].rearrange("p (o d) -> p o d", o=1).broadcast_to([128, 7, 128]), op=ALU.mult),
                     reads=[r_pf, r_c], writes=[r_t1])
                for a in range(2):
                    for hf in range(2):
                        o0 = a * 64 + hf * 32
                        i0 = a * 64 + (1 - hf) * 32
                        S.op("vector", lambda e, o0=o0, i0=i0, s_t=s_t: e.tensor_tensor(
                            out=t2[:].rearrange("p (h d) -> p h d", h=7)[:, :, o0:o0 + 32], in0=pf[:].rearrange("p (h d) -> p h d", h=7)[:, :, i0:i0 + 32],
                            in1=s_t[:, o0:o0 + 32].rearrange("p (o d) -> p o d", o=1).broadcast_to([128, 7, 32]), op=ALU.mult),
                             reads=[r_pf, r_s], updates=[r_t2])
                S.op("vector", lambda e: e.tensor_tensor(out=Rb[:], in0=t1[:], in1=t2[:], op=ALU.add), reads=[r_t1, r_t2], writes=[r_Rb])
                S.op("gpsimd", lambda e, t=t: e.tensor_copy(out=tm[:, t, 0:256], in_=Rb[:, 256:512]), reads=[r_Rb], updates=[r_tm[t]])
                for c in range(7):
                    S.op("tensor", lambda e, c=c: e.transpose(out=pT7[:, c, :], in_=Rb[:, c * 128:(c + 1) * 128], identity=idb[:]),
                         reads=[r_Rb, r_idb], updates=[r_pT7] if c else (), writes=() if c else [r_pT7])
                S.op("scalar", lambda e, t=t: e.copy(out=qkT[:, :, t * 128:(t + 1) * 128], in_=pT7[:]), reads=[r_pT7], writes=[r_qkT[t]])
        C.stack = saved
        S.barrier()

        stS = contextlib.ExitStack()
        C.stack = stS
        with stS:
            oacc, _ = C.sb([128, NT, 256], F32, "oacc")
            r_oacc = [Res("oacc%d" % t) for t in range(NT)]
            Sst = [C.sb([128, 128], F32, "Sst%d" % i) for i in range(4)]
            Sbf = [C.sb([128, 128], BF16, "Sbf%d" % i) for i in range(4)]
            for i in range(4):
                S.op("gpsimd", lambda e, i=i: e.memset(Sst[i][0][:], 0.0), writes=[Sst[i][1]])
                S.op("gpsimd", lambda e, i=i: e.memset(Sbf[i][0][:], 0.0), writes=[Sbf[i][1]])
            PTs = [C.sb([128, 128], BF16, "PTs%d" % i) for i in range(2)]
            qs = [C.sb([128, 128], BF16, "qs%d" % i) for i in range(2)]
            ks = [C.sb([128, 128], BF16, "ks%d" % i) for i in range(2)]
            pSc = [C.ps([128, 128], F32, "pSc")] * 2
            pO = [C.ps([128, 128], F32, "pO%d" % i) for i in range(2)]
            pU = [C.ps([128, 128], F32, "pU")] * 2
            cnt = [0]

            def ret_step(t, h, dr):
                ix = dr * 2 + h
                k = cnt[0]
                cnt[0] += 1
                tok = slice(t * 128, (t + 1) * 128)
                p_s, r_ps = pSc[k % 2]
                p_o, r_po = pO[k % 2]
                p_u, r_pu = pU[k % 2]
                PT, r_PT = PTs[k % 2]
                q_s, r_qs = qs[k % 2]
                k_s, r_ks = ks[k % 2]
                S.op("tensor", lambda e: e.matmul(out=p_s[:], lhsT=qkT[:, 2 + h, tok], rhs=qkT[:, h, tok], start=True, stop=True),
                     reads=[r_qkT[t]], writes=[r_ps])
                S.op("vector", lambda e: e.tensor_tensor(out=PT[:], in0=p_s[:], in1=dmask[:, ix, :], op=ALU.mult), reads=[r_ps, r_dmask], writes=[r_PT])
                S.op("vector", lambda e: e.tensor_tensor(out=q_s[:], in0=qkT[:, h, tok], in1=qdec[:, ix, :], op=ALU.mult), reads=[r_qkT[t], r_qdec], writes=[r_qs])
                S.op("vector", lambda e: e.tensor_scalar(out=k_s[:], in0=tm[:, t, h * 128:(h + 1) * 128], scalar1=kdec[:, ix:ix + 1], scalar2=None, op0=ALU.mult),
                     reads=[r_tm[t], r_kdec], writes=[r_ks])
                vv = tm[:, t, 256 + h * 128:256 + (h + 1) * 128]
                S.op("tensor", lambda e: e.matmul(out=p_o[:], lhsT=PT[:], rhs=vv, start=True, stop=False), reads=[r_PT, r_tm[t]], writes=[r_po])
                S.op("tensor", lambda e: e.matmul(out=p_o[:], lhsT=q_s[:], rhs=Sbf[ix][0][:], start=False, stop=True), reads=[r_qs, Sbf[ix][1]], updates=[r_po])
                S.op("tensor", lambda e: e.matmul(out=p_u[:], lhsT=k_s[:], rhs=vv, start=True, stop=True), reads=[r_ks, r_tm[t]], writes=[r_pu])
                S.op("vector", lambda e: e.scalar_tensor_tensor(out=Sst[ix][0][:], in0=Sst[ix][0][:], scalar=cdec[:, ix:ix + 1], in1=p_u[:], op0=ALU.mult, op1=ALU.add),
                     reads=[r_pu, r_cdec], writes=[Sst[ix][1]])
                S.op("scalar", lambda e: e.copy(out=Sbf[ix][0][:], in_=Sst[ix][0][:]), reads=[Sst[ix][1]], writes=[Sbf[ix][1]])
                return p_o, r_po

            for t in range(NT):
                for h in range(2):
                    p_o, r_po = ret_step(t, h, 0)
                    S.op("scalar", lambda e, t=t, h=h, p_o=p_o: e.copy(out=oacc[:, t, h * 128:(h + 1) * 128], in_=p_o[:]), reads=[r_po], updates=[r_oacc[t]])

            mgt = [C.sb([128, 512], BF16, "mgt%d" % i) for i in range(2)]
            otot, r_otot = C.sb([128, 256], F32, "otot")
            sgt, r_sgt = C.sb([128, 256], F32, "sgt")
            jk, r_jk = C.sb([128, 128], F32, "jk2")
            ssh, r_ssh = C.sb([128, 2], F32, "ssh")
            ssc, r_ssc = C.sb([128, 640], F32, "ssc")
            Pb, r_Pb = C.sb([128, 640], BF16, "Pb")
            PTa, r_PTa = C.sb([128, 5, 128], BF16, "PTa")
            mx, r_mx = C.sb([128, 1], F32, "mx")
            nmx, r_nmx = C.sb([128, 1], F32, "nmx")
            rs, r_rs = C.sb([128, 1], F32, "rs")
            esk, r_esk = C.sb([128, 1], F32, "esk")
            pA, r_pA = C.ps([128, 384], F32, "pA")
            pB, r_pB = C.ps([128, 256], F32, "pB")
            pPT, r_pPT = C.ps([128, 5, 128], BF16, "pPT")
            pOa, r_pOa = C.ps([128, 128], F32, "pOa")
            order = [1, 0] + list(range(NT - 1, 1, -1))
            for oi, t in enumerate(order):
                m_t, r_m = mgt[oi % 2]
                S.op("scalar", lambda e, t=t: e.activation(out=sgt[:], in_=tm[:, t, 512:768], func=AF.Silu), reads=[r_tm[t]], writes=[r_sgt])
                S.op("gpsimd", lambda e: e.memset(ssh[:], 0.0), writes=[r_ssh])
                for h in range(2):
                    p_o, r_po = ret_step(t, h, 1)
                    hs = slice(h * 128, (h + 1) * 128)
                    S.op("vector", lambda e, t=t, hs=hs, p_o=p_o: e.tensor_tensor(out=otot[:, hs], in0=p_o[:], in1=oacc[:, t, hs], op=ALU.add),
                         reads=[r_po, r_oacc[t]], updates=[r_otot] if h else (), writes=() if h else [r_otot])
                    S.op("scalar", lambda e, hs=hs, h=h: e.activation(out=jk[:], in_=otot[:, hs], func=AF.Square, accum_out=ssh[:, h:h + 1]),
                         reads=[r_otot], writes=[r_jk], updates=[r_ssh])
                S.op("vector", lambda e: e.tensor_scalar(out=ssh[:], in0=ssh[:], scalar1=1.0 / 128, scalar2=EPS, op0=ALU.mult, op1=ALU.add), reads=[r_ssh], writes=[r_ssh])
                S.op("scalar", lambda e: e.activation(out=ssh[:], in_=ssh[:], func=AF.Sqrt), reads=[r_ssh], writes=[r_ssh])
                S.op("vector", lambda e: e.reciprocal(out=ssh[:], in_=ssh[:]), reads=[r_ssh], writes=[r_ssh])
                for h in range(2):
                    hs = slice(h * 128, (h + 1) * 128)
                    S.op("vector", lambda e, hs=hs, h=h, m_t=m_t: e.scalar_tensor_tensor(out=m_t[:, hs], in0=otot[:, hs], scalar=ssh[:, h:h + 1], in1=sgt[:, hs], op0=ALU.mult, op1=ALU.mult),
                         reads=[r_otot, r_ssh, r_sgt], updates=[r_m] if h else (), writes=() if h else [r_m])
                if t >= 2:
                    n = t - 2
                    lo = max(n - 1, 0)
                    hi = min(n + 1, 31)
                    loc = list(range(lo + 2, hi + 3))
                else:
                    loc = []
                ktiles = loc + [0, 1]
                nl = len(loc)
                nk = len(ktiles)
                rk_reads = [r_qkT[kt] for kt in ktiles]
                rv_reads = [r_tm[kt] for kt in ktiles]
                for g in range(2):
                    qT = qkT[:, 4 + g, t * 128:(t + 1) * 128]
                    if nl:
                        S.op("tensor", lambda e, qT=qT, loc=loc, nl=nl: e.matmul(out=pA[:, 0:nl * 128], lhsT=qT, rhs=qkT[:, 6, loc[0] * 128:(loc[-1] + 1) * 128], start=True, stop=True),
                             reads=[r_qkT[t]] + rk_reads, writes=[r_pA])
                    S.op("tensor", lambda e, qT=qT: e.matmul(out=pB[:], lhsT=qT, rhs=qkT[:, 6, 0:256], start=True, stop=True), reads=[r_qkT[t]] + rk_reads, writes=[r_pB])
                    for li, kt in enumerate(loc):
                        rel = kt - t
                        dst = ssc[:, li * 128:(li + 1) * 128]
                        src = pA[:, li * 128:(li + 1) * 128]
                        if rel == 0:
                            S.op("vector", lambda e, dst=dst, src=src: e.tensor_copy(out=dst, in_=src), reads=[r_pA], updates=[r_ssc])
                        else:
                            mi = 4 if rel < 0 else 5
                            S.op("vector", lambda e, dst=dst, src=src, mi=mi: e.tensor_tensor(out=dst, in0=src, in1=cm[:, mi, :], op=ALU.add), reads=[r_pA, r_cm], updates=[r_ssc])
                    S.op("scalar", lambda e, nl=nl: e.copy(out=ssc[:, nl * 128:nl * 128 + 256], in_=pB[:]), reads=[r_pB], updates=[r_ssc])
                    W = nk * 128
                    S.op("vector", lambda e, W=W: e.reduce_max(out=mx[:], in_=ssc[:, 0:W], axis=AX.X), reads=[r_ssc], writes=[r_mx])
                    S.op("vector", lambda e, g=g: e.tensor_scalar(out=mx[:], in0=mx[:], scalar1=SC, scalar2=skt[:, g:g + 1], op0=ALU.mult, op1=ALU.max), reads=[r_mx, r_skt], writes=[r_mx])
                    S.op("vector", lambda e: e.tensor_scalar(out=nmx[:], in0=mx[:], scalar1=-1.0, scalar2=None, op0=ALU.mult), reads=[r_mx], writes=[r_nmx])
                    S.op("gpsimd", lambda e: e.memset(rs[:], 0.0), writes=[r_rs])
                    S.op("scalar", lambda e, W=W: e.activation(out=Pb[:, 0:W], in_=ssc[:, 0:W], func=AF.Exp, bias=nmx[:, 0:1], scale=SC, accum_out=rs[:]),
                         reads=[r_ssc, r_nmx], writes=[r_Pb], updates=[r_rs])
                    S.op("scalar", lambda e, g=g: e.activation(out=esk[:], in_=skt[:, g:g + 1], func=AF.Exp, bias=nmx[:, 0:1], scale=1.0), reads=[r_skt, r_nmx], writes=[r_esk])
                    S.op("vector", lambda e: e.tensor_tensor(out=rs[:], in0=rs[:], in1=esk[:], op=ALU.add), reads=[r_esk], updates=[r_rs])
                    S.op("vector", lambda e: e.reciprocal(out=rs[:], in_=rs[:]), reads=[], updates=[r_rs])
                    for ki in range(nk):
                        S.op("tensor", lambda e, ki=ki: e.transpose(out=pPT[:, ki, :], in_=Pb[:, ki * 128:(ki + 1) * 128], identity=idb[:]),
                             reads=[r_Pb, r_idb], updates=[r_pPT] if ki else (), writes=() if ki else [r_pPT])
                    S.op("scalar", lambda e, nk=nk: e.copy(out=PTa[:, 0:nk, :], in_=pPT[:, 0:nk, :]), reads=[r_pPT], writes=[r_PTa])
                    for ki, kt in enumerate(ktiles):
                        S.op("tensor", lambda e, ki=ki, kt=kt, nk=nk: e.matmul(out=pOa[:], lhsT=PTa[:, ki, :], rhs=tm[:, kt, 768:896], start=(ki == 0), stop=(ki == nk - 1)),
                             reads=[r_PTa] + rv_reads, updates=[r_pOa] if ki else (), writes=() if ki else [r_pOa])
                    S.op("vector", lambda e, g=g, m_t=m_t: e.tensor_scalar(out=m_t[:, 256 + g * 128:256 + (g + 1) * 128], in0=pOa[:], scalar1=rs[:, 0:1], scalar2=None, op0=ALU.mult),
                         reads=[r_pOa, r_rs], updates=[r_m])
                S.dma(mgo[t * 128:(t + 1) * 128, :], m_t[:], reads=[r_m])
        C.stack = saved


def even_consts():
    p = np.arange(128, dtype=np.float32)[:, None]
    f = np.arange(128, dtype=np.float32)[None, :]
    SC = np.float32(128 ** -0.5)
    cmat = np.stack([np.maximum(f - p, 0), np.maximum(p - f, 0), (f >= p) * SC, (p >= f) * SC,
                     np.where(f >= p, 0.0, NEG), np.where(f <= p, 0.0, NEG)], 1).astype(np.float32)
    crow = np.concatenate([f[0] + 1, 128 - f[0]])[None].astype(np.float32)
    ccol = np.concatenate([127 - p, p], 1).astype(np.float32)
    t = np.arange(L)
    row = (t // 64).astype(np.float32)
    col = (t % 64).astype(np.float32)
    inv = (10000.0 ** (-np.arange(0, 64, 2, dtype=np.float32) / 64)).astype(np.float32)
    ar = row[:, None] * inv[None]
    ac = col[:, None] * inv[None]
    cos = np.concatenate([np.cos(ar), np.cos(ar), np.cos(ac), np.cos(ac)], 1)
    sin = np.concatenate([-np.sin(ar), np.sin(ar), -np.sin(ac), np.sin(ac)], 1)
    ropec = np.concatenate([np.ones((NCTX, 128)), cos], 0).astype(np.float32)
    ropes = np.concatenate([np.zeros((NCTX, 128)), sin], 0).astype(np.float32)
    return dict(cmat=np.ascontiguousarray(cmat), crow=crow, ccol=np.ascontiguousarray(ccol), ropec=ropec, ropes=ropes)


def emit_odd(C, A, debug=False):
    nc, S = C.nc, C.S
    NT = 34
    NTOK = NT * 128
    xs, modv, nw, wc, lbl, lsel, hnw, cmat, mgo = (A[k] for k in "xs modv nw wc lbl lsel hnw cmat mgo".split())
    if debug:
        dbg = C.dout("dbg", [128, 8, 512])
        dbg2 = C.dout("dbg2", [128, 8, 512])
    with C.scope():
        idb, r_idb, idf, r_idf = make_identity(C)
        cm, r_cm = C.sb([128, 2, 128], F32, "cm")
        S.dma(cm[:], cmat, writes=[r_cm])
        ones, r_ones = C.sb([128, 128], F32, "ones")
        S.op("gpsimd", lambda e: e.memset(ones[:], 1.0), writes=[r_ones])
        hn, r_hn = C.sb([128, 128], F32, "hn")
        S.dma(hn[:], hnw.partition_broadcast(128), writes=[r_hn])
        lgt, r_lgt = C.sb([128, 4, 4], F32, "lgt")
        for l_ in range(4):
            S.dma(lgt[:, l_, :], lbl[l_, :].rearrange("(h d) -> d h", d=128), updates=[r_lgt], allow_slow_non_contiguous=True)
        sel, r_sel = C.sb([128, 4], F32, "sel")
        S.dma(sel[:], lsel.partition_broadcast(128), writes=[r_sel])
        mxl, r_mxl = C.sb([128, 4], F32, "mxl")
        S.op("vector", lambda e: e.tensor_tensor(out=mxl[:], in0=lgt[:, 0, :], in1=lgt[:, 1, :], op=ALU.max), reads=[r_lgt], writes=[r_mxl])
        S.op("vector", lambda e: e.tensor_tensor(out=mxl[:], in0=mxl[:], in1=lgt[:, 2, :], op=ALU.max), reads=[r_lgt], writes=[r_mxl])
        S.op("vector", lambda e: e.tensor_tensor(out=mxl[:], in0=mxl[:], in1=lgt[:, 3, :], op=ALU.max), reads=[r_lgt], writes=[r_mxl])
        for l_ in range(4):
            S.op("vector", lambda e, l_=l_: e.tensor_tensor(out=lgt[:, l_, :], in0=lgt[:, l_, :], in1=mxl[:], op=ALU.subtract), reads=[r_mxl], updates=[r_lgt])
        S.op("scalar", lambda e: e.activation(out=lgt[:], in_=lgt[:], func=AF.Exp), reads=[], updates=[r_lgt])
        den, r_den = C.sb([128, 4], F32, "den")
        lb, r_lb = C.sb([128, 4], F32, "lb")
        oml, r_oml = C.sb([128, 4], F32, "oml")
        tl, r_tl = C.sb([128, 4], F32, "tl")
        S.op("gpsimd", lambda e: e.memset(den[:], 0.0), writes=[r_den])
        S.op("gpsimd", lambda e: e.memset(lb[:], 0.0), writes=[r_lb])
        for l_ in range(4):
            S.op("vector", lambda e, l_=l_: e.tensor_tensor(out=den[:], in0=den[:], in1=lgt[:, l_, :], op=ALU.add), reads=[r_lgt], writes=[r_den])
            S.op("vector", lambda e, l_=l_: e.tensor_scalar(out=tl[:], in0=lgt[:, l_, :], scalar1=sel[:, l_:l_ + 1], scalar2=None, op0=ALU.mult), reads=[r_lgt, r_sel], writes=[r_tl])
            S.op("vector", lambda e: e.tensor_tensor(out=lb[:], in0=lb[:], in1=tl[:], op=ALU.add), reads=[r_tl], writes=[r_lb])
        S.op("vector", lambda e: e.reciprocal(out=den[:], in_=den[:]), reads=[], updates=[r_den])
        S.op("vector", lambda e: e.tensor_tensor(out=lb[:], in0=lb[:], in1=den[:], op=ALU.mult), reads=[r_den], writes=[r_lb])
        S.op("vector", lambda e: e.tensor_scalar(out=oml[:], in0=lb[:], scalar1=-1.0, scalar2=1.0, op0=ALU.mult, op1=ALU.add), reads=[r_lb], writes=[r_oml])

        wcb, r_wcb = C.sb([128, 8, 2560], BF16, "wcb")
        load_weight_bf16(C, wcb, r_wcb, wc, 2560)
        NI = NormIn(C, xs, modv, nw, idb, r_idb, nps=1)
        ofw, _ = C.sb([128, NT, 512], F32, "ofw")
        r_ofw = [Res("ofw%d" % t) for t in range(NT)]
        pQ, r_pQ = C.ps([128, 4, 128], F32, "pQ")
        pZ, r_pZ = C.ps([128, 4, 128], F32, "pZ")
        pV, r_pV = C.ps([128, 512], F32, "pV")
        pG, r_pG = pV, r_pV
        pS, r_pS = C.ps([128, 4, 128], F32, "pS")
        _pK, r_pK = C.ps([128, 8, 128], BF16, "pK")
        pK = _pK.rearrange("p (h c) j -> p h c j", c=2)
        pUu, r_pUu = C.ps([128, 4, 128], F32, "pUu")
        pOo, r_pOo = C.ps([128, 4, 128], F32, "pOo")
        NB = 2
        bufs = []
        for i_ in range(NB):
            d_ = {}
            for nm_ in ("sf", "ff", "kk", "cs", "E1", "E2", "qsb"):
                d_[nm_] = C.sb([128, 4, 128], F32, "%s%d" % (nm_, i_))
            d_["rr"] = C.sb([128, 4, 2], F32, "rr%d" % i_)
            d_["aa"] = C.sb([128, 4, 2, 3], F32, "aa%d" % i_)
            d_["qtP"] = C.sb([128, 4, 2, 128], BF16, "qtP%d" % i_)
            d_["ktP"] = C.sb([128, 4, 2, 128], BF16, "ktP%d" % i_)
            S.op("vector", lambda e, t_=d_["qtP"][0]: e.memset(t_[:].rearrange("p a b c -> p (a b c)"), 0.0), writes=[d_["qtP"][1]])
            S.op("vector", lambda e, t_=d_["ktP"][0]: e.memset(t_[:].rearrange("p a b c -> p (a b c)"), 0.0), writes=[d_["ktP"][1]])
            d_["vb"] = C.sb([128, 512], BF16, "vb%d" % i_)
            d_["sgg"] = C.sb([128, 512], F32, "sgg%d" % i_)
            d_["otot"] = C.sb([128, 512], F32, "hotot%d" % i_)
            d_["ssh"] = C.sb([128, 4], F32, "hssh%d" % i_)
            bufs.append(d_)
        PT_l = [C.sb([128, 4, 128], BF16, "PT%d" % i_) for i_ in range(1)] * 2
        kTM_l = [C.sb([128, 4, 2, 128], BF16, "kTM%d" % i_) for i_ in range(1)] * 2
        tU_l = [C.sb([128, 4, 128], F32, "tU%d" % i_) for i_ in range(1)] * 2
        Sall, r_Sall = C.sb([128, 4, 128], F32, "Sall")
        Spp = [C.sb([128, 2, 4, 128], BF16, "hSpp%d" % i) for i in range(2)]
        jk, r_jk = C.sb([128, 128], F32, "hjk")
        mgt = [C.sb([128, 512], BF16, "hmg%d" % i) for i in range(2)]
        kcnt = [0]

        tilek = [0]

        def do_tile(oi, t, dr, zc0):
            B_ = bufs[tilek[0] % NB]
            tilek[0] += 1
            sf, r_sf = B_["sf"]
            ff, r_ff = B_["ff"]
            kk, r_kk = B_["kk"]
            cs, r_cs = B_["cs"]
            uu, r_uu = B_["sf"]
            E1, r_E1 = B_["E1"]
            E2, r_E2 = B_["E2"]
            qsb, r_qsb = B_["qsb"]
            rr, r_rr = B_["rr"]
            aa, r_aa = B_["aa"]
            qtP, r_qtP = B_["qtP"]
            ktP, r_ktP = B_["ktP"]
            vb, r_vb = B_["vb"]
            sgg, r_sgg = B_["sgg"]
            otot, r_otot = B_["otot"]
            ssh, r_ssh = B_["ssh"]
            h_T, r_hT = NI.tile(t)
            for h in range(4):
                for c in range(8):
                    S.op("tensor", lambda e, c=c, h=h, h_T=h_T: e.matmul(out=pQ[:, h, :], lhsT=wcb[:, c, h * 128:(h + 1) * 128], rhs=h_T[:, c, :], start=(c == 0), stop=(c == 7)),
                         reads=[r_hT, r_wcb], updates=[r_pQ] if (c or h) else (), writes=() if (c or h) else [r_pQ])
            for h in range(4):
                for c in range(8):
                    S.op("tensor", lambda e, c=c, h=h, zc0=zc0, h_T=h_T: e.matmul(out=pZ[:, h, :], lhsT=wcb[:, c, zc0 + h * 128:zc0 + (h + 1) * 128], rhs=h_T[:, c, :], start=(c == 0), stop=(c == 7)),
                         reads=[r_hT, r_wcb], updates=[r_pZ] if (c or h) else (), writes=() if (c or h) else [r_pZ])
            for c in range(8):
                S.op("tensor", lambda e, c=c, h_T=h_T: e.matmul(out=pV[:], lhsT=h_T[:, c, :], rhs=wcb[:, c, 1536:2048], start=(c == 0), stop=(c == 7)),
                     reads=[r_hT, r_wcb], updates=[r_pV] if c else (), writes=() if c else [r_pV])
            S.op("scalar", lambda e: e.copy(out=vb[:], in_=pV[:]), reads=[r_pV], writes=[r_vb])
            S.op("scalar", lambda e: e.copy(out=qsb[:], in_=pQ[:]), reads=[r_pQ], writes=[r_qsb])
            if dr == 1:
                for c in range(8):
                    S.op("tensor", lambda e, c=c, h_T=h_T: e.matmul(out=pG[:], lhsT=h_T[:, c, :], rhs=wcb[:, c, 2048:2560], start=(c == 0), stop=(c == 7)),
                         reads=[r_hT, r_wcb], updates=[r_pG] if c else (), writes=() if c else [r_pG])
                S.op("scalar", lambda e: e.activation(out=sgg[:], in_=pG[:], func=AF.Silu), reads=[r_pG], writes=[r_sgg])
            S.op("scalar", lambda e: e.activation(out=sf[:], in_=pZ[:], func=AF.Sigmoid), reads=[r_pZ], writes=[r_sf])
            for h in range(4):
                S.op("vector", lambda e, h=h: e.tensor_scalar(out=ff[:, h, :], in0=sf[:, h, :], scalar1=oml[:, h:h + 1], scalar2=lb[:, h:h + 1], op0=ALU.mult, op1=ALU.add),
                     reads=[r_sf, r_oml, r_lb], updates=[r_ff] if h else (), writes=() if h else [r_ff])
            S.op("vector", lambda e: e.tensor_scalar(out=kk[:], in0=ff[:], scalar1=-1.0, scalar2=1.0, op0=ALU.mult, op1=ALU.add), reads=[r_ff], writes=[r_kk])
            S.op("scalar", lambda e: e.activation(out=ff[:], in_=ff[:], func=AF.Ln), reads=[], updates=[r_ff])
            for h in range(4):
                S.op("vector", lambda e, h=h: e.tensor_tensor_scan(out=cs[:, h, :], data0=ones[:], data1=ff[:, h, :], initial=0.0, op0=ALU.mult, op1=ALU.add),
                     reads=[r_ff, r_ones], updates=[r_cs] if h else (), writes=() if h else [r_cs])
            if dr == 0:
                u, r_u = cs, r_cs
            else:
                S.op("vector", lambda e: e.tensor_tensor(out=uu[:], in0=ff[:], in1=cs[:], op=ALU.subtract), reads=[r_ff, r_cs], writes=[r_uu])
                u, r_u = uu, r_uu
            S.op("vector", lambda e, u=u: e.tensor_copy(out=rr[:], in_=u[:].rearrange("p h (c s) -> p h c s", c=2)[:, :, :, 31]), reads=[r_u], writes=[r_rr])
            for c in range(2):
                for h in range(4):
                    a1 = aa[:, h, c, 0:1]
                    a2 = aa[:, h, c, 1:2]
                    if dr == 0:
                        if c == 0:
                            S.op("vector", lambda e, a1=a1, h=h, c=c: e.tensor_copy(out=a1, in_=rr[:, h, c:c + 1]), reads=[r_rr], updates=[r_aa])
                        else:
                            S.op("vector", lambda e, a1=a1, h=h, c=c: e.tensor_tensor(out=a1, in0=rr[:, h, c:c + 1], in1=cs[:, h, 63:64], op=ALU.subtract), reads=[r_rr, r_cs], updates=[r_aa])
                        S.op("vector", lambda e, a2=a2, h=h, c=c: e.tensor_tensor(out=a2, in0=cs[:, h, c * 64 + 63:c * 64 + 64], in1=rr[:, h, c:c + 1], op=ALU.subtract), reads=[r_rr, r_cs], updates=[r_aa])
                    else:
                        S.op("vector", lambda e, a1=a1, h=h, c=c: e.tensor_tensor(out=a1, in0=rr[:, h, c:c + 1], in1=cs[:, h, c * 64 + 63:c * 64 + 64], op=ALU.add), reads=[r_rr, r_cs], updates=[r_aa])
                        if c == 0:
                            S.op("vector", lambda e, a2=a2, h=h, c=c: e.tensor_scalar(out=a2, in0=rr[:, h, c:c + 1], scalar1=-1.0, scalar2=None, op0=ALU.mult), reads=[r_rr], updates=[r_aa])
                        else:
                            S.op("vector", lambda e, a2=a2, h=h, c=c: e.scalar_tensor_tensor(out=a2, in0=rr[:, h, c:c + 1], scalar=-1.0, in1=cs[:, h, 63:64], op0=ALU.mult, op1=ALU.subtract),
                                 reads=[r_rr, r_cs], updates=[r_aa])
            S.op("scalar", lambda e: e.activation(out=aa[:, :, :, 0:2], in_=aa[:, :, :, 0:2], func=AF.Exp), reads=[], updates=[r_aa])
            S.op("vector", lambda e: e.tensor_tensor(out=aa[:, :, :, 2], in0=aa[:, :, :, 0], in1=aa[:, :, :, 1], op=ALU.mult), reads=[], updates=[r_aa])
            for h in range(4):
                for c in range(2):
                    S.op("vector", lambda e, h=h, c=c, u=u: e.tensor_scalar(out=E1[:, h, c * 64:(c + 1) * 64], in0=u[:, h, c * 64:(c + 1) * 64], scalar1=rr[:, h, c:c + 1], scalar2=None, op0=ALU.subtract),
                         reads=[r_u, r_rr], updates=[r_E1] if (h or c) else (), writes=() if (h or c) else [r_E1])
            S.op("scalar", lambda e: e.activation(out=E2[:], in_=E1[:], func=AF.Exp, scale=-1.0), reads=[r_E1], writes=[r_E2])
            S.op("scalar", lambda e: e.activation(out=E1[:], in_=E1[:], func=AF.Exp), reads=[r_E2], updates=[r_E1])
            for c in range(2):
                cs_ = slice(c * 64, (c + 1) * 64)
                S.op("vector", lambda e, c=c, cs_=cs_: e.tensor_tensor(out=qtP[:, :, c, cs_], in0=qsb[:, :, cs_], in1=E1[:, :, cs_], op=ALU.mult), reads=[r_qsb, r_E1], updates=[r_qtP])
                S.op("vector", lambda e, c=c, cs_=cs_: e.tensor_tensor(out=ktP[:, :, c, cs_], in0=kk[:, :, cs_], in1=E2[:, :, cs_], op=ALU.mult), reads=[r_kk, r_E2], updates=[r_ktP])
            if debug and dr == 0 and t == 0:
                dtile, r_dt = C.sb([128, 8, 512], F32, "dtile")
                S.op("gpsimd", lambda e: e.memset(dtile[:], 0.0), writes=[r_dt])
                S.op("vector", lambda e: e.tensor_copy(out=dtile[:, 0, 0:4], in_=lb[:]), reads=[r_lb], updates=[r_dt])
                S.op("vector", lambda e: e.tensor_copy(out=dtile[:, 1, :], in_=ff[:].rearrange("p a b -> p (a b)")), reads=[r_ff], updates=[r_dt])
                S.op("vector", lambda e: e.tensor_copy(out=dtile[:, 2, :], in_=cs[:].rearrange("p a b -> p (a b)")), reads=[r_cs], updates=[r_dt])
                S.op("vector", lambda e: e.tensor_copy(out=dtile[:, 3, :], in_=E1[:].rearrange("p a b -> p (a b)")), reads=[r_E1], updates=[r_dt])
                S.op("vector", lambda e: e.tensor_copy(out=dtile[:, 4, :], in_=E2[:].rearrange("p a b -> p (a b)")), reads=[r_E2], updates=[r_dt])
                S.op("vector", lambda e: e.tensor_copy(out=dtile[:, 5, 0:24], in_=aa[:].rearrange("p a b c -> p (a b c)")), reads=[r_aa], updates=[r_dt])
                S.op("vector", lambda e: e.tensor_copy(out=dtile[:, 6, :], in_=qtP[:, 0:2, :, :].rearrange("p a b c -> p (a b c)")), reads=[r_qtP], updates=[r_dt])
                S.op("vector", lambda e: e.tensor_copy(out=dtile[:, 7, :], in_=pQ[:].rearrange("p a b -> p (a b)")), reads=[r_pQ], updates=[r_dt])
                S.dma(dbg, dtile[:], reads=[r_dt])
            yield
            corder = [0, 1] if dr == 0 else [1, 0]
            kx = kcnt[0]
            kcnt[0] += 1
            Sp, r_Sp = Spp[kx % 2]
            PT, r_PT = PT_l[kx % 2]
            kTM, r_kTM = kTM_l[kx % 2]
            tU, r_tU = tU_l[kx % 2]
            first = True
            for h in range(4):
                for c in range(2):
                    S.op("tensor", lambda e, c=c, h=h: e.matmul(out=pS[:, h, c * 64:(c + 1) * 64], lhsT=ktP[:, h, c, :], rhs=qtP[:, h, c, c * 64:(c + 1) * 64], start=True, stop=True),
                         reads=[r_ktP, r_qtP], updates=() if first else [r_pS], writes=[r_pS] if first else ())
                    first = False
            S.op("vector", lambda e, dr=dr: e.tensor_tensor(out=PT[:], in0=pS[:], in1=cm[:, dr:dr + 1, :].broadcast_to([128, 4, 128]), op=ALU.mult), reads=[r_pS, r_cm], writes=[r_PT])
            first = True
            for h in range(4):
                for c in range(2):
                    S.op("tensor", lambda e, c=c, h=h: e.transpose(out=pK[:, h, c, :], in_=ktP[:, h, c, :], identity=idb[:]), reads=[r_ktP, r_idb],
                         updates=() if first else [r_pK], writes=[r_pK] if first else ())
                    first = False
            S.op("scalar", lambda e: e.copy(out=kTM[:], in_=pK[:]), reads=[r_pK], writes=[r_kTM])
            for ci, c in enumerate(corder):
                for h in range(4):
                    S.op("tensor", lambda e, c=c, h=h: e.matmul(out=pUu[:, h, :], lhsT=kTM[:, h, c, :], rhs=vb[:, h * 128:(h + 1) * 128], start=True, stop=True),
                         reads=[r_kTM, r_vb], updates=[r_pUu] if h else (), writes=() if h else [r_pUu])
                a1b = aa[:, :, c, 0:1].broadcast_to([128, 4, 128])
                a2b = aa[:, :, c, 1:2].broadcast_to([128, 4, 128])
                a3b = aa[:, :, c, 2:3].broadcast_to([128, 4, 128])
                S.op("vector", lambda e, c=c, a1b=a1b: e.tensor_tensor(out=Sp[:, c, :, :], in0=Sall[:], in1=a1b, op=ALU.mult),
                     reads=[r_Sall, r_aa], updates=[r_Sp] if ci else (), writes=() if ci else [r_Sp])
                S.op("vector", lambda e, a2b=a2b: e.tensor_tensor(out=tU[:], in0=pUu[:], in1=a2b, op=ALU.mult), reads=[r_pUu, r_aa], writes=[r_tU])
                S.op("vector", lambda e, a3b=a3b: e.tensor_tensor(out=Sall[:], in0=Sall[:], in1=a3b, op=ALU.mult), reads=[r_aa], writes=[r_Sall])
                S.op("gpsimd", lambda e: e.tensor_tensor(out=Sall[:], in0=Sall[:], in1=tU[:], op=ALU.add), reads=[r_tU], writes=[r_Sall])
            for h in range(4):
                vv = vb[:, h * 128:(h + 1) * 128]
                S.op("tensor", lambda e, vv=vv, h=h: e.matmul(out=pOo[:, h, :], lhsT=PT[:, h, :], rhs=vv, start=True, stop=False), reads=[r_PT, r_vb],
                     updates=[r_pOo] if h else (), writes=() if h else [r_pOo])
                for c in range(2):
                    S.op("tensor", lambda e, c=c, h=h: e.matmul(out=pOo[:, h, :], lhsT=qtP[:, h, c, :], rhs=Sp[:, c, h, :], start=False, stop=(c == 1)), reads=[r_qtP, r_Sp], updates=[r_pOo])
            pOf = pOo[:].rearrange("p h e -> p (h e)")
            if dr == 0:
                S.op("scalar", lambda e, t=t: e.copy(out=ofw[:, t, :], in_=pOf), reads=[r_pOo], writes=[r_ofw[t]])
            else:
                S.op("vector", lambda e, t=t: e.tensor_tensor(out=otot[:], in0=pOf, in1=ofw[:, t, :], op=ALU.add), reads=[r_pOo, r_ofw[t]], writes=[r_otot])
            if debug and dr == 0 and t == 0:
                dt2, r_dt2 = C.sb([128, 8, 512], F32, "dtile2")
                S.op("gpsimd", lambda e: e.memset(dt2[:], 0.0), writes=[r_dt2])
                S.op("vector", lambda e: e.tensor_copy(out=dt2[:, 0, 0:128], in_=PT[:]), reads=[r_PT], updates=[r_dt2])
                S.op("vector", lambda e: e.tensor_copy(out=dt2[:, 1, 0:128], in_=pOo[:]), reads=[r_pOo], updates=[r_dt2])
                S.op("vector", lambda e: e.tensor_copy(out=dt2[:, 2, :], in_=ofw[:, 0, :]), reads=[r_ofw[0]], updates=[r_dt2])
                S.op("vector", lambda e: e.tensor_copy(out=dt2[:, 3, :], in_=vb[:]), reads=[r_vb], updates=[r_dt2])
                S.op("vector", lambda e: e.tensor_copy(out=dt2[:, 4, 0:128], in_=pS[:]), reads=[r_pS], updates=[r_dt2])
                S.op("vector", lambda e: e.tensor_copy(out=dt2[:, 5, 0:256], in_=kTM[:].rearrange("p a b -> p (a b)")), reads=[r_kTM], updates=[r_dt2])
                S.op("vector", lambda e: e.tensor_copy(out=dt2[:, 6, 0:128], in_=Sst[3][0][:]), reads=[Sst[3][1]], updates=[r_dt2])
                S.dma(dbg2, dt2[:], reads=[r_dt2])
            if dr == 1:
                m_t, r_m = mgt[oi % 2]
                S.op("gpsimd", lambda e: e.memset(ssh[:], 0.0), writes=[r_ssh])
                for h in range(4):
                    hs = slice(h * 128, (h + 1) * 128)
                    S.op("scalar", lambda e, hs=hs, h=h: e.activation(out=jk[:], in_=otot[:, hs], func=AF.Square, accum_out=ssh[:, h:h + 1]), reads=[r_otot], writes=[r_jk], updates=[r_ssh])
                S.op("vector", lambda e: e.tensor_scalar(out=ssh[:], in0=ssh[:], scalar1=1.0 / 128, scalar2=EPS, op0=ALU.mult, op1=ALU.add), reads=[r_ssh], writes=[r_ssh])
                S.op("scalar", lambda e: e.activation(out=ssh[:], in_=ssh[:], func=AF.Sqrt), reads=[r_ssh], writes=[r_ssh])
                S.op("vector", lambda e: e.reciprocal(out=ssh[:], in_=ssh[:]), reads=[r_ssh], writes=[r_ssh])
                for h in range(4):
                    hs = slice(h * 128, (h + 1) * 128)
                    S.op("vector", lambda e, hs=hs, h=h: e.scalar_tensor_tensor(out=otot[:, hs], in0=otot[:, hs], scalar=ssh[:, h:h + 1], in1=hn[:], op0=ALU.mult, op1=ALU.mult),
                         reads=[r_ssh, r_hn], updates=[r_otot])
                S.op("vector", lambda e, m_t=m_t: e.tensor_tensor(out=m_t[:], in0=otot[:], in1=sgg[:], op=ALU.mult), reads=[r_otot, r_sgg], writes=[r_m])
                S.dma(mgo[t * 128:(t + 1) * 128, :], m_t[:], reads=[r_m])

        for dr in range(2):
            order = list(range(NT)) if dr == 0 else [1, 0] + list(range(NT - 1, 1, -1))
            zc0 = 512 + dr * 512
            S.op("vector", lambda e: e.memset(Sall[:].rearrange("p a b -> p (a b)"), 0.0), writes=[r_Sall])
            prev_g = None
            for oi, t in enumerate(order):
                g_ = do_tile(oi, t, dr, zc0)
                next(g_)
                if prev_g is not None:
                    next(prev_g, None)
                prev_g = g_
            next(prev_g, None)


def odd_consts():
    p = np.arange(128)[:, None]
    f = np.arange(128)[None, :]
    same = (p // 64) == (f // 64)
    cmat = np.stack([(same & (p <= f)), (same & (p >= f))], 1).astype(np.float32)
    return dict(cmat=np.ascontiguousarray(cmat))


def emit_mod(C, cv, aws, abs_, modd):
    nc, S = C.nc, C.S
    with C.scope():
        ct, r_ct = C.sb([128, 8, 2], F32, "ct")
        for b_ in range(2):
            S.dma(ct[:, :, b_], cv[b_, :].rearrange("(c p) -> p c", p=128), updates=[r_ct], allow_slow_non_contiguous=True)
        sc, r_sc = C.sb([128, 8, 2], F32, "sc")
        S.op("scalar", lambda e: e.activation(out=sc[:], in_=ct[:], func=AF.Silu), reads=[r_ct], writes=[r_sc])
        wts = [C.sb([128, 8, 1536], F32, "aw%d" % i) for i in range(2)]
        bt, r_bt = C.sb([2, 6144], F32, "bt")
        ots = [C.sb([2, 6144], F32, "ot%d" % i) for i in range(2)]
        pss = [C.ps([2, 512], F32, "pm%d" % i) for i in range(2)]
        k = 0
        for l in range(DEPTH):
            ot, r_ot = ots[l % 2]
            S.dma(bt[:], abs_[l].partition_broadcast(2), writes=[r_bt])
            for cb in range(4):
                w, r_w = wts[k % 2]
                for c in range(8):
                    S.dma(w[:, c, :], aws[l][c * 128:(c + 1) * 128, cb * 1536:(cb + 1) * 1536], updates=[r_w] if c else (), writes=() if c else [r_w],
                          eng=("sync" if c % 2 == 0 else "gpsimd"))
                for j in range(3):
                    p, r_p = pss[(k * 3 + j) % 2]
                    for c in range(8):
                        S.op("tensor", lambda e, c=c, j=j, p=p, w=w: e.matmul(out=p[:], lhsT=sc[:, c, :], rhs=w[:, c, j * 512:(j + 1) * 512], start=(c == 0), stop=(c == 7)),
                             reads=[r_sc, r_w], updates=[r_p] if c else (), writes=() if c else [r_p])
                    col = cb * 1536 + j * 512
                    S.op("vector", lambda e, p=p, col=col, ot=ot: e.tensor_tensor(out=ot[:, col:col + 512], in0=p[:], in1=bt[:, col:col + 512], op=ALU.add),
                         reads=[r_p, r_bt], updates=[r_ot])
                k += 1
            S.dma(modd[l], ot[:], reads=[r_ot])


def emit_final(C, xsrc, fnw, out):
    nc, S = C.nc, C.S
    with C.scope():
        fw_t, r_fw = C.sb([128, D], F32, "fnw_sb")
        S.dma(fw_t[:], fnw.partition_broadcast(128), writes=[r_fw])
        ssf, r_ssf = C.sb([128, 1], F32, "ssf")
        jk, r_jk = C.sb([128, D], F32, "jk")
        xt = [C.sb([128, D], F32, "fx%d" % i) for i in range(2)]
        ob = [C.sb([128, D], F32, "ob%d" % i) for i in range(2)]
        for t in range(32):
            x_t, r_x = xt[t % 2]
            o_t, r_o = ob[t % 2]
            S.dma(x_t[:], xsrc[256 + t * 128:256 + (t + 1) * 128, :], reads=[C.r_xf[(t + 2) // 8]], writes=[r_x])
            S.op("gpsimd", lambda e: e.memset(ssf[:], 0.0), writes=[r_ssf])
            S.op("scalar", lambda e, x_t=x_t: e.activation(out=jk[:], in_=x_t[:], func=AF.Square, accum_out=ssf[:]), reads=[r_x], writes=[r_jk], updates=[r_ssf])
            S.op("vector", lambda e: e.tensor_scalar(out=ssf[:], in0=ssf[:], scalar1=1.0 / D, scalar2=EPS, op0=ALU.mult, op1=ALU.add), reads=[r_ssf], writes=[r_ssf])
            S.op("scalar", lambda e: e.activation(out=ssf[:], in_=ssf[:], func=AF.Sqrt), reads=[r_ssf], writes=[r_ssf])
            S.op("vector", lambda e: e.reciprocal(out=ssf[:], in_=ssf[:]), reads=[r_ssf], writes=[r_ssf])
            S.op("vector", lambda e, x_t=x_t, o_t=o_t: e.scalar_tensor_tensor(out=o_t[:], in0=x_t[:], scalar=ssf[:, 0:1], in1=fw_t[:], op0=ALU.mult, op1=ALU.mult),
                 reads=[r_x, r_ssf, r_fw], writes=[r_o])
            S.dma(out[t * 128:(t + 1) * 128, :], o_t[:], reads=[r_o], eng="gpsimd")


GROUPS = [[0, 1], [2, 3], [4, 5], [6, 7]]


def build_fused(n_layers=DEPTH, dbg_out=False):
    C = Ctx()
    nc, S = C.nc, C.S
    NTOK = 34 * 128
    xs_in = C.din("xs", [NTOK, D])
    cv = C.din("cv", [2, D])
    aws = [C.din("aw%d" % l, [D, 6144]) for l in range(DEPTH)]
    abs_ = [C.din("ab%d" % l, [1, 6144]) for l in range(DEPTH)]
    nwa = C.din("nwa", [2 * DEPTH, D])
    fnw = C.din("fnw", [1, D])
    wce = [C.din("wce%d" % p, [D, 1536]) for p in range(2)]
    draw = [C.din("draw%d" % p, [1, 4]) for p in range(2)]
    sink = [C.din("sink%d" % p, [1, 2]) for p in range(2)]
    wco = [C.din("wco%d" % p, [D, 2560]) for p in range(2)]
    lbl = C.din("lbl", [4, 512])
    lsel = [C.din("lsel%d" % p, [1, 4]) for p in range(2)]
    hnw = [C.din("hnw%d" % p, [1, 128]) for p in range(2)]
    wo = [C.din("wo%d" % l, [D, D]) for l in range(DEPTH)]
    ropec = C.din("ropec", [NTOK, 128])
    ropes = C.din("ropes", [NTOK, 128])
    cmat_e = C.din("cmat_e", [128, 6, 128])
    crow = C.din("crow", [1, 256])
    ccol = C.din("ccol", [128, 2])
    cmat_o = C.din("cmat_o", [128, 2, 128])
    wr = [C.din("wr%d" % l, [D, 36]) for l in range(DEPTH)]
    br = [C.din("br%d" % l, [1, 36]) for l in range(DEPTH)]
    w1 = [C.din("w1_%d" % l, [16, D, 512]) for l in range(DEPTH)]
    w3 = [C.din("w3_%d" % l, [16, D, 512]) for l in range(DEPTH)]
    w2 = [C.din("w2_%d" % l, [16, 512, D]) for l in range(DEPTH)]
    out = C.dout("out", [L, D])
    if dbg_out:
        xdbg = C.dout("xdbg", [NTOK, D])
    xfull = C.dram("xfull", [NTOK, D])
    ypart = C.dram("ypart", [NTOK, D])
    mgl = C.dram("mgl", [NTOK, 512], BF16)
    mgall = C.dram("mgall", [2 * NTOK, 512], BF16)
    moddt = C.dram("modd", [DEPTH, 2, 6144])
    modd = [moddt[l] for l in range(DEPTH)]
    with C.stack:
        C.prefix = "cp_"
        for t in range(34):
            S.dma(xfull[t * 128:(t + 1) * 128, :], xs_in[t * 128:(t + 1) * 128, :], updates=[C.r_xf[t // 8]], eng=("sync" if t % 2 == 0 else "gpsimd"))
        C.prefix = "mod_"
        emit_mod(C, cv, aws, abs_, modd)
        for l in range(n_layers):
            p = l // 2
            modv = modd[l].rearrange("s (k d) -> s k d", k=6)
            C.prefix = "L%dmix_" % l
            if l % 2 == 0:
                emit_even(C, dict(xs=xfull, modv=modv, nw=nwa[2 * l:2 * l + 1, :], wc=wce[p], ropec=ropec, ropes=ropes, draw=draw[p], sink=sink[p],
                                  cmat=cmat_e, crow=crow, ccol=ccol, mgo=mgl))
            else:
                emit_odd(C, dict(xs=xfull, modv=modv, nw=nwa[2 * l:2 * l + 1, :], wc=wco[p], lbl=lbl, lsel=lsel[p], hnw=hnw[p], cmat=cmat_o, mgo=mgl))
            ag = []
            for st_k in range(0, NTOK, 2048):
                R_k = min(2048, NTOK - st_k)
                ag.append(lambda g, st_k=st_k, R_k=R_k: g.collective_compute("AllGather", ALU.bypass, replica_groups=GROUPS, ins=[mgl[st_k:st_k + R_k, :].opt()],
                                                                              outs=[mgall[2 * st_k:2 * st_k + 2 * R_k, :].opt()]))
            S.collective(ag)
            def ar_chunk(k_):
                st_k = k_ * 1024
                R_k = min(1024, NTOK - st_k)
                S.cc_async(lambda g, st_k=st_k, R_k=R_k: g.collective_compute("AllReduce", ALU.add, replica_groups=GROUPS, ins=[ypart[st_k:st_k + R_k, :].opt()],
                                                                               outs=[xfull[st_k:st_k + R_k, :].opt()]),
                           reads=[C.r_yp[k_]], writes=[C.r_xf[k_]])
            for half in range(2):
                C.prefix = "L%dmoe%d_" % (l, half)
                emit_moe(C, dict(xs=xfull, mgall=mgall, wo=wo[l], modv=modv, nw=nwa[2 * l + 1:2 * l + 2, :], wr=wr[l], br=br[l], w1=w1[l], w3=w3[l], w2=w2[l], ypart=ypart),
                         tiles=list(range(half * 17, (half + 1) * 17)))
                for k_ in ([0, 1] if half == 0 else [2, 3, 4]):
                    ar_chunk(k_)
        C.prefix = "fin_"
        emit_final(C, xfull, fnw, out)
        if dbg_out:
            for t in range(34):
                S.dma(xdbg[t * 128:(t + 1) * 128, :], xfull[t * 128:(t + 1) * 128, :], reads=[C.r_xf[t // 8]])
        S.emit()
    return nc


_NC = {}


def make_in_maps(x, c, ctx, c_ctx, ada_w, ada_b, norm_w, final_norm_w, ev_w_in, ev_w_out, ret_decay_raw, att_sink,
                 od_w_in, od_w_out, hg_lb_logits, hg_norm_w, moe_wg, moe_bg, moe_we, moe_be, moe_w1, moe_w3, moe_w2):
    f32 = np.float32
    A_ = lambda v: np.ascontiguousarray(np.asarray(v, dtype=f32))
    g = {k: np.asarray(v, dtype=f32) for k, v in dict(x=x, c=c, ctx=ctx, c_ctx=c_ctx, ada_w=ada_w, ada_b=ada_b, norm_w=norm_w, final_norm_w=final_norm_w,
                                                         ev_w_in=ev_w_in, ev_w_out=ev_w_out, ret_decay_raw=ret_decay_raw, att_sink=att_sink, od_w_in=od_w_in,
                                                         od_w_out=od_w_out, hg_lb_logits=hg_lb_logits, hg_norm_w=hg_norm_w, moe_wg=moe_wg, moe_bg=moe_bg,
                                                         moe_we=moe_we, moe_be=moe_be, moe_w1=moe_w1, moe_w3=moe_w3, moe_w2=moe_w2).items()}
    ec = even_consts()
    oc = odd_consts()
    shared = dict(nwa=A_(g['norm_w'].reshape(2 * DEPTH, D)), fnw=A_(g['final_norm_w'][None]), ropec=ec['ropec'], ropes=ec['ropes'], cmat_e=ec['cmat'],
                  crow=ec['crow'], ccol=ec['ccol'], cmat_o=oc['cmat'])
    for l in range(DEPTH):
        shared["aw%d" % l] = A_(g['ada_w'][l])
        shared["ab%d" % l] = A_(g['ada_b'][l][None])
    per_s = []
    for s in range(2):
        d = {}
        for p in range(2):
            win = g['ev_w_in'][p]
            d["wce%d" % p] = A_(np.concatenate([win[:, s * 256:(s + 1) * 256], win[:, 512 + s * 256:512 + (s + 1) * 256],
                                                win[:, 2048 + s * 256:2048 + (s + 1) * 256], win[:, 2560 + s * 128:2560 + (s + 1) * 128],
                                                win[:, 1024 + s * 256:1024 + (s + 1) * 256], win[:, 1536 + s * 256:1536 + (s + 1) * 256],
                                                win[:, 2816 + s * 128:2816 + (s + 1) * 128]], 1))
            d["draw%d" % p] = A_(g['ret_decay_raw'][p][:, s * 2:(s + 1) * 2].reshape(1, 4))
            d["sink%d" % p] = A_(g['att_sink'][p][s * 2:(s + 1) * 2][None])
            wino = g['od_w_in'][p]
            d["wco%d" % p] = A_(np.concatenate([wino[:, k * 1024 + s * 512:k * 1024 + (s + 1) * 512] for k in range(5)], 1))
            lo = 2 * p + 1
            d["lsel%d" % p] = np.array([[0.0] + [1.0 if m <= lo else 0.0 for m in range(1, 4)]], f32)
            d["hnw%d" % p] = A_(g['hg_norm_w'][p][None])
        d["lbl"] = A_(g['hg_lb_logits'][:, s * 512:(s + 1) * 512])
        go = [2 * s, 2 * s + 1, 2 * (1 - s), 2 * (1 - s) + 1]
        for l in range(DEPTH):
            d["wr%d" % l] = A_(np.concatenate([g['moe_wg'][l][:, go]] + [g['moe_we'][l][gi] for gi in go], 1))
            d["br%d" % l] = A_(np.concatenate([g['moe_bg'][l][go]] + [g['moe_be'][l][gi] for gi in go])[None])
            d["w1_%d" % l] = A_(g['moe_w1'][l][s * 16:(s + 1) * 16])
            d["w3_%d" % l] = A_(g['moe_w3'][l][s * 16:(s + 1) * 16])
            d["w2_%d" % l] = A_(g['moe_w2'][l][s * 16:(s + 1) * 16])
        per_s.append(d)
    for l in range(DEPTH):
        if l % 2 == 0:
            w = g['ev_w_out'][l // 2]
            shared["wo%d" % l] = A_(np.concatenate([w[0:256], w[512:768], w[256:512], w[768:1024]], 0))
        else:
            shared["wo%d" % l] = A_(g['od_w_out'][l // 2])
    maps = []
    for i in range(8):
        b, s = i // 2, i % 2
        d = dict(shared)
        d.update(per_s[s])
        d["xs"] = A_(np.concatenate([g['ctx'][b], g['x'][b]], 0))
        d["cv"] = A_(np.stack([g['c_ctx'], g['c'][b]]))
        maps.append(d)
    return maps


def kernel(x, c, ctx, c_ctx, ada_w, ada_b, norm_w, final_norm_w, ev_w_in, ev_w_out, ret_decay_raw, att_sink,
           od_w_in, od_w_out, hg_lb_logits, hg_norm_w, moe_wg, moe_bg, moe_we, moe_be, moe_w1, moe_w3, moe_w2):
    if "fused" not in _NC:
        _NC["fused"] = build_fused()
    maps = make_in_maps(x, c, ctx, c_ctx, ada_w, ada_b, norm_w, final_norm_w, ev_w_in, ev_w_out, ret_decay_raw, att_sink,
                        od_w_in, od_w_out, hg_lb_logits, hg_norm_w, moe_wg, moe_bg, moe_we, moe_be, moe_w1, moe_w3, moe_w2)
    res = run_bass_kernel_spmd(_NC["fused"], maps, core_ids=list(range(8)))
    return np.stack([res.results[2 * b]["out"] for b in range(B)], 0).astype(np.float32)
```

```python
import contextlib
import numpy as np
import ml_dtypes
import concourse.bass as bass
import concourse.mybir as mybir
from concourse.bass_utils import run_bass_kernel_spmd

F32 = mybir.dt.float32
BF16 = mybir.dt.bfloat16
ALU = mybir.AluOpType
AF = mybir.ActivationFunctionType
AX = mybir.AxisListType
ENGINES = ("tensor", "vector", "scalar", "gpsimd", "sync")

D = 1024
L = 4096
NCTX = 256
B = 4
DEPTH = 4
EPS = 1e-6
NEG = -1e30


class Res:
    __slots__ = ("name", "writers", "readers")

    def __init__(self, name=""):
        self.name = name
        self.writers = []
        self.readers = []


class Op:
    __slots__ = ("eng", "fn", "deps", "sig", "sigval", "is_dma", "dsem", "dval", "cc", "ccval")

    def __init__(self, eng, fn, is_dma=False):
        self.eng = eng
        self.fn = fn
        self.deps = []
        self.sig = False
        self.sigval = 0
        self.is_dma = is_dma
        self.dsem = None
        self.dval = 0
        self.cc = False
        self.ccval = 0


class Sched:
    def __init__(self, nc, n_dma_sems=32):
        self.nc = nc
        self.ops = {e: [] for e in ENGINES}
        self.n_dma_sems = n_dma_sems
        self.dma_rr = 0
        self.dma_last = [None] * n_dma_sems
        self.dma_cnt = [0] * n_dma_sems
        self.nops = 0
        self.cc_count = 0

    def _add_dep(self, op, dep):
        if dep is None or dep is op:
            return
        if dep.eng == op.eng and not dep.is_dma and not op.is_dma and op.eng == "tensor":
            return
        if dep not in op.deps:
            op.deps.append(dep)
            dep.sig = True

    def op(self, eng, fn, reads=(), writes=(), updates=(), is_dma=False):
        o = Op(eng, fn, is_dma)
        for r in reads:
            for w in r.writers:
                self._add_dep(o, w)
        for r in list(writes) + list(updates):
            for w in r.writers:
                self._add_dep(o, w)
            for rd in r.readers:
                if rd.eng == eng and not rd.is_dma and not is_dma:
                    continue
                self._add_dep(o, rd)
        if is_dma:
            k = self.dma_rr
            self.dma_rr = (self.dma_rr + 1) % self.n_dma_sems
            self._add_dep(o, self.dma_last[k])
            self.dma_cnt[k] += 16
            o.dsem = k
            o.dval = self.dma_cnt[k]
            self.dma_last[k] = o
        for r in reads:
            r.readers = [x for x in r.readers if not (x.eng == eng and not x.is_dma and not is_dma)] + [o]
        for r in writes:
            r.writers = [o]
            r.readers = []
        for r in updates:
            r.writers = [x for x in r.writers if not (x.eng == eng and not x.is_dma and not is_dma)] + [o]
            r.readers = []
        self.ops[eng].append(o)
        self.nops += 1
        return o

    def barrier(self):
        lasts = []
        for e in ENGINES:
            for o in reversed(self.ops[e]):
                if not o.is_dma and not o.cc:
                    lasts.append(o)
                    break
        lasts += [o for o in self.dma_last if o is not None]
        for e in ENGINES:
            o = Op(e, lambda eng: eng.nop())
            for d in lasts:
                if d.eng == e and not d.is_dma:
                    continue
                if d not in o.deps:
                    o.deps.append(d)
                    d.sig = True
            self.ops[e].append(o)

    def collective(self, fn):
        self.barrier()
        fns = fn if isinstance(fn, (list, tuple)) else [fn]
        for f in fns:
            o = Op("gpsimd", f)
            o.cc = True
            self.cc_count += 1
            o.ccval = self.cc_count
            self.ops["gpsimd"].append(o)
        for e in ENGINES:
            w = Op(e, lambda eng: eng.nop())
            w.deps.append(o)
            self.ops[e].append(w)

    def cc_async(self, fn, reads=(), writes=()):
        o = self.op("gpsimd", fn, reads=reads, writes=writes)
        o.cc = True
        self.cc_count += 1
        o.ccval = self.cc_count
        return o

    def dma(self, out, in_, reads=(), writes=(), updates=(), eng="sync", **kw):
        return self.op(eng, lambda e: e.dma_start(out=out, in_=in_, **kw), reads, writes, updates, is_dma=True)

    def emit(self):
        nc = self.nc
        cnt = {e: 0 for e in ENGINES}
        for e in ENGINES:
            for o in self.ops[e]:
                if o.sig and not o.is_dma and not o.cc:
                    cnt[e] += 1
                    o.sigval = cnt[e]
        with contextlib.ExitStack() as st:
            esem = {e: st.enter_context(nc.semaphore("s_" + e)) for e in ENGINES}
            dsem = [st.enter_context(nc.semaphore("d_%d" % i)) for i in range(self.n_dma_sems)]
            ccsem = st.enter_context(nc.semaphore("s_cc"))
            block = st.enter_context(nc.Block())

            def make(ename):
                def body(eng):
                    waited = {}
                    for o in self.ops[ename]:
                        for d in o.deps:
                            if d.cc:
                                key, val, sem = ("c", 0), d.ccval, ccsem
                            elif d.is_dma:
                                key, val, sem = ("d", d.dsem), d.dval, dsem[d.dsem]
                            else:
                                key, val, sem = ("e", d.eng), d.sigval, esem[d.eng]
                            if waited.get(key, 0) < val:
                                eng.wait_ge(sem, val)
                                waited[key] = val
                        ins = o.fn(eng)
                        if o.cc:
                            ins.then_inc(ccsem, 1)
                        elif o.is_dma:
                            ins.then_inc(dsem[o.dsem], 16)
                        elif o.sig:
                            ins.then_inc(esem[ename], 1)
                    if ename == "sync":
                        for k in range(self.n_dma_sems):
                            if self.dma_cnt[k] > 0:
                                eng.wait_ge(dsem[k], self.dma_cnt[k])
                        for e2 in ENGINES:
                            if e2 != "sync" and cnt[e2] > 0:
                                eng.wait_ge(esem[e2], cnt[e2])
                return body

            for e in ENGINES:
                getattr(block, e)(make(e))


class Ctx:
    def __init__(self, name=""):
        self.nc = bass.Bass("TRN2", target_bir_lowering=False)
        self.S = Sched(self.nc)
        self.stack = contextlib.ExitStack()
        self.n = 0
        self.prefix = ""
        self.r_xf = [Res("xf%d" % k) for k in range(5)]
        self.r_yp = [Res("yp%d" % k) for k in range(5)]

    @contextlib.contextmanager
    def scope(self):
        saved = self.stack
        st = contextlib.ExitStack()
        self.stack = st
        with st:
            yield
        self.stack = saved
        self.S.barrier()

    def dram(self, name, shape, dt=F32):
        return self.nc.dram_tensor(name, list(shape), dt).ap()

    def sb(self, shape, dt, name=None):
        self.n += 1
        nm = self.prefix + (name or "sb") + "_%d" % self.n
        t = self.stack.enter_context(self.nc.sbuf_tensor(nm, list(shape), dt))
        return t, Res(nm)

    def ps(self, shape, dt, name=None):
        self.n += 1
        full = 512 if dt == F32 else 1024
        nm = self.prefix + (name or "ps") + "_%d" % self.n
        t = self.stack.enter_context(self.nc.psum_tensor(nm, [128, full], dt))
        n = int(np.prod(shape[1:]))
        v = t[0:shape[0], 0:n]
        if len(shape) == 3:
            v = v.rearrange("p (a b) -> p a b", a=shape[1])
        return v, Res(name or ("ps%d" % self.n))

    def din(self, name, shape, dt=F32):
        return self.nc.dram_tensor(name, list(shape), dt, kind="ExternalInput").ap()

    def dout(self, name, shape, dt=F32):
        return self.nc.dram_tensor(name, list(shape), dt, kind="ExternalOutput").ap()


def make_identity(C, dt=BF16):
    S = C.S
    idf, r_idf = C.sb([128, 128], F32)
    S.op("gpsimd", lambda e: e.memset(idf[:], 0.0), writes=[r_idf])
    S.op("gpsimd", lambda e: e.affine_select(out=idf[:], in_=idf[:], pattern=[[-1, 128]], compare_op=ALU.not_equal,
                                             fill=1.0, base=0, channel_multiplier=1), updates=[r_idf])
    if dt == F32:
        return idf, r_idf
    idb, r_idb = C.sb([128, 128], dt)
    S.op("vector", lambda e: e.tensor_copy(out=idb[:], in_=idf[:]), reads=[r_idf], writes=[r_idb])
    return idb, r_idb, idf, r_idf


def emit_moe(C, A, tiles, NEXP=16):
    nc, S = C.nc, C.S
    NT = len(tiles)
    NTOK = NT * 128
    skip_outproj = False
    xs, mgall, wo, modv, nw, wr, br, w1, w3, w2, ypart = (A[k] for k in "xs mgall wo modv nw wr br w1 w3 w2 ypart".split())
    with C.scope():
        idb, r_idb, idf, r_idf = make_identity(C)
        acc, _ = C.sb([128, NT, D], F32, "acc")
        r_acc = [[Res("acc%d_%d" % (t, h)) for h in range(2)] for t in range(NT)]
        h2T, _ = C.sb([128, 8, NTOK], BF16, "h2T")
        r_h2T = [Res("h2T%d" % t) for t in range(NT)]
        gates, _ = C.sb([128, NT, 32], F32, "gates")
        r_gates = [Res("g%d" % t) for t in range(NT)]
        gmlp = [C.sb([128, D], F32, "gmlp%d" % i) for i in range(2)]
        for i in range(2):
            S.dma(gmlp[i][0][:], modv[i, 5:6, :].partition_broadcast(128), writes=[gmlp[i][1]])
        wrt, r_wrt = C.sb([128, 8, 36], F32, "wrt")
        S.dma(wrt[:], wr.rearrange("(c p) j -> p c j", p=128), writes=[r_wrt])
        brt, r_brt = C.sb([128, 36], F32, "brt")
        S.dma(brt[:], br.partition_broadcast(128), writes=[r_brt])

        stA = contextlib.ExitStack()
        C_stack_saved = C.stack
        C.stack = stA
        with stA:
            wob, r_wob = C.sb([128, 8, D], BF16, "wob")
            if not skip_outproj:
                load_weight_bf16(C, wob, r_wob, wo, D)
            gmsa, r_gmsa = C.sb([128, D], F32, "gmsa")
            w2e, r_w2e = C.sb([128, D], F32, "w2e")
            sh2, r_sh2 = C.sb([128, D], F32, "sh2")
            nwt, r_nwt = C.sb([128, D], F32, "nwt")
            S.dma(nwt[:], nw.partition_broadcast(128), writes=[r_nwt])
            xt = [C.sb([128, D], F32, "xt%d" % i) for i in range(2)]
            mgt = [C.sb([128, D], BF16, "mgt%d" % i) for i in range(2)]
            mgT = [C.sb([128, 8, 128], BF16, "mgT%d" % i) for i in range(2)]
            junk, r_junk = C.sb([128, D], F32, "junk")
            tmpA_l = [C.sb([128, D], F32, "tmpA%d" % i) for i in range(2)]
            h2f_l = [C.sb([128, D], F32, "h2f%d" % i) for i in range(2)]
            h2b_l = [C.sb([128, D], BF16, "h2b%d" % i) for i in range(2)]
            h2Tf_l = [C.sb([128, 8, 128], F32, "h2Tf%d" % i) for i in range(2)]
            sm_l = [{k: C.sb([128, n], F32, "sm%d_" % i + k) for k, n in
                     dict(ss=1, rstd=1, lg=36, m4=1, ng=1, ex4=4, se=1, gp=1, oh=4, pen=4, msk=32, top8=8, sel=32, nt1=1, d21=1,
                          e21=1, coef=1, wf=32).items()} for i in range(2)]
            pT = [C.ps([128, 8, 128], BF16, "pT%d" % i) for i in range(2)]
            pTf = [C.ps([128, 4, 128], F32, "pTf%d" % i) for i in range(2)]
            pY = [C.ps([128, 512], F32, "pY%d" % i) for i in range(2)]
            pL, r_pL = C.ps([128, 36], F32, "pL")
            prev = [None]

            def do_tile(t):
                gt = tiles[t]
                tmpA, r_tmpA = tmpA_l[t % 2]
                h2f, r_h2f = h2f_l[t % 2]
                h2b, r_h2b = h2b_l[t % 2]
                h2Tf, r_h2Tf = h2Tf_l[t % 2]
                sm = sm_l[t % 2]
                si = 0 if gt < 2 else 1
                if si != prev[0]:
                    prev[0] = si
                    S.dma(gmsa[:], modv[si, 2:3, :].partition_broadcast(128), writes=[r_gmsa])
                    S.dma(sh2[:], modv[si, 3:4, :].partition_broadcast(128), writes=[r_sh2])
                    S.dma(w2e[:], modv[si, 4:5, :].partition_broadcast(128), writes=[r_w2e])
                    S.op("vector", lambda e: e.scalar_tensor_tensor(out=w2e[:], in0=w2e[:], scalar=1.0, in1=nwt[:], op0=ALU.add, op1=ALU.mult),
                         reads=[r_w2e, r_nwt], writes=[r_w2e])
                x_t, r_x = xt[t % 2]
                S.dma(x_t[:], xs[gt * 128:(gt + 1) * 128, :], reads=[C.r_xf[gt // 8]], writes=[r_x])
                a_t = acc[:, t, :]
                ra = r_acc[t]
                if not skip_outproj:
                    m_t, r_m = mgt[t % 2]
                    mT, r_mT = mgT[t % 2]
                    p_T, r_pT = pT[t % 2]
                    st_k = (gt // 16) * 2048
                    R_k = min(2048, 4352 - st_k)
                    r0 = 2 * st_k + gt * 128 - st_k
                    S.dma(m_t[:, 0:512], mgall[r0:r0 + 128, :], writes=[r_m], eng="gpsimd")
                    S.dma(m_t[:, 512:1024], mgall[r0 + R_k:r0 + R_k + 128, :], updates=[r_m], eng="gpsimd")
                    for c in range(8):
                        S.op("tensor", lambda e, c=c, p_T=p_T, m_t=m_t: e.transpose(out=p_T[:, c, :], in_=m_t[:, c * 128:(c + 1) * 128], identity=idb[:]),
                             reads=[r_m, r_idb], updates=[r_pT] if c else (), writes=() if c else [r_pT])
                    S.op("scalar", lambda e, mT=mT, p_T=p_T: e.copy(out=mT[:], in_=p_T[:]), reads=[r_pT], writes=[r_mT])
                    for hf in range(2):
                        p_Y, r_pY = pY[hf]
                        for c in range(8):
                            S.op("tensor", lambda e, c=c, hf=hf, p_Y=p_Y, mT=mT: e.matmul(out=p_Y[:], lhsT=mT[:, c, :], rhs=wob[:, c, hf * 512:(hf + 1) * 512],
                                                                                         start=(c == 0), stop=(c == 7)),
                                 reads=[r_mT, r_wob], updates=[r_pY] if c else (), writes=() if c else [r_pY])
                        sl = slice(hf * 512, (hf + 1) * 512)
                        S.op("vector", lambda e, p_Y=p_Y, sl=sl: e.tensor_tensor(out=tmpA[:, sl], in0=p_Y[:], in1=gmsa[:, sl], op=ALU.mult),
                             reads=[r_pY, r_gmsa], updates=[r_tmpA])
                        S.op("gpsimd", lambda e, sl=sl, a_t=a_t, x_t=x_t: e.tensor_tensor(out=a_t[:, sl], in0=tmpA[:, sl], in1=x_t[:, sl], op=ALU.add),
                             reads=[r_tmpA, r_x], writes=[ra[hf]])
                else:
                    for hf in range(2):
                        sl = slice(hf * 512, (hf + 1) * 512)
                        S.op("gpsimd", lambda e, sl=sl, a_t=a_t, x_t=x_t: e.tensor_copy(out=a_t[:, sl], in_=x_t[:, sl]), reads=[r_x], writes=[ra[hf]])
                ss, r_ss = sm["ss"]
                rstd, r_rstd = sm["rstd"]
                T_se, R_se = sm["se"]
                S.op("gpsimd", lambda e: e.memset(ss[:], 0.0), writes=[r_ss])
                S.op("gpsimd", lambda e: e.memset(T_se[:], 0.0), writes=[R_se])
                S.op("scalar", lambda e, a_t=a_t: e.activation(out=junk[:], in_=a_t, func=AF.Square, accum_out=ss[:]),
                     reads=ra, writes=[r_junk], updates=[r_ss])
                S.op("vector", lambda e: e.tensor_scalar(out=rstd[:], in0=ss[:], scalar1=1.0 / D, scalar2=EPS, op0=ALU.mult, op1=ALU.add),
                     reads=[r_ss], writes=[r_rstd])
                S.op("scalar", lambda e: e.activation(out=rstd[:], in_=rstd[:], func=AF.Sqrt), reads=[r_rstd], writes=[r_rstd])
                S.op("vector", lambda e: e.reciprocal(out=rstd[:], in_=rstd[:]), reads=[r_rstd], writes=[r_rstd])
                S.op("vector", lambda e, a_t=a_t: e.scalar_tensor_tensor(out=h2f[:], in0=a_t, scalar=rstd[:, 0:1], in1=w2e[:], op0=ALU.mult, op1=ALU.mult),
                     reads=ra + [r_rstd, r_w2e], writes=[r_h2f])
                S.op("gpsimd", lambda e: e.tensor_tensor(out=h2f[:], in0=h2f[:], in1=sh2[:], op=ALU.add), reads=[r_h2f, r_sh2], writes=[r_h2f])
                for hf in range(2):
                    sl = slice(hf * 512, (hf + 1) * 512)
                    S.op("scalar", lambda e, a_t=a_t, sl=sl: e.mul(out=a_t[:, sl], in_=a_t[:, sl], mul=0.5), reads=[], writes=[ra[hf]])
                S.op("vector", lambda e: e.tensor_copy(out=h2b[:], in_=h2f[:]), reads=[r_h2f], writes=[r_h2b])
                p_T, r_pT = pT[(t + 1) % 2]
                for c in range(8):
                    S.op("tensor", lambda e, c=c, p_T=p_T: e.transpose(out=p_T[:, c, :], in_=h2b[:, c * 128:(c + 1) * 128], identity=idb[:]),
                         reads=[r_h2b, r_idb], updates=[r_pT] if c else (), writes=() if c else [r_pT])
                S.op("scalar", lambda e, p_T=p_T, t=t: e.copy(out=h2T[:, :, t * 128:(t + 1) * 128], in_=p_T[:]), reads=[r_pT], writes=[r_h2T[t]])
                for g4 in range(2):
                    p_f, r_pf = pTf[g4]
                    for c in range(4):
                        cc = g4 * 4 + c
                        S.op("tensor", lambda e, c=c, cc=cc, p_f=p_f: e.transpose(out=p_f[:, c, :], in_=h2f[:, cc * 128:(cc + 1) * 128], identity=idf[:]),
                             reads=[r_h2f, r_idf], updates=[r_pf] if c else (), writes=() if c else [r_pf])
                    S.op("vector" if g4 else "scalar", (lambda e, p_f=p_f, g4=g4: e.tensor_copy(out=h2Tf[:, g4 * 4:(g4 + 1) * 4, :], in_=p_f[:])) if g4 else
                         (lambda e, p_f=p_f, g4=g4: e.copy(out=h2Tf[:, g4 * 4:(g4 + 1) * 4, :], in_=p_f[:])),
                         reads=[r_pf], updates=[r_h2Tf])
                for c in range(8):
                    S.op("tensor", lambda e, c=c: e.matmul(out=pL[:], lhsT=h2Tf[:, c, :], rhs=wrt[:, c, :], start=(c == 0), stop=(c == 7)),
                         reads=[r_h2Tf, r_wrt], updates=[r_pL] if c else (), writes=() if c else [r_pL])
                def T(k):
                    return sm[k][0]

                def Rr(k):
                    return sm[k][1]
                V = lambda fn, reads, writes: S.op("vector", fn, reads=reads, writes=writes)
                V(lambda e: e.tensor_tensor(out=T("lg")[:], in0=pL[:], in1=brt[:], op=ALU.add), [r_pL, r_brt], [Rr("lg")])
                V(lambda e: e.reduce_max(out=T("m4")[:], in_=T("lg")[:, 0:4], axis=AX.X), [Rr("lg")], [Rr("m4")])
                V(lambda e: e.tensor_scalar(out=T("ng")[:], in0=T("m4")[:], scalar1=-1.0, scalar2=None, op0=ALU.mult), [Rr("m4")], [Rr("ng")])
                S.op("scalar", lambda e: e.activation(out=T("ex4")[:], in_=T("lg")[:, 0:4], func=AF.Exp, bias=T("ng")[:, 0:1], scale=1.0, accum_out=T("se")[:]),
                     reads=[Rr("lg"), Rr("ng")], writes=[Rr("ex4")], updates=[Rr("se")])
                V(lambda e: e.reciprocal(out=T("gp")[:], in_=T("se")[:]), [Rr("se")], [Rr("gp")])
                V(lambda e: e.tensor_scalar(out=T("oh")[:], in0=T("lg")[:, 0:4], scalar1=T("m4")[:, 0:1], scalar2=None, op0=ALU.is_ge), [Rr("lg"), Rr("m4")], [Rr("oh")])
                V(lambda e: e.tensor_scalar(out=T("pen")[:], in0=T("oh")[:], scalar1=-1.0, scalar2=-NEG, op0=ALU.add, op1=ALU.mult), [Rr("oh")], [Rr("pen")])
                V(lambda e: e.tensor_tensor(out=T("msk")[:].rearrange("p (g x) -> p g x", g=4), in0=T("lg")[:, 4:36].rearrange("p (g x) -> p g x", g=4),
                                            in1=T("pen")[:].rearrange("p (g o) -> p g o", o=1).broadcast_to([128, 4, 8]), op=ALU.add),
                  [Rr("lg"), Rr("pen")], [Rr("msk")])
                V(lambda e: e.max(out=T("top8")[:], in_=T("msk")[:]), [Rr("msk")], [Rr("top8")])
                V(lambda e: e.tensor_scalar(out=T("sel")[:], in0=T("msk")[:], scalar1=T("top8")[:, 1:2], scalar2=None, op0=ALU.is_ge), [Rr("msk"), Rr("top8")], [Rr("sel")])
                V(lambda e: e.tensor_scalar(out=T("nt1")[:], in0=T("top8")[:, 0:1], scalar1=-1.0, scalar2=None, op0=ALU.mult), [Rr("top8")], [Rr("nt1")])
                V(lambda e: e.tensor_tensor(out=T("d21")[:], in0=T("top8")[:, 1:2], in1=T("top8")[:, 0:1], op=ALU.subtract), [Rr("top8")], [Rr("d21")])
                S.op("scalar", lambda e: e.activation(out=T("e21")[:], in_=T("d21")[:], func=AF.Exp), reads=[Rr("d21")], writes=[Rr("e21")])
                S.op("scalar", lambda e: e.activation(out=T("wf")[:], in_=T("msk")[:], func=AF.Exp, bias=T("nt1")[:, 0:1], scale=1.0),
                     reads=[Rr("msk"), Rr("nt1")], writes=[Rr("wf")])
                V(lambda e: e.tensor_scalar(out=T("coef")[:], in0=T("e21")[:], scalar1=1.0, scalar2=None, op0=ALU.add), [Rr("e21")], [Rr("coef")])
                V(lambda e: e.reciprocal(out=T("coef")[:], in_=T("coef")[:]), [Rr("coef")], [Rr("coef")])
                V(lambda e: e.tensor_tensor(out=T("coef")[:], in0=T("coef")[:], in1=T("gp")[:], op=ALU.mult), [Rr("coef"), Rr("gp")], [Rr("coef")])
                V(lambda e, t=t: e.scalar_tensor_tensor(out=gates[:, t, :], in0=T("wf")[:], scalar=T("coef")[:, 0:1], in1=T("sel")[:], op0=ALU.mult, op1=ALU.mult),
                  [Rr("wf"), Rr("coef"), Rr("sel")], [r_gates[t]])
            for t in range(NT):
                do_tile(t)
        C.stack = C_stack_saved
        S.barrier()

        stB = contextlib.ExitStack()
        C.stack = stB
        with stB:
            NST = 3
            stg = [C.sb([128, 4, 512], F32, "stg%d" % i) for i in range(NST)]
            w1b = [C.sb([128, 8, 512], BF16, "w1b%d" % i) for i in range(2)]
            w3b = [C.sb([128, 8, 512], BF16, "w3b%d" % i) for i in range(2)]
            w2b = [C.sb([128, 4, D], BF16, "w2b%d" % i) for i in range(2)]
            sg = [C.sb([128, 512], F32, "sg%d" % i) for i in range(2)]
            act = [C.sb([128, 4, 512], BF16, "act%d" % i) for i in range(2)]
            tmpB = [C.sb([128, 512], F32, "tmpB%d" % i) for i in range(2)]
            pU1 = [C.ps([128, 512], F32, "pU1_%d" % i) for i in range(2)]
            pU3 = [C.ps([128, 512], F32, "pU3_%d" % i) for i in range(2)]
            pYB = [C.ps([128, 512], F32, "pYB%d" % i) for i in range(3)]
            chunks = []
            t0 = 0
            while t0 < NT:
                chunks.append(list(range(t0, min(t0 + 4, NT))))
                t0 += 4
            si_ = 0
            ny = 0
            nact = 0
            sic = [0]

            def load_piece(ex_, pi):
                pb_ = ex_ % 2
                if pi < 4:
                    wsrc, dst = ((w1, w1b[pb_]), (w3, w3b[pb_]))[pi // 2]
                    half = pi % 2
                    src = wsrc[ex_, half * 512:(half + 1) * 512, :].rearrange("(c p) f -> p c f", p=128)
                    dstap, r_dst = dst[0][:, half * 4:(half + 1) * 4, :], dst[1]
                else:
                    half = pi - 4
                    src = w2[ex_, half * 256:(half + 1) * 256, :].rearrange("(c p) f -> p c f", p=128)
                    dstap, r_dst = w2b[pb_][0][:, half * 2:(half + 1) * 2, :], w2b[pb_][1]
                s_t, r_s = stg[sic[0] % NST]
                sic[0] += 1
                sv = s_t[:] if pi < 4 else s_t[:].rearrange("p (c a) f -> p c (a f)", a=2)
                S.dma(sv, src, writes=[r_s], eng="sync")
                pending.append((sv, r_s, dstap, r_dst))

            pending = []

            def cast_pending():
                while pending:
                    sv, r_s, dstap, r_dst = pending.pop(0)
                    S.op("scalar", lambda e, sv=sv, dstap=dstap: e.copy(out=dstap, in_=sv), reads=[r_s], updates=[r_dst])

            for pi in range(6):
                load_piece(0, pi)
                if len(pending) >= 2:
                    cast_pending()
            cast_pending()
            sched_next = {0: [0, 1], 1: [2], 2: [3, 4], 3: [5]} if len(chunks) >= 4 else {0: [0, 1, 2, 3, 4, 5]}
            for ex in range(NEXP):
                pb = ex % 2
                W1, rW1 = w1b[pb]
                W3, rW3 = w3b[pb]
                W2, rW2 = w2b[pb]
                for ci_, ch in enumerate(chunks):
                    cast_pending()
                    if ex + 1 < NEXP:
                        for pi in sched_next.get(ci_, []):
                            load_piece(ex + 1, pi)
                    n = len(ch) * 128
                    c0 = ch[0] * 128
                    a_t, r_a = act[nact % 2]
                    nact += 1
                    rh = [r_h2T[t] for t in ch]
                    for fc in range(4):
                        p1, r_p1 = pU1[fc % 2]
                        p3, r_p3 = pU3[fc % 2]
                        for c in range(8):
                            S.op("tensor", lambda e, c=c, fc=fc, p1=p1, W1=W1, c0=c0, n=n: e.matmul(out=p1[:, 0:n], lhsT=W1[:, c, fc * 128:(fc + 1) * 128], rhs=h2T[:, c, c0:c0 + n],
                                                                                                       start=(c == 0), stop=(c == 7)),
                                 reads=rh + [rW1], updates=[r_p1] if c else (), writes=() if c else [r_p1])
                        for c in range(8):
                            S.op("tensor", lambda e, c=c, fc=fc, p3=p3, W3=W3, c0=c0, n=n: e.matmul(out=p3[:, 0:n], lhsT=W3[:, c, fc * 128:(fc + 1) * 128], rhs=h2T[:, c, c0:c0 + n],
                                                                                                       start=(c == 0), stop=(c == 7)),
                                 reads=rh + [rW3], updates=[r_p3] if c else (), writes=() if c else [r_p3])
                        s_g, r_sg = sg[fc % 2]
                        S.op("scalar", lambda e, s_g=s_g, p1=p1, n=n: e.activation(out=s_g[:, 0:n], in_=p1[:, 0:n], func=AF.Silu), reads=[r_p1], writes=[r_sg])
                        S.op("vector", lambda e, s_g=s_g, p3=p3, a_t=a_t, fc=fc, n=n: e.tensor_tensor(out=a_t[:, fc, 0:n], in0=s_g[:, 0:n], in1=p3[:, 0:n], op=ALU.mult),
                             reads=[r_sg, r_p3], updates=[r_a] if fc else (), writes=() if fc else [r_a])
                    for ti, t in enumerate(ch):
                        gm = gmlp[0 if tiles[t] < 2 else 1]
                        for hf in range(2):
                            p_y, r_py = pYB[ny % 3]
                            tb, r_tb = tmpB[ny % 2]
                            ny += 1
                            for fc in range(4):
                                S.op("tensor", lambda e, fc=fc, p_y=p_y, a_t=a_t, ti=ti, W2=W2, hf=hf: e.matmul(out=p_y[:], lhsT=a_t[:, fc, ti * 128:(ti + 1) * 128],
                                                                                                                  rhs=W2[:, fc, hf * 512:(hf + 1) * 512], start=(fc == 0), stop=(fc == 3)),
                                     reads=[r_a, rW2], updates=[r_py] if fc else (), writes=() if fc else [r_py])
                            sl = slice(hf * 512, (hf + 1) * 512)
                            S.op("vector", lambda e, p_y=p_y, tb=tb, t=t, ex=ex, gm=gm, sl=sl: e.scalar_tensor_tensor(out=tb[:], in0=p_y[:], scalar=gates[:, t, ex:ex + 1], in1=gm[0][:, sl],
                                                                                                                        op0=ALU.mult, op1=ALU.mult),
                                 reads=[r_py, r_gates[t], gm[1]], writes=[r_tb])
                            S.op("gpsimd", lambda e, tb=tb, t=t, sl=sl: e.tensor_tensor(out=acc[:, t, sl], in0=acc[:, t, sl], in1=tb[:], op=ALU.add),
                                 reads=[r_tb], writes=[r_acc[t][hf]])
                cast_pending()
        C.stack = C_stack_saved
        for t in range(NT):
            gt = tiles[t]
            S.dma(ypart[gt * 128:(gt + 1) * 128, :], acc[:, t, :], reads=r_acc[t], updates=[C.r_yp[gt // 8]])


class NormIn:
    def __init__(self, C, xs, modv, nw, idb, r_idb, nps=2):
        self.C, self.xs, self.modv = C, xs, modv
        S = C.S
        self.idb, self.r_idb = idb, r_idb
        self.w1e, self.r_w1e = C.sb([128, D], F32, "w1e")
        self.sh1, self.r_sh1 = C.sb([128, D], F32, "sh1")
        self.nwt, self.r_nwt = C.sb([128, D], F32, "nwt1")
        S.dma(self.nwt[:], nw.partition_broadcast(128), writes=[self.r_nwt])
        self.xt = [C.sb([128, D], F32, "nxt%d" % i) for i in range(2)]
        self.tmp_l = [C.sb([128, D], F32, "ntmp%d" % i) for i in range(2)]
        self.hb_l = [C.sb([128, D], BF16, "nhb%d" % i) for i in range(2)]
        self.ss_l = [C.sb([128, 1], F32, "nss%d" % i) for i in range(2)]
        self.rstd_l = [C.sb([128, 1], F32, "nrstd%d" % i) for i in range(2)]
        self.pT = [C.ps([128, 8, 128], BF16, "npT%d" % i) for i in range(nps)]
        if nps == 1:
            self.pT = self.pT * 2
        self.hT = [C.sb([128, 8, 128], BF16, "nhT%d" % i) for i in range(2)]
        self.cur = None
        self.k = 0

    def load_mod(self, si):
        S, modv = self.C.S, self.modv
        S.dma(self.sh1[:], modv[si, 0:1, :].partition_broadcast(128), writes=[self.r_sh1])
        S.dma(self.w1e[:], modv[si, 1:2, :].partition_broadcast(128), writes=[self.r_w1e])
        S.op("vector", lambda e: e.scalar_tensor_tensor(out=self.w1e[:], in0=self.w1e[:], scalar=1.0, in1=self.nwt[:], op0=ALU.add, op1=ALU.mult),
             reads=[self.r_w1e, self.r_nwt], writes=[self.r_w1e])

    def tile(self, t):
        S = self.C.S
        need = 0 if t < 2 else 1
        if need != self.cur:
            self.load_mod(need)
            self.cur = need
        k = self.k
        self.k += 1
        x_t, r_x = self.xt[k % 2]
        S.dma(x_t[:], self.xs[t * 128:(t + 1) * 128, :], reads=[self.C.r_xf[t // 8]], writes=[r_x])
        ss, r_ss = self.ss_l[k % 2]
        rstd, r_rstd = self.rstd_l[k % 2]
        tmp, r_tmp = self.tmp_l[k % 2]
        hb, r_hb = self.hb_l[k % 2]
        S.op("gpsimd", lambda e: e.memset(ss[:], 0.0), writes=[r_ss])
        S.op("scalar", lambda e: e.activation(out=tmp[:], in_=x_t[:], func=AF.Square, accum_out=ss[:]), reads=[r_x], writes=[r_tmp], updates=[r_ss])
        S.op("vector", lambda e: e.tensor_scalar(out=rstd[:], in0=ss[:], scalar1=1.0 / D, scalar2=EPS, op0=ALU.mult, op1=ALU.add), reads=[r_ss], writes=[r_rstd])
        S.op("scalar", lambda e: e.activation(out=rstd[:], in_=rstd[:], func=AF.Sqrt), reads=[r_rstd], writes=[r_rstd])
        S.op("vector", lambda e: e.reciprocal(out=rstd[:], in_=rstd[:]), reads=[r_rstd], writes=[r_rstd])
        S.op("vector", lambda e: e.scalar_tensor_tensor(out=tmp[:], in0=x_t[:], scalar=rstd[:, 0:1], in1=self.w1e[:], op0=ALU.mult, op1=ALU.mult),
             reads=[r_x, r_rstd, self.r_w1e], writes=[r_tmp])
        S.op("gpsimd", lambda e: e.tensor_tensor(out=hb[:], in0=tmp[:], in1=self.sh1[:], op=ALU.add), reads=[r_tmp, self.r_sh1], writes=[r_hb])
        p_T, r_pT = self.pT[k % 2]
        h_T, r_hT = self.hT[k % 2]
        for c in range(8):
            S.op("tensor", lambda e, c=c: e.transpose(out=p_T[:, c, :], in_=hb[:, c * 128:(c + 1) * 128], identity=self.idb[:]),
                 reads=[r_hb, self.r_idb], updates=[r_pT] if c else (), writes=() if c else [r_pT])
        S.op("scalar", lambda e: e.copy(out=h_T[:], in_=p_T[:]), reads=[r_pT], writes=[r_hT])
        return h_T, r_hT


def load_weight_bf16(C, dst, r_dst, src, ncols, col0=0, eng_cast="gpsimd"):
    S = C.S
    saved = C.stack
    tmpst = contextlib.ExitStack()
    C.stack = tmpst
    with tmpst:
        stgs = [C.sb([128, max(2048, ncols)], F32, "lwst%d_%d" % (C.n, i)) for i in range(2)]
        step = max(1, 2048 // ncols)
        k = 0
        for c0 in range(0, 8, step):
            nck = min(step, 8 - c0)
            s_t, r_s = stgs[k % 2]
            sv = s_t[:, 0:nck * ncols].rearrange("p (c f) -> p c f", c=nck)
            S.dma(sv, src[c0 * 128:(c0 + nck) * 128, :].rearrange("(c p) f -> p c f", p=128), writes=[r_s], eng=("sync" if k % 2 == 0 else "gpsimd"))
            S.op(eng_cast, lambda e, sv=sv, c0=c0, nck=nck: e.tensor_copy(out=dst[:, c0:c0 + nck, col0:col0 + ncols], in_=sv), reads=[r_s], updates=[r_dst])
            k += 1
    C.stack = saved
    S.barrier()


def emit_even(C, A):
    nc, S = C.nc, C.S
    NT = 34
    NTOK = NT * 128
    xs, modv, nw, wc, ropec, ropes, draw, sink, cmat, crow, ccol, mgo = (A[k] for k in "xs modv nw wc ropec ropes draw sink cmat crow ccol mgo".split())
    SC = 128 ** -0.5
    with C.scope():
        idb, r_idb, idf, r_idf = make_identity(C)
        qkT, _ = C.sb([128, 7, NTOK], BF16, "qkT")
        r_qkT = [Res("qkT%d" % t) for t in range(NT)]
        tm, _ = C.sb([128, NT, 896], BF16, "tm")
        r_tm = [Res("tm%d" % t) for t in range(NT)]
        cm, r_cm = C.sb([128, 6, 128], F32, "cm")
        S.dma(cm[:], cmat, writes=[r_cm])
        cr, r_cr = C.sb([128, 256], F32, "cr")
        S.dma(cr[:], crow.partition_broadcast(128), writes=[r_cr])
        cc, r_cc = C.sb([128, 2], F32, "cc")
        S.dma(cc[:], ccol, writes=[r_cc])
        lg, r_lg = C.sb([128, 4], F32, "lg")
        S.dma(lg[:], draw.partition_broadcast(128), writes=[r_lg])
        S.op("scalar", lambda e: e.activation(out=lg[:], in_=lg[:], func=AF.Exp), reads=[r_lg], writes=[r_lg])
        S.op("vector", lambda e: e.tensor_scalar(out=lg[:], in0=lg[:], scalar1=-1.0, scalar2=None, op0=ALU.mult), reads=[r_lg], writes=[r_lg])
        skt, r_skt = C.sb([128, 2], F32, "skt")
        S.dma(skt[:], sink.partition_broadcast(128), writes=[r_skt])
        dmask, r_dmask = C.sb([128, 4, 128], F32, "dmask")
        qdec, r_qdec = C.sb([128, 4, 128], F32, "qdec")
        kdec, r_kdec = C.sb([128, 4], F32, "kdec")
        cdec, r_cdec = C.sb([128, 4], F32, "cdec")
        c128, r_c128 = C.sb([128, 1], F32, "c128")
        S.op("gpsimd", lambda e: e.memset(c128[:], 128.0), writes=[r_c128])
        for dr in range(2):
            for h in range(2):
                ix = dr * 2 + h
                lgc = lg[:, ix:ix + 1]
                S.op("vector", lambda e, ix=ix, dr=dr, lgc=lgc: e.tensor_scalar(out=dmask[:, ix, :], in0=cm[:, dr, :], scalar1=lgc, scalar2=None, op0=ALU.mult),
                     reads=[r_cm, r_lg], updates=[r_dmask])
                S.op("scalar", lambda e, ix=ix: e.activation(out=dmask[:, ix, :], in_=dmask[:, ix, :], func=AF.Exp), reads=[], updates=[r_dmask])
                S.op("vector", lambda e, ix=ix, dr=dr: e.tensor_tensor(out=dmask[:, ix, :], in0=dmask[:, ix, :], in1=cm[:, 2 + dr, :], op=ALU.mult),
                     reads=[r_cm], updates=[r_dmask])
                S.op("vector", lambda e, ix=ix, dr=dr, lgc=lgc: e.tensor_scalar(out=qdec[:, ix, :], in0=cr[:, dr * 128:(dr + 1) * 128], scalar1=lgc, scalar2=None, op0=ALU.mult),
                     reads=[r_cr, r_lg], updates=[r_qdec])
                S.op("scalar", lambda e, ix=ix: e.activation(out=qdec[:, ix, :], in_=qdec[:, ix, :], func=AF.Exp), reads=[], updates=[r_qdec])
                S.op("vector", lambda e, ix=ix, dr=dr, lgc=lgc: e.tensor_scalar(out=kdec[:, ix:ix + 1], in0=cc[:, dr:dr + 1], scalar1=lgc, scalar2=None, op0=ALU.mult),
                     reads=[r_cc, r_lg], updates=[r_kdec])
                S.op("vector", lambda e, ix=ix, lgc=lgc: e.tensor_scalar(out=cdec[:, ix:ix + 1], in0=c128[:], scalar1=lgc, scalar2=None, op0=ALU.mult),
                     reads=[r_c128, r_lg], updates=[r_cdec])
        S.op("scalar", lambda e: e.activation(out=kdec[:], in_=kdec[:], func=AF.Exp), reads=[], updates=[r_kdec])
        S.op("scalar", lambda e: e.activation(out=cdec[:], in_=cdec[:], func=AF.Exp), reads=[], updates=[r_cdec])
        S.op("vector", lambda e: e.tensor_scalar(out=kdec[:], in0=kdec[:], scalar1=SC, scalar2=None, op0=ALU.mult), reads=[], updates=[r_kdec])

        stP = contextlib.ExitStack()
        saved = C.stack
        C.stack = stP
        with stP:
            wcb, r_wcb = C.sb([128, 8, 1536], BF16, "wcb")
            load_weight_bf16(C, wcb, r_wcb, wc, 1536)
            NI = NormIn(C, xs, modv, nw, idb, r_idb)
            cosT = [C.sb([128, 128], F32, "cos%d" % i) for i in range(2)]
            sinT = [C.sb([128, 128], F32, "sin%d" % i) for i in range(2)]
            pP = [C.ps([128, 512], F32, "pP%d" % i) for i in range(3)]
            pT7, r_pT7 = C.ps([128, 7, 128], BF16, "pT7")
            pf, r_pf = C.sb([128, 896], F32, "pf")
            t1, r_t1 = C.sb([128, 896], F32, "t1")
            t2, r_t2 = C.sb([128, 896], F32, "t2")
            Rb, r_Rb = C.sb([128, 896], BF16, "Rb")
            for t in range(NT):
                h_T, r_hT = NI.tile(t)
                c_t, r_c = cosT[t % 2]
                s_t, r_s = sinT[t % 2]
                S.dma(c_t[:], ropec[t * 128:(t + 1) * 128, :], writes=[r_c], eng="gpsimd")
                S.dma(s_t[:], ropes[t * 128:(t + 1) * 128, :], writes=[r_s], eng="gpsimd")
                for nb in range(3):
                    p, r_p = pP[nb]
                    for c in range(8):
                        S.op("tensor", lambda e, c=c, nb=nb, p=p, h_T=h_T: e.matmul(out=p[:], lhsT=h_T[:, c, :], rhs=wcb[:, c, nb * 512:(nb + 1) * 512], start=(c == 0), stop=(c == 7)),
                             reads=[r_hT, r_wcb], updates=[pP[nb][1]] if c else (), writes=() if c else [pP[nb][1]])
                S.op("scalar", lambda e: e.copy(out=pf[:, 0:512], in_=pP[0][0][:]), reads=[pP[0][1]], writes=[r_pf])
                S.op("scalar", lambda e: e.copy(out=pf[:, 512:896], in_=pP[1][0][:, 0:384]), reads=[pP[1][1]], updates=[r_pf])
                S.op("scalar", lambda e, t=t: e.copy(out=tm[:, t, 256:384], in_=pP[1][0][:, 384:512]), reads=[pP[1][1]], updates=[r_tm[t]])
                S.op("scalar", lambda e, t=t: e.copy(out=tm[:, t, 384:896], in_=pP[2][0][:]), reads=[pP[2][1]], updates=[r_tm[t]])
                S.op("vector", lambda e, c_t=c_t: e.tensor_tensor(out=t1[:].rearrange("p (h d) -> p h d", h=7), in0=pf[:].rearrange("p (h d) -> p h d", h=7),
                                                                  in1=c_t[:].rearrange("p (o d) -> p o d", o=1).broadcast_to([128, 7, 128]), op=ALU.mult),
                     reads=[r_pf, r_c], writes=[r_t1])
                for a in range(2):
                    for hf in range(2):
                        o0 = a * 64 + hf * 32
                        i0 = a * 64 + (1 - hf) * 32
                        S.op("vector", lambda e, o0=o0, i0=i0, s_t=s_t: e.tensor_tensor(
                            out=t2[:].rearrange("p (h d) -> p h d", h=7)[:, :, o0:o0 + 32], in0=pf[:].rearrange("p (h d) -> p h d", h=7)[:, :, i0:i0 + 32],
                            in1=s_t[:, o0:o0 + 32].rearrange("p (o d) -> p o d", o=1).broadcast_to([128, 7, 32]), op=ALU.mult),
                             reads=[r_pf, r_s], updates=[r_t2])
                S.op("vector", lambda e: e.tensor_tensor(out=Rb[:], in0=t1[:], in1=t2[:], op=ALU.add), reads=[r_t1, r_t2], writes=[r_Rb])
                S.op("gpsimd", lambda e, t=t: e.tensor_copy(out=tm[:, t, 0:256], in_=Rb[:, 256:512]), reads=[r_Rb], updates=[r_tm[t]])
                for c in range(7):
                    S.op("tensor", lambda e, c=c: e.transpose(out=pT7[:, c, :], in_=Rb[:, c * 128:(c + 1) * 128], identity=idb[:]),
                         reads=[r_Rb, r_idb], updates=[r_pT7] if c else (), writes=() if c else [r_pT7])
                S.op("scalar", lambda e, t=t: e.copy(out=qkT[:, :, t * 128:(t + 1) * 128], in_=pT7[:]), reads=[r_pT7], writes=[r_qkT[t]])
        C.stack = saved
        S.barrier()

        stS = contextlib.ExitStack()
        C.stack = stS
        with stS:
            oacc, _ = C.sb([128, NT, 256], F32, "oacc")
            r_oacc = [Res("oacc%d" % t) for t in range(NT)]
            Sst = [C.sb([128, 128], F32, "Sst%d" % i) for i in range(4)]
            Sbf = [C.sb([128, 128], BF16, "Sbf%d" % i) for i in range(4)]
            for i in range(4):
                S.op("gpsimd", lambda e, i=i: e.memset(Sst[i][0][:], 0.0), writes=[Sst[i][1]])
                S.op("gpsimd", lambda e, i=i: e.memset(Sbf[i][0][:], 0.0), writes=[Sbf[i][1]])
            PTs = [C.sb([128, 128], BF16, "PTs%d" % i) for i in range(2)]
            qs = [C.sb([128, 128], BF16, "qs%d" % i) for i in range(2)]
            ks = [C.sb([128, 128], BF16, "ks%d" % i) for i in range(2)]
            pSc = [C.ps([128, 128], F32, "pSc")] * 2
            pO = [C.ps([128, 128], F32, "pO%d" % i) for i in range(2)]
            pU = [C.ps([128, 128], F32, "pU")] * 2
            cnt = [0]

            def ret_step(t, h, dr):
                ix = dr * 2 + h
                k = cnt[0]
                cnt[0] += 1
                tok = slice(t * 128, (t + 1) * 128)
                p_s, r_ps = pSc[k % 2]
                p_o, r_po = pO[k % 2]
                p_u, r_pu = pU[k % 2]
                PT, r_PT = PTs[k % 2]
                q_s, r_qs = qs[k % 2]
                k_s, r_ks = ks[k % 2]
                S.op("tensor", lambda e: e.matmul(out=p_s[:], lhsT=qkT[:, 2 + h, tok], rhs=qkT[:, h, tok], start=True, stop=True),
                     reads=[r_qkT[t]], writes=[r_ps])
                S.op("vector", lambda e: e.tensor_tensor(out=PT[:], in0=p_s[:], in1=dmask[:, ix, :], op=ALU.mult), reads=[r_ps, r_dmask], writes=[r_PT])
                S.op("vector", lambda e: e.tensor_tensor(out=q_s[:], in0=qkT[:, h, tok], in1=qdec[:, ix, :], op=ALU.mult), reads=[r_qkT[t], r_qdec], writes=[r_qs])
                S.op("vector", lambda e: e.tensor_scalar(out=k_s[:], in0=tm[:, t, h * 128:(h + 1) * 128], scalar1=kdec[:, ix:ix + 1], scalar2=None, op0=ALU.mult),
                     reads=[r_tm[t], r_kdec], writes=[r_ks])
                vv = tm[:, t, 256 + h * 128:256 + (h + 1) * 128]
                S.op("tensor", lambda e: e.matmul(out=p_o[:], lhsT=PT[:], rhs=vv, start=True, stop=False), reads=[r_PT, r_tm[t]], writes=[r_po])
                S.op("tensor", lambda e: e.matmul(out=p_o[:], lhsT=q_s[:], rhs=Sbf[ix][0][:], start=False, stop=True), reads=[r_qs, Sbf[ix][1]], updates=[r_po])
                S.op("tensor", lambda e: e.matmul(out=p_u[:], lhsT=k_s[:], rhs=vv, start=True, stop=True), reads=[r_ks, r_tm[t]], writes=[r_pu])
                S.op("vector", lambda e: e.scalar_tensor_tensor(out=Sst[ix][0][:], in0=Sst[ix][0][:], scalar=cdec[:, ix:ix + 1], in1=p_u[:], op0=ALU.mult, op1=ALU.add),
                     reads=[r_pu, r_cdec], writes=[Sst[ix][1]])
                S.op("scalar", lambda e: e.copy(out=Sbf[ix][0][:], in_=Sst[ix][0][:]), reads=[Sst[ix][1]], writes=[Sbf[ix][1]])
                return p_o, r_po

            for t in range(NT):
                for h in range(2):
                    p_o, r_po = ret_step(t, h, 0)
                    S.op("scalar", lambda e, t=t, h=h, p_o=p_o: e.copy(out=oacc[:, t, h * 128:(h + 1) * 128], in_=p_o[:]), reads=[r_po], updates=[r_oacc[t]])

            mgt = [C.sb([128, 512], BF16, "mgt%d" % i) for i in range(2)]
            otot, r_otot = C.sb([128, 256], F32, "otot")
            sgt, r_sgt = C.sb([128, 256], F32, "sgt")
            jk, r_jk = C.sb([128, 128], F32, "jk2")
            ssh, r_ssh = C.sb([128, 2], F32, "ssh")
            ssc, r_ssc = C.sb([128, 640], F32, "ssc")
            Pb, r_Pb = C.sb([128, 640], BF16, "Pb")
            PTa, r_PTa = C.sb([128, 5, 128], BF16, "PTa")
            mx, r_mx = C.sb([128, 1], F32, "mx")
            nmx, r_nmx = C.sb([128, 1], F32, "nmx")
            rs, r_rs = C.sb([128, 1], F32, "rs")
            esk, r_esk = C.sb([128, 1], F32, "esk")
            pA, r_pA = C.ps([128, 384], F32, "pA")
            pB, r_pB = C.ps([128, 256], F32, "pB")
            pPT, r_pPT = C.ps([128, 5, 128], BF16, "pPT")
            pOa, r_pOa = C.ps([128, 128], F32, "pOa")
            order = [1, 0] + list(range(NT - 1, 1, -1))
            for oi, t in enumerate(order):
                m_t, r_m = mgt[oi % 2]
                S.op("scalar", lambda e, t=t: e.activation(out=sgt[:], in_=tm[:, t, 512:768], func=AF.Silu), reads=[r_tm[t]], writes=[r_sgt])
                S.op("gpsimd", lambda e: e.memset(ssh[:], 0.0), writes=[r_ssh])
                for h in range(2):
                    p_o, r_po = ret_step(t, h, 1)
                    hs = slice(h * 128, (h + 1) * 128)
                    S.op("vector", lambda e, t=t, hs=hs, p_o=p_o: e.tensor_tensor(out=otot[:, hs], in0=p_o[:], in1=oacc[:, t, hs], op=ALU.add),
                         reads=[r_po, r_oacc[t]], updates=[r_otot] if h else (), writes=() if h else [r_otot])
                    S.op("scalar", lambda e, hs=hs, h=h: e.activation(out=jk[:], in_=otot[:, hs], func=AF.Square, accum_out=ssh[:, h:h + 1]),
                         reads=[r_otot], writes=[r_jk], updates=[r_ssh])
                S.op("vector", lambda e: e.tensor_scalar(out=ssh[:], in0=ssh[:], scalar1=1.0 / 128, scalar2=EPS, op0=ALU.mult, op1=ALU.add), reads=[r_ssh], writes=[r_ssh])
                S.op("scalar", lambda e: e.activation(out=ssh[:], in_=ssh[:], func=AF.Sqrt), reads=[r_ssh], writes=[r_ssh])
                S.op("vector", lambda e: e.reciprocal(out=ssh[:], in_=ssh[:]), reads=[r_ssh], writes=[r_ssh])
                for h in range(2):
                    hs = slice(h * 128, (h + 1) * 128)
                    S.op("vector", lambda e, hs=hs, h=h, m_t=m_t: e.scalar_tensor_tensor(out=m_t[:, hs], in0=otot[:, hs], scalar=ssh[:, h:h + 1], in1=sgt[:, hs], op0=ALU.mult, op1=ALU.mult),
                         reads=[r_otot, r_ssh, r_sgt], updates=[r_m] if h else (), writes=() if h else [r_m])
                if t >= 2:
                    n = t - 2
                    lo = max(n - 1, 0)
                    hi = min(n + 1, 31)
                    loc = list(range(lo + 2, hi + 3))
                else:
                    loc = []
                ktiles = loc + [0, 1]
                nl = len(loc)
                nk = len(ktiles)
                rk_reads = [r_qkT[kt] for kt in ktiles]
                rv_reads = [r_tm[kt] for kt in ktiles]
                for g in range(2):
                    qT = qkT[:, 4 + g, t * 128:(t + 1) * 128]
                    if nl:
                        S.op("tensor", lambda e, qT=qT, loc=loc, nl=nl: e.matmul(out=pA[:, 0:nl * 128], lhsT=qT, rhs=qkT[:, 6, loc[0] * 128:(loc[-1] + 1) * 128], start=True, stop=True),
                             reads=[r_qkT[t]] + rk_reads, writes=[r_pA])
                    S.op("tensor", lambda e, qT=qT: e.matmul(out=pB[:], lhsT=qT, rhs=qkT[:, 6, 0:256], start=True, stop=True), reads=[r_qkT[t]] + rk_reads, writes=[r_pB])
                    for li, kt in enumerate(loc):
                        rel = kt - t
                        dst = ssc[:, li * 128:(li + 1) * 128]
                        src = pA[:, li * 128:(li + 1) * 128]
                        if rel == 0:
                            S.op("vector", lambda e, dst=dst, src=src: e.tensor_copy(out=dst, in_=src), reads=[r_pA], updates=[r_ssc])
                        else:
                            mi = 4 if rel < 0 else 5
                            S.op("vector", lambda e, dst=dst, src=src, mi=mi: e.tensor_tensor(out=dst, in0=src, in1=cm[:, mi, :], op=ALU.add), reads=[r_pA, r_cm], updates=[r_ssc])
                    S.op("scalar", lambda e, nl=nl: e.copy(out=ssc[:, nl * 128:nl * 128 + 256], in_=pB[:]), reads=[r_pB], updates=[r_ssc])
                    W = nk * 128
                    S.op("vector", lambda e, W=W: e.reduce_max(out=mx[:], in_=ssc[:, 0:W], axis=AX.X), reads=[r_ssc], writes=[r_mx])
                    S.op("vector", lambda e, g=g: e.tensor_scalar(out=mx[:], in0=mx[:], scalar1=SC, scalar2=skt[:, g:g + 1], op0=ALU.mult, op1=ALU.max), reads=[r_mx, r_skt], writes=[r_mx])
                    S.op("vector", lambda e: e.tensor_scalar(out=nmx[:], in0=mx[:], scalar1=-1.0, scalar2=None, op0=ALU.mult), reads=[r_mx], writes=[r_nmx])
                    S.op("gpsimd", lambda e: e.memset(rs[:], 0.0), writes=[r_rs])
                    S.op("scalar", lambda e, W=W: e.activation(out=Pb[:, 0:W], in_=ssc[:, 0:W], func=AF.Exp, bias=nmx[:, 0:1], scale=SC, accum_out=rs[:]),
                         reads=[r_ssc, r_nmx], writes=[r_Pb], updates=[r_rs])
                    S.op("scalar", lambda e, g=g: e.activation(out=esk[:], in_=skt[:, g:g + 1], func=AF.Exp, bias=nmx[:, 0:1], scale=1.0), reads=[r_skt, r_nmx], writes=[r_esk])
                    S.op("vector", lambda e: e.tensor_tensor(out=rs[:], in0=rs[:], in1=esk[:], op=ALU.add), reads=[r_esk], updates=[r_rs])
                    S.op("vector", lambda e: e.reciprocal(out=rs[:], in_=rs[:]), reads=[], updates=[r_rs])
                    for ki in range(nk):
                        S.op("tensor", lambda e, ki=ki: e.transpose(out=pPT[:, ki, :], in_=Pb[:, ki * 128:(ki + 1) * 128], identity=idb[:]),
                             reads=[r_Pb, r_idb], updates=[r_pPT] if ki else (), writes=() if ki else [r_pPT])
                    S.op("scalar", lambda e, nk=nk: e.copy(out=PTa[:, 0:nk, :], in_=pPT[:, 0:nk, :]), reads=[r_pPT], writes=[r_PTa])
                    for ki, kt in enumerate(ktiles):
                        S.op("tensor", lambda e, ki=ki, kt=kt, nk=nk: e.matmul(out=pOa[:], lhsT=PTa[:, ki, :], rhs=tm[:, kt, 768:896], start=(ki == 0), stop=(ki == nk - 1)),
                             reads=[r_PTa] + rv_reads, updates=[r_pOa] if ki else (), writes=() if ki else [r_pOa])
                    S.op("vector", lambda e, g=g, m_t=m_t: e.tensor_scalar(out=m_t[:, 256 + g * 128:256 + (g + 1) * 128], in0=pOa[:], scalar1=rs[:, 0:1], scalar2=None, op0=ALU.mult),
                         reads=[r_pOa, r_rs], updates=[r_m])
                S.dma(mgo[t * 128:(t + 1) * 128, :], m_t[:], reads=[r_m])
        C.stack = saved


def even_consts():
    p = np.arange(128, dtype=np.float32)[:, None]
    f = np.arange(128, dtype=np.float32)[None, :]
    SC = np.float32(128 ** -0.5)
    cmat = np.stack([np.maximum(f - p, 0), np.maximum(p - f, 0), (f >= p) * SC, (p >= f) * SC,
                     np.where(f >= p, 0.0, NEG), np.where(f <= p, 0.0, NEG)], 1).astype(np.float32)
    crow = np.concatenate([f[0] + 1, 128 - f[0]])[None].astype(np.float32)
    ccol = np.concatenate([127 - p, p], 1).astype(np.float32)
    t = np.arange(L)
    row = (t // 64).astype(np.float32)
    col = (t % 64).astype(np.float32)
    inv = (10000.0 ** (-np.arange(0, 64, 2, dtype=np.float32) / 64)).astype(np.float32)
    ar = row[:, None] * inv[None]
    ac = col[:, None] * inv[None]
    cos = np.concatenate([np.cos(ar), np.cos(ar), np.cos(ac), np.cos(ac)], 1)
    sin = np.concatenate([-np.sin(ar), np.sin(ar), -np.sin(ac), np.sin(ac)], 1)
    ropec = np.concatenate([np.ones((NCTX, 128)), cos], 0).astype(np.float32)
    ropes = np.concatenate([np.zeros((NCTX, 128)), sin], 0).astype(np.float32)
    return dict(cmat=np.ascontiguousarray(cmat), crow=crow, ccol=np.ascontiguousarray(ccol), ropec=ropec, ropes=ropes)


def emit_odd(C, A, debug=False):
    nc, S = C.nc, C.S
    NT = 34
    NTOK = NT * 128
    xs, modv, nw, wc, lbl, lsel, hnw, cmat, mgo = (A[k] for k in "xs modv nw wc lbl lsel hnw cmat mgo".split())
    if debug:
        dbg = C.dout("dbg", [128, 8, 512])
        dbg2 = C.dout("dbg2", [128, 8, 512])
    with C.scope():
        idb, r_idb, idf, r_idf = make_identity(C)
        cm, r_cm = C.sb([128, 2, 128], F32, "cm")
        S.dma(cm[:], cmat, writes=[r_cm])
        ones, r_ones = C.sb([128, 128], F32, "ones")
        S.op("gpsimd", lambda e: e.memset(ones[:], 1.0), writes=[r_ones])
        hn, r_hn = C.sb([128, 128], F32, "hn")
        S.dma(hn[:], hnw.partition_broadcast(128), writes=[r_hn])
        lgt, r_lgt = C.sb([128, 4, 4], F32, "lgt")
        for l_ in range(4):
            S.dma(lgt[:, l_, :], lbl[l_, :].rearrange("(h d) -> d h", d=128), updates=[r_lgt], allow_slow_non_contiguous=True)
        sel, r_sel = C.sb([128, 4], F32, "sel")
        S.dma(sel[:], lsel.partition_broadcast(128), writes=[r_sel])
        mxl, r_mxl = C.sb([128, 4], F32, "mxl")
        S.op("vector", lambda e: e.tensor_tensor(out=mxl[:], in0=lgt[:, 0, :], in1=lgt[:, 1, :], op=ALU.max), reads=[r_lgt], writes=[r_mxl])
        S.op("vector", lambda e: e.tensor_tensor(out=mxl[:], in0=mxl[:], in1=lgt[:, 2, :], op=ALU.max), reads=[r_lgt], writes=[r_mxl])
        S.op("vector", lambda e: e.tensor_tensor(out=mxl[:], in0=mxl[:], in1=lgt[:, 3, :], op=ALU.max), reads=[r_lgt], writes=[r_mxl])
        for l_ in range(4):
            S.op("vector", lambda e, l_=l_: e.tensor_tensor(out=lgt[:, l_, :], in0=lgt[:, l_, :], in1=mxl[:], op=ALU.subtract), reads=[r_mxl], updates=[r_lgt])
        S.op("scalar", lambda e: e.activation(out=lgt[:], in_=lgt[:], func=AF.Exp), reads=[], updates=[r_lgt])
        den, r_den = C.sb([128, 4], F32, "den")
        lb, r_lb = C.sb([128, 4], F32, "lb")
        oml, r_oml = C.sb([128, 4], F32, "oml")
        tl, r_tl = C.sb([128, 4], F32, "tl")
        S.op("gpsimd", lambda e: e.memset(den[:], 0.0), writes=[r_den])
        S.op("gpsimd", lambda e: e.memset(lb[:], 0.0), writes=[r_lb])
        for l_ in range(4):
            S.op("vector", lambda e, l_=l_: e.tensor_tensor(out=den[:], in0=den[:], in1=lgt[:, l_, :], op=ALU.add), reads=[r_lgt], writes=[r_den])
            S.op("vector", lambda e, l_=l_: e.tensor_scalar(out=tl[:], in0=lgt[:, l_, :], scalar1=sel[:, l_:l_ + 1], scalar2=None, op0=ALU.mult), reads=[r_lgt, r_sel], writes=[r_tl])
            S.op("vector", lambda e: e.tensor_tensor(out=lb[:], in0=lb[:], in1=tl[:], op=ALU.add), reads=[r_tl], writes=[r_lb])
        S.op("vector", lambda e: e.reciprocal(out=den[:], in_=den[:]), reads=[], updates=[r_den])
        S.op("vector", lambda e: e.tensor_tensor(out=lb[:], in0=lb[:], in1=den[:], op=ALU.mult), reads=[r_den], writes=[r_lb])
        S.op("vector", lambda e: e.tensor_scalar(out=oml[:], in0=lb[:], scalar1=-1.0, scalar2=1.0, op0=ALU.mult, op1=ALU.add), reads=[r_lb], writes=[r_oml])

        wcb, r_wcb = C.sb([128, 8, 2560], BF16, "wcb")
        load_weight_bf16(C, wcb, r_wcb, wc, 2560)
        NI = NormIn(C, xs, modv, nw, idb, r_idb, nps=1)
        ofw, _ = C.sb([128, NT, 512], F32, "ofw")
        r_ofw = [Res("ofw%d" % t) for t in range(NT)]
        pQ, r_pQ = C.ps([128, 4, 128], F32, "pQ")
        pZ, r_pZ = C.ps([128, 4, 128], F32, "pZ")
        pV, r_pV = C.ps([128, 512], F32, "pV")
        pG, r_pG = pV, r_pV
        pS, r_pS = C.ps([128, 4, 128], F32, "pS")
        _pK, r_pK = C.ps([128, 8, 128], BF16, "pK")
        pK = _pK.rearrange("p (h c) j -> p h c j", c=2)
        pUu, r_pUu = C.ps([128, 4, 128], F32, "pUu")
        pOo, r_pOo = C.ps([128, 4, 128], F32, "pOo")
        NB = 2
        bufs = []
        for i_ in range(NB):
            d_ = {}
            for nm_ in ("sf", "ff", "kk", "cs", "E1", "E2", "qsb"):
                d_[nm_] = C.sb([128, 4, 128], F32, "%s%d" % (nm_, i_))
            d_["rr"] = C.sb([128, 4, 2], F32, "rr%d" % i_)
            d_["aa"] = C.sb([128, 4, 2, 3], F32, "aa%d" % i_)
            d_["qtP"] = C.sb([128, 4, 2, 128], BF16, "qtP%d" % i_)
            d_["ktP"] = C.sb([128, 4, 2, 128], BF16, "ktP%d" % i_)
            S.op("vector", lambda e, t_=d_["qtP"][0]: e.memset(t_[:].rearrange("p a b c -> p (a b c)"), 0.0), writes=[d_["qtP"][1]])
            S.op("vector", lambda e, t_=d_["ktP"][0]: e.memset(t_[:].rearrange("p a b c -> p (a b c)"), 0.0), writes=[d_["ktP"][1]])
            d_["vb"] = C.sb([128, 512], BF16, "vb%d" % i_)
            d_["sgg"] = C.sb([128, 512], F32, "sgg%d" % i_)
            d_["otot"] = C.sb([128, 512], F32, "hotot%d" % i_)
            d_["ssh"] = C.sb([128, 4], F32, "hssh%d" % i_)
            bufs.append(d_)
        PT_l = [C.sb([128, 4, 128], BF16, "PT%d" % i_) for i_ in range(1)] * 2
        kTM_l = [C.sb([128, 4, 2, 128], BF16, "kTM%d" % i_) for i_ in range(1)] * 2
        tU_l = [C.sb([128, 4, 128], F32, "tU%d" % i_) for i_ in range(1)] * 2
        Sall, r_Sall = C.sb([128, 4, 128], F32, "Sall")
        Spp = [C.sb([128, 2, 4, 128], BF16, "hSpp%d" % i) for i in range(2)]
        jk, r_jk = C.sb([128, 128], F32, "hjk")
        mgt = [C.sb([128, 512], BF16, "hmg%d" % i) for i in range(2)]
        kcnt = [0]

        tilek = [0]

        def do_tile(oi, t, dr, zc0):
            B_ = bufs[tilek[0] % NB]
            tilek[0] += 1
            sf, r_sf = B_["sf"]
            ff, r_ff = B_["ff"]
            kk, r_kk = B_["kk"]
            cs, r_cs = B_["cs"]
            uu, r_uu = B_["sf"]
            E1, r_E1 = B_["E1"]
            E2, r_E2 = B_["E2"]
            qsb, r_qsb = B_["qsb"]
            rr, r_rr = B_["rr"]
            aa, r_aa = B_["aa"]
            qtP, r_qtP = B_["qtP"]
            ktP, r_ktP = B_["ktP"]
            vb, r_vb = B_["vb"]
            sgg, r_sgg = B_["sgg"]
            otot, r_otot = B_["otot"]
            ssh, r_ssh = B_["ssh"]
            h_T, r_hT = NI.tile(t)
            yield 'S'
            for h in range(4):
                for c in range(8):
                    S.op("tensor", lambda e, c=c, h=h, h_T=h_T: e.matmul(out=pQ[:, h, :], lhsT=wcb[:, c, h * 128:(h + 1) * 128], rhs=h_T[:, c, :], start=(c == 0), stop=(c == 7)),
                         reads=[r_hT, r_wcb], updates=[r_pQ] if (c or h) else (), writes=() if (c or h) else [r_pQ])
            for h in range(4):
                for c in range(8):
                    S.op("tensor", lambda e, c=c, h=h, zc0=zc0, h_T=h_T: e.matmul(out=pZ[:, h, :], lhsT=wcb[:, c, zc0 + h * 128:zc0 + (h + 1) * 128], rhs=h_T[:, c, :], start=(c == 0), stop=(c == 7)),
                         reads=[r_hT, r_wcb], updates=[r_pZ] if (c or h) else (), writes=() if (c or h) else [r_pZ])
            for c in range(8):
                S.op("tensor", lambda e, c=c, h_T=h_T: e.matmul(out=pV[:], lhsT=h_T[:, c, :], rhs=wcb[:, c, 1536:2048], start=(c == 0), stop=(c == 7)),
                     reads=[r_hT, r_wcb], updates=[r_pV] if c else (), writes=() if c else [r_pV])
            S.op("scalar", lambda e: e.copy(out=vb[:], in_=pV[:]), reads=[r_pV], writes=[r_vb])
            S.op("scalar", lambda e: e.copy(out=qsb[:], in_=pQ[:]), reads=[r_pQ], writes=[r_qsb])
            if dr == 1:
                for c in range(8):
                    S.op("tensor", lambda e, c=c, h_T=h_T: e.matmul(out=pG[:], lhsT=h_T[:, c, :], rhs=wcb[:, c, 2048:2560], start=(c == 0), stop=(c == 7)),
                         reads=[r_hT, r_wcb], updates=[r_pG] if c else (), writes=() if c else [r_pG])
                S.op("scalar", lambda e: e.activation(out=sgg[:], in_=pG[:], func=AF.Silu), reads=[r_pG], writes=[r_sgg])
            yield 'S'
            S.op("scalar", lambda e: e.activation(out=sf[:], in_=pZ[:], func=AF.Sigmoid), reads=[r_pZ], writes=[r_sf])
            yield 'S'
            for h in range(4):
                S.op("vector", lambda e, h=h: e.tensor_scalar(out=ff[:, h, :], in0=sf[:, h, :], scalar1=oml[:, h:h + 1], scalar2=lb[:, h:h + 1], op0=ALU.mult, op1=ALU.add),
                     reads=[r_sf, r_oml, r_lb], updates=[r_ff] if h else (), writes=() if h else [r_ff])
            S.op("vector", lambda e: e.tensor_scalar(out=kk[:], in0=ff[:], scalar1=-1.0, scalar2=1.0, op0=ALU.mult, op1=ALU.add), reads=[r_ff], writes=[r_kk])
            yield 'S'
            S.op("scalar", lambda e: e.activation(out=ff[:], in_=ff[:], func=AF.Ln), reads=[], updates=[r_ff])
            yield 'S'
            for h in range(4):
                S.op("vector", lambda e, h=h: e.tensor_tensor_scan(out=cs[:, h, :], data0=ones[:], data1=ff[:, h, :], initial=0.0, op0=ALU.mult, op1=ALU.add),
                     reads=[r_ff, r_ones], updates=[r_cs] if h else (), writes=() if h else [r_cs])
            if dr == 0:
                u, r_u = cs, r_cs
            else:
                S.op("vector", lambda e: e.tensor_tensor(out=uu[:], in0=ff[:], in1=cs[:], op=ALU.subtract), reads=[r_ff, r_cs], writes=[r_uu])
                u, r_u = uu, r_uu
            S.op("vector", lambda e, u=u: e.tensor_copy(out=rr[:], in_=u[:].rearrange("p h (c s) -> p h c s", c=2)[:, :, :, 31]), reads=[r_u], writes=[r_rr])
            for c in range(2):
                for h in range(4):
                    a1 = aa[:, h, c, 0:1]
                    a2 = aa[:, h, c, 1:2]
                    if dr == 0:
                        if c == 0:
                            S.op("vector", lambda e, a1=a1, h=h, c=c: e.tensor_copy(out=a1, in_=rr[:, h, c:c + 1]), reads=[r_rr], updates=[r_aa])
                        else:
                            S.op("vector", lambda e, a1=a1, h=h, c=c: e.tensor_tensor(out=a1, in0=rr[:, h, c:c + 1], in1=cs[:, h, 63:64], op=ALU.subtract), reads=[r_rr, r_cs], updates=[r_aa])
                        S.op("vector", lambda e, a2=a2, h=h, c=c: e.tensor_tensor(out=a2, in0=cs[:, h, c * 64 + 63:c * 64 + 64], in1=rr[:, h, c:c + 1], op=ALU.subtract), reads=[r_rr, r_cs], updates=[r_aa])
                    else:
                        S.op("vector", lambda e, a1=a1, h=h, c=c: e.tensor_tensor(out=a1, in0=rr[:, h, c:c + 1], in1=cs[:, h, c * 64 + 63:c * 64 + 64], op=ALU.add), reads=[r_rr, r_cs], updates=[r_aa])
                        if c == 0:
                            S.op("vector", lambda e, a2=a2, h=h, c=c: e.tensor_scalar(out=a2, in0=rr[:, h, c:c + 1], scalar1=-1.0, scalar2=None, op0=ALU.mult), reads=[r_rr], updates=[r_aa])
                        else:
                            S.op("vector", lambda e, a2=a2, h=h, c=c: e.scalar_tensor_tensor(out=a2, in0=rr[:, h, c:c + 1], scalar=-1.0, in1=cs[:, h, 63:64], op0=ALU.mult, op1=ALU.subtract),
                                 reads=[r_rr, r_cs], updates=[r_aa])
            yield 'S'
            S.op("scalar", lambda e: e.activation(out=aa[:, :, :, 0:2], in_=aa[:, :, :, 0:2], func=AF.Exp), reads=[], updates=[r_aa])
            yield 'S'
            S.op("vector", lambda e: e.tensor_tensor(out=aa[:, :, :, 2], in0=aa[:, :, :, 0], in1=aa[:, :, :, 1], op=ALU.mult), reads=[], updates=[r_aa])
            for h in range(4):
                for c in range(2):
                    S.op("vector", lambda e, h=h, c=c, u=u: e.tensor_scalar(out=E1[:, h, c * 64:(c + 1) * 64], in0=u[:, h, c * 64:(c + 1) * 64], scalar1=rr[:, h, c:c + 1], scalar2=None, op0=ALU.subtract),
                         reads=[r_u, r_rr], updates=[r_E1] if (h or c) else (), writes=() if (h or c) else [r_E1])
            yield 'S'
            S.op("scalar", lambda e: e.activation(out=E2[:], in_=E1[:], func=AF.Exp, scale=-1.0), reads=[r_E1], writes=[r_E2])
            S.op("scalar", lambda e: e.activation(out=E1[:], in_=E1[:], func=AF.Exp), reads=[r_E2], updates=[r_E1])
            yield 'S'
            for c in range(2):
                cs_ = slice(c * 64, (c + 1) * 64)
                S.op("vector", lambda e, c=c, cs_=cs_: e.tensor_tensor(out=qtP[:, :, c, cs_], in0=qsb[:, :, cs_], in1=E1[:, :, cs_], op=ALU.mult), reads=[r_qsb, r_E1], updates=[r_qtP])
                S.op("vector", lambda e, c=c, cs_=cs_: e.tensor_tensor(out=ktP[:, :, c, cs_], in0=kk[:, :, cs_], in1=E2[:, :, cs_], op=ALU.mult), reads=[r_kk, r_E2], updates=[r_ktP])
            if debug and dr == 0 and t == 0:
                dtile, r_dt = C.sb([128, 8, 512], F32, "dtile")
                S.op("gpsimd", lambda e: e.memset(dtile[:], 0.0), writes=[r_dt])
                S.op("vector", lambda e: e.tensor_copy(out=dtile[:, 0, 0:4], in_=lb[:]), reads=[r_lb], updates=[r_dt])
                S.op("vector", lambda e: e.tensor_copy(out=dtile[:, 1, :], in_=ff[:].rearrange("p a b -> p (a b)")), reads=[r_ff], updates=[r_dt])
                S.op("vector", lambda e: e.tensor_copy(out=dtile[:, 2, :], in_=cs[:].rearrange("p a b -> p (a b)")), reads=[r_cs], updates=[r_dt])
                S.op("vector", lambda e: e.tensor_copy(out=dtile[:, 3, :], in_=E1[:].rearrange("p a b -> p (a b)")), reads=[r_E1], updates=[r_dt])
                S.op("vector", lambda e: e.tensor_copy(out=dtile[:, 4, :], in_=E2[:].rearrange("p a b -> p (a b)")), reads=[r_E2], updates=[r_dt])
                S.op("vector", lambda e: e.tensor_copy(out=dtile[:, 5, 0:24], in_=aa[:].rearrange("p a b c -> p (a b c)")), reads=[r_aa], updates=[r_dt])
                S.op("vector", lambda e: e.tensor_copy(out=dtile[:, 6, :], in_=qtP[:, 0:2, :, :].rearrange("p a b c -> p (a b c)")), reads=[r_qtP], updates=[r_dt])
                S.op("vector", lambda e: e.tensor_copy(out=dtile[:, 7, :], in_=pQ[:].rearrange("p a b -> p (a b)")), reads=[r_pQ], updates=[r_dt])
                S.dma(dbg, dtile[:], reads=[r_dt])
            yield 'SPLIT'
            corder = [0, 1] if dr == 0 else [1, 0]
            kx = kcnt[0]
            kcnt[0] += 1
            Sp, r_Sp = Spp[kx % 2]
            PT, r_PT = PT_l[kx % 2]
            kTM, r_kTM = kTM_l[kx % 2]
            tU, r_tU = tU_l[kx % 2]
            first = True
            for h in range(4):
                for c in range(2):
                    S.op("tensor", lambda e, c=c, h=h: e.matmul(out=pS[:, h, c * 64:(c + 1) * 64], lhsT=ktP[:, h, c, :], rhs=qtP[:, h, c, c * 64:(c + 1) * 64], start=True, stop=True),
                         reads=[r_ktP, r_qtP], updates=() if first else [r_pS], writes=[r_pS] if first else ())
                    first = False
            yield 'S'
            S.op("vector", lambda e, dr=dr: e.tensor_tensor(out=PT[:], in0=pS[:], in1=cm[:, dr:dr + 1, :].broadcast_to([128, 4, 128]), op=ALU.mult), reads=[r_pS, r_cm], writes=[r_PT])
            yield 'S'
            first = True
            for h in range(4):
                for c in range(2):
                    S.op("tensor", lambda e, c=c, h=h: e.transpose(out=pK[:, h, c, :], in_=ktP[:, h, c, :], identity=idb[:]), reads=[r_ktP, r_idb],
                         updates=() if first else [r_pK], writes=[r_pK] if first else ())
                    first = False
            yield 'S'
            S.op("scalar", lambda e: e.copy(out=kTM[:], in_=pK[:]), reads=[r_pK], writes=[r_kTM])
            yield 'S'
            for ci, c in enumerate(corder):
                for h in range(4):
                    S.op("tensor", lambda e, c=c, h=h: e.matmul(out=pUu[:, h, :], lhsT=kTM[:, h, c, :], rhs=vb[:, h * 128:(h + 1) * 128], start=True, stop=True),
                         reads=[r_kTM, r_vb], updates=[r_pUu] if h else (), writes=() if h else [r_pUu])
                yield 'S'
                a1b = aa[:, :, c, 0:1].broadcast_to([128, 4, 128])
                a2b = aa[:, :, c, 1:2].broadcast_to([128, 4, 128])
                a3b = aa[:, :, c, 2:3].broadcast_to([128, 4, 128])
                S.op("vector", lambda e, c=c, a1b=a1b: e.tensor_tensor(out=Sp[:, c, :, :], in0=Sall[:], in1=a1b, op=ALU.mult),
                     reads=[r_Sall, r_aa], updates=[r_Sp] if ci else (), writes=() if ci else [r_Sp])
                S.op("vector", lambda e, a2b=a2b: e.tensor_tensor(out=tU[:], in0=pUu[:], in1=a2b, op=ALU.mult), reads=[r_pUu, r_aa], writes=[r_tU])
                S.op("vector", lambda e, a3b=a3b: e.tensor_tensor(out=Sall[:], in0=Sall[:], in1=a3b, op=ALU.mult), reads=[r_aa], writes=[r_Sall])
                S.op("gpsimd", lambda e: e.tensor_tensor(out=Sall[:], in0=Sall[:], in1=tU[:], op=ALU.add), reads=[r_tU], writes=[r_Sall])
            yield 'S'
            for h in range(4):
                vv = vb[:, h * 128:(h + 1) * 128]
                S.op("tensor", lambda e, vv=vv, h=h: e.matmul(out=pOo[:, h, :], lhsT=PT[:, h, :], rhs=vv, start=True, stop=False), reads=[r_PT, r_vb],
                     updates=[r_pOo] if h else (), writes=() if h else [r_pOo])
                for c in range(2):
                    S.op("tensor", lambda e, c=c, h=h: e.matmul(out=pOo[:, h, :], lhsT=qtP[:, h, c, :], rhs=Sp[:, c, h, :], start=False, stop=(c == 1)), reads=[r_qtP, r_Sp], updates=[r_pOo])
            yield 'S'
            pOf = pOo[:].rearrange("p h e -> p (h e)")
            if dr == 0:
                S.op("scalar", lambda e, t=t: e.copy(out=ofw[:, t, :], in_=pOf), reads=[r_pOo], writes=[r_ofw[t]])
            else:
                S.op("vector", lambda e, t=t: e.tensor_tensor(out=otot[:], in0=pOf, in1=ofw[:, t, :], op=ALU.add), reads=[r_pOo, r_ofw[t]], writes=[r_otot])
            if debug and dr == 0 and t == 0:
                dt2, r_dt2 = C.sb([128, 8, 512], F32, "dtile2")
                S.op("gpsimd", lambda e: e.memset(dt2[:], 0.0), writes=[r_dt2])
                S.op("vector", lambda e: e.tensor_copy(out=dt2[:, 0, 0:128], in_=PT[:]), reads=[r_PT], updates=[r_dt2])
                S.op("vector", lambda e: e.tensor_copy(out=dt2[:, 1, 0:128], in_=pOo[:]), reads=[r_pOo], updates=[r_dt2])
                S.op("vector", lambda e: e.tensor_copy(out=dt2[:, 2, :], in_=ofw[:, 0, :]), reads=[r_ofw[0]], updates=[r_dt2])
                S.op("vector", lambda e: e.tensor_copy(out=dt2[:, 3, :], in_=vb[:]), reads=[r_vb], updates=[r_dt2])
                S.op("vector", lambda e: e.tensor_copy(out=dt2[:, 4, 0:128], in_=pS[:]), reads=[r_pS], updates=[r_dt2])
                S.op("vector", lambda e: e.tensor_copy(out=dt2[:, 5, 0:256], in_=kTM[:].rearrange("p a b -> p (a b)")), reads=[r_kTM], updates=[r_dt2])
                S.op("vector", lambda e: e.tensor_copy(out=dt2[:, 6, 0:128], in_=Sst[3][0][:]), reads=[Sst[3][1]], updates=[r_dt2])
                S.dma(dbg2, dt2[:], reads=[r_dt2])
            if dr == 1:
                m_t, r_m = mgt[oi % 2]
                S.op("gpsimd", lambda e: e.memset(ssh[:], 0.0), writes=[r_ssh])
                for h in range(4):
                    hs = slice(h * 128, (h + 1) * 128)
                    S.op("scalar", lambda e, hs=hs, h=h: e.activation(out=jk[:], in_=otot[:, hs], func=AF.Square, accum_out=ssh[:, h:h + 1]), reads=[r_otot], writes=[r_jk], updates=[r_ssh])
                S.op("vector", lambda e: e.tensor_scalar(out=ssh[:], in0=ssh[:], scalar1=1.0 / 128, scalar2=EPS, op0=ALU.mult, op1=ALU.add), reads=[r_ssh], writes=[r_ssh])
                S.op("scalar", lambda e: e.activation(out=ssh[:], in_=ssh[:], func=AF.Sqrt), reads=[r_ssh], writes=[r_ssh])
                S.op("vector", lambda e: e.reciprocal(out=ssh[:], in_=ssh[:]), reads=[r_ssh], writes=[r_ssh])
                for h in range(4):
                    hs = slice(h * 128, (h + 1) * 128)
                    S.op("vector", lambda e, hs=hs, h=h: e.scalar_tensor_tensor(out=otot[:, hs], in0=otot[:, hs], scalar=ssh[:, h:h + 1], in1=hn[:], op0=ALU.mult, op1=ALU.mult),
                         reads=[r_ssh, r_hn], updates=[r_otot])
                S.op("vector", lambda e, m_t=m_t: e.tensor_tensor(out=m_t[:], in0=otot[:], in1=sgg[:], op=ALU.mult), reads=[r_otot, r_sgg], writes=[r_m])
                S.dma(mgo[t * 128:(t + 1) * 128, :], m_t[:], reads=[r_m])

        for dr in range(2):
            order = list(range(NT)) if dr == 0 else [1, 0] + list(range(NT - 1, 1, -1))
            zc0 = 512 + dr * 512
            S.op("vector", lambda e: e.memset(Sall[:].rearrange("p a b -> p (a b)"), 0.0), writes=[r_Sall])
            cur = None
            for oi, t in enumerate(order):
                nxt = do_tile(oi, t, dr, zc0)
                a_done = False
                b_done = cur is None
                while not (a_done and b_done):
                    if not a_done:
                        a_done = next(nxt) == 'SPLIT'
                    if not b_done:
                        b_done = next(cur, 'END') == 'END'
                cur = nxt
            while next(cur, 'END') != 'END':
                pass


def odd_consts():
    p = np.arange(128)[:, None]
    f = np.arange(128)[None, :]
    same = (p // 64) == (f // 64)
    cmat = np.stack([(same & (p <= f)), (same & (p >= f))], 1).astype(np.float32)
    return dict(cmat=np.ascontiguousarray(cmat))


def emit_mod(C, cv, aws, abs_, modd):
    nc, S = C.nc, C.S
    with C.scope():
        ct, r_ct = C.sb([128, 8, 2], F32, "ct")
        for b_ in range(2):
            S.dma(ct[:, :, b_], cv[b_, :].rearrange("(c p) -> p c", p=128), updates=[r_ct], allow_slow_non_contiguous=True)
        sc, r_sc = C.sb([128, 8, 2], F32, "sc")
        S.op("scalar", lambda e: e.activation(out=sc[:], in_=ct[:], func=AF.Silu), reads=[r_ct], writes=[r_sc])
        wts = [C.sb([128, 8, 1536], F32, "aw%d" % i) for i in range(2)]
        bt, r_bt = C.sb([2, 6144], F32, "bt")
        ots = [C.sb([2, 6144], F32, "ot%d" % i) for i in range(2)]
        pss = [C.ps([2, 512], F32, "pm%d" % i) for i in range(2)]
        k = 0
        for l in range(DEPTH):
            ot, r_ot = ots[l % 2]
            S.dma(bt[:], abs_[l].partition_broadcast(2), writes=[r_bt])
            for cb in range(4):
                w, r_w = wts[k % 2]
                for c in range(8):
                    S.dma(w[:, c, :], aws[l][c * 128:(c + 1) * 128, cb * 1536:(cb + 1) * 1536], updates=[r_w] if c else (), writes=() if c else [r_w],
                          eng=("sync" if c % 2 == 0 else "gpsimd"))
                for j in range(3):
                    p, r_p = pss[(k * 3 + j) % 2]
                    for c in range(8):
                        S.op("tensor", lambda e, c=c, j=j, p=p, w=w: e.matmul(out=p[:], lhsT=sc[:, c, :], rhs=w[:, c, j * 512:(j + 1) * 512], start=(c == 0), stop=(c == 7)),
                             reads=[r_sc, r_w], updates=[r_p] if c else (), writes=() if c else [r_p])
                    col = cb * 1536 + j * 512
                    S.op("vector", lambda e, p=p, col=col, ot=ot: e.tensor_tensor(out=ot[:, col:col + 512], in0=p[:], in1=bt[:, col:col + 512], op=ALU.add),
                         reads=[r_p, r_bt], updates=[r_ot])
                k += 1
            S.dma(modd[l], ot[:], reads=[r_ot])


def emit_final(C, xsrc, fnw, out):
    nc, S = C.nc, C.S
    with C.scope():
        fw_t, r_fw = C.sb([128, D], F32, "fnw_sb")
        S.dma(fw_t[:], fnw.partition_broadcast(128), writes=[r_fw])
        ssf, r_ssf = C.sb([128, 1], F32, "ssf")
        jk, r_jk = C.sb([128, D], F32, "jk")
        xt = [C.sb([128, D], F32, "fx%d" % i) for i in range(2)]
        ob = [C.sb([128, D], F32, "ob%d" % i) for i in range(2)]
        for t in range(32):
            x_t, r_x = xt[t % 2]
            o_t, r_o = ob[t % 2]
            S.dma(x_t[:], xsrc[256 + t * 128:256 + (t + 1) * 128, :], reads=[C.r_xf[(t + 2) // 8]], writes=[r_x])
            S.op("gpsimd", lambda e: e.memset(ssf[:], 0.0), writes=[r_ssf])
            S.op("scalar", lambda e, x_t=x_t: e.activation(out=jk[:], in_=x_t[:], func=AF.Square, accum_out=ssf[:]), reads=[r_x], writes=[r_jk], updates=[r_ssf])
            S.op("vector", lambda e: e.tensor_scalar(out=ssf[:], in0=ssf[:], scalar1=1.0 / D, scalar2=EPS, op0=ALU.mult, op1=ALU.add), reads=[r_ssf], writes=[r_ssf])
            S.op("scalar", lambda e: e.activation(out=ssf[:], in_=ssf[:], func=AF.Sqrt), reads=[r_ssf], writes=[r_ssf])
            S.op("vector", lambda e: e.reciprocal(out=ssf[:], in_=ssf[:]), reads=[r_ssf], writes=[r_ssf])
            S.op("vector", lambda e, x_t=x_t, o_t=o_t: e.scalar_tensor_tensor(out=o_t[:], in0=x_t[:], scalar=ssf[:, 0:1], in1=fw_t[:], op0=ALU.mult, op1=ALU.mult),
                 reads=[r_x, r_ssf, r_fw], writes=[r_o])
            S.dma(out[t * 128:(t + 1) * 128, :], o_t[:], reads=[r_o], eng="gpsimd")


GROUPS = [[0, 1], [2, 3], [4, 5], [6, 7]]


def build_fused(n_layers=DEPTH, dbg_out=False):
    C = Ctx()
    nc, S = C.nc, C.S
    NTOK = 34 * 128
    xs_in = C.din("xs", [NTOK, D])
    cv = C.din("cv", [2, D])
    aws = [C.din("aw%d" % l, [D, 6144]) for l in range(DEPTH)]
    abs_ = [C.din("ab%d" % l, [1, 6144]) for l in range(DEPTH)]
    nwa = C.din("nwa", [2 * DEPTH, D])
    fnw = C.din("fnw", [1, D])
    wce = [C.din("wce%d" % p, [D, 1536]) for p in range(2)]
    draw = [C.din("draw%d" % p, [1, 4]) for p in range(2)]
    sink = [C.din("sink%d" % p, [1, 2]) for p in range(2)]
    wco = [C.din("wco%d" % p, [D, 2560]) for p in range(2)]
    lbl = C.din("lbl", [4, 512])
    lsel = [C.din("lsel%d" % p, [1, 4]) for p in range(2)]
    hnw = [C.din("hnw%d" % p, [1, 128]) for p in range(2)]
    wo = [C.din("wo%d" % l, [D, D]) for l in range(DEPTH)]
    ropec = C.din("ropec", [NTOK, 128])
    ropes = C.din("ropes", [NTOK, 128])
    cmat_e = C.din("cmat_e", [128, 6, 128])
    crow = C.din("crow", [1, 256])
    ccol = C.din("ccol", [128, 2])
    cmat_o = C.din("cmat_o", [128, 2, 128])
    wr = [C.din("wr%d" % l, [D, 36]) for l in range(DEPTH)]
    br = [C.din("br%d" % l, [1, 36]) for l in range(DEPTH)]
    w1 = [C.din("w1_%d" % l, [16, D, 512]) for l in range(DEPTH)]
    w3 = [C.din("w3_%d" % l, [16, D, 512]) for l in range(DEPTH)]
    w2 = [C.din("w2_%d" % l, [16, 512, D]) for l in range(DEPTH)]
    out = C.dout("out", [L, D])
    if dbg_out:
        xdbg = C.dout("xdbg", [NTOK, D])
    xfull = C.dram("xfull", [NTOK, D])
    ypart = C.dram("ypart", [NTOK, D])
    mgl = C.dram("mgl", [NTOK, 512], BF16)
    mgall = C.dram("mgall", [2 * NTOK, 512], BF16)
    moddt = C.dram("modd", [DEPTH, 2, 6144])
    modd = [moddt[l] for l in range(DEPTH)]
    with C.stack:
        C.prefix = "cp_"
        for t in range(34):
            S.dma(xfull[t * 128:(t + 1) * 128, :], xs_in[t * 128:(t + 1) * 128, :], updates=[C.r_xf[t // 8]], eng=("sync" if t % 2 == 0 else "gpsimd"))
        C.prefix = "mod_"
        emit_mod(C, cv, aws, abs_, modd)
        for l in range(n_layers):
            p = l // 2
            modv = modd[l].rearrange("s (k d) -> s k d", k=6)
            C.prefix = "L%dmix_" % l
            if l % 2 == 0:
                emit_even(C, dict(xs=xfull, modv=modv, nw=nwa[2 * l:2 * l + 1, :], wc=wce[p], ropec=ropec, ropes=ropes, draw=draw[p], sink=sink[p],
                                  cmat=cmat_e, crow=crow, ccol=ccol, mgo=mgl))
            else:
                emit_odd(C, dict(xs=xfull, modv=modv, nw=nwa[2 * l:2 * l + 1, :], wc=wco[p], lbl=lbl, lsel=lsel[p], hnw=hnw[p], cmat=cmat_o, mgo=mgl))
            ag = []
            for st_k in range(0, NTOK, 2048):
                R_k = min(2048, NTOK - st_k)
                ag.append(lambda g, st_k=st_k, R_k=R_k: g.collective_compute("AllGather", ALU.bypass, replica_groups=GROUPS, ins=[mgl[st_k:st_k + R_k, :].opt()],
                                                                              outs=[mgall[2 * st_k:2 * st_k + 2 * R_k, :].opt()]))
            S.collective(ag)
            def ar_chunk(k_):
                st_k = k_ * 1024
                R_k = min(1024, NTOK - st_k)
                S.cc_async(lambda g, st_k=st_k, R_k=R_k: g.collective_compute("AllReduce", ALU.add, replica_groups=GROUPS, ins=[ypart[st_k:st_k + R_k, :].opt()],
                                                                               outs=[xfull[st_k:st_k + R_k, :].opt()]),
                           reads=[C.r_yp[k_]], writes=[C.r_xf[k_]])
            for half in range(2):
                C.prefix = "L%dmoe%d_" % (l, half)
                emit_moe(C, dict(xs=xfull, mgall=mgall, wo=wo[l], modv=modv, nw=nwa[2 * l + 1:2 * l + 2, :], wr=wr[l], br=br[l], w1=w1[l], w3=w3[l], w2=w2[l], ypart=ypart),
                         tiles=list(range(half * 17, (half + 1) * 17)))
                for k_ in ([0, 1] if half == 0 else [2, 3, 4]):
                    ar_chunk(k_)
        C.prefix = "fin_"
        emit_final(C, xfull, fnw, out)
        if dbg_out:
            for t in range(34):
                S.dma(xdbg[t * 128:(t + 1) * 128, :], xfull[t * 128:(t + 1) * 128, :], reads=[C.r_xf[t // 8]])
        S.emit()
    return nc


_NC = {}


def make_in_maps(x, c, ctx, c_ctx, ada_w, ada_b, norm_w, final_norm_w, ev_w_in, ev_w_out, ret_decay_raw, att_sink,
                 od_w_in, od_w_out, hg_lb_logits, hg_norm_w, moe_wg, moe_bg, moe_we, moe_be, moe_w1, moe_w3, moe_w2):
    f32 = np.float32
    A_ = lambda v: np.ascontiguousarray(np.asarray(v, dtype=f32))
    g = {k: np.asarray(v, dtype=f32) for k, v in dict(x=x, c=c, ctx=ctx, c_ctx=c_ctx, ada_w=ada_w, ada_b=ada_b, norm_w=norm_w, final_norm_w=final_norm_w,
                                                         ev_w_in=ev_w_in, ev_w_out=ev_w_out, ret_decay_raw=ret_decay_raw, att_sink=att_sink, od_w_in=od_w_in,
                                                         od_w_out=od_w_out, hg_lb_logits=hg_lb_logits, hg_norm_w=hg_norm_w, moe_wg=moe_wg, moe_bg=moe_bg,
                                                         moe_we=moe_we, moe_be=moe_be, moe_w1=moe_w1, moe_w3=moe_w3, moe_w2=moe_w2).items()}
    ec = even_consts()
    oc = odd_consts()
    shared = dict(nwa=A_(g['norm_w'].reshape(2 * DEPTH, D)), fnw=A_(g['final_norm_w'][None]), ropec=ec['ropec'], ropes=ec['ropes'], cmat_e=ec['cmat'],
                  crow=ec['crow'], ccol=ec['ccol'], cmat_o=oc['cmat'])
    for l in range(DEPTH):
        shared["aw%d" % l] = A_(g['ada_w'][l])
        shared["ab%d" % l] = A_(g['ada_b'][l][None])
    per_s = []
    for s in range(2):
        d = {}
        for p in range(2):
            win = g['ev_w_in'][p]
            d["wce%d" % p] = A_(np.concatenate([win[:, s * 256:(s + 1) * 256], win[:, 512 + s * 256:512 + (s + 1) * 256],
                                                win[:, 2048 + s * 256:2048 + (s + 1) * 256], win[:, 2560 + s * 128:2560 + (s + 1) * 128],
                                                win[:, 1024 + s * 256:1024 + (s + 1) * 256], win[:, 1536 + s * 256:1536 + (s + 1) * 256],
                                                win[:, 2816 + s * 128:2816 + (s + 1) * 128]], 1))
            d["draw%d" % p] = A_(g['ret_decay_raw'][p][:, s * 2:(s + 1) * 2].reshape(1, 4))
            d["sink%d" % p] = A_(g['att_sink'][p][s * 2:(s + 1) * 2][None])
            wino = g['od_w_in'][p]
            d["wco%d" % p] = A_(np.concatenate([wino[:, k * 1024 + s * 512:k * 1024 + (s + 1) * 512] for k in range(5)], 1))
            lo = 2 * p + 1
            d["lsel%d" % p] = np.array([[0.0] + [1.0 if m <= lo else 0.0 for m in range(1, 4)]], f32)
            d["hnw%d" % p] = A_(g['hg_norm_w'][p][None])
        d["lbl"] = A_(g['hg_lb_logits'][:, s * 512:(s + 1) * 512])
        go = [2 * s, 2 * s + 1, 2 * (1 - s), 2 * (1 - s) + 1]
        for l in range(DEPTH):
            d["wr%d" % l] = A_(np.concatenate([g['moe_wg'][l][:, go]] + [g['moe_we'][l][gi] for gi in go], 1))
            d["br%d" % l] = A_(np.concatenate([g['moe_bg'][l][go]] + [g['moe_be'][l][gi] for gi in go])[None])
            d["w1_%d" % l] = A_(g['moe_w1'][l][s * 16:(s + 1) * 16])
            d["w3_%d" % l] = A_(g['moe_w3'][l][s * 16:(s + 1) * 16])
            d["w2_%d" % l] = A_(g['moe_w2'][l][s * 16:(s + 1) * 16])
        per_s.append(d)
    for l in range(DEPTH):
        if l % 2 == 0:
            w = g['ev_w_out'][l // 2]
            shared["wo%d" % l] = A_(np.concatenate([w[0:256], w[512:768], w[256:512], w[768:1024]], 0))
        else:
            shared["wo%d" % l] = A_(g['od_w_out'][l // 2])
    maps = []
    for i in range(8):
        b, s = i // 2, i % 2
        d = dict(shared)
        d.update(per_s[s])
        d["xs"] = A_(np.concatenate([g['ctx'][b], g['x'][b]], 0))
        d["cv"] = A_(np.stack([g['c_ctx'], g['c'][b]]))
        maps.append(d)
    return maps


def kernel(x, c, ctx, c_ctx, ada_w, ada_b, norm_w, final_norm_w, ev_w_in, ev_w_out, ret_decay_raw, att_sink,
           od_w_in, od_w_out, hg_lb_logits, hg_norm_w, moe_wg, moe_bg, moe_we, moe_be, moe_w1, moe_w3, moe_w2):
    if "fused" not in _NC:
        _NC["fused"] = build_fused()
    maps = make_in_maps(x, c, ctx, c_ctx, ada_w, ada_b, norm_w, final_norm_w, ev_w_in, ev_w_out, ret_decay_raw, att_sink,
                        od_w_in, od_w_out, hg_lb_logits, hg_norm_w, moe_wg, moe_bg, moe_we, moe_be, moe_w1, moe_w3, moe_w2)
    res = run_bass_kernel_spmd(_NC["fused"], maps, core_ids=list(range(8)))
    return np.stack([res.results[2 * b]["out"] for b in range(B)], 0).astype(np.float32)
```

```python
import contextlib
import numpy as np
import ml_dtypes
import concourse.bass as bass
import concourse.mybir as mybir
from concourse.bass_utils import run_bass_kernel_spmd

F32 = mybir.dt.float32
BF16 = mybir.dt.bfloat16
ALU = mybir.AluOpType
AF = mybir.ActivationFunctionType
AX = mybir.AxisListType
ENGINES = ("tensor", "vector", "scalar", "gpsimd", "sync")

D = 1024
L = 4096
NCTX = 256
B = 4
DEPTH = 4
EPS = 1e-6
NEG = -1e30


class Res:
    __slots__ = ("name", "writers", "readers")

    def __init__(self, name=""):
        self.name = name
        self.writers = []
        self.readers = []


class Op:
    __slots__ = ("eng", "fn", "deps", "sig", "sigval", "is_dma", "dsem", "dval", "cc", "ccval")

    def __init__(self, eng, fn, is_dma=False):
        self.eng = eng
        self.fn = fn
        self.deps = []
        self.sig = False
        self.sigval = 0
        self.is_dma = is_dma
        self.dsem = None
        self.dval = 0
        self.cc = False
        self.ccval = 0


class Sched:
    def __init__(self, nc, n_dma_sems=32):
        self.nc = nc
        self.ops = {e: [] for e in ENGINES}
        self.n_dma_sems = n_dma_sems
        self.dma_rr = 0
        self.dma_rr2 = 0
        self.n_sync_sems = 20
        self.dma_last = [None] * n_dma_sems
        self.dma_cnt = [0] * n_dma_sems
        self.nops = 0
        self.cc_count = 0

    def _add_dep(self, op, dep):
        if dep is None or dep is op:
            return
        if dep.eng == op.eng and not dep.is_dma and not op.is_dma and op.eng == "tensor":
            return
        if dep not in op.deps:
            op.deps.append(dep)
            dep.sig = True

    def op(self, eng, fn, reads=(), writes=(), updates=(), is_dma=False):
        o = Op(eng, fn, is_dma)
        for r in reads:
            for w in r.writers:
                self._add_dep(o, w)
        for r in list(writes) + list(updates):
            for w in r.writers:
                self._add_dep(o, w)
            for rd in r.readers:
                if rd.eng == eng and not rd.is_dma and not is_dma:
                    continue
                self._add_dep(o, rd)
        if is_dma:
            if eng == "sync":
                k = self.dma_rr % self.n_sync_sems
                self.dma_rr += 1
            else:
                k = self.n_sync_sems + self.dma_rr2 % (self.n_dma_sems - self.n_sync_sems)
                self.dma_rr2 += 1
            self._add_dep(o, self.dma_last[k])
            self.dma_cnt[k] += 16
            o.dsem = k
            o.dval = self.dma_cnt[k]
            self.dma_last[k] = o
        for r in reads:
            r.readers = [x for x in r.readers if not (x.eng == eng and not x.is_dma and not is_dma)] + [o]
        for r in writes:
            r.writers = [o]
            r.readers = []
        for r in updates:
            r.writers = [x for x in r.writers if not (x.eng == eng and not x.is_dma and not is_dma)] + [o]
            r.readers = []
        self.ops[eng].append(o)
        self.nops += 1
        return o

    def barrier(self):
        lasts = []
        for e in ENGINES:
            for o in reversed(self.ops[e]):
                if not o.is_dma and not o.cc:
                    lasts.append(o)
                    break
        lasts += [o for o in self.dma_last if o is not None]
        for e in ENGINES:
            o = Op(e, lambda eng: eng.nop())
            for d in lasts:
                if d.eng == e and not d.is_dma:
                    continue
                if d not in o.deps:
                    o.deps.append(d)
                    d.sig = True
            self.ops[e].append(o)

    def collective(self, fn):
        self.barrier()
        fns = fn if isinstance(fn, (list, tuple)) else [fn]
        for f in fns:
            o = Op("gpsimd", f)
            o.cc = True
            self.cc_count += 1
            o.ccval = self.cc_count
            self.ops["gpsimd"].append(o)
        for e in ENGINES:
            w = Op(e, lambda eng: eng.nop())
            w.deps.append(o)
            self.ops[e].append(w)

    def cc_async(self, fn, reads=(), writes=()):
        o = self.op("gpsimd", fn, reads=reads, writes=writes)
        o.cc = True
        self.cc_count += 1
        o.ccval = self.cc_count
        return o

    def dma(self, out, in_, reads=(), writes=(), updates=(), eng="sync", **kw):
        return self.op(eng, lambda e: e.dma_start(out=out, in_=in_, **kw), reads, writes, updates, is_dma=True)

    def emit(self):
        nc = self.nc
        cnt = {e: 0 for e in ENGINES}
        for e in ENGINES:
            for o in self.ops[e]:
                if o.sig and not o.is_dma and not o.cc:
                    cnt[e] += 1
                    o.sigval = cnt[e]
        with contextlib.ExitStack() as st:
            esem = {e: st.enter_context(nc.semaphore("s_" + e)) for e in ENGINES}
            dsem = [st.enter_context(nc.semaphore("d_%d" % i)) for i in range(self.n_dma_sems)]
            ccsem = st.enter_context(nc.semaphore("s_cc"))
            block = st.enter_context(nc.Block())

            def make(ename):
                def body(eng):
                    waited = {}
                    for o in self.ops[ename]:
                        for d in o.deps:
                            if d.cc:
                                key, val, sem = ("c", 0), d.ccval, ccsem
                            elif d.is_dma:
                                key, val, sem = ("d", d.dsem), d.dval, dsem[d.dsem]
                            else:
                                key, val, sem = ("e", d.eng), d.sigval, esem[d.eng]
                            if waited.get(key, 0) < val:
                                eng.wait_ge(sem, val)
                                waited[key] = val
                        ins = o.fn(eng)
                        if o.cc:
                            ins.then_inc(ccsem, 1)
                        elif o.is_dma:
                            ins.then_inc(dsem[o.dsem], 16)
                        elif o.sig:
                            ins.then_inc(esem[ename], 1)
                    if ename == "sync":
                        for k in range(self.n_dma_sems):
                            if self.dma_cnt[k] > 0:
                                eng.wait_ge(dsem[k], self.dma_cnt[k])
                        for e2 in ENGINES:
                            if e2 != "sync" and cnt[e2] > 0:
                                eng.wait_ge(esem[e2], cnt[e2])
                return body

            for e in ENGINES:
                getattr(block, e)(make(e))


class Ctx:
    def __init__(self, name=""):
        self.nc = bass.Bass("TRN2", target_bir_lowering=False)
        self.S = Sched(self.nc)
        self.stack = contextlib.ExitStack()
        self.n = 0
        self.prefix = ""
        self.r_xf = [Res("xf%d" % k) for k in range(5)]
        self.r_yp = [Res("yp%d" % k) for k in range(5)]

    @contextlib.contextmanager
    def scope(self):
        saved = self.stack
        st = contextlib.ExitStack()
        self.stack = st
        with st:
            yield
        self.stack = saved
        self.S.barrier()

    def dram(self, name, shape, dt=F32):
        return self.nc.dram_tensor(name, list(shape), dt).ap()

    def sb(self, shape, dt, name=None):
        self.n += 1
        nm = self.prefix + (name or "sb") + "_%d" % self.n
        t = self.stack.enter_context(self.nc.sbuf_tensor(nm, list(shape), dt))
        return t, Res(nm)

    def ps(self, shape, dt, name=None):
        self.n += 1
        full = 512 if dt == F32 else 1024
        nm = self.prefix + (name or "ps") + "_%d" % self.n
        t = self.stack.enter_context(self.nc.psum_tensor(nm, [128, full], dt))
        n = int(np.prod(shape[1:]))
        v = t[0:shape[0], 0:n]
        if len(shape) == 3:
            v = v.rearrange("p (a b) -> p a b", a=shape[1])
        return v, Res(name or ("ps%d" % self.n))

    def din(self, name, shape, dt=F32):
        return self.nc.dram_tensor(name, list(shape), dt, kind="ExternalInput").ap()

    def dout(self, name, shape, dt=F32):
        return self.nc.dram_tensor(name, list(shape), dt, kind="ExternalOutput").ap()


def make_identity(C, dt=BF16):
    S = C.S
    idf, r_idf = C.sb([128, 128], F32)
    S.op("gpsimd", lambda e: e.memset(idf[:], 0.0), writes=[r_idf])
    S.op("gpsimd", lambda e: e.affine_select(out=idf[:], in_=idf[:], pattern=[[-1, 128]], compare_op=ALU.not_equal,
                                             fill=1.0, base=0, channel_multiplier=1), updates=[r_idf])
    if dt == F32:
        return idf, r_idf
    idb, r_idb = C.sb([128, 128], dt)
    S.op("vector", lambda e: e.tensor_copy(out=idb[:], in_=idf[:]), reads=[r_idf], writes=[r_idb])
    return idb, r_idb, idf, r_idf


def emit_moe(C, A, tiles, NEXP=16):
    nc, S = C.nc, C.S
    NT = len(tiles)
    NTOK = NT * 128
    skip_outproj = False
    xs, mgall, wo, modv, nw, wr, br, w1, w3, w2, ypart = (A[k] for k in "xs mgall wo modv nw wr br w1 w3 w2 ypart".split())
    with C.scope():
        idb, r_idb, idf, r_idf = make_identity(C)
        acc, _ = C.sb([128, NT, D], F32, "acc")
        r_acc = [[Res("acc%d_%d" % (t, h)) for h in range(2)] for t in range(NT)]
        h2T, _ = C.sb([128, 8, NTOK], BF16, "h2T")
        r_h2T = [Res("h2T%d" % t) for t in range(NT)]
        gates, _ = C.sb([128, NT, 32], F32, "gates")
        r_gates = [Res("g%d" % t) for t in range(NT)]
        gmlp = [C.sb([128, D], F32, "gmlp%d" % i) for i in range(2)]
        for i in range(2):
            S.dma(gmlp[i][0][:], modv[i, 5:6, :].partition_broadcast(128), writes=[gmlp[i][1]])
        wrt, r_wrt = C.sb([128, 8, 36], F32, "wrt")
        S.dma(wrt[:], wr.rearrange("(c p) j -> p c j", p=128), writes=[r_wrt])
        brt, r_brt = C.sb([128, 36], F32, "brt")
        S.dma(brt[:], br.partition_broadcast(128), writes=[r_brt])

        stA = contextlib.ExitStack()
        C_stack_saved = C.stack
        C.stack = stA
        with stA:
            wob, r_wob = C.sb([128, 8, D], BF16, "wob")
            if not skip_outproj:
                load_weight_bf16(C, wob, r_wob, wo, D)
            gmsa, r_gmsa = C.sb([128, D], F32, "gmsa")
            w2e, r_w2e = C.sb([128, D], F32, "w2e")
            sh2, r_sh2 = C.sb([128, D], F32, "sh2")
            nwt, r_nwt = C.sb([128, D], F32, "nwt")
            S.dma(nwt[:], nw.partition_broadcast(128), writes=[r_nwt])
            xt = [C.sb([128, D], F32, "xt%d" % i) for i in range(2)]
            mgt = [C.sb([128, D], BF16, "mgt%d" % i) for i in range(2)]
            mgT = [C.sb([128, 8, 128], BF16, "mgT%d" % i) for i in range(2)]
            junk, r_junk = C.sb([128, D], F32, "junk")
            tmpA_l = [C.sb([128, D], F32, "tmpA%d" % i) for i in range(2)]
            h2f_l = [C.sb([128, D], F32, "h2f%d" % i) for i in range(2)]
            h2b_l = [C.sb([128, D], BF16, "h2b%d" % i) for i in range(2)]
            h2Tf_l = [C.sb([128, 8, 128], F32, "h2Tf%d" % i) for i in range(2)]
            sm_l = [{k: C.sb([128, n], F32, "sm%d_" % i + k) for k, n in
                     dict(ss=1, rstd=1, lg=36, m4=1, ng=1, ex4=4, se=1, gp=1, oh=4, pen=4, msk=32, top8=8, sel=32, nt1=1, d21=1,
                          e21=1, coef=1, wf=32).items()} for i in range(2)]
            pT = [C.ps([128, 8, 128], BF16, "pT%d" % i) for i in range(2)]
            pTf = [C.ps([128, 4, 128], F32, "pTf%d" % i) for i in range(2)]
            pY = [C.ps([128, 512], F32, "pY%d" % i) for i in range(2)]
            pL, r_pL = C.ps([128, 36], F32, "pL")
            prev = [None]

            def do_tile(t):
                gt = tiles[t]
                tmpA, r_tmpA = tmpA_l[t % 2]
                h2f, r_h2f = h2f_l[t % 2]
                h2b, r_h2b = h2b_l[t % 2]
                h2Tf, r_h2Tf = h2Tf_l[t % 2]
                sm = sm_l[t % 2]
                si = 0 if gt < 2 else 1
                if si != prev[0]:
                    prev[0] = si
                    S.dma(gmsa[:], modv[si, 2:3, :].partition_broadcast(128), writes=[r_gmsa])
                    S.dma(sh2[:], modv[si, 3:4, :].partition_broadcast(128), writes=[r_sh2])
                    S.dma(w2e[:], modv[si, 4:5, :].partition_broadcast(128), writes=[r_w2e])
                    S.op("vector", lambda e: e.scalar_tensor_tensor(out=w2e[:], in0=w2e[:], scalar=1.0, in1=nwt[:], op0=ALU.add, op1=ALU.mult),
                         reads=[r_w2e, r_nwt], writes=[r_w2e])
                x_t, r_x = xt[t % 2]
                S.dma(x_t[:], xs[gt * 128:(gt + 1) * 128, :], reads=[C.r_xf[gt // 8]], writes=[r_x])
                a_t = acc[:, t, :]
                ra = r_acc[t]
                if not skip_outproj:
                    m_t, r_m = mgt[t % 2]
                    mT, r_mT = mgT[t % 2]
                    p_T, r_pT = pT[t % 2]
                    st_k = (gt // 16) * 2048
                    R_k = min(2048, 4352 - st_k)
                    r0 = 2 * st_k + gt * 128 - st_k
                    S.dma(m_t[:, 0:512], mgall[r0:r0 + 128, :], writes=[r_m], eng="gpsimd")
                    S.dma(m_t[:, 512:1024], mgall[r0 + R_k:r0 + R_k + 128, :], updates=[r_m], eng="gpsimd")
                    for c in range(8):
                        S.op("tensor", lambda e, c=c, p_T=p_T, m_t=m_t: e.transpose(out=p_T[:, c, :], in_=m_t[:, c * 128:(c + 1) * 128], identity=idb[:]),
                             reads=[r_m, r_idb], updates=[r_pT] if c else (), writes=() if c else [r_pT])
                    S.op("scalar", lambda e, mT=mT, p_T=p_T: e.copy(out=mT[:], in_=p_T[:]), reads=[r_pT], writes=[r_mT])
                    for hf in range(2):
                        p_Y, r_pY = pY[hf]
                        for c in range(8):
                            S.op("tensor", lambda e, c=c, hf=hf, p_Y=p_Y, mT=mT: e.matmul(out=p_Y[:], lhsT=mT[:, c, :], rhs=wob[:, c, hf * 512:(hf + 1) * 512],
                                                                                         start=(c == 0), stop=(c == 7)),
                                 reads=[r_mT, r_wob], updates=[r_pY] if c else (), writes=() if c else [r_pY])
                        sl = slice(hf * 512, (hf + 1) * 512)
                        S.op("vector", lambda e, p_Y=p_Y, sl=sl: e.tensor_tensor(out=tmpA[:, sl], in0=p_Y[:], in1=gmsa[:, sl], op=ALU.mult),
                             reads=[r_pY, r_gmsa], updates=[r_tmpA])
                        S.op("gpsimd", lambda e, sl=sl, a_t=a_t, x_t=x_t: e.tensor_tensor(out=a_t[:, sl], in0=tmpA[:, sl], in1=x_t[:, sl], op=ALU.add),
                             reads=[r_tmpA, r_x], writes=[ra[hf]])
                else:
                    for hf in range(2):
                        sl = slice(hf * 512, (hf + 1) * 512)
                        S.op("gpsimd", lambda e, sl=sl, a_t=a_t, x_t=x_t: e.tensor_copy(out=a_t[:, sl], in_=x_t[:, sl]), reads=[r_x], writes=[ra[hf]])
                ss, r_ss = sm["ss"]
                rstd, r_rstd = sm["rstd"]
                T_se, R_se = sm["se"]
                S.op("gpsimd", lambda e: e.memset(ss[:], 0.0), writes=[r_ss])
                S.op("gpsimd", lambda e: e.memset(T_se[:], 0.0), writes=[R_se])
                S.op("scalar", lambda e, a_t=a_t: e.activation(out=junk[:], in_=a_t, func=AF.Square, accum_out=ss[:]),
                     reads=ra, writes=[r_junk], updates=[r_ss])
                S.op("vector", lambda e: e.tensor_scalar(out=rstd[:], in0=ss[:], scalar1=1.0 / D, scalar2=EPS, op0=ALU.mult, op1=ALU.add),
                     reads=[r_ss], writes=[r_rstd])
                S.op("scalar", lambda e: e.activation(out=rstd[:], in_=rstd[:], func=AF.Sqrt), reads=[r_rstd], writes=[r_rstd])
                S.op("vector", lambda e: e.reciprocal(out=rstd[:], in_=rstd[:]), reads=[r_rstd], writes=[r_rstd])
                S.op("vector", lambda e, a_t=a_t: e.scalar_tensor_tensor(out=h2f[:], in0=a_t, scalar=rstd[:, 0:1], in1=w2e[:], op0=ALU.mult, op1=ALU.mult),
                     reads=ra + [r_rstd, r_w2e], writes=[r_h2f])
                S.op("gpsimd", lambda e: e.tensor_tensor(out=h2f[:], in0=h2f[:], in1=sh2[:], op=ALU.add), reads=[r_h2f, r_sh2], writes=[r_h2f])
                for hf in range(2):
                    sl = slice(hf * 512, (hf + 1) * 512)
                    S.op("scalar", lambda e, a_t=a_t, sl=sl: e.mul(out=a_t[:, sl], in_=a_t[:, sl], mul=0.5), reads=[], writes=[ra[hf]])
                S.op("vector", lambda e: e.tensor_copy(out=h2b[:], in_=h2f[:]), reads=[r_h2f], writes=[r_h2b])
                p_T, r_pT = pT[(t + 1) % 2]
                for c in range(8):
                    S.op("tensor", lambda e, c=c, p_T=p_T: e.transpose(out=p_T[:, c, :], in_=h2b[:, c * 128:(c + 1) * 128], identity=idb[:]),
                         reads=[r_h2b, r_idb], updates=[r_pT] if c else (), writes=() if c else [r_pT])
                S.op("scalar", lambda e, p_T=p_T, t=t: e.copy(out=h2T[:, :, t * 128:(t + 1) * 128], in_=p_T[:]), reads=[r_pT], writes=[r_h2T[t]])
                for g4 in range(2):
                    p_f, r_pf = pTf[g4]
                    for c in range(4):
                        cc = g4 * 4 + c
                        S.op("tensor", lambda e, c=c, cc=cc, p_f=p_f: e.transpose(out=p_f[:, c, :], in_=h2f[:, cc * 128:(cc + 1) * 128], identity=idf[:]),
                             reads=[r_h2f, r_idf], updates=[r_pf] if c else (), writes=() if c else [r_pf])
                    S.op("vector" if g4 else "scalar", (lambda e, p_f=p_f, g4=g4: e.tensor_copy(out=h2Tf[:, g4 * 4:(g4 + 1) * 4, :], in_=p_f[:])) if g4 else
                         (lambda e, p_f=p_f, g4=g4: e.copy(out=h2Tf[:, g4 * 4:(g4 + 1) * 4, :], in_=p_f[:])),
                         reads=[r_pf], updates=[r_h2Tf])
                for c in range(8):
                    S.op("tensor", lambda e, c=c: e.matmul(out=pL[:], lhsT=h2Tf[:, c, :], rhs=wrt[:, c, :], start=(c == 0), stop=(c == 7)),
                         reads=[r_h2Tf, r_wrt], updates=[r_pL] if c else (), writes=() if c else [r_pL])
                def T(k):
                    return sm[k][0]

                def Rr(k):
                    return sm[k][1]
                V = lambda fn, reads, writes: S.op("vector", fn, reads=reads, writes=writes)
                V(lambda e: e.tensor_tensor(out=T("lg")[:], in0=pL[:], in1=brt[:], op=ALU.add), [r_pL, r_brt], [Rr("lg")])
                V(lambda e: e.reduce_max(out=T("m4")[:], in_=T("lg")[:, 0:4], axis=AX.X), [Rr("lg")], [Rr("m4")])
                V(lambda e: e.tensor_scalar(out=T("ng")[:], in0=T("m4")[:], scalar1=-1.0, scalar2=None, op0=ALU.mult), [Rr("m4")], [Rr("ng")])
                S.op("scalar", lambda e: e.activation(out=T("ex4")[:], in_=T("lg")[:, 0:4], func=AF.Exp, bias=T("ng")[:, 0:1], scale=1.0, accum_out=T("se")[:]),
                     reads=[Rr("lg"), Rr("ng")], writes=[Rr("ex4")], updates=[Rr("se")])
                V(lambda e: e.reciprocal(out=T("gp")[:], in_=T("se")[:]), [Rr("se")], [Rr("gp")])
                V(lambda e: e.tensor_scalar(out=T("oh")[:], in0=T("lg")[:, 0:4], scalar1=T("m4")[:, 0:1], scalar2=None, op0=ALU.is_ge), [Rr("lg"), Rr("m4")], [Rr("oh")])
                V(lambda e: e.tensor_scalar(out=T("pen")[:], in0=T("oh")[:], scalar1=-1.0, scalar2=-NEG, op0=ALU.add, op1=ALU.mult), [Rr("oh")], [Rr("pen")])
                V(lambda e: e.tensor_tensor(out=T("msk")[:].rearrange("p (g x) -> p g x", g=4), in0=T("lg")[:, 4:36].rearrange("p (g x) -> p g x", g=4),
                                            in1=T("pen")[:].rearrange("p (g o) -> p g o", o=1).broadcast_to([128, 4, 8]), op=ALU.add),
                  [Rr("lg"), Rr("pen")], [Rr("msk")])
                V(lambda e: e.max(out=T("top8")[:], in_=T("msk")[:]), [Rr("msk")], [Rr("top8")])
                V(lambda e: e.tensor_scalar(out=T("sel")[:], in0=T("msk")[:], scalar1=T("top8")[:, 1:2], scalar2=None, op0=ALU.is_ge), [Rr("msk"), Rr("top8")], [Rr("sel")])
                V(lambda e: e.tensor_scalar(out=T("nt1")[:], in0=T("top8")[:, 0:1], scalar1=-1.0, scalar2=None, op0=ALU.mult), [Rr("top8")], [Rr("nt1")])
                V(lambda e: e.tensor_tensor(out=T("d21")[:], in0=T("top8")[:, 1:2], in1=T("top8")[:, 0:1], op=ALU.subtract), [Rr("top8")], [Rr("d21")])
                S.op("scalar", lambda e: e.activation(out=T("e21")[:], in_=T("d21")[:], func=AF.Exp), reads=[Rr("d21")], writes=[Rr("e21")])
                S.op("scalar", lambda e: e.activation(out=T("wf")[:], in_=T("msk")[:], func=AF.Exp, bias=T("nt1")[:, 0:1], scale=1.0),
                     reads=[Rr("msk"), Rr("nt1")], writes=[Rr("wf")])
                V(lambda e: e.tensor_scalar(out=T("coef")[:], in0=T("e21")[:], scalar1=1.0, scalar2=None, op0=ALU.add), [Rr("e21")], [Rr("coef")])
                V(lambda e: e.reciprocal(out=T("coef")[:], in_=T("coef")[:]), [Rr("coef")], [Rr("coef")])
                V(lambda e: e.tensor_tensor(out=T("coef")[:], in0=T("coef")[:], in1=T("gp")[:], op=ALU.mult), [Rr("coef"), Rr("gp")], [Rr("coef")])
                V(lambda e, t=t: e.scalar_tensor_tensor(out=gates[:, t, :], in0=T("wf")[:], scalar=T("coef")[:, 0:1], in1=T("sel")[:], op0=ALU.mult, op1=ALU.mult),
                  [Rr("wf"), Rr("coef"), Rr("sel")], [r_gates[t]])
            for t in range(NT):
                do_tile(t)
        C.stack = C_stack_saved
        S.barrier()

        stB = contextlib.ExitStack()
        C.stack = stB
        with stB:
            NST = 3
            stg = [C.sb([128, 4, 512], F32, "stg%d" % i) for i in range(NST)]
            w1b = [C.sb([128, 8, 512], BF16, "w1b%d" % i) for i in range(2)]
            w3b = [C.sb([128, 8, 512], BF16, "w3b%d" % i) for i in range(2)]
            w2b = [C.sb([128, 4, D], BF16, "w2b%d" % i) for i in range(2)]
            sg = [C.sb([128, 512], F32, "sg%d" % i) for i in range(2)]
            act = [C.sb([128, 4, 512], BF16, "act%d" % i) for i in range(2)]
            tmpB = [C.sb([128, 512], F32, "tmpB%d" % i) for i in range(2)]
            pU1 = [C.ps([128, 512], F32, "pU1_%d" % i) for i in range(2)]
            pU3 = [C.ps([128, 512], F32, "pU3_%d" % i) for i in range(2)]
            pYB = [C.ps([128, 512], F32, "pYB%d" % i) for i in range(3)]
            chunks = []
            t0 = 0
            while t0 < NT:
                chunks.append(list(range(t0, min(t0 + 4, NT))))
                t0 += 4
            si_ = 0
            ny = 0
            nact = 0
            sic = [0]

            def load_piece(ex_, pi):
                pb_ = ex_ % 2
                if pi < 4:
                    wsrc, dst = ((w1, w1b[pb_]), (w3, w3b[pb_]))[pi // 2]
                    half = pi % 2
                    src = wsrc[ex_, half * 512:(half + 1) * 512, :].rearrange("(c p) f -> p c f", p=128)
                    dstap, r_dst = dst[0][:, half * 4:(half + 1) * 4, :], dst[1]
                else:
                    half = pi - 4
                    src = w2[ex_, half * 256:(half + 1) * 256, :].rearrange("(c p) f -> p c f", p=128)
                    dstap, r_dst = w2b[pb_][0][:, half * 2:(half + 1) * 2, :], w2b[pb_][1]
                s_t, r_s = stg[sic[0] % NST]
                sic[0] += 1
                sv = s_t[:] if pi < 4 else s_t[:].rearrange("p (c a) f -> p c (a f)", a=2)
                S.dma(sv, src, writes=[r_s], eng="sync")
                pending.append((sv, r_s, dstap, r_dst))

            pending = []

            def cast_pending():
                while pending:
                    sv, r_s, dstap, r_dst = pending.pop(0)
                    S.op("scalar", lambda e, sv=sv, dstap=dstap: e.copy(out=dstap, in_=sv), reads=[r_s], updates=[r_dst])

            for pi in range(6):
                load_piece(0, pi)
                if len(pending) >= 2:
                    cast_pending()
            cast_pending()
            sched_next = {0: [0, 1], 1: [2], 2: [3, 4], 3: [5]} if len(chunks) >= 4 else {0: [0, 1, 2, 3, 4, 5]}
            for ex in range(NEXP):
                pb = ex % 2
                W1, rW1 = w1b[pb]
                W3, rW3 = w3b[pb]
                W2, rW2 = w2b[pb]
                for ci_, ch in enumerate(chunks):
                    cast_pending()
                    if ex + 1 < NEXP:
                        for pi in sched_next.get(ci_, []):
                            load_piece(ex + 1, pi)
                    n = len(ch) * 128
                    c0 = ch[0] * 128
                    a_t, r_a = act[nact % 2]
                    nact += 1
                    rh = [r_h2T[t] for t in ch]
                    for fc in range(4):
                        p1, r_p1 = pU1[fc % 2]
                        p3, r_p3 = pU3[fc % 2]
                        for c in range(8):
                            S.op("tensor", lambda e, c=c, fc=fc, p1=p1, W1=W1, c0=c0, n=n: e.matmul(out=p1[:, 0:n], lhsT=W1[:, c, fc * 128:(fc + 1) * 128], rhs=h2T[:, c, c0:c0 + n],
                                                                                                       start=(c == 0), stop=(c == 7)),
                                 reads=rh + [rW1], updates=[r_p1] if c else (), writes=() if c else [r_p1])
                        for c in range(8):
                            S.op("tensor", lambda e, c=c, fc=fc, p3=p3, W3=W3, c0=c0, n=n: e.matmul(out=p3[:, 0:n], lhsT=W3[:, c, fc * 128:(fc + 1) * 128], rhs=h2T[:, c, c0:c0 + n],
                                                                                                       start=(c == 0), stop=(c == 7)),
                                 reads=rh + [rW3], updates=[r_p3] if c else (), writes=() if c else [r_p3])
                        s_g, r_sg = sg[fc % 2]
                        S.op("scalar", lambda e, s_g=s_g, p1=p1, n=n: e.activation(out=s_g[:, 0:n], in_=p1[:, 0:n], func=AF.Silu), reads=[r_p1], writes=[r_sg])
                        S.op("vector", lambda e, s_g=s_g, p3=p3, a_t=a_t, fc=fc, n=n: e.tensor_tensor(out=a_t[:, fc, 0:n], in0=s_g[:, 0:n], in1=p3[:, 0:n], op=ALU.mult),
                             reads=[r_sg, r_p3], updates=[r_a] if fc else (), writes=() if fc else [r_a])
                    for ti, t in enumerate(ch):
                        gm = gmlp[0 if tiles[t] < 2 else 1]
                        for hf in range(2):
                            p_y, r_py = pYB[ny % 3]
                            tb, r_tb = tmpB[ny % 2]
                            ny += 1
                            for fc in range(4):
                                S.op("tensor", lambda e, fc=fc, p_y=p_y, a_t=a_t, ti=ti, W2=W2, hf=hf: e.matmul(out=p_y[:], lhsT=a_t[:, fc, ti * 128:(ti + 1) * 128],
                                                                                                                  rhs=W2[:, fc, hf * 512:(hf + 1) * 512], start=(fc == 0), stop=(fc == 3)),
                                     reads=[r_a, rW2], updates=[r_py] if fc else (), writes=() if fc else [r_py])
                            sl = slice(hf * 512, (hf + 1) * 512)
                            S.op("vector", lambda e, p_y=p_y, tb=tb, t=t, ex=ex, gm=gm, sl=sl: e.scalar_tensor_tensor(out=tb[:], in0=p_y[:], scalar=gates[:, t, ex:ex + 1], in1=gm[0][:, sl],
                                                                                                                        op0=ALU.mult, op1=ALU.mult),
                                 reads=[r_py, r_gates[t], gm[1]], writes=[r_tb])
                            S.op("gpsimd", lambda e, tb=tb, t=t, sl=sl: e.tensor_tensor(out=acc[:, t, sl], in0=acc[:, t, sl], in1=tb[:], op=ALU.add),
                                 reads=[r_tb], writes=[r_acc[t][hf]])
                cast_pending()
        C.stack = C_stack_saved
        for t in range(NT):
            gt = tiles[t]
            S.dma(ypart[gt * 128:(gt + 1) * 128, :], acc[:, t, :], reads=r_acc[t], updates=[C.r_yp[gt // 8]])


class NormIn:
    def __init__(self, C, xs, modv, nw, idb, r_idb, nps=2):
        self.C, self.xs, self.modv = C, xs, modv
        S = C.S
        self.idb, self.r_idb = idb, r_idb
        self.w1e, self.r_w1e = C.sb([128, D], F32, "w1e")
        self.sh1, self.r_sh1 = C.sb([128, D], F32, "sh1")
        self.nwt, self.r_nwt = C.sb([128, D], F32, "nwt1")
        S.dma(self.nwt[:], nw.partition_broadcast(128), writes=[self.r_nwt])
        self.xt = [C.sb([128, D], F32, "nxt%d" % i) for i in range(2)]
        self.tmp_l = [C.sb([128, D], F32, "ntmp%d" % i) for i in range(2)]
        self.hb_l = [C.sb([128, D], BF16, "nhb%d" % i) for i in range(2)]
        self.ss_l = [C.sb([128, 1], F32, "nss%d" % i) for i in range(2)]
        self.rstd_l = [C.sb([128, 1], F32, "nrstd%d" % i) for i in range(2)]
        self.pT = [C.ps([128, 8, 128], BF16, "npT%d" % i) for i in range(nps)]
        if nps == 1:
            self.pT = self.pT * 2
        self.hT = [C.sb([128, 8, 128], BF16, "nhT%d" % i) for i in range(2)]
        self.cur = None
        self.k = 0

    def load_mod(self, si):
        S, modv = self.C.S, self.modv
        S.dma(self.sh1[:], modv[si, 0:1, :].partition_broadcast(128), writes=[self.r_sh1])
        S.dma(self.w1e[:], modv[si, 1:2, :].partition_broadcast(128), writes=[self.r_w1e])
        S.op("vector", lambda e: e.scalar_tensor_tensor(out=self.w1e[:], in0=self.w1e[:], scalar=1.0, in1=self.nwt[:], op0=ALU.add, op1=ALU.mult),
             reads=[self.r_w1e, self.r_nwt], writes=[self.r_w1e])

    def tile(self, t):
        S = self.C.S
        need = 0 if t < 2 else 1
        if need != self.cur:
            self.load_mod(need)
            self.cur = need
        k = self.k
        self.k += 1
        x_t, r_x = self.xt[k % 2]
        S.dma(x_t[:], self.xs[t * 128:(t + 1) * 128, :], reads=[self.C.r_xf[t // 8]], writes=[r_x])
        ss, r_ss = self.ss_l[k % 2]
        rstd, r_rstd = self.rstd_l[k % 2]
        tmp, r_tmp = self.tmp_l[k % 2]
        hb, r_hb = self.hb_l[k % 2]
        S.op("gpsimd", lambda e: e.memset(ss[:], 0.0), writes=[r_ss])
        S.op("scalar", lambda e: e.activation(out=tmp[:], in_=x_t[:], func=AF.Square, accum_out=ss[:]), reads=[r_x], writes=[r_tmp], updates=[r_ss])
        S.op("vector", lambda e: e.tensor_scalar(out=rstd[:], in0=ss[:], scalar1=1.0 / D, scalar2=EPS, op0=ALU.mult, op1=ALU.add), reads=[r_ss], writes=[r_rstd])
        S.op("scalar", lambda e: e.activation(out=rstd[:], in_=rstd[:], func=AF.Sqrt), reads=[r_rstd], writes=[r_rstd])
        S.op("vector", lambda e: e.reciprocal(out=rstd[:], in_=rstd[:]), reads=[r_rstd], writes=[r_rstd])
        S.op("vector", lambda e: e.scalar_tensor_tensor(out=tmp[:], in0=x_t[:], scalar=rstd[:, 0:1], in1=self.w1e[:], op0=ALU.mult, op1=ALU.mult),
             reads=[r_x, r_rstd, self.r_w1e], writes=[r_tmp])
        S.op("gpsimd", lambda e: e.tensor_tensor(out=hb[:], in0=tmp[:], in1=self.sh1[:], op=ALU.add), reads=[r_tmp, self.r_sh1], writes=[r_hb])
        p_T, r_pT = self.pT[k % 2]
        h_T, r_hT = self.hT[k % 2]
        for c in range(8):
            S.op("tensor", lambda e, c=c: e.transpose(out=p_T[:, c, :], in_=hb[:, c * 128:(c + 1) * 128], identity=self.idb[:]),
                 reads=[r_hb, self.r_idb], updates=[r_pT] if c else (), writes=() if c else [r_pT])
        S.op("scalar", lambda e: e.copy(out=h_T[:], in_=p_T[:]), reads=[r_pT], writes=[r_hT])
        return h_T, r_hT


def load_weight_bf16(C, dst, r_dst, src, ncols, col0=0, eng_cast="gpsimd"):
    S = C.S
    saved = C.stack
    tmpst = contextlib.ExitStack()
    C.stack = tmpst
    with tmpst:
        stgs = [C.sb([128, max(2048, ncols)], F32, "lwst%d_%d" % (C.n, i)) for i in range(2)]
        step = max(1, 2048 // ncols)
        k = 0
        for c0 in range(0, 8, step):
            nck = min(step, 8 - c0)
            s_t, r_s = stgs[k % 2]
            sv = s_t[:, 0:nck * ncols].rearrange("p (c f) -> p c f", c=nck)
            S.dma(sv, src[c0 * 128:(c0 + nck) * 128, :].rearrange("(c p) f -> p c f", p=128), writes=[r_s], eng=("sync" if k % 2 == 0 else "gpsimd"))
            S.op(eng_cast, lambda e, sv=sv, c0=c0, nck=nck: e.tensor_copy(out=dst[:, c0:c0 + nck, col0:col0 + ncols], in_=sv), reads=[r_s], updates=[r_dst])
            k += 1
    C.stack = saved
    S.barrier()


def emit_even(C, A):
    nc, S = C.nc, C.S
    NT = 34
    NTOK = NT * 128
    xs, modv, nw, wc, ropec, ropes, draw, sink, cmat, crow, ccol, mgo = (A[k] for k in "xs modv nw wc ropec ropes draw sink cmat crow ccol mgo".split())
    SC = 128 ** -0.5
    with C.scope():
        idb, r_idb, idf, r_idf = make_identity(C)
        qkT, _ = C.sb([128, 7, NTOK], BF16, "qkT")
        r_qkT = [Res("qkT%d" % t) for t in range(NT)]
        tm, _ = C.sb([128, NT, 896], BF16, "tm")
        r_tm = [Res("tm%d" % t) for t in range(NT)]
        cm, r_cm = C.sb([128, 6, 128], F32, "cm")
        S.dma(cm[:], cmat, writes=[r_cm])
        cr, r_cr = C.sb([128, 256], F32, "cr")
        S.dma(cr[:], crow.partition_broadcast(128), writes=[r_cr])
        cc, r_cc = C.sb([128, 2], F32, "cc")
        S.dma(cc[:], ccol, writes=[r_cc])
        lg, r_lg = C.sb([128, 4], F32, "lg")
        S.dma(lg[:], draw.partition_broadcast(128), writes=[r_lg])
        S.op("scalar", lambda e: e.activation(out=lg[:], in_=lg[:], func=AF.Exp), reads=[r_lg], writes=[r_lg])
        S.op("vector", lambda e: e.tensor_scalar(out=lg[:], in0=lg[:], scalar1=-1.0, scalar2=None, op0=ALU.mult), reads=[r_lg], writes=[r_lg])
        skt, r_skt = C.sb([128, 2], F32, "skt")
        S.dma(skt[:], sink.partition_broadcast(128), writes=[r_skt])
        dmask, r_dmask = C.sb([128, 4, 128], F32, "dmask")
        qdec, r_qdec = C.sb([128, 4, 128], F32, "qdec")
        kdec, r_kdec = C.sb([128, 4], F32, "kdec")
        cdec, r_cdec = C.sb([128, 4], F32, "cdec")
        c128, r_c128 = C.sb([128, 1], F32, "c128")
        S.op("gpsimd", lambda e: e.memset(c128[:], 128.0), writes=[r_c128])
        for dr in range(2):
            for h in range(2):
                ix = dr * 2 + h
                lgc = lg[:, ix:ix + 1]
                S.op("vector", lambda e, ix=ix, dr=dr, lgc=lgc: e.tensor_scalar(out=dmask[:, ix, :], in0=cm[:, dr, :], scalar1=lgc, scalar2=None, op0=ALU.mult),
                     reads=[r_cm, r_lg], updates=[r_dmask])
                S.op("scalar", lambda e, ix=ix: e.activation(out=dmask[:, ix, :], in_=dmask[:, ix, :], func=AF.Exp), reads=[], updates=[r_dmask])
                S.op("vector", lambda e, ix=ix, dr=dr: e.tensor_tensor(out=dmask[:, ix, :], in0=dmask[:, ix, :], in1=cm[:, 2 + dr, :], op=ALU.mult),
                     reads=[r_cm], updates=[r_dmask])
                S.op("vector", lambda e, ix=ix, dr=dr, lgc=lgc: e.tensor_scalar(out=qdec[:, ix, :], in0=cr[:, dr * 128:(dr + 1) * 128], scalar1=lgc, scalar2=None, op0=ALU.mult),
                     reads=[r_cr, r_lg], updates=[r_qdec])
                S.op("scalar", lambda e, ix=ix: e.activation(out=qdec[:, ix, :], in_=qdec[:, ix, :], func=AF.Exp), reads=[], updates=[r_qdec])
                S.op("vector", lambda e, ix=ix, dr=dr, lgc=lgc: e.tensor_scalar(out=kdec[:, ix:ix + 1], in0=cc[:, dr:dr + 1], scalar1=lgc, scalar2=None, op0=ALU.mult),
                     reads=[r_cc, r_lg], updates=[r_kdec])
                S.op("vector", lambda e, ix=ix, lgc=lgc: e.tensor_scalar(out=cdec[:, ix:ix + 1], in0=c128[:], scalar1=lgc, scalar2=None, op0=ALU.mult),
                     reads=[r_c128, r_lg], updates=[r_cdec])
        S.op("scalar", lambda e: e.activation(out=kdec[:], in_=kdec[:], func=AF.Exp), reads=[], updates=[r_kdec])
        S.op("scalar", lambda e: e.activation(out=cdec[:], in_=cdec[:], func=AF.Exp), reads=[], updates=[r_cdec])
        S.op("vector", lambda e: e.tensor_scalar(out=kdec[:], in0=kdec[:], scalar1=SC, scalar2=None, op0=ALU.mult), reads=[], updates=[r_kdec])

        stP = contextlib.ExitStack()
        saved = C.stack
        C.stack = stP
        with stP:
            wcb, r_wcb = C.sb([128, 8, 1536], BF16, "wcb")
            load_weight_bf16(C, wcb, r_wcb, wc, 1536)
            NI = NormIn(C, xs, modv, nw, idb, r_idb)
            cosT = [C.sb([128, 128], F32, "cos%d" % i) for i in range(2)]
            sinT = [C.sb([128, 128], F32, "sin%d" % i) for i in range(2)]
            pP = [C.ps([128, 512], F32, "pP%d" % i) for i in range(3)]
            pT7, r_pT7 = C.ps([128, 7, 128], BF16, "pT7")
            pf, r_pf = C.sb([128, 896], F32, "pf")
            t1, r_t1 = C.sb([128, 896], F32, "t1")
            t2, r_t2 = C.sb([128, 896], F32, "t2")
            Rb, r_Rb = C.sb([128, 896], BF16, "Rb")
            for t in range(NT):
                h_T, r_hT = NI.tile(t)
                c_t, r_c = cosT[t % 2]
                s_t, r_s = sinT[t % 2]
                S.dma(c_t[:], ropec[t * 128:(t + 1) * 128, :], writes=[r_c], eng="gpsimd")
                S.dma(s_t[:], ropes[t * 128:(t + 1) * 128, :], writes=[r_s], eng="gpsimd")
                for nb in range(3):
                    p, r_p = pP[nb]
                    for c in range(8):
                        S.op("tensor", lambda e, c=c, nb=nb, p=p, h_T=h_T: e.matmul(out=p[:], lhsT=h_T[:, c, :], rhs=wcb[:, c, nb * 512:(nb + 1) * 512], start=(c == 0), stop=(c == 7)),
                             reads=[r_hT, r_wcb], updates=[pP[nb][1]] if c else (), writes=() if c else [pP[nb][1]])
                S.op("scalar", lambda e: e.copy(out=pf[:, 0:512], in_=pP[0][0][:]), reads=[pP[0][1]], writes=[r_pf])
                S.op("scalar", lambda e: e.copy(out=pf[:, 512:896], in_=pP[1][0][:, 0:384]), reads=[pP[1][1]], updates=[r_pf])
                S.op("scalar", lambda e, t=t: e.copy(out=tm[:, t, 256:384], in_=pP[1][0][:, 384:512]), reads=[pP[1][1]], updates=[r_tm[t]])
                S.op("scalar", lambda e, t=t: e.copy(out=tm[:, t, 384:896], in_=pP[2][0][:]), reads=[pP[2][1]], updates=[r_tm[t]])
                S.op("vector", lambda e, c_t=c_t: e.tensor_tensor(out=t1[:].rearrange("p (h d) -> p h d", h=7), in0=pf[:].rearrange("p (h d) -> p h d", h=7),
                                                                  in1=c_t[:].rearrange("p (o d) -> p o d", o=1).broadcast_to([128, 7, 128]), op=ALU.mult),
                     reads=[r_pf, r_c], writes=[r_t1])
                for a in range(2):
                    for hf in range(2):
                        o0 = a * 64 + hf * 32
                        i0 = a * 64 + (1 - hf) * 32
                        S.op("vector", lambda e, o0=o0, i0=i0, s_t=s_t: e.tensor_tensor(
                            out=t2[:].rearrange("p (h d) -> p h d", h=7)[:, :, o0:o0 + 32], in0=pf[:].rearrange("p (h d) -> p h d", h=7)[:, :, i0:i0 + 32],
                            in1=s_t[:, o0:o0 + 32].rearrange("p (o d) -> p o d", o=1).broadcast_to([128, 7, 32]), op=ALU.mult),
                             reads=[r_pf, r_s], updates=[r_t2])
                S.op("vector", lambda e: e.tensor_tensor(out=Rb[:], in0=t1[:], in1=t2[:], op=ALU.add), reads=[r_t1, r_t2], writes=[r_Rb])
                S.op("gpsimd", lambda e, t=t: e.tensor_copy(out=tm[:, t, 0:256], in_=Rb[:, 256:512]), reads=[r_Rb], updates=[r_tm[t]])
                for c in range(7):
                    S.op("tensor", lambda e, c=c: e.transpose(out=pT7[:, c, :], in_=Rb[:, c * 128:(c + 1) * 128], identity=idb[:]),
                         reads=[r_Rb, r_idb], updates=[r_pT7] if c else (), writes=() if c else [r_pT7])
                S.op("scalar", lambda e, t=t: e.copy(out=qkT[:, :, t * 128:(t + 1) * 128], in_=pT7[:]), reads=[r_pT7], writes=[r_qkT[t]])
        C.stack = saved
        S.barrier()

        stS = contextlib.ExitStack()
        C.stack = stS
        with stS:
            oacc, _ = C.sb([128, NT, 256], F32, "oacc")
            r_oacc = [Res("oacc%d" % t) for t in range(NT)]
            Sst = [C.sb([128, 128], F32, "Sst%d" % i) for i in range(4)]
            Sbf = [C.sb([128, 128], BF16, "Sbf%d" % i) for i in range(4)]
            for i in range(4):
                S.op("gpsimd", lambda e, i=i: e.memset(Sst[i][0][:], 0.0), writes=[Sst[i][1]])
                S.op("gpsimd", lambda e, i=i: e.memset(Sbf[i][0][:], 0.0), writes=[Sbf[i][1]])
            PTs = [C.sb([128, 128], BF16, "PTs%d" % i) for i in range(2)]
            qs = [C.sb([128, 128], BF16, "qs%d" % i) for i in range(2)]
            ks = [C.sb([128, 128], BF16, "ks%d" % i) for i in range(2)]
            pSc = [C.ps([128, 128], F32, "pSc")] * 2
            pO = [C.ps([128, 128], F32, "pO%d" % i) for i in range(2)]
            pU = [C.ps([128, 128], F32, "pU")] * 2
            cnt = [0]

            def ret_step(t, h, dr):
                ix = dr * 2 + h
                k = cnt[0]
                cnt[0] += 1
                tok = slice(t * 128, (t + 1) * 128)
                p_s, r_ps = pSc[k % 2]
                p_o, r_po = pO[k % 2]
                p_u, r_pu = pU[k % 2]
                PT, r_PT = PTs[k % 2]
                q_s, r_qs = qs[k % 2]
                k_s, r_ks = ks[k % 2]
                S.op("tensor", lambda e: e.matmul(out=p_s[:], lhsT=qkT[:, 2 + h, tok], rhs=qkT[:, h, tok], start=True, stop=True),
                     reads=[r_qkT[t]], writes=[r_ps])
                S.op("vector", lambda e: e.tensor_tensor(out=PT[:], in0=p_s[:], in1=dmask[:, ix, :], op=ALU.mult), reads=[r_ps, r_dmask], writes=[r_PT])
                S.op("vector", lambda e: e.tensor_tensor(out=q_s[:], in0=qkT[:, h, tok], in1=qdec[:, ix, :], op=ALU.mult), reads=[r_qkT[t], r_qdec], writes=[r_qs])
                S.op("vector", lambda e: e.tensor_scalar(out=k_s[:], in0=tm[:, t, h * 128:(h + 1) * 128], scalar1=kdec[:, ix:ix + 1], scalar2=None, op0=ALU.mult),
                     reads=[r_tm[t], r_kdec], writes=[r_ks])
                vv = tm[:, t, 256 + h * 128:256 + (h + 1) * 128]
                S.op("tensor", lambda e: e.matmul(out=p_o[:], lhsT=PT[:], rhs=vv, start=True, stop=False), reads=[r_PT, r_tm[t]], writes=[r_po])
                S.op("tensor", lambda e: e.matmul(out=p_o[:], lhsT=q_s[:], rhs=Sbf[ix][0][:], start=False, stop=True), reads=[r_qs, Sbf[ix][1]], updates=[r_po])
                S.op("tensor", lambda e: e.matmul(out=p_u[:], lhsT=k_s[:], rhs=vv, start=True, stop=True), reads=[r_ks, r_tm[t]], writes=[r_pu])
                S.op("vector", lambda e: e.scalar_tensor_tensor(out=Sst[ix][0][:], in0=Sst[ix][0][:], scalar=cdec[:, ix:ix + 1], in1=p_u[:], op0=ALU.mult, op1=ALU.add),
                     reads=[r_pu, r_cdec], writes=[Sst[ix][1]])
                S.op("scalar", lambda e: e.copy(out=Sbf[ix][0][:], in_=Sst[ix][0][:]), reads=[Sst[ix][1]], writes=[Sbf[ix][1]])
                return p_o, r_po

            for t in range(NT):
                for h in range(2):
                    p_o, r_po = ret_step(t, h, 0)
                    S.op("scalar", lambda e, t=t, h=h, p_o=p_o: e.copy(out=oacc[:, t, h * 128:(h + 1) * 128], in_=p_o[:]), reads=[r_po], updates=[r_oacc[t]])

            mgt = [C.sb([128, 512], BF16, "mgt%d" % i) for i in range(2)]
            otot, r_otot = C.sb([128, 256], F32, "otot")
            sgt, r_sgt = C.sb([128, 256], F32, "sgt")
            jk, r_jk = C.sb([128, 128], F32, "jk2")
            ssh, r_ssh = C.sb([128, 2], F32, "ssh")
            ssc, r_ssc = C.sb([128, 640], F32, "ssc")
            Pb, r_Pb = C.sb([128, 640], BF16, "Pb")
            PTa, r_PTa = C.sb([128, 5, 128], BF16, "PTa")
            mx, r_mx = C.sb([128, 1], F32, "mx")
            nmx, r_nmx = C.sb([128, 1], F32, "nmx")
            rs, r_rs = C.sb([128, 1], F32, "rs")
            esk, r_esk = C.sb([128, 1], F32, "esk")
            pA, r_pA = C.ps([128, 384], F32, "pA")
            pB, r_pB = C.ps([128, 256], F32, "pB")
            pPT, r_pPT = C.ps([128, 5, 128], BF16, "pPT")
            pOa, r_pOa = C.ps([128, 128], F32, "pOa")
            order = [1, 0] + list(range(NT - 1, 1, -1))
            for oi, t in enumerate(order):
                m_t, r_m = mgt[oi % 2]
                S.op("scalar", lambda e, t=t: e.activation(out=sgt[:], in_=tm[:, t, 512:768], func=AF.Silu), reads=[r_tm[t]], writes=[r_sgt])
                S.op("gpsimd", lambda e: e.memset(ssh[:], 0.0), writes=[r_ssh])
                for h in range(2):
                    p_o, r_po = ret_step(t, h, 1)
                    hs = slice(h * 128, (h + 1) * 128)
                    S.op("vector", lambda e, t=t, hs=hs, p_o=p_o: e.tensor_tensor(out=otot[:, hs], in0=p_o[:], in1=oacc[:, t, hs], op=ALU.add),
                         reads=[r_po, r_oacc[t]], updates=[r_otot] if h else (), writes=() if h else [r_otot])
                    S.op("scalar", lambda e, hs=hs, h=h: e.activation(out=jk[:], in_=otot[:, hs], func=AF.Square, accum_out=ssh[:, h:h + 1]),
                         reads=[r_otot], writes=[r_jk], updates=[r_ssh])
                S.op("vector", lambda e: e.tensor_scalar(out=ssh[:], in0=ssh[:], scalar1=1.0 / 128, scalar2=EPS, op0=ALU.mult, op1=ALU.add), reads=[r_ssh], writes=[r_ssh])
                S.op("scalar", lambda e: e.activation(out=ssh[:], in_=ssh[:], func=AF.Sqrt), reads=[r_ssh], writes=[r_ssh])
                S.op("vector", lambda e: e.reciprocal(out=ssh[:], in_=ssh[:]), reads=[r_ssh], writes=[r_ssh])
                for h in range(2):
                    hs = slice(h * 128, (h + 1) * 128)
                    S.op("vector", lambda e, hs=hs, h=h, m_t=m_t: e.scalar_tensor_tensor(out=m_t[:, hs], in0=otot[:, hs], scalar=ssh[:, h:h + 1], in1=sgt[:, hs], op0=ALU.mult, op1=ALU.mult),
                         reads=[r_otot, r_ssh, r_sgt], updates=[r_m] if h else (), writes=() if h else [r_m])
                if t >= 2:
                    n = t - 2
                    lo = max(n - 1, 0)
                    hi = min(n + 1, 31)
                    loc = list(range(lo + 2, hi + 3))
                else:
                    loc = []
                ktiles = loc + [0, 1]
                nl = len(loc)
                nk = len(ktiles)
                rk_reads = [r_qkT[kt] for kt in ktiles]
                rv_reads = [r_tm[kt] for kt in ktiles]
                for g in range(2):
                    qT = qkT[:, 4 + g, t * 128:(t + 1) * 128]
                    if nl:
                        S.op("tensor", lambda e, qT=qT, loc=loc, nl=nl: e.matmul(out=pA[:, 0:nl * 128], lhsT=qT, rhs=qkT[:, 6, loc[0] * 128:(loc[-1] + 1) * 128], start=True, stop=True),
                             reads=[r_qkT[t]] + rk_reads, writes=[r_pA])
                    S.op("tensor", lambda e, qT=qT: e.matmul(out=pB[:], lhsT=qT, rhs=qkT[:, 6, 0:256], start=True, stop=True), reads=[r_qkT[t]] + rk_reads, writes=[r_pB])
                    for li, kt in enumerate(loc):
                        rel = kt - t
                        dst = ssc[:, li * 128:(li + 1) * 128]
                        src = pA[:, li * 128:(li + 1) * 128]
                        if rel == 0:
                            S.op("vector", lambda e, dst=dst, src=src: e.tensor_copy(out=dst, in_=src), reads=[r_pA], updates=[r_ssc])
                        else:
                            mi = 4 if rel < 0 else 5
                            S.op("vector", lambda e, dst=dst, src=src, mi=mi: e.tensor_tensor(out=dst, in0=src, in1=cm[:, mi, :], op=ALU.add), reads=[r_pA, r_cm], updates=[r_ssc])
                    S.op("scalar", lambda e, nl=nl: e.copy(out=ssc[:, nl * 128:nl * 128 + 256], in_=pB[:]), reads=[r_pB], updates=[r_ssc])
                    W = nk * 128
                    S.op("vector", lambda e, W=W: e.reduce_max(out=mx[:], in_=ssc[:, 0:W], axis=AX.X), reads=[r_ssc], writes=[r_mx])
                    S.op("vector", lambda e, g=g: e.tensor_scalar(out=mx[:], in0=mx[:], scalar1=SC, scalar2=skt[:, g:g + 1], op0=ALU.mult, op1=ALU.max), reads=[r_mx, r_skt], writes=[r_mx])
                    S.op("vector", lambda e: e.tensor_scalar(out=nmx[:], in0=mx[:], scalar1=-1.0, scalar2=None, op0=ALU.mult), reads=[r_mx], writes=[r_nmx])
                    S.op("gpsimd", lambda e: e.memset(rs[:], 0.0), writes=[r_rs])
                    S.op("scalar", lambda e, W=W: e.activation(out=Pb[:, 0:W], in_=ssc[:, 0:W], func=AF.Exp, bias=nmx[:, 0:1], scale=SC, accum_out=rs[:]),
                         reads=[r_ssc, r_nmx], writes=[r_Pb], updates=[r_rs])
                    S.op("scalar", lambda e, g=g: e.activation(out=esk[:], in_=skt[:, g:g + 1], func=AF.Exp, bias=nmx[:, 0:1], scale=1.0), reads=[r_skt, r_nmx], writes=[r_esk])
                    S.op("vector", lambda e: e.tensor_tensor(out=rs[:], in0=rs[:], in1=esk[:], op=ALU.add), reads=[r_esk], updates=[r_rs])
                    S.op("vector", lambda e: e.reciprocal(out=rs[:], in_=rs[:]), reads=[], updates=[r_rs])
                    for ki in range(nk):
                        S.op("tensor", lambda e, ki=ki: e.transpose(out=pPT[:, ki, :], in_=Pb[:, ki * 128:(ki + 1) * 128], identity=idb[:]),
                             reads=[r_Pb, r_idb], updates=[r_pPT] if ki else (), writes=() if ki else [r_pPT])
                    S.op("scalar", lambda e, nk=nk: e.copy(out=PTa[:, 0:nk, :], in_=pPT[:, 0:nk, :]), reads=[r_pPT], writes=[r_PTa])
                    for ki, kt in enumerate(ktiles):
                        S.op("tensor", lambda e, ki=ki, kt=kt, nk=nk: e.matmul(out=pOa[:], lhsT=PTa[:, ki, :], rhs=tm[:, kt, 768:896], start=(ki == 0), stop=(ki == nk - 1)),
                             reads=[r_PTa] + rv_reads, updates=[r_pOa] if ki else (), writes=() if ki else [r_pOa])
                    S.op("vector", lambda e, g=g, m_t=m_t: e.tensor_scalar(out=m_t[:, 256 + g * 128:256 + (g + 1) * 128], in0=pOa[:], scalar1=rs[:, 0:1], scalar2=None, op0=ALU.mult),
                         reads=[r_pOa, r_rs], updates=[r_m])
                S.dma(mgo[t * 128:(t + 1) * 128, :], m_t[:], reads=[r_m])
        C.stack = saved


def even_consts():
    p = np.arange(128, dtype=np.float32)[:, None]
    f = np.arange(128, dtype=np.float32)[None, :]
    SC = np.float32(128 ** -0.5)
    cmat = np.stack([np.maximum(f - p, 0), np.maximum(p - f, 0), (f >= p) * SC, (p >= f) * SC,
                     np.where(f >= p, 0.0, NEG), np.where(f <= p, 0.0, NEG)], 1).astype(np.float32)
    crow = np.concatenate([f[0] + 1, 128 - f[0]])[None].astype(np.float32)
    ccol = np.concatenate([127 - p, p], 1).astype(np.float32)
    t = np.arange(L)
    row = (t // 64).astype(np.float32)
    col = (t % 64).astype(np.float32)
    inv = (10000.0 ** (-np.arange(0, 64, 2, dtype=np.float32) / 64)).astype(np.float32)
    ar = row[:, None] * inv[None]
    ac = col[:, None] * inv[None]
    cos = np.concatenate([np.cos(ar), np.cos(ar), np.cos(ac), np.cos(ac)], 1)
    sin = np.concatenate([-np.sin(ar), np.sin(ar), -np.sin(ac), np.sin(ac)], 1)
    ropec = np.concatenate([np.ones((NCTX, 128)), cos], 0).astype(np.float32)
    ropes = np.concatenate([np.zeros((NCTX, 128)), sin], 0).astype(np.float32)
    return dict(cmat=np.ascontiguousarray(cmat), crow=crow, ccol=np.ascontiguousarray(ccol), ropec=ropec, ropes=ropes)


def emit_odd(C, A, debug=False):
    nc, S = C.nc, C.S
    NT = 34
    NTOK = NT * 128
    xs, modv, nw, wc, lbl, lsel, hnw, cmat, mgo = (A[k] for k in "xs modv nw wc lbl lsel hnw cmat mgo".split())
    if debug:
        dbg = C.dout("dbg", [128, 8, 512])
        dbg2 = C.dout("dbg2", [128, 8, 512])
    with C.scope():
        idb, r_idb, idf, r_idf = make_identity(C)
        cm, r_cm = C.sb([128, 2, 128], F32, "cm")
        S.dma(cm[:], cmat, writes=[r_cm])
        ones, r_ones = C.sb([128, 128], F32, "ones")
        S.op("gpsimd", lambda e: e.memset(ones[:], 1.0), writes=[r_ones])
        hn, r_hn = C.sb([128, 128], F32, "hn")
        S.dma(hn[:], hnw.partition_broadcast(128), writes=[r_hn])
        lgt, r_lgt = C.sb([128, 4, 4], F32, "lgt")
        for l_ in range(4):
            S.dma(lgt[:, l_, :], lbl[l_, :].rearrange("(h d) -> d h", d=128), updates=[r_lgt], allow_slow_non_contiguous=True)
        sel, r_sel = C.sb([128, 4], F32, "sel")
        S.dma(sel[:], lsel.partition_broadcast(128), writes=[r_sel])
        mxl, r_mxl = C.sb([128, 4], F32, "mxl")
        S.op("vector", lambda e: e.tensor_tensor(out=mxl[:], in0=lgt[:, 0, :], in1=lgt[:, 1, :], op=ALU.max), reads=[r_lgt], writes=[r_mxl])
        S.op("vector", lambda e: e.tensor_tensor(out=mxl[:], in0=mxl[:], in1=lgt[:, 2, :], op=ALU.max), reads=[r_lgt], writes=[r_mxl])
        S.op("vector", lambda e: e.tensor_tensor(out=mxl[:], in0=mxl[:], in1=lgt[:, 3, :], op=ALU.max), reads=[r_lgt], writes=[r_mxl])
        for l_ in range(4):
            S.op("vector", lambda e, l_=l_: e.tensor_tensor(out=lgt[:, l_, :], in0=lgt[:, l_, :], in1=mxl[:], op=ALU.subtract), reads=[r_mxl], updates=[r_lgt])
        S.op("scalar", lambda e: e.activation(out=lgt[:], in_=lgt[:], func=AF.Exp), reads=[], updates=[r_lgt])
        den, r_den = C.sb([128, 4], F32, "den")
        lb, r_lb = C.sb([128, 4], F32, "lb")
        oml, r_oml = C.sb([128, 4], F32, "oml")
        tl, r_tl = C.sb([128, 4], F32, "tl")
        S.op("gpsimd", lambda e: e.memset(den[:], 0.0), writes=[r_den])
        S.op("gpsimd", lambda e: e.memset(lb[:], 0.0), writes=[r_lb])
        for l_ in range(4):
            S.op("vector", lambda e, l_=l_: e.tensor_tensor(out=den[:], in0=den[:], in1=lgt[:, l_, :], op=ALU.add), reads=[r_lgt], writes=[r_den])
            S.op("vector", lambda e, l_=l_: e.tensor_scalar(out=tl[:], in0=lgt[:, l_, :], scalar1=sel[:, l_:l_ + 1], scalar2=None, op0=ALU.mult), reads=[r_lgt, r_sel], writes=[r_tl])
            S.op("vector", lambda e: e.tensor_tensor(out=lb[:], in0=lb[:], in1=tl[:], op=ALU.add), reads=[r_tl], writes=[r_lb])
        S.op("vector", lambda e: e.reciprocal(out=den[:], in_=den[:]), reads=[], updates=[r_den])
        S.op("vector", lambda e: e.tensor_tensor(out=lb[:], in0=lb[:], in1=den[:], op=ALU.mult), reads=[r_den], writes=[r_lb])
        S.op("vector", lambda e: e.tensor_scalar(out=oml[:], in0=lb[:], scalar1=-1.0, scalar2=1.0, op0=ALU.mult, op1=ALU.add), reads=[r_lb], writes=[r_oml])

        wcb, r_wcb = C.sb([128, 8, 2560], BF16, "wcb")
        load_weight_bf16(C, wcb, r_wcb, wc, 2560)
        NI = NormIn(C, xs, modv, nw, idb, r_idb, nps=1)
        ofw, _ = C.sb([128, NT, 512], F32, "ofw")
        r_ofw = [Res("ofw%d" % t) for t in range(NT)]
        pQ, r_pQ = C.ps([128, 4, 128], F32, "pQ")
        pZ, r_pZ = C.ps([128, 4, 128], F32, "pZ")
        pV, r_pV = C.ps([128, 512], F32, "pV")
        pG, r_pG = pV, r_pV
        pS, r_pS = C.ps([128, 4, 128], F32, "pS")
        _pK, r_pK = C.ps([128, 8, 128], BF16, "pK")
        pK = _pK.rearrange("p (h c) j -> p h c j", c=2)
        pUu, r_pUu = C.ps([128, 4, 128], F32, "pUu")
        pOo, r_pOo = C.ps([128, 4, 128], F32, "pOo")
        NB = 2
        bufs = []
        for i_ in range(NB):
            d_ = {}
            for nm_ in ("sf", "ff", "kk", "cs", "E1", "E2", "qsb"):
                d_[nm_] = C.sb([128, 4, 128], F32, "%s%d" % (nm_, i_))
            d_["rr"] = C.sb([128, 4, 2], F32, "rr%d" % i_)
            d_["aa"] = C.sb([128, 4, 2, 3], F32, "aa%d" % i_)
            d_["qtP"] = C.sb([128, 4, 2, 128], BF16, "qtP%d" % i_)
            d_["ktP"] = C.sb([128, 4, 2, 128], BF16, "ktP%d" % i_)
            S.op("vector", lambda e, t_=d_["qtP"][0]: e.memset(t_[:].rearrange("p a b c -> p (a b c)"), 0.0), writes=[d_["qtP"][1]])
            S.op("vector", lambda e, t_=d_["ktP"][0]: e.memset(t_[:].rearrange("p a b c -> p (a b c)"), 0.0), writes=[d_["ktP"][1]])
            d_["vb"] = C.sb([128, 512], BF16, "vb%d" % i_)
            d_["sgg"] = C.sb([128, 512], F32, "sgg%d" % i_)
            d_["otot"] = C.sb([128, 512], F32, "hotot%d" % i_)
            d_["ssh"] = C.sb([128, 4], F32, "hssh%d" % i_)
            bufs.append(d_)
        PT_l = [C.sb([128, 4, 128], BF16, "PT%d" % i_) for i_ in range(1)] * 2
        kTM_l = [C.sb([128, 4, 2, 128], BF16, "kTM%d" % i_) for i_ in range(1)] * 2
        tU_l = [C.sb([128, 4, 128], F32, "tU%d" % i_) for i_ in range(1)] * 2
        Sall, r_Sall = C.sb([128, 4, 128], F32, "Sall")
        Spp = [C.sb([128, 2, 4, 128], BF16, "hSpp%d" % i) for i in range(2)]
        jk, r_jk = C.sb([128, 128], F32, "hjk")
        mgt = [C.sb([128, 512], BF16, "hmg%d" % i) for i in range(2)]
        kcnt = [0]

        tilek = [0]

        def do_tile(oi, t, dr, zc0):
            B_ = bufs[tilek[0] % NB]
            tilek[0] += 1
            sf, r_sf = B_["sf"]
            ff, r_ff = B_["ff"]
            kk, r_kk = B_["kk"]
            cs, r_cs = B_["cs"]
            uu, r_uu = B_["sf"]
            E1, r_E1 = B_["E1"]
            E2, r_E2 = B_["E2"]
            qsb, r_qsb = B_["qsb"]
            rr, r_rr = B_["rr"]
            aa, r_aa = B_["aa"]
            qtP, r_qtP = B_["qtP"]
            ktP, r_ktP = B_["ktP"]
            vb, r_vb = B_["vb"]
            sgg, r_sgg = B_["sgg"]
            otot, r_otot = B_["otot"]
            ssh, r_ssh = B_["ssh"]
            h_T, r_hT = NI.tile(t)
            yield 'S'
            for h in range(4):
                for c in range(8):
                    S.op("tensor", lambda e, c=c, h=h, h_T=h_T: e.matmul(out=pQ[:, h, :], lhsT=wcb[:, c, h * 128:(h + 1) * 128], rhs=h_T[:, c, :], start=(c == 0), stop=(c == 7)),
                         reads=[r_hT, r_wcb], updates=[r_pQ] if (c or h) else (), writes=() if (c or h) else [r_pQ])
            for h in range(4):
                for c in range(8):
                    S.op("tensor", lambda e, c=c, h=h, zc0=zc0, h_T=h_T: e.matmul(out=pZ[:, h, :], lhsT=wcb[:, c, zc0 + h * 128:zc0 + (h + 1) * 128], rhs=h_T[:, c, :], start=(c == 0), stop=(c == 7)),
                         reads=[r_hT, r_wcb], updates=[r_pZ] if (c or h) else (), writes=() if (c or h) else [r_pZ])
            for c in range(8):
                S.op("tensor", lambda e, c=c, h_T=h_T: e.matmul(out=pV[:], lhsT=h_T[:, c, :], rhs=wcb[:, c, 1536:2048], start=(c == 0), stop=(c == 7)),
                     reads=[r_hT, r_wcb], updates=[r_pV] if c else (), writes=() if c else [r_pV])
            S.op("scalar", lambda e: e.copy(out=vb[:], in_=pV[:]), reads=[r_pV], writes=[r_vb])
            S.op("scalar", lambda e: e.copy(out=qsb[:], in_=pQ[:]), reads=[r_pQ], writes=[r_qsb])
            if dr == 1:
                for c in range(8):
                    S.op("tensor", lambda e, c=c, h_T=h_T: e.matmul(out=pG[:], lhsT=h_T[:, c, :], rhs=wcb[:, c, 2048:2560], start=(c == 0), stop=(c == 7)),
                         reads=[r_hT, r_wcb], updates=[r_pG] if c else (), writes=() if c else [r_pG])
                S.op("scalar", lambda e: e.activation(out=sgg[:], in_=pG[:], func=AF.Silu), reads=[r_pG], writes=[r_sgg])
            yield 'S'
            S.op("scalar", lambda e: e.activation(out=sf[:], in_=pZ[:], func=AF.Sigmoid), reads=[r_pZ], writes=[r_sf])
            yield 'S'
            for h in range(4):
                S.op("vector", lambda e, h=h: e.tensor_scalar(out=ff[:, h, :], in0=sf[:, h, :], scalar1=oml[:, h:h + 1], scalar2=lb[:, h:h + 1], op0=ALU.mult, op1=ALU.add),
                     reads=[r_sf, r_oml, r_lb], updates=[r_ff] if h else (), writes=() if h else [r_ff])
            S.op("vector", lambda e: e.tensor_scalar(out=kk[:], in0=ff[:], scalar1=-1.0, scalar2=1.0, op0=ALU.mult, op1=ALU.add), reads=[r_ff], writes=[r_kk])
            yield 'S'
            S.op("scalar", lambda e: e.activation(out=ff[:], in_=ff[:], func=AF.Ln), reads=[], updates=[r_ff])
            yield 'S'
            for h in range(4):
                S.op("vector", lambda e, h=h: e.tensor_tensor_scan(out=cs[:, h, :], data0=ones[:], data1=ff[:, h, :], initial=0.0, op0=ALU.mult, op1=ALU.add),
                     reads=[r_ff, r_ones], updates=[r_cs] if h else (), writes=() if h else [r_cs])
            if dr == 0:
                u, r_u = cs, r_cs
            else:
                S.op("vector", lambda e: e.tensor_tensor(out=uu[:], in0=ff[:], in1=cs[:], op=ALU.subtract), reads=[r_ff, r_cs], writes=[r_uu])
                u, r_u = uu, r_uu
            S.op("vector", lambda e, u=u: e.tensor_copy(out=rr[:], in_=u[:].rearrange("p h (c s) -> p h c s", c=2)[:, :, :, 31]), reads=[r_u], writes=[r_rr])
            for c in range(2):
                for h in range(4):
                    a1 = aa[:, h, c, 0:1]
                    a2 = aa[:, h, c, 1:2]
                    if dr == 0:
                        if c == 0:
                            S.op("vector", lambda e, a1=a1, h=h, c=c: e.tensor_copy(out=a1, in_=rr[:, h, c:c + 1]), reads=[r_rr], updates=[r_aa])
                        else:
                            S.op("vector", lambda e, a1=a1, h=h, c=c: e.tensor_tensor(out=a1, in0=rr[:, h, c:c + 1], in1=cs[:, h, 63:64], op=ALU.subtract), reads=[r_rr, r_cs], updates=[r_aa])
                        S.op("vector", lambda e, a2=a2, h=h, c=c: e.tensor_tensor(out=a2, in0=cs[:, h, c * 64 + 63:c * 64 + 64], in1=rr[:, h, c:c + 1], op=ALU.subtract), reads=[r_rr, r_cs], updates=[r_aa])
                    else:
                        S.op("vector", lambda e, a1=a1, h=h, c=c: e.tensor_tensor(out=a1, in0=rr[:, h, c:c + 1], in1=cs[:, h, c * 64 + 63:c * 64 + 64], op=ALU.add), reads=[r_rr, r_cs], updates=[r_aa])
                        if c == 0:
                            S.op("vector", lambda e, a2=a2, h=h, c=c: e.tensor_scalar(out=a2, in0=rr[:, h, c:c + 1], scalar1=-1.0, scalar2=None, op0=ALU.mult), reads=[r_rr], updates=[r_aa])
                        else:
                            S.op("vector", lambda e, a2=a2, h=h, c=c: e.scalar_tensor_tensor(out=a2, in0=rr[:, h, c:c + 1], scalar=-1.0, in1=cs[:, h, 63:64], op0=ALU.mult, op1=ALU.subtract),
                                 reads=[r_rr, r_cs], updates=[r_aa])
            yield 'S'
            S.op("scalar", lambda e: e.activation(out=aa[:, :, :, 0:2], in_=aa[:, :, :, 0:2], func=AF.Exp), reads=[], updates=[r_aa])
            yield 'S'
            S.op("vector", lambda e: e.tensor_tensor(out=aa[:, :, :, 2], in0=aa[:, :, :, 0], in1=aa[:, :, :, 1], op=ALU.mult), reads=[], updates=[r_aa])
            for h in range(4):
                for c in range(2):
                    S.op("vector", lambda e, h=h, c=c, u=u: e.tensor_scalar(out=E1[:, h, c * 64:(c + 1) * 64], in0=u[:, h, c * 64:(c + 1) * 64], scalar1=rr[:, h, c:c + 1], scalar2=None, op0=ALU.subtract),
                         reads=[r_u, r_rr], updates=[r_E1] if (h or c) else (), writes=() if (h or c) else [r_E1])
            yield 'S'
            S.op("scalar", lambda e: e.activation(out=E2[:], in_=E1[:], func=AF.Exp, scale=-1.0), reads=[r_E1], writes=[r_E2])
            S.op("scalar", lambda e: e.activation(out=E1[:], in_=E1[:], func=AF.Exp), reads=[r_E2], updates=[r_E1])
            yield 'S'
            for c in range(2):
                cs_ = slice(c * 64, (c + 1) * 64)
                S.op("vector", lambda e, c=c, cs_=cs_: e.tensor_tensor(out=qtP[:, :, c, cs_], in0=qsb[:, :, cs_], in1=E1[:, :, cs_], op=ALU.mult), reads=[r_qsb, r_E1], updates=[r_qtP])
                S.op("vector", lambda e, c=c, cs_=cs_: e.tensor_tensor(out=ktP[:, :, c, cs_], in0=kk[:, :, cs_], in1=E2[:, :, cs_], op=ALU.mult), reads=[r_kk, r_E2], updates=[r_ktP])
            if debug and dr == 0 and t == 0:
                dtile, r_dt = C.sb([128, 8, 512], F32, "dtile")
                S.op("gpsimd", lambda e: e.memset(dtile[:], 0.0), writes=[r_dt])
                S.op("vector", lambda e: e.tensor_copy(out=dtile[:, 0, 0:4], in_=lb[:]), reads=[r_lb], updates=[r_dt])
                S.op("vector", lambda e: e.tensor_copy(out=dtile[:, 1, :], in_=ff[:].rearrange("p a b -> p (a b)")), reads=[r_ff], updates=[r_dt])
                S.op("vector", lambda e: e.tensor_copy(out=dtile[:, 2, :], in_=cs[:].rearrange("p a b -> p (a b)")), reads=[r_cs], updates=[r_dt])
                S.op("vector", lambda e: e.tensor_copy(out=dtile[:, 3, :], in_=E1[:].rearrange("p a b -> p (a b)")), reads=[r_E1], updates=[r_dt])
                S.op("vector", lambda e: e.tensor_copy(out=dtile[:, 4, :], in_=E2[:].rearrange("p a b -> p (a b)")), reads=[r_E2], updates=[r_dt])
                S.op("vector", lambda e: e.tensor_copy(out=dtile[:, 5, 0:24], in_=aa[:].rearrange("p a b c -> p (a b c)")), reads=[r_aa], updates=[r_dt])
                S.op("vector", lambda e: e.tensor_copy(out=dtile[:, 6, :], in_=qtP[:, 0:2, :, :].rearrange("p a b c -> p (a b c)")), reads=[r_qtP], updates=[r_dt])
                S.op("vector", lambda e: e.tensor_copy(out=dtile[:, 7, :], in_=pQ[:].rearrange("p a b -> p (a b)")), reads=[r_pQ], updates=[r_dt])
                S.dma(dbg, dtile[:], reads=[r_dt])
            yield 'SPLIT'
            corder = [0, 1] if dr == 0 else [1, 0]
            kx = kcnt[0]
            kcnt[0] += 1
            Sp, r_Sp = Spp[kx % 2]
            PT, r_PT = PT_l[kx % 2]
            kTM, r_kTM = kTM_l[kx % 2]
            tU, r_tU = tU_l[kx % 2]
            first = True
            for h in range(4):
                for c in range(2):
                    S.op("tensor", lambda e, c=c, h=h: e.matmul(out=pS[:, h, c * 64:(c + 1) * 64], lhsT=ktP[:, h, c, :], rhs=qtP[:, h, c, c * 64:(c + 1) * 64], start=True, stop=True),
                         reads=[r_ktP, r_qtP], updates=() if first else [r_pS], writes=[r_pS] if first else ())
                    first = False
            yield 'S'
            S.op("vector", lambda e, dr=dr: e.tensor_tensor(out=PT[:], in0=pS[:], in1=cm[:, dr:dr + 1, :].broadcast_to([128, 4, 128]), op=ALU.mult), reads=[r_pS, r_cm], writes=[r_PT])
            yield 'S'
            first = True
            for h in range(4):
                for c in range(2):
                    S.op("tensor", lambda e, c=c, h=h: e.transpose(out=pK[:, h, c, :], in_=ktP[:, h, c, :], identity=idb[:]), reads=[r_ktP, r_idb],
                         updates=() if first else [r_pK], writes=[r_pK] if first else ())
                    first = False
            yield 'S'
            S.op("scalar", lambda e: e.copy(out=kTM[:], in_=pK[:]), reads=[r_pK], writes=[r_kTM])
            yield 'S'
            for ci, c in enumerate(corder):
                for h in range(4):
                    S.op("tensor", lambda e, c=c, h=h: e.matmul(out=pUu[:, h, :], lhsT=kTM[:, h, c, :], rhs=vb[:, h * 128:(h + 1) * 128], start=True, stop=True),
                         reads=[r_kTM, r_vb], updates=[r_pUu] if h else (), writes=() if h else [r_pUu])
                yield 'S'
                a1b = aa[:, :, c, 0:1].broadcast_to([128, 4, 128])
                a2b = aa[:, :, c, 1:2].broadcast_to([128, 4, 128])
                a3b = aa[:, :, c, 2:3].broadcast_to([128, 4, 128])
                S.op("vector", lambda e, c=c, a1b=a1b: e.tensor_tensor(out=Sp[:, c, :, :], in0=Sall[:], in1=a1b, op=ALU.mult),
                     reads=[r_Sall, r_aa], updates=[r_Sp] if ci else (), writes=() if ci else [r_Sp])
                S.op("vector", lambda e, a2b=a2b: e.tensor_tensor(out=tU[:], in0=pUu[:], in1=a2b, op=ALU.mult), reads=[r_pUu, r_aa], writes=[r_tU])
                S.op("vector", lambda e, a3b=a3b: e.tensor_tensor(out=Sall[:], in0=Sall[:], in1=a3b, op=ALU.mult), reads=[r_aa], writes=[r_Sall])
                S.op("gpsimd", lambda e: e.tensor_tensor(out=Sall[:], in0=Sall[:], in1=tU[:], op=ALU.add), reads=[r_tU], writes=[r_Sall])
            yield 'S'
            for h in range(4):
                vv = vb[:, h * 128:(h + 1) * 128]
                S.op("tensor", lambda e, vv=vv, h=h: e.matmul(out=pOo[:, h, :], lhsT=PT[:, h, :], rhs=vv, start=True, stop=False), reads=[r_PT, r_vb],
                     updates=[r_pOo] if h else (), writes=() if h else [r_pOo])
                for c in range(2):
                    S.op("tensor", lambda e, c=c, h=h: e.matmul(out=pOo[:, h, :], lhsT=qtP[:, h, c, :], rhs=Sp[:, c, h, :], start=False, stop=(c == 1)), reads=[r_qtP, r_Sp], updates=[r_pOo])
            yield 'S'
            pOf = pOo[:].rearrange("p h e -> p (h e)")
            if dr == 0:
                S.op("scalar", lambda e, t=t: e.copy(out=ofw[:, t, :], in_=pOf), reads=[r_pOo], writes=[r_ofw[t]])
            else:
                S.op("vector", lambda e, t=t: e.tensor_tensor(out=otot[:], in0=pOf, in1=ofw[:, t, :], op=ALU.add), reads=[r_pOo, r_ofw[t]], writes=[r_otot])
            if debug and dr == 0 and t == 0:
                dt2, r_dt2 = C.sb([128, 8, 512], F32, "dtile2")
                S.op("gpsimd", lambda e: e.memset(dt2[:], 0.0), writes=[r_dt2])
                S.op("vector", lambda e: e.tensor_copy(out=dt2[:, 0, 0:128], in_=PT[:]), reads=[r_PT], updates=[r_dt2])
                S.op("vector", lambda e: e.tensor_copy(out=dt2[:, 1, 0:128], in_=pOo[:]), reads=[r_pOo], updates=[r_dt2])
                S.op("vector", lambda e: e.tensor_copy(out=dt2[:, 2, :], in_=ofw[:, 0, :]), reads=[r_ofw[0]], updates=[r_dt2])
                S.op("vector", lambda e: e.tensor_copy(out=dt2[:, 3, :], in_=vb[:]), reads=[r_vb], updates=[r_dt2])
                S.op("vector", lambda e: e.tensor_copy(out=dt2[:, 4, 0:128], in_=pS[:]), reads=[r_pS], updates=[r_dt2])
                S.op("vector", lambda e: e.tensor_copy(out=dt2[:, 5, 0:256], in_=kTM[:].rearrange("p a b -> p (a b)")), reads=[r_kTM], updates=[r_dt2])
                S.op("vector", lambda e: e.tensor_copy(out=dt2[:, 6, 0:128], in_=Sst[3][0][:]), reads=[Sst[3][1]], updates=[r_dt2])
                S.dma(dbg2, dt2[:], reads=[r_dt2])
            if dr == 1:
                m_t, r_m = mgt[oi % 2]
                S.op("gpsimd", lambda e: e.memset(ssh[:], 0.0), writes=[r_ssh])
                for h in range(4):
                    hs = slice(h * 128, (h + 1) * 128)
                    S.op("scalar", lambda e, hs=hs, h=h: e.activation(out=jk[:], in_=otot[:, hs], func=AF.Square, accum_out=ssh[:, h:h + 1]), reads=[r_otot], writes=[r_jk], updates=[r_ssh])
                S.op("vector", lambda e: e.tensor_scalar(out=ssh[:], in0=ssh[:], scalar1=1.0 / 128, scalar2=EPS, op0=ALU.mult, op1=ALU.add), reads=[r_ssh], writes=[r_ssh])
                S.op("scalar", lambda e: e.activation(out=ssh[:], in_=ssh[:], func=AF.Sqrt), reads=[r_ssh], writes=[r_ssh])
                S.op("vector", lambda e: e.reciprocal(out=ssh[:], in_=ssh[:]), reads=[r_ssh], writes=[r_ssh])
                for h in range(4):
                    hs = slice(h * 128, (h + 1) * 128)
                    S.op("vector", lambda e, hs=hs, h=h: e.scalar_tensor_tensor(out=otot[:, hs], in0=otot[:, hs], scalar=ssh[:, h:h + 1], in1=hn[:], op0=ALU.mult, op1=ALU.mult),
                         reads=[r_ssh, r_hn], updates=[r_otot])
                S.op("vector", lambda e, m_t=m_t: e.tensor_tensor(out=m_t[:], in0=otot[:], in1=sgg[:], op=ALU.mult), reads=[r_otot, r_sgg], writes=[r_m])
                S.dma(mgo[t * 128:(t + 1) * 128, :], m_t[:], reads=[r_m])

        for dr in range(2):
            order = list(range(NT)) if dr == 0 else [1, 0] + list(range(NT - 1, 1, -1))
            zc0 = 512 + dr * 512
            S.op("vector", lambda e: e.memset(Sall[:].rearrange("p a b -> p (a b)"), 0.0), writes=[r_Sall])
            cur = None
            for oi, t in enumerate(order):
                nxt = do_tile(oi, t, dr, zc0)
                a_done = False
                b_done = cur is None
                while not (a_done and b_done):
                    if not a_done:
                        a_done = next(nxt) == 'SPLIT'
                    if not b_done:
                        b_done = next(cur, 'END') == 'END'
                cur = nxt
            while next(cur, 'END') != 'END':
                pass


def odd_consts():
    p = np.arange(128)[:, None]
    f = np.arange(128)[None, :]
    same = (p // 64) == (f // 64)
    cmat = np.stack([(same & (p <= f)), (same & (p >= f))], 1).astype(np.float32)
    return dict(cmat=np.ascontiguousarray(cmat))


def emit_mod(C, cv, aws, abs_, modd):
    nc, S = C.nc, C.S
    with C.scope():
        ct, r_ct = C.sb([128, 8, 2], F32, "ct")
        for b_ in range(2):
            S.dma(ct[:, :, b_], cv[b_, :].rearrange("(c p) -> p c", p=128), updates=[r_ct], allow_slow_non_contiguous=True)
        sc, r_sc = C.sb([128, 8, 2], F32, "sc")
        S.op("scalar", lambda e: e.activation(out=sc[:], in_=ct[:], func=AF.Silu), reads=[r_ct], writes=[r_sc])
        wts = [C.sb([128, 8, 1536], F32, "aw%d" % i) for i in range(2)]
        bt, r_bt = C.sb([2, 6144], F32, "bt")
        ots = [C.sb([2, 6144], F32, "ot%d" % i) for i in range(2)]
        pss = [C.ps([2, 512], F32, "pm%d" % i) for i in range(2)]
        k = 0
        for l in range(DEPTH):
            ot, r_ot = ots[l % 2]
            S.dma(bt[:], abs_[l].partition_broadcast(2), writes=[r_bt])
            for cb in range(4):
                w, r_w = wts[k % 2]
                for c in range(8):
                    S.dma(w[:, c, :], aws[l][c * 128:(c + 1) * 128, cb * 1536:(cb + 1) * 1536], updates=[r_w] if c else (), writes=() if c else [r_w],
                          eng=("sync" if c % 2 == 0 else "gpsimd"))
                for j in range(3):
                    p, r_p = pss[(k * 3 + j) % 2]
                    for c in range(8):
                        S.op("tensor", lambda e, c=c, j=j, p=p, w=w: e.matmul(out=p[:], lhsT=sc[:, c, :], rhs=w[:, c, j * 512:(j + 1) * 512], start=(c == 0), stop=(c == 7)),
                             reads=[r_sc, r_w], updates=[r_p] if c else (), writes=() if c else [r_p])
                    col = cb * 1536 + j * 512
                    S.op("vector", lambda e, p=p, col=col, ot=ot: e.tensor_tensor(out=ot[:, col:col + 512], in0=p[:], in1=bt[:, col:col + 512], op=ALU.add),
                         reads=[r_p, r_bt], updates=[r_ot])
                k += 1
            S.dma(modd[l], ot[:], reads=[r_ot])


def emit_final(C, xsrc, fnw, out):
    nc, S = C.nc, C.S
    with C.scope():
        fw_t, r_fw = C.sb([128, D], F32, "fnw_sb")
        S.dma(fw_t[:], fnw.partition_broadcast(128), writes=[r_fw])
        ssf, r_ssf = C.sb([128, 1], F32, "ssf")
        jk, r_jk = C.sb([128, D], F32, "jk")
        xt = [C.sb([128, D], F32, "fx%d" % i) for i in range(2)]
        ob = [C.sb([128, D], F32, "ob%d" % i) for i in range(2)]
        for t in range(32):
            x_t, r_x = xt[t % 2]
            o_t, r_o = ob[t % 2]
            S.dma(x_t[:], xsrc[256 + t * 128:256 + (t + 1) * 128, :], reads=[C.r_xf[(t + 2) // 8]], writes=[r_x])
            S.op("gpsimd", lambda e: e.memset(ssf[:], 0.0), writes=[r_ssf])
            S.op("scalar", lambda e, x_t=x_t: e.activation(out=jk[:], in_=x_t[:], func=AF.Square, accum_out=ssf[:]), reads=[r_x], writes=[r_jk], updates=[r_ssf])
            S.op("vector", lambda e: e.tensor_scalar(out=ssf[:], in0=ssf[:], scalar1=1.0 / D, scalar2=EPS, op0=ALU.mult, op1=ALU.add), reads=[r_ssf], writes=[r_ssf])
            S.op("scalar", lambda e: e.activation(out=ssf[:], in_=ssf[:], func=AF.Sqrt), reads=[r_ssf], writes=[r_ssf])
            S.op("vector", lambda e: e.reciprocal(out=ssf[:], in_=ssf[:]), reads=[r_ssf], writes=[r_ssf])
            S.op("vector", lambda e, x_t=x_t, o_t=o_t: e.scalar_tensor_tensor(out=o_t[:], in0=x_t[:], scalar=ssf[:, 0:1], in1=fw_t[:], op0=ALU.mult, op1=ALU.mult),
                 reads=[r_x, r_ssf, r_fw], writes=[r_o])
            S.dma(out[t * 128:(t + 1) * 128, :], o_t[:], reads=[r_o], eng="gpsimd")


GROUPS = [[0, 1], [2, 3], [4, 5], [6, 7]]


def build_fused(n_layers=DEPTH, dbg_out=False):
    C = Ctx()
    nc, S = C.nc, C.S
    NTOK = 34 * 128
    xs_in = C.din("xs", [NTOK, D])
    cv = C.din("cv", [2, D])
    aws = [C.din("aw%d" % l, [D, 6144]) for l in range(DEPTH)]
    abs_ = [C.din("ab%d" % l, [1, 6144]) for l in range(DEPTH)]
    nwa = C.din("nwa", [2 * DEPTH, D])
    fnw = C.din("fnw", [1, D])
    wce = [C.din("wce%d" % p, [D, 1536]) for p in range(2)]
    draw = [C.din("draw%d" % p, [1, 4]) for p in range(2)]
    sink = [C.din("sink%d" % p, [1, 2]) for p in range(2)]
    wco = [C.din("wco%d" % p, [D, 2560]) for p in range(2)]
    lbl = C.din("lbl", [4, 512])
    lsel = [C.din("lsel%d" % p, [1, 4]) for p in range(2)]
    hnw = [C.din("hnw%d" % p, [1, 128]) for p in range(2)]
    wo = [C.din("wo%d" % l, [D, D]) for l in range(DEPTH)]
    ropec = C.din("ropec", [NTOK, 128])
    ropes = C.din("ropes", [NTOK, 128])
    cmat_e = C.din("cmat_e", [128, 6, 128])
    crow = C.din("crow", [1, 256])
    ccol = C.din("ccol", [128, 2])
    cmat_o = C.din("cmat_o", [128, 2, 128])
    wr = [C.din("wr%d" % l, [D, 36]) for l in range(DEPTH)]
    br = [C.din("br%d" % l, [1, 36]) for l in range(DEPTH)]
    w1 = [C.din("w1_%d" % l, [16, D, 512]) for l in range(DEPTH)]
    w3 = [C.din("w3_%d" % l, [16, D, 512]) for l in range(DEPTH)]
    w2 = [C.din("w2_%d" % l, [16, 512, D]) for l in range(DEPTH)]
    out = C.dout("out", [L, D])
    if dbg_out:
        xdbg = C.dout("xdbg", [NTOK, D])
    xfull = C.dram("xfull", [NTOK, D])
    ypart = C.dram("ypart", [NTOK, D])
    mgl = C.dram("mgl", [NTOK, 512], BF16)
    mgall = C.dram("mgall", [2 * NTOK, 512], BF16)
    moddt = C.dram("modd", [DEPTH, 2, 6144])
    modd = [moddt[l] for l in range(DEPTH)]
    with C.stack:
        C.prefix = "cp_"
        for t in range(34):
            S.dma(xfull[t * 128:(t + 1) * 128, :], xs_in[t * 128:(t + 1) * 128, :], updates=[C.r_xf[t // 8]], eng=("sync" if t % 2 == 0 else "gpsimd"))
        C.prefix = "mod_"
        emit_mod(C, cv, aws, abs_, modd)
        for l in range(n_layers):
            p = l // 2
            modv = modd[l].rearrange("s (k d) -> s k d", k=6)
            C.prefix = "L%dmix_" % l
            if l % 2 == 0:
                emit_even(C, dict(xs=xfull, modv=modv, nw=nwa[2 * l:2 * l + 1, :], wc=wce[p], ropec=ropec, ropes=ropes, draw=draw[p], sink=sink[p],
                                  cmat=cmat_e, crow=crow, ccol=ccol, mgo=mgl))
            else:
                emit_odd(C, dict(xs=xfull, modv=modv, nw=nwa[2 * l:2 * l + 1, :], wc=wco[p], lbl=lbl, lsel=lsel[p], hnw=hnw[p], cmat=cmat_o, mgo=mgl))
            ag = []
            for st_k in range(0, NTOK, 2048):
                R_k = min(2048, NTOK - st_k)
                ag.append(lambda g, st_k=st_k, R_k=R_k: g.collective_compute("AllGather", ALU.bypass, replica_groups=GROUPS, ins=[mgl[st_k:st_k + R_k, :].opt()],
                                                                              outs=[mgall[2 * st_k:2 * st_k + 2 * R_k, :].opt()]))
            S.collective(ag)
            def ar_chunk(k_):
                st_k = k_ * 1024
                R_k = min(1024, NTOK - st_k)
                S.cc_async(lambda g, st_k=st_k, R_k=R_k: g.collective_compute("AllReduce", ALU.add, replica_groups=GROUPS, ins=[ypart[st_k:st_k + R_k, :].opt()],
                                                                               outs=[xfull[st_k:st_k + R_k, :].opt()]),
                           reads=[C.r_yp[k_]], writes=[C.r_xf[k_]])
            for half in range(2):
                C.prefix = "L%dmoe%d_" % (l, half)
                emit_moe(C, dict(xs=xfull, mgall=mgall, wo=wo[l], modv=modv, nw=nwa[2 * l + 1:2 * l + 2, :], wr=wr[l], br=br[l], w1=w1[l], w3=w3[l], w2=w2[l], ypart=ypart),
                         tiles=list(range(half * 17, (half + 1) * 17)))
                for k_ in ([0, 1] if half == 0 else [2, 3, 4]):
                    ar_chunk(k_)
        C.prefix = "fin_"
        emit_final(C, xfull, fnw, out)
        if dbg_out:
            for t in range(34):
                S.dma(xdbg[t * 128:(t + 1) * 128, :], xfull[t * 128:(t + 1) * 128, :], reads=[C.r_xf[t // 8]])
        S.emit()
    return nc


_NC = {}


def make_in_maps(x, c, ctx, c_ctx, ada_w, ada_b, norm_w, final_norm_w, ev_w_in, ev_w_out, ret_decay_raw, att_sink,
                 od_w_in, od_w_out, hg_lb_logits, hg_norm_w, moe_wg, moe_bg, moe_we, moe_be, moe_w1, moe_w3, moe_w2):
    f32 = np.float32
    A_ = lambda v: np.ascontiguousarray(np.asarray(v, dtype=f32))
    g = {k: np.asarray(v, dtype=f32) for k, v in dict(x=x, c=c, ctx=ctx, c_ctx=c_ctx, ada_w=ada_w, ada_b=ada_b, norm_w=norm_w, final_norm_w=final_norm_w,
                                                         ev_w_in=ev_w_in, ev_w_out=ev_w_out, ret_decay_raw=ret_decay_raw, att_sink=att_sink, od_w_in=od_w_in,
                                                         od_w_out=od_w_out, hg_lb_logits=hg_lb_logits, hg_norm_w=hg_norm_w, moe_wg=moe_wg, moe_bg=moe_bg,
                                                         moe_we=moe_we, moe_be=moe_be, moe_w1=moe_w1, moe_w3=moe_w3, moe_w2=moe_w2).items()}
    ec = even_consts()
    oc = odd_consts()
    shared = dict(nwa=A_(g['norm_w'].reshape(2 * DEPTH, D)), fnw=A_(g['final_norm_w'][None]), ropec=ec['ropec'], ropes=ec['ropes'], cmat_e=ec['cmat'],
                  crow=ec['crow'], ccol=ec['ccol'], cmat_o=oc['cmat'])
    for l in range(DEPTH):
        shared["aw%d" % l] = A_(g['ada_w'][l])
        shared["ab%d" % l] = A_(g['ada_b'][l][None])
    per_s = []
    for s in range(2):
        d = {}
        for p in range(2):
            win = g['ev_w_in'][p]
            d["wce%d" % p] = A_(np.concatenate([win[:, s * 256:(s + 1) * 256], win[:, 512 + s * 256:512 + (s + 1) * 256],
                                                win[:, 2048 + s * 256:2048 + (s + 1) * 256], win[:, 2560 + s * 128:2560 + (s + 1) * 128],
                                                win[:, 1024 + s * 256:1024 + (s + 1) * 256], win[:, 1536 + s * 256:1536 + (s + 1) * 256],
                                                win[:, 2816 + s * 128:2816 + (s + 1) * 128]], 1))
            d["draw%d" % p] = A_(g['ret_decay_raw'][p][:, s * 2:(s + 1) * 2].reshape(1, 4))
            d["sink%d" % p] = A_(g['att_sink'][p][s * 2:(s + 1) * 2][None])
            wino = g['od_w_in'][p]
            d["wco%d" % p] = A_(np.concatenate([wino[:, k * 1024 + s * 512:k * 1024 + (s + 1) * 512] for k in range(5)], 1))
            lo = 2 * p + 1
            d["lsel%d" % p] = np.array([[0.0] + [1.0 if m <= lo else 0.0 for m in range(1, 4)]], f32)
            d["hnw%d" % p] = A_(g['hg_norm_w'][p][None])
        d["lbl"] = A_(g['hg_lb_logits'][:, s * 512:(s + 1) * 512])
        go = [2 * s, 2 * s + 1, 2 * (1 - s), 2 * (1 - s) + 1]
        for l in range(DEPTH):
            d["wr%d" % l] = A_(np.concatenate([g['moe_wg'][l][:, go]] + [g['moe_we'][l][gi] for gi in go], 1))
            d["br%d" % l] = A_(np.concatenate([g['moe_bg'][l][go]] + [g['moe_be'][l][gi] for gi in go])[None])
            d["w1_%d" % l] = A_(g['moe_w1'][l][s * 16:(s + 1) * 16])
            d["w3_%d" % l] = A_(g['moe_w3'][l][s * 16:(s + 1) * 16])
            d["w2_%d" % l] = A_(g['moe_w2'][l][s * 16:(s + 1) * 16])
        per_s.append(d)
    for l in range(DEPTH):
        if l % 2 == 0:
            w = g['ev_w_out'][l // 2]
            shared["wo%d" % l] = A_(np.concatenate([w[0:256], w[512:768], w[256:512], w[768:1024]], 0))
        else:
            shared["wo%d" % l] = A_(g['od_w_out'][l // 2])
    maps = []
    for i in range(8):
        b, s = i // 2, i % 2
        d = dict(shared)
        d.update(per_s[s])
        d["xs"] = A_(np.concatenate([g['ctx'][b], g['x'][b]], 0))
        d["cv"] = A_(np.stack([g['c_ctx'], g['c'][b]]))
        maps.append(d)
    return maps


def kernel(x, c, ctx, c_ctx, ada_w, ada_b, norm_w, final_norm_w, ev_w_in, ev_w_out, ret_decay_raw, att_sink,
           od_w_in, od_w_out, hg_lb_logits, hg_norm_w, moe_wg, moe_bg, moe_we, moe_be, moe_w1, moe_w3, moe_w2):
    if "fused" not in _NC:
        _NC["fused"] = build_fused()
    maps = make_in_maps(x, c, ctx, c_ctx, ada_w, ada_b, norm_w, final_norm_w, ev_w_in, ev_w_out, ret_decay_raw, att_sink,
                        od_w_in, od_w_out, hg_lb_logits, hg_norm_w, moe_wg, moe_bg, moe_we, moe_be, moe_w1, moe_w3, moe_w2)
    res = run_bass_kernel_spmd(_NC["fused"], maps, core_ids=list(range(8)))
    return np.stack([res.results[2 * b]["out"] for b in range(B)], 0).astype(np.float32)
```

```python
import contextlib
import numpy as np
import ml_dtypes
import concourse.bass as bass
import concourse.mybir as mybir
from concourse.bass_utils import run_bass_kernel_spmd

F32 = mybir.dt.float32
BF16 = mybir.dt.bfloat16
ALU = mybir.AluOpType
AF = mybir.ActivationFunctionType
AX = mybir.AxisListType
ENGINES = ("tensor", "vector", "scalar", "gpsimd", "sync")

D = 1024
L = 4096
NCTX = 256
B = 4
DEPTH = 4
EPS = 1e-6
NEG = -1e30


class Res:
    __slots__ = ("name", "writers", "readers")

    def __init__(self, name=""):
        self.name = name
        self.writers = []
        self.readers = []


class Op:
    __slots__ = ("eng", "fn", "deps", "sig", "sigval", "is_dma", "dsem", "dval", "cc", "ccval")

    def __init__(self, eng, fn, is_dma=False):
        self.eng = eng
        self.fn = fn
        self.deps = []
        self.sig = False
        self.sigval = 0
        self.is_dma = is_dma
        self.dsem = None
        self.dval = 0
        self.cc = False
        self.ccval = 0


class Sched:
    def __init__(self, nc, n_dma_sems=32):
        self.nc = nc
        self.ops = {e: [] for e in ENGINES}
        self.n_dma_sems = n_dma_sems
        self.dma_rr = 0
        self.dma_rr2 = 0
        self.n_sync_sems = 20
        self.dma_last = [None] * n_dma_sems
        self.dma_cnt = [0] * n_dma_sems
        self.nops = 0
        self.cc_count = 0

    def _add_dep(self, op, dep):
        if dep is None or dep is op:
            return
        if dep.eng == op.eng and not dep.is_dma and not op.is_dma and op.eng == "tensor":
            return
        if dep not in op.deps:
            op.deps.append(dep)
            dep.sig = True

    def op(self, eng, fn, reads=(), writes=(), updates=(), is_dma=False):
        o = Op(eng, fn, is_dma)
        for r in reads:
            for w in r.writers:
                self._add_dep(o, w)
        for r in list(writes) + list(updates):
            for w in r.writers:
                self._add_dep(o, w)
            for rd in r.readers:
                if rd.eng == eng and not rd.is_dma and not is_dma:
                    continue
                self._add_dep(o, rd)
        if is_dma:
            if eng == "sync":
                k = self.dma_rr % self.n_sync_sems
                self.dma_rr += 1
            else:
                k = self.n_sync_sems + self.dma_rr2 % (self.n_dma_sems - self.n_sync_sems)
                self.dma_rr2 += 1
            self._add_dep(o, self.dma_last[k])
            self.dma_cnt[k] += 16
            o.dsem = k
            o.dval = self.dma_cnt[k]
            self.dma_last[k] = o
        for r in reads:
            r.readers = [x for x in r.readers if not (x.eng == eng and not x.is_dma and not is_dma)] + [o]
        for r in writes:
            r.writers = [o]
            r.readers = []
        for r in updates:
            r.writers = [x for x in r.writers if not (x.eng == eng and not x.is_dma and not is_dma)] + [o]
            r.readers = []
        self.ops[eng].append(o)
        self.nops += 1
        return o

    def barrier(self):
        lasts = []
        for e in ENGINES:
            for o in reversed(self.ops[e]):
                if not o.is_dma and not o.cc:
                    lasts.append(o)
                    break
        lasts += [o for o in self.dma_last if o is not None]
        for e in ENGINES:
            o = Op(e, lambda eng: eng.nop())
            for d in lasts:
                if d.eng == e and not d.is_dma:
                    continue
                if d not in o.deps:
                    o.deps.append(d)
                    d.sig = True
            self.ops[e].append(o)

    def collective(self, fn):
        self.barrier()
        fns = fn if isinstance(fn, (list, tuple)) else [fn]
        for f in fns:
            o = Op("gpsimd", f)
            o.cc = True
            self.cc_count += 1
            o.ccval = self.cc_count
            self.ops["gpsimd"].append(o)
        for e in ENGINES:
            w = Op(e, lambda eng: eng.nop())
            w.deps.append(o)
            self.ops[e].append(w)

    def cc_async(self, fn, reads=(), writes=()):
        o = self.op("gpsimd", fn, reads=reads, writes=writes)
        o.cc = True
        self.cc_count += 1
        o.ccval = self.cc_count
        return o

    def dma(self, out, in_, reads=(), writes=(), updates=(), eng="sync", **kw):
        return self.op(eng, lambda e: e.dma_start(out=out, in_=in_, **kw), reads, writes, updates, is_dma=True)

    def emit(self):
        nc = self.nc
        cnt = {e: 0 for e in ENGINES}
        for e in ENGINES:
            for o in self.ops[e]:
                if o.sig and not o.is_dma and not o.cc:
                    cnt[e] += 1
                    o.sigval = cnt[e]
        with contextlib.ExitStack() as st:
            esem = {e: st.enter_context(nc.semaphore("s_" + e)) for e in ENGINES}
            dsem = [st.enter_context(nc.semaphore("d_%d" % i)) for i in range(self.n_dma_sems)]
            ccsem = st.enter_context(nc.semaphore("s_cc"))
            block = st.enter_context(nc.Block())

            def make(ename):
                def body(eng):
                    waited = {}
                    for o in self.ops[ename]:
                        for d in o.deps:
                            if d.cc:
                                key, val, sem = ("c", 0), d.ccval, ccsem
                            elif d.is_dma:
                                key, val, sem = ("d", d.dsem), d.dval, dsem[d.dsem]
                            else:
                                key, val, sem = ("e", d.eng), d.sigval, esem[d.eng]
                            if waited.get(key, 0) < val:
                                eng.wait_ge(sem, val)
                                waited[key] = val
                        ins = o.fn(eng)
                        if o.cc:
                            ins.then_inc(ccsem, 1)
                        elif o.is_dma:
                            ins.then_inc(dsem[o.dsem], 16)
                        elif o.sig:
                            ins.then_inc(esem[ename], 1)
                    if ename == "sync":
                        for k in range(self.n_dma_sems):
                            if self.dma_cnt[k] > 0:
                                eng.wait_ge(dsem[k], self.dma_cnt[k])
                        for e2 in ENGINES:
                            if e2 != "sync" and cnt[e2] > 0:
                                eng.wait_ge(esem[e2], cnt[e2])
                return body

            for e in ENGINES:
                getattr(block, e)(make(e))


class Ctx:
    def __init__(self, name=""):
        self.nc = bass.Bass("TRN2", target_bir_lowering=False)
        self.S = Sched(self.nc)
        self.stack = contextlib.ExitStack()
        self.n = 0
        self.prefix = ""
        self.r_xf = [Res("xf%d" % k) for k in range(5)]
        self.r_yp = [Res("yp%d" % k) for k in range(5)]

    @contextlib.contextmanager
    def scope(self):
        saved = self.stack
        st = contextlib.ExitStack()
        self.stack = st
        with st:
            yield
        self.stack = saved
        self.S.barrier()

    def dram(self, name, shape, dt=F32):
        return self.nc.dram_tensor(name, list(shape), dt).ap()

    def sb(self, shape, dt, name=None):
        self.n += 1
        nm = self.prefix + (name or "sb") + "_%d" % self.n
        t = self.stack.enter_context(self.nc.sbuf_tensor(nm, list(shape), dt))
        return t, Res(nm)

    def ps(self, shape, dt, name=None):
        self.n += 1
        full = 512 if dt == F32 else 1024
        nm = self.prefix + (name or "ps") + "_%d" % self.n
        t = self.stack.enter_context(self.nc.psum_tensor(nm, [128, full], dt))
        n = int(np.prod(shape[1:]))
        v = t[0:shape[0], 0:n]
        if len(shape) == 3:
            v = v.rearrange("p (a b) -> p a b", a=shape[1])
        return v, Res(name or ("ps%d" % self.n))

    def din(self, name, shape, dt=F32):
        return self.nc.dram_tensor(name, list(shape), dt, kind="ExternalInput").ap()

    def dout(self, name, shape, dt=F32):
        return self.nc.dram_tensor(name, list(shape), dt, kind="ExternalOutput").ap()


def run_pipelined(gens, offset):
    it = iter(gens)
    pending = next(it, None)
    active = []
    while active or pending is not None:
        if pending is not None and (not active or active[-1][1] >= offset):
            active.append([pending, 0])
            pending = next(it, None)
        for a_ in list(active):
            r = next(a_[0], 'END')
            a_[1] += 1
            if r == 'END':
                active.remove(a_)


def make_identity(C, dt=BF16):
    S = C.S
    idf, r_idf = C.sb([128, 128], F32)
    S.op("gpsimd", lambda e: e.memset(idf[:], 0.0), writes=[r_idf])
    S.op("gpsimd", lambda e: e.affine_select(out=idf[:], in_=idf[:], pattern=[[-1, 128]], compare_op=ALU.not_equal,
                                             fill=1.0, base=0, channel_multiplier=1), updates=[r_idf])
    if dt == F32:
        return idf, r_idf
    idb, r_idb = C.sb([128, 128], dt)
    S.op("vector", lambda e: e.tensor_copy(out=idb[:], in_=idf[:]), reads=[r_idf], writes=[r_idb])
    return idb, r_idb, idf, r_idf


def emit_moe(C, A, tiles, NEXP=16):
    nc, S = C.nc, C.S
    NT = len(tiles)
    NTOK = NT * 128
    skip_outproj = False
    xs, mgall, wo, modv, nw, wr, br, w1, w3, w2, ypart = (A[k] for k in "xs mgall wo modv nw wr br w1 w3 w2 ypart".split())
    with C.scope():
        idb, r_idb, idf, r_idf = make_identity(C)
        acc, _ = C.sb([128, NT, D], F32, "acc")
        r_acc = [[Res("acc%d_%d" % (t, h)) for h in range(2)] for t in range(NT)]
        h2T, _ = C.sb([128, 8, NTOK], BF16, "h2T")
        r_h2T = [Res("h2T%d" % t) for t in range(NT)]
        gates, _ = C.sb([128, NT, 32], F32, "gates")
        r_gates = [Res("g%d" % t) for t in range(NT)]
        gmlp = [C.sb([128, D], F32, "gmlp%d" % i) for i in range(2)]
        for i in range(2):
            S.dma(gmlp[i][0][:], modv[i, 5:6, :].partition_broadcast(128), writes=[gmlp[i][1]])
        wrt, r_wrt = C.sb([128, 8, 36], F32, "wrt")
        S.dma(wrt[:], wr.rearrange("(c p) j -> p c j", p=128), writes=[r_wrt])
        brt, r_brt = C.sb([128, 36], F32, "brt")
        S.dma(brt[:], br.partition_broadcast(128), writes=[r_brt])

        stA = contextlib.ExitStack()
        C_stack_saved = C.stack
        C.stack = stA
        with stA:
            wob, r_wob = C.sb([128, 8, D], BF16, "wob")
            if not skip_outproj:
                load_weight_bf16(C, wob, r_wob, wo, D)
            gmsa, r_gmsa = C.sb([128, D], F32, "gmsa")
            w2e, r_w2e = C.sb([128, D], F32, "w2e")
            sh2, r_sh2 = C.sb([128, D], F32, "sh2")
            nwt, r_nwt = C.sb([128, D], F32, "nwt")
            S.dma(nwt[:], nw.partition_broadcast(128), writes=[r_nwt])
            xt = [C.sb([128, D], F32, "xt%d" % i) for i in range(2)]
            mgt = [C.sb([128, D], BF16, "mgt%d" % i) for i in range(2)]
            mgT = [C.sb([128, 8, 128], BF16, "mgT%d" % i) for i in range(2)]
            junk, r_junk = C.sb([128, D], F32, "junk")
            tmpA_l = [C.sb([128, D], F32, "tmpA%d" % i) for i in range(2)]
            h2f_l = [C.sb([128, D], F32, "h2f%d" % i) for i in range(2)]
            h2b_l = [C.sb([128, D], BF16, "h2b%d" % i) for i in range(2)]
            h2Tf_l = [C.sb([128, 8, 128], F32, "h2Tf%d" % i) for i in range(2)]
            sm_l = [{k: C.sb([128, n], F32, "sm%d_" % i + k) for k, n in
                     dict(ss=1, rstd=1, lg=36, m4=1, ng=1, ex4=4, se=1, gp=1, oh=4, pen=4, msk=32, top8=8, sel=32, nt1=1, d21=1,
                          e21=1, coef=1, wf=32).items()} for i in range(2)]
            pT = [C.ps([128, 8, 128], BF16, "pT%d" % i) for i in range(2)]
            pTf = [C.ps([128, 4, 128], F32, "pTf%d" % i) for i in range(2)]
            pY = [C.ps([128, 512], F32, "pY%d" % i) for i in range(2)]
            pL, r_pL = C.ps([128, 36], F32, "pL")
            prev = [None]

            def do_tile(t):
                gt = tiles[t]
                tmpA, r_tmpA = tmpA_l[t % 2]
                h2f, r_h2f = h2f_l[t % 2]
                h2b, r_h2b = h2b_l[t % 2]
                h2Tf, r_h2Tf = h2Tf_l[t % 2]
                sm = sm_l[t % 2]
                si = 0 if gt < 2 else 1
                if si != prev[0]:
                    prev[0] = si
                    S.dma(gmsa[:], modv[si, 2:3, :].partition_broadcast(128), writes=[r_gmsa])
                    S.dma(sh2[:], modv[si, 3:4, :].partition_broadcast(128), writes=[r_sh2])
                    S.dma(w2e[:], modv[si, 4:5, :].partition_broadcast(128), writes=[r_w2e])
                    S.op("vector", lambda e: e.scalar_tensor_tensor(out=w2e[:], in0=w2e[:], scalar=1.0, in1=nwt[:], op0=ALU.add, op1=ALU.mult),
                         reads=[r_w2e, r_nwt], writes=[r_w2e])
                x_t, r_x = xt[t % 2]
                S.dma(x_t[:], xs[gt * 128:(gt + 1) * 128, :], reads=[C.r_xf[gt // 8]], writes=[r_x])
                a_t = acc[:, t, :]
                ra = r_acc[t]
                if not skip_outproj:
                    m_t, r_m = mgt[t % 2]
                    mT, r_mT = mgT[t % 2]
                    p_T, r_pT = pT[t % 2]
                    st_k = (gt // 16) * 2048
                    R_k = min(2048, 4352 - st_k)
                    r0 = 2 * st_k + gt * 128 - st_k
                    S.dma(m_t[:, 0:512], mgall[r0:r0 + 128, :], writes=[r_m], eng="gpsimd")
                    S.dma(m_t[:, 512:1024], mgall[r0 + R_k:r0 + R_k + 128, :], updates=[r_m], eng="gpsimd")
                    yield 'S'
                    for c in range(8):
                        S.op("tensor", lambda e, c=c, p_T=p_T, m_t=m_t: e.transpose(out=p_T[:, c, :], in_=m_t[:, c * 128:(c + 1) * 128], identity=idb[:]),
                             reads=[r_m, r_idb], updates=[r_pT] if c else (), writes=() if c else [r_pT])
                    S.op("scalar", lambda e, mT=mT, p_T=p_T: e.copy(out=mT[:], in_=p_T[:]), reads=[r_pT], writes=[r_mT])
                    yield 'S'
                    for hf in range(2):
                        p_Y, r_pY = pY[hf]
                        for c in range(8):
                            S.op("tensor", lambda e, c=c, hf=hf, p_Y=p_Y, mT=mT: e.matmul(out=p_Y[:], lhsT=mT[:, c, :], rhs=wob[:, c, hf * 512:(hf + 1) * 512],
                                                                                         start=(c == 0), stop=(c == 7)),
                                 reads=[r_mT, r_wob], updates=[r_pY] if c else (), writes=() if c else [r_pY])
                        sl = slice(hf * 512, (hf + 1) * 512)
                        S.op("vector", lambda e, p_Y=p_Y, sl=sl: e.tensor_tensor(out=tmpA[:, sl], in0=p_Y[:], in1=gmsa[:, sl], op=ALU.mult),
                             reads=[r_pY, r_gmsa], updates=[r_tmpA])
                        S.op("gpsimd", lambda e, sl=sl, a_t=a_t, x_t=x_t: e.tensor_tensor(out=a_t[:, sl], in0=tmpA[:, sl], in1=x_t[:, sl], op=ALU.add),
                             reads=[r_tmpA, r_x], writes=[ra[hf]])
                else:
                    for hf in range(2):
                        sl = slice(hf * 512, (hf + 1) * 512)
                        S.op("gpsimd", lambda e, sl=sl, a_t=a_t, x_t=x_t: e.tensor_copy(out=a_t[:, sl], in_=x_t[:, sl]), reads=[r_x], writes=[ra[hf]])
                yield 'S'
                ss, r_ss = sm["ss"]
                rstd, r_rstd = sm["rstd"]
                T_se, R_se = sm["se"]
                S.op("gpsimd", lambda e: e.memset(ss[:], 0.0), writes=[r_ss])
                S.op("gpsimd", lambda e: e.memset(T_se[:], 0.0), writes=[R_se])
                S.op("scalar", lambda e, a_t=a_t: e.activation(out=junk[:], in_=a_t, func=AF.Square, accum_out=ss[:]),
                     reads=ra, writes=[r_junk], updates=[r_ss])
                S.op("vector", lambda e: e.tensor_scalar(out=rstd[:], in0=ss[:], scalar1=1.0 / D, scalar2=EPS, op0=ALU.mult, op1=ALU.add),
                     reads=[r_ss], writes=[r_rstd])
                S.op("scalar", lambda e: e.activation(out=rstd[:], in_=rstd[:], func=AF.Sqrt), reads=[r_rstd], writes=[r_rstd])
                S.op("vector", lambda e: e.reciprocal(out=rstd[:], in_=rstd[:]), reads=[r_rstd], writes=[r_rstd])
                yield 'S'
                S.op("vector", lambda e, a_t=a_t: e.scalar_tensor_tensor(out=h2f[:], in0=a_t, scalar=rstd[:, 0:1], in1=w2e[:], op0=ALU.mult, op1=ALU.mult),
                     reads=ra + [r_rstd, r_w2e], writes=[r_h2f])
                S.op("gpsimd", lambda e: e.tensor_tensor(out=h2f[:], in0=h2f[:], in1=sh2[:], op=ALU.add), reads=[r_h2f, r_sh2], writes=[r_h2f])
                for hf in range(2):
                    sl = slice(hf * 512, (hf + 1) * 512)
                    S.op("scalar", lambda e, a_t=a_t, sl=sl: e.mul(out=a_t[:, sl], in_=a_t[:, sl], mul=0.5), reads=[], writes=[ra[hf]])
                S.op("vector", lambda e: e.tensor_copy(out=h2b[:], in_=h2f[:]), reads=[r_h2f], writes=[r_h2b])
                yield 'S'
                p_T, r_pT = pT[(t + 1) % 2]
                for c in range(8):
                    S.op("tensor", lambda e, c=c, p_T=p_T: e.transpose(out=p_T[:, c, :], in_=h2b[:, c * 128:(c + 1) * 128], identity=idb[:]),
                         reads=[r_h2b, r_idb], updates=[r_pT] if c else (), writes=() if c else [r_pT])
                S.op("scalar", lambda e, p_T=p_T, t=t: e.copy(out=h2T[:, :, t * 128:(t + 1) * 128], in_=p_T[:]), reads=[r_pT], writes=[r_h2T[t]])
                yield 'S'
                for g4 in range(2):
                    p_f, r_pf = pTf[g4]
                    for c in range(4):
                        cc = g4 * 4 + c
                        S.op("tensor", lambda e, c=c, cc=cc, p_f=p_f: e.transpose(out=p_f[:, c, :], in_=h2f[:, cc * 128:(cc + 1) * 128], identity=idf[:]),
                             reads=[r_h2f, r_idf], updates=[r_pf] if c else (), writes=() if c else [r_pf])
                    S.op("vector" if g4 else "scalar", (lambda e, p_f=p_f, g4=g4: e.tensor_copy(out=h2Tf[:, g4 * 4:(g4 + 1) * 4, :], in_=p_f[:])) if g4 else
                         (lambda e, p_f=p_f, g4=g4: e.copy(out=h2Tf[:, g4 * 4:(g4 + 1) * 4, :], in_=p_f[:])),
                         reads=[r_pf], updates=[r_h2Tf])
                yield 'S'
                for c in range(8):
                    S.op("tensor", lambda e, c=c: e.matmul(out=pL[:], lhsT=h2Tf[:, c, :], rhs=wrt[:, c, :], start=(c == 0), stop=(c == 7)),
                         reads=[r_h2Tf, r_wrt], updates=[r_pL] if c else (), writes=() if c else [r_pL])
                yield 'S'
                def T(k):
                    return sm[k][0]

                def Rr(k):
                    return sm[k][1]
                V = lambda fn, reads, writes: S.op("vector", fn, reads=reads, writes=writes)
                V(lambda e: e.tensor_tensor(out=T("lg")[:], in0=pL[:], in1=brt[:], op=ALU.add), [r_pL, r_brt], [Rr("lg")])
                V(lambda e: e.reduce_max(out=T("m4")[:], in_=T("lg")[:, 0:4], axis=AX.X), [Rr("lg")], [Rr("m4")])
                V(lambda e: e.tensor_scalar(out=T("ng")[:], in0=T("m4")[:], scalar1=-1.0, scalar2=None, op0=ALU.mult), [Rr("m4")], [Rr("ng")])
                S.op("scalar", lambda e: e.activation(out=T("ex4")[:], in_=T("lg")[:, 0:4], func=AF.Exp, bias=T("ng")[:, 0:1], scale=1.0, accum_out=T("se")[:]),
                     reads=[Rr("lg"), Rr("ng")], writes=[Rr("ex4")], updates=[Rr("se")])
                yield 'S'
                V(lambda e: e.reciprocal(out=T("gp")[:], in_=T("se")[:]), [Rr("se")], [Rr("gp")])
                V(lambda e: e.tensor_scalar(out=T("oh")[:], in0=T("lg")[:, 0:4], scalar1=T("m4")[:, 0:1], scalar2=None, op0=ALU.is_ge), [Rr("lg"), Rr("m4")], [Rr("oh")])
                V(lambda e: e.tensor_scalar(out=T("pen")[:], in0=T("oh")[:], scalar1=-1.0, scalar2=-NEG, op0=ALU.add, op1=ALU.mult), [Rr("oh")], [Rr("pen")])
                V(lambda e: e.tensor_tensor(out=T("msk")[:].rearrange("p (g x) -> p g x", g=4), in0=T("lg")[:, 4:36].rearrange("p (g x) -> p g x", g=4),
                                            in1=T("pen")[:].rearrange("p (g o) -> p g o", o=1).broadcast_to([128, 4, 8]), op=ALU.add),
                  [Rr("lg"), Rr("pen")], [Rr("msk")])
                V(lambda e: e.max(out=T("top8")[:], in_=T("msk")[:]), [Rr("msk")], [Rr("top8")])
                V(lambda e: e.tensor_scalar(out=T("sel")[:], in0=T("msk")[:], scalar1=T("top8")[:, 1:2], scalar2=None, op0=ALU.is_ge), [Rr("msk"), Rr("top8")], [Rr("sel")])
                V(lambda e: e.tensor_scalar(out=T("nt1")[:], in0=T("top8")[:, 0:1], scalar1=-1.0, scalar2=None, op0=ALU.mult), [Rr("top8")], [Rr("nt1")])
                V(lambda e: e.tensor_tensor(out=T("d21")[:], in0=T("top8")[:, 1:2], in1=T("top8")[:, 0:1], op=ALU.subtract), [Rr("top8")], [Rr("d21")])
                yield 'S'
                S.op("scalar", lambda e: e.activation(out=T("e21")[:], in_=T("d21")[:], func=AF.Exp), reads=[Rr("d21")], writes=[Rr("e21")])
                S.op("scalar", lambda e: e.activation(out=T("wf")[:], in_=T("msk")[:], func=AF.Exp, bias=T("nt1")[:, 0:1], scale=1.0),
                     reads=[Rr("msk"), Rr("nt1")], writes=[Rr("wf")])
                yield 'S'
                V(lambda e: e.tensor_scalar(out=T("coef")[:], in0=T("e21")[:], scalar1=1.0, scalar2=None, op0=ALU.add), [Rr("e21")], [Rr("coef")])
                V(lambda e: e.reciprocal(out=T("coef")[:], in_=T("coef")[:]), [Rr("coef")], [Rr("coef")])
                V(lambda e: e.tensor_tensor(out=T("coef")[:], in0=T("coef")[:], in1=T("gp")[:], op=ALU.mult), [Rr("coef"), Rr("gp")], [Rr("coef")])
                V(lambda e, t=t: e.scalar_tensor_tensor(out=gates[:, t, :], in0=T("wf")[:], scalar=T("coef")[:, 0:1], in1=T("sel")[:], op0=ALU.mult, op1=ALU.mult),
                  [Rr("wf"), Rr("coef"), Rr("sel")], [r_gates[t]])
            run_pipelined((do_tile(t) for t in range(NT)), 6)
        C.stack = C_stack_saved
        S.barrier()

        stB = contextlib.ExitStack()
        C.stack = stB
        with stB:
            NST = 3
            stg = [C.sb([128, 4, 512], F32, "stg%d" % i) for i in range(NST)]
            w1b = [C.sb([128, 8, 512], BF16, "w1b%d" % i) for i in range(2)]
            w3b = [C.sb([128, 8, 512], BF16, "w3b%d" % i) for i in range(2)]
            w2b = [C.sb([128, 4, D], BF16, "w2b%d" % i) for i in range(2)]
            sg = [C.sb([128, 512], F32, "sg%d" % i) for i in range(2)]
            act = [C.sb([128, 4, 512], BF16, "act%d" % i) for i in range(2)]
            tmpB = [C.sb([128, 512], F32, "tmpB%d" % i) for i in range(2)]
            pU1 = [C.ps([128, 512], F32, "pU1_%d" % i) for i in range(2)]
            pU3 = [C.ps([128, 512], F32, "pU3_%d" % i) for i in range(2)]
            pYB = [C.ps([128, 512], F32, "pYB%d" % i) for i in range(3)]
            chunks = []
            t0 = 0
            while t0 < NT:
                chunks.append(list(range(t0, min(t0 + 4, NT))))
                t0 += 4
            si_ = 0
            ny = 0
            nact = 0
            sic = [0]

            def load_piece(ex_, pi):
                pb_ = ex_ % 2
                if pi < 4:
                    wsrc, dst = ((w1, w1b[pb_]), (w3, w3b[pb_]))[pi // 2]
                    half = pi % 2
                    src = wsrc[ex_, half * 512:(half + 1) * 512, :].rearrange("(c p) f -> p c f", p=128)
                    dstap, r_dst = dst[0][:, half * 4:(half + 1) * 4, :], dst[1]
                else:
                    half = pi - 4
                    src = w2[ex_, half * 256:(half + 1) * 256, :].rearrange("(c p) f -> p c f", p=128)
                    dstap, r_dst = w2b[pb_][0][:, half * 2:(half + 1) * 2, :], w2b[pb_][1]
                s_t, r_s = stg[sic[0] % NST]
                sic[0] += 1
                sv = s_t[:] if pi < 4 else s_t[:].rearrange("p (c a) f -> p c (a f)", a=2)
                S.dma(sv, src, writes=[r_s], eng="sync")
                pending.append((sv, r_s, dstap, r_dst))

            pending = []

            def cast_pending():
                while pending:
                    sv, r_s, dstap, r_dst = pending.pop(0)
                    S.op("scalar", lambda e, sv=sv, dstap=dstap: e.copy(out=dstap, in_=sv), reads=[r_s], updates=[r_dst])

            for pi in range(6):
                load_piece(0, pi)
                if len(pending) >= 2:
                    cast_pending()
            cast_pending()
            sched_next = {0: [0, 1], 1: [2], 2: [3, 4], 3: [5]} if len(chunks) >= 4 else {0: [0, 1, 2, 3, 4, 5]}
            for ex in range(NEXP):
                pb = ex % 2
                W1, rW1 = w1b[pb]
                W3, rW3 = w3b[pb]
                W2, rW2 = w2b[pb]
                for ci_, ch in enumerate(chunks):
                    cast_pending()
                    if ex + 1 < NEXP:
                        for pi in sched_next.get(ci_, []):
                            load_piece(ex + 1, pi)
                    n = len(ch) * 128
                    c0 = ch[0] * 128
                    a_t, r_a = act[nact % 2]
                    nact += 1
                    rh = [r_h2T[t] for t in ch]
                    for fc in range(4):
                        p1, r_p1 = pU1[fc % 2]
                        p3, r_p3 = pU3[fc % 2]
                        for c in range(8):
                            S.op("tensor", lambda e, c=c, fc=fc, p1=p1, W1=W1, c0=c0, n=n: e.matmul(out=p1[:, 0:n], lhsT=W1[:, c, fc * 128:(fc + 1) * 128], rhs=h2T[:, c, c0:c0 + n],
                                                                                                       start=(c == 0), stop=(c == 7)),
                                 reads=rh + [rW1], updates=[r_p1] if c else (), writes=() if c else [r_p1])
                        for c in range(8):
                            S.op("tensor", lambda e, c=c, fc=fc, p3=p3, W3=W3, c0=c0, n=n: e.matmul(out=p3[:, 0:n], lhsT=W3[:, c, fc * 128:(fc + 1) * 128], rhs=h2T[:, c, c0:c0 + n],
                                                                                                       start=(c == 0), stop=(c == 7)),
                                 reads=rh + [rW3], updates=[r_p3] if c else (), writes=() if c else [r_p3])
                        s_g, r_sg = sg[fc % 2]
                        S.op("scalar", lambda e, s_g=s_g, p1=p1, n=n: e.activation(out=s_g[:, 0:n], in_=p1[:, 0:n], func=AF.Silu), reads=[r_p1], writes=[r_sg])
                        S.op("vector", lambda e, s_g=s_g, p3=p3, a_t=a_t, fc=fc, n=n: e.tensor_tensor(out=a_t[:, fc, 0:n], in0=s_g[:, 0:n], in1=p3[:, 0:n], op=ALU.mult),
                             reads=[r_sg, r_p3], updates=[r_a] if fc else (), writes=() if fc else [r_a])
                    for ti, t in enumerate(ch):
                        gm = gmlp[0 if tiles[t] < 2 else 1]
                        for hf in range(2):
                            p_y, r_py = pYB[ny % 3]
                            tb, r_tb = tmpB[ny % 2]
                            ny += 1
                            for fc in range(4):
                                S.op("tensor", lambda e, fc=fc, p_y=p_y, a_t=a_t, ti=ti, W2=W2, hf=hf: e.matmul(out=p_y[:], lhsT=a_t[:, fc, ti * 128:(ti + 1) * 128],
                                                                                                                  rhs=W2[:, fc, hf * 512:(hf + 1) * 512], start=(fc == 0), stop=(fc == 3)),
                                     reads=[r_a, rW2], updates=[r_py] if fc else (), writes=() if fc else [r_py])
                            sl = slice(hf * 512, (hf + 1) * 512)
                            S.op("vector", lambda e, p_y=p_y, tb=tb, t=t, ex=ex, gm=gm, sl=sl: e.scalar_tensor_tensor(out=tb[:], in0=p_y[:], scalar=gates[:, t, ex:ex + 1], in1=gm[0][:, sl],
                                                                                                                        op0=ALU.mult, op1=ALU.mult),
                                 reads=[r_py, r_gates[t], gm[1]], writes=[r_tb])
                            S.op("gpsimd", lambda e, tb=tb, t=t, sl=sl: e.tensor_tensor(out=acc[:, t, sl], in0=acc[:, t, sl], in1=tb[:], op=ALU.add),
                                 reads=[r_tb], writes=[r_acc[t][hf]])
                cast_pending()
        C.stack = C_stack_saved
        for t in range(NT):
            gt = tiles[t]
            S.dma(ypart[gt * 128:(gt + 1) * 128, :], acc[:, t, :], reads=r_acc[t], updates=[C.r_yp[gt // 8]])


class NormIn:
    def __init__(self, C, xs, modv, nw, idb, r_idb, nps=2):
        self.C, self.xs, self.modv = C, xs, modv
        S = C.S
        self.idb, self.r_idb = idb, r_idb
        self.w1e, self.r_w1e = C.sb([128, D], F32, "w1e")
        self.sh1, self.r_sh1 = C.sb([128, D], F32, "sh1")
        self.nwt, self.r_nwt = C.sb([128, D], F32, "nwt1")
        S.dma(self.nwt[:], nw.partition_broadcast(128), writes=[self.r_nwt])
        self.xt = [C.sb([128, D], F32, "nxt%d" % i) for i in range(2)]
        self.tmp_l = [C.sb([128, D], F32, "ntmp%d" % i) for i in range(2)]
        self.hb_l = [C.sb([128, D], BF16, "nhb%d" % i) for i in range(2)]
        self.ss_l = [C.sb([128, 1], F32, "nss%d" % i) for i in range(2)]
        self.rstd_l = [C.sb([128, 1], F32, "nrstd%d" % i) for i in range(2)]
        self.pT = [C.ps([128, 8, 128], BF16, "npT%d" % i) for i in range(nps)]
        if nps == 1:
            self.pT = self.pT * 2
        self.hT = [C.sb([128, 8, 128], BF16, "nhT%d" % i) for i in range(2)]
        self.cur = None
        self.k = 0

    def load_mod(self, si):
        S, modv = self.C.S, self.modv
        S.dma(self.sh1[:], modv[si, 0:1, :].partition_broadcast(128), writes=[self.r_sh1])
        S.dma(self.w1e[:], modv[si, 1:2, :].partition_broadcast(128), writes=[self.r_w1e])
        S.op("vector", lambda e: e.scalar_tensor_tensor(out=self.w1e[:], in0=self.w1e[:], scalar=1.0, in1=self.nwt[:], op0=ALU.add, op1=ALU.mult),
             reads=[self.r_w1e, self.r_nwt], writes=[self.r_w1e])

    def tile(self, t):
        S = self.C.S
        need = 0 if t < 2 else 1
        if need != self.cur:
            self.load_mod(need)
            self.cur = need
        k = self.k
        self.k += 1
        x_t, r_x = self.xt[k % 2]
        S.dma(x_t[:], self.xs[t * 128:(t + 1) * 128, :], reads=[self.C.r_xf[t // 8]], writes=[r_x])
        ss, r_ss = self.ss_l[k % 2]
        rstd, r_rstd = self.rstd_l[k % 2]
        tmp, r_tmp = self.tmp_l[k % 2]
        hb, r_hb = self.hb_l[k % 2]
        S.op("gpsimd", lambda e: e.memset(ss[:], 0.0), writes=[r_ss])
        S.op("scalar", lambda e: e.activation(out=tmp[:], in_=x_t[:], func=AF.Square, accum_out=ss[:]), reads=[r_x], writes=[r_tmp], updates=[r_ss])
        S.op("vector", lambda e: e.tensor_scalar(out=rstd[:], in0=ss[:], scalar1=1.0 / D, scalar2=EPS, op0=ALU.mult, op1=ALU.add), reads=[r_ss], writes=[r_rstd])
        S.op("scalar", lambda e: e.activation(out=rstd[:], in_=rstd[:], func=AF.Sqrt), reads=[r_rstd], writes=[r_rstd])
        S.op("vector", lambda e: e.reciprocal(out=rstd[:], in_=rstd[:]), reads=[r_rstd], writes=[r_rstd])
        S.op("vector", lambda e: e.scalar_tensor_tensor(out=tmp[:], in0=x_t[:], scalar=rstd[:, 0:1], in1=self.w1e[:], op0=ALU.mult, op1=ALU.mult),
             reads=[r_x, r_rstd, self.r_w1e], writes=[r_tmp])
        S.op("gpsimd", lambda e: e.tensor_tensor(out=hb[:], in0=tmp[:], in1=self.sh1[:], op=ALU.add), reads=[r_tmp, self.r_sh1], writes=[r_hb])
        p_T, r_pT = self.pT[k % 2]
        h_T, r_hT = self.hT[k % 2]
        for c in range(8):
            S.op("tensor", lambda e, c=c: e.transpose(out=p_T[:, c, :], in_=hb[:, c * 128:(c + 1) * 128], identity=self.idb[:]),
                 reads=[r_hb, self.r_idb], updates=[r_pT] if c else (), writes=() if c else [r_pT])
        S.op("scalar", lambda e: e.copy(out=h_T[:], in_=p_T[:]), reads=[r_pT], writes=[r_hT])
        return h_T, r_hT


def load_weight_bf16(C, dst, r_dst, src, ncols, col0=0, eng_cast="gpsimd"):
    S = C.S
    saved = C.stack
    tmpst = contextlib.ExitStack()
    C.stack = tmpst
    with tmpst:
        stgs = [C.sb([128, max(2048, ncols)], F32, "lwst%d_%d" % (C.n, i)) for i in range(2)]
        step = max(1, 2048 // ncols)
        k = 0
        for c0 in range(0, 8, step):
            nck = min(step, 8 - c0)
            s_t, r_s = stgs[k % 2]
            sv = s_t[:, 0:nck * ncols].rearrange("p (c f) -> p c f", c=nck)
            S.dma(sv, src[c0 * 128:(c0 + nck) * 128, :].rearrange("(c p) f -> p c f", p=128), writes=[r_s], eng=("sync" if k % 2 == 0 else "gpsimd"))
            S.op(eng_cast, lambda e, sv=sv, c0=c0, nck=nck: e.tensor_copy(out=dst[:, c0:c0 + nck, col0:col0 + ncols], in_=sv), reads=[r_s], updates=[r_dst])
            k += 1
    C.stack = saved
    S.barrier()


def emit_even(C, A):
    nc, S = C.nc, C.S
    NT = 34
    NTOK = NT * 128
    xs, modv, nw, wc, ropec, ropes, draw, sink, cmat, crow, ccol, mgo = (A[k] for k in "xs modv nw wc ropec ropes draw sink cmat crow ccol mgo".split())
    SC = 128 ** -0.5
    with C.scope():
        idb, r_idb, idf, r_idf = make_identity(C)
        qkT, _ = C.sb([128, 7, NTOK], BF16, "qkT")
        r_qkT = [Res("qkT%d" % t) for t in range(NT)]
        tm, _ = C.sb([128, NT, 896], BF16, "tm")
        r_tm = [Res("tm%d" % t) for t in range(NT)]
        cm, r_cm = C.sb([128, 6, 128], F32, "cm")
        S.dma(cm[:], cmat, writes=[r_cm])
        cr, r_cr = C.sb([128, 256], F32, "cr")
        S.dma(cr[:], crow.partition_broadcast(128), writes=[r_cr])
        cc, r_cc = C.sb([128, 2], F32, "cc")
        S.dma(cc[:], ccol, writes=[r_cc])
        lg, r_lg = C.sb([128, 4], F32, "lg")
        S.dma(lg[:], draw.partition_broadcast(128), writes=[r_lg])
        S.op("scalar", lambda e: e.activation(out=lg[:], in_=lg[:], func=AF.Exp), reads=[r_lg], writes=[r_lg])
        S.op("vector", lambda e: e.tensor_scalar(out=lg[:], in0=lg[:], scalar1=-1.0, scalar2=None, op0=ALU.mult), reads=[r_lg], writes=[r_lg])
        skt, r_skt = C.sb([128, 2], F32, "skt")
        S.dma(skt[:], sink.partition_broadcast(128), writes=[r_skt])
        dmask, r_dmask = C.sb([128, 4, 128], F32, "dmask")
        qdec, r_qdec = C.sb([128, 4, 128], F32, "qdec")
        kdec, r_kdec = C.sb([128, 4], F32, "kdec")
        cdec, r_cdec = C.sb([128, 4], F32, "cdec")
        c128, r_c128 = C.sb([128, 1], F32, "c128")
        S.op("gpsimd", lambda e: e.memset(c128[:], 128.0), writes=[r_c128])
        for dr in range(2):
            for h in range(2):
                ix = dr * 2 + h
                lgc = lg[:, ix:ix + 1]
                S.op("vector", lambda e, ix=ix, dr=dr, lgc=lgc: e.tensor_scalar(out=dmask[:, ix, :], in0=cm[:, dr, :], scalar1=lgc, scalar2=None, op0=ALU.mult),
                     reads=[r_cm, r_lg], updates=[r_dmask])
                S.op("scalar", lambda e, ix=ix: e.activation(out=dmask[:, ix, :], in_=dmask[:, ix, :], func=AF.Exp), reads=[], updates=[r_dmask])
                S.op("vector", lambda e, ix=ix, dr=dr: e.tensor_tensor(out=dmask[:, ix, :], in0=dmask[:, ix, :], in1=cm[:, 2 + dr, :], op=ALU.mult),
                     reads=[r_cm], updates=[r_dmask])
                S.op("vector", lambda e, ix=ix, dr=dr, lgc=lgc: e.tensor_scalar(out=qdec[:, ix, :], in0=cr[:, dr * 128:(dr + 1) * 128], scalar1=lgc, scalar2=None, op0=ALU.mult),
                     reads=[r_cr, r_lg], updates=[r_qdec])
                S.op("scalar", lambda e, ix=ix: e.activation(out=qdec[:, ix, :], in_=qdec[:, ix, :], func=AF.Exp), reads=[], updates=[r_qdec])
                S.op("vector", lambda e, ix=ix, dr=dr, lgc=lgc: e.tensor_scalar(out=kdec[:, ix:ix + 1], in0=cc[:, dr:dr + 1], scalar1=lgc, scalar2=None, op0=ALU.mult),
                     reads=[r_cc, r_lg], updates=[r_kdec])
                S.op("vector", lambda e, ix=ix, lgc=lgc: e.tensor_scalar(out=cdec[:, ix:ix + 1], in0=c128[:], scalar1=lgc, scalar2=None, op0=ALU.mult),
                     reads=[r_c128, r_lg], updates=[r_cdec])
        S.op("scalar", lambda e: e.activation(out=kdec[:], in_=kdec[:], func=AF.Exp), reads=[], updates=[r_kdec])
        S.op("scalar", lambda e: e.activation(out=cdec[:], in_=cdec[:], func=AF.Exp), reads=[], updates=[r_cdec])
        S.op("vector", lambda e: e.tensor_scalar(out=kdec[:], in0=kdec[:], scalar1=SC, scalar2=None, op0=ALU.mult), reads=[], updates=[r_kdec])

        stP = contextlib.ExitStack()
        saved = C.stack
        C.stack = stP
        with stP:
            wcb, r_wcb = C.sb([128, 8, 1536], BF16, "wcb")
            load_weight_bf16(C, wcb, r_wcb, wc, 1536)
            NI = NormIn(C, xs, modv, nw, idb, r_idb)
            cosT = [C.sb([128, 128], F32, "cos%d" % i) for i in range(2)]
            sinT = [C.sb([128, 128], F32, "sin%d" % i) for i in range(2)]
            pP = [C.ps([128, 512], F32, "pP%d" % i) for i in range(3)]
            pT7, r_pT7 = C.ps([128, 7, 128], BF16, "pT7")
            pf, r_pf = C.sb([128, 896], F32, "pf")
            t1, r_t1 = C.sb([128, 896], F32, "t1")
            t2, r_t2 = C.sb([128, 896], F32, "t2")
            Rb, r_Rb = C.sb([128, 896], BF16, "Rb")
            for t in range(NT):
                h_T, r_hT = NI.tile(t)
                c_t, r_c = cosT[t % 2]
                s_t, r_s = sinT[t % 2]
                S.dma(c_t[:], ropec[t * 128:(t + 1) * 128, :], writes=[r_c], eng="gpsimd")
                S.dma(s_t[:], ropes[t * 128:(t + 1) * 128, :], writes=[r_s], eng="gpsimd")
                for nb in range(3):
                    p, r_p = pP[nb]
                    for c in range(8):
                        S.op("tensor", lambda e, c=c, nb=nb, p=p, h_T=h_T: e.matmul(out=p[:], lhsT=h_T[:, c, :], rhs=wcb[:, c, nb * 512:(nb + 1) * 512], start=(c == 0), stop=(c == 7)),
                             reads=[r_hT, r_wcb], updates=[pP[nb][1]] if c else (), writes=() if c else [pP[nb][1]])
                S.op("scalar", lambda e: e.copy(out=pf[:, 0:512], in_=pP[0][0][:]), reads=[pP[0][1]], writes=[r_pf])
                S.op("scalar", lambda e: e.copy(out=pf[:, 512:896], in_=pP[1][0][:, 0:384]), reads=[pP[1][1]], updates=[r_pf])
                S.op("scalar", lambda e, t=t: e.copy(out=tm[:, t, 256:384], in_=pP[1][0][:, 384:512]), reads=[pP[1][1]], updates=[r_tm[t]])
                S.op("scalar", lambda e, t=t: e.copy(out=tm[:, t, 384:896], in_=pP[2][0][:]), reads=[pP[2][1]], updates=[r_tm[t]])
                S.op("vector", lambda e, c_t=c_t: e.tensor_tensor(out=t1[:].rearrange("p (h d) -> p h d", h=7), in0=pf[:].rearrange("p (h d) -> p h d", h=7),
                                                                  in1=c_t[:].rearrange("p (o d) -> p o d", o=1).broadcast_to([128, 7, 128]), op=ALU.mult),
                     reads=[r_pf, r_c], writes=[r_t1])
                for a in range(2):
                    for hf in range(2):
                        o0 = a * 64 + hf * 32
                        i0 = a * 64 + (1 - hf) * 32
                        S.op("vector", lambda e, o0=o0, i0=i0, s_t=s_t: e.tensor_tensor(
                            out=t2[:].rearrange("p (h d) -> p h d", h=7)[:, :, o0:o0 + 32], in0=pf[:].rearrange("p (h d) -> p h d", h=7)[:, :, i0:i0 + 32],
                            in1=s_t[:, o0:o0 + 32].rearrange("p (o d) -> p o d", o=1).broadcast_to([128, 7, 32]), op=ALU.mult),
                             reads=[r_pf, r_s], updates=[r_t2])
                S.op("vector", lambda e: e.tensor_tensor(out=Rb[:], in0=t1[:], in1=t2[:], op=ALU.add), reads=[r_t1, r_t2], writes=[r_Rb])
                S.op("gpsimd", lambda e, t=t: e.tensor_copy(out=tm[:, t, 0:256], in_=Rb[:, 256:512]), reads=[r_Rb], updates=[r_tm[t]])
                for c in range(7):
                    S.op("tensor", lambda e, c=c: e.transpose(out=pT7[:, c, :], in_=Rb[:, c * 128:(c + 1) * 128], identity=idb[:]),
                         reads=[r_Rb, r_idb], updates=[r_pT7] if c else (), writes=() if c else [r_pT7])
                S.op("scalar", lambda e, t=t: e.copy(out=qkT[:, :, t * 128:(t + 1) * 128], in_=pT7[:]), reads=[r_pT7], writes=[r_qkT[t]])
        C.stack = saved
        S.barrier()

        stS = contextlib.ExitStack()
        C.stack = stS
        with stS:
            oacc, _ = C.sb([128, NT, 256], F32, "oacc")
            r_oacc = [Res("oacc%d" % t) for t in range(NT)]
            Sst = [C.sb([128, 128], F32, "Sst%d" % i) for i in range(4)]
            Sbf = [C.sb([128, 128], BF16, "Sbf%d" % i) for i in range(4)]
            for i in range(4):
                S.op("gpsimd", lambda e, i=i: e.memset(Sst[i][0][:], 0.0), writes=[Sst[i][1]])
                S.op("gpsimd", lambda e, i=i: e.memset(Sbf[i][0][:], 0.0), writes=[Sbf[i][1]])
            PTs = [C.sb([128, 128], BF16, "PTs%d" % i) for i in range(2)]
            qs = [C.sb([128, 128], BF16, "qs%d" % i) for i in range(2)]
            ks = [C.sb([128, 128], BF16, "ks%d" % i) for i in range(2)]
            pSc = [C.ps([128, 128], F32, "pSc")] * 2
            pO = [C.ps([128, 128], F32, "pO%d" % i) for i in range(2)]
            pU = [C.ps([128, 128], F32, "pU")] * 2
            cnt = [0]

            def ret_step(t, h, dr):
                ix = dr * 2 + h
                k = cnt[0]
                cnt[0] += 1
                tok = slice(t * 128, (t + 1) * 128)
                p_s, r_ps = pSc[k % 2]
                p_o, r_po = pO[k % 2]
                p_u, r_pu = pU[k % 2]
                PT, r_PT = PTs[k % 2]
                q_s, r_qs = qs[k % 2]
                k_s, r_ks = ks[k % 2]
                S.op("tensor", lambda e: e.matmul(out=p_s[:], lhsT=qkT[:, 2 + h, tok], rhs=qkT[:, h, tok], start=True, stop=True),
                     reads=[r_qkT[t]], writes=[r_ps])
                S.op("vector", lambda e: e.tensor_tensor(out=PT[:], in0=p_s[:], in1=dmask[:, ix, :], op=ALU.mult), reads=[r_ps, r_dmask], writes=[r_PT])
                S.op("vector", lambda e: e.tensor_tensor(out=q_s[:], in0=qkT[:, h, tok], in1=qdec[:, ix, :], op=ALU.mult), reads=[r_qkT[t], r_qdec], writes=[r_qs])
                S.op("vector", lambda e: e.tensor_scalar(out=k_s[:], in0=tm[:, t, h * 128:(h + 1) * 128], scalar1=kdec[:, ix:ix + 1], scalar2=None, op0=ALU.mult),
                     reads=[r_tm[t], r_kdec], writes=[r_ks])
                vv = tm[:, t, 256 + h * 128:256 + (h + 1) * 128]
                S.op("tensor", lambda e: e.matmul(out=p_o[:], lhsT=PT[:], rhs=vv, start=True, stop=False), reads=[r_PT, r_tm[t]], writes=[r_po])
                S.op("tensor", lambda e: e.matmul(out=p_o[:], lhsT=q_s[:], rhs=Sbf[ix][0][:], start=False, stop=True), reads=[r_qs, Sbf[ix][1]], updates=[r_po])
                S.op("tensor", lambda e: e.matmul(out=p_u[:], lhsT=k_s[:], rhs=vv, start=True, stop=True), reads=[r_ks, r_tm[t]], writes=[r_pu])
                S.op("vector", lambda e: e.scalar_tensor_tensor(out=Sst[ix][0][:], in0=Sst[ix][0][:], scalar=cdec[:, ix:ix + 1], in1=p_u[:], op0=ALU.mult, op1=ALU.add),
                     reads=[r_pu, r_cdec], writes=[Sst[ix][1]])
                S.op("scalar", lambda e: e.copy(out=Sbf[ix][0][:], in_=Sst[ix][0][:]), reads=[Sst[ix][1]], writes=[Sbf[ix][1]])
                return p_o, r_po

            for t in range(NT):
                for h in range(2):
                    p_o, r_po = ret_step(t, h, 0)
                    S.op("scalar", lambda e, t=t, h=h, p_o=p_o: e.copy(out=oacc[:, t, h * 128:(h + 1) * 128], in_=p_o[:]), reads=[r_po], updates=[r_oacc[t]])

            mgt = [C.sb([128, 512], BF16, "mgt%d" % i) for i in range(2)]
            otot, r_otot = C.sb([128, 256], F32, "otot")
            sgt, r_sgt = C.sb([128, 256], F32, "sgt")
            jk, r_jk = C.sb([128, 128], F32, "jk2")
            ssh, r_ssh = C.sb([128, 2], F32, "ssh")
            ssc, r_ssc = C.sb([128, 640], F32, "ssc")
            Pb, r_Pb = C.sb([128, 640], BF16, "Pb")
            PTa, r_PTa = C.sb([128, 5, 128], BF16, "PTa")
            mx, r_mx = C.sb([128, 1], F32, "mx")
            nmx, r_nmx = C.sb([128, 1], F32, "nmx")
            rs, r_rs = C.sb([128, 1], F32, "rs")
            esk, r_esk = C.sb([128, 1], F32, "esk")
            pA, r_pA = C.ps([128, 384], F32, "pA")
            pB, r_pB = C.ps([128, 256], F32, "pB")
            pPT, r_pPT = C.ps([128, 5, 128], BF16, "pPT")
            pOa, r_pOa = C.ps([128, 128], F32, "pOa")
            order = [1, 0] + list(range(NT - 1, 1, -1))
            for oi, t in enumerate(order):
                m_t, r_m = mgt[oi % 2]
                S.op("scalar", lambda e, t=t: e.activation(out=sgt[:], in_=tm[:, t, 512:768], func=AF.Silu), reads=[r_tm[t]], writes=[r_sgt])
                S.op("gpsimd", lambda e: e.memset(ssh[:], 0.0), writes=[r_ssh])
                for h in range(2):
                    p_o, r_po = ret_step(t, h, 1)
                    hs = slice(h * 128, (h + 1) * 128)
                    S.op("vector", lambda e, t=t, hs=hs, p_o=p_o: e.tensor_tensor(out=otot[:, hs], in0=p_o[:], in1=oacc[:, t, hs], op=ALU.add),
                         reads=[r_po, r_oacc[t]], updates=[r_otot] if h else (), writes=() if h else [r_otot])
                    S.op("scalar", lambda e, hs=hs, h=h: e.activation(out=jk[:], in_=otot[:, hs], func=AF.Square, accum_out=ssh[:, h:h + 1]),
                         reads=[r_otot], writes=[r_jk], updates=[r_ssh])
                S.op("vector", lambda e: e.tensor_scalar(out=ssh[:], in0=ssh[:], scalar1=1.0 / 128, scalar2=EPS, op0=ALU.mult, op1=ALU.add), reads=[r_ssh], writes=[r_ssh])
                S.op("scalar", lambda e: e.activation(out=ssh[:], in_=ssh[:], func=AF.Sqrt), reads=[r_ssh], writes=[r_ssh])
                S.op("vector", lambda e: e.reciprocal(out=ssh[:], in_=ssh[:]), reads=[r_ssh], writes=[r_ssh])
                for h in range(2):
                    hs = slice(h * 128, (h + 1) * 128)
                    S.op("vector", lambda e, hs=hs, h=h, m_t=m_t: e.scalar_tensor_tensor(out=m_t[:, hs], in0=otot[:, hs], scalar=ssh[:, h:h + 1], in1=sgt[:, hs], op0=ALU.mult, op1=ALU.mult),
                         reads=[r_otot, r_ssh, r_sgt], updates=[r_m] if h else (), writes=() if h else [r_m])
                if t >= 2:
                    n = t - 2
                    lo = max(n - 1, 0)
                    hi = min(n + 1, 31)
                    loc = list(range(lo + 2, hi + 3))
                else:
                    loc = []
                ktiles = loc + [0, 1]
                nl = len(loc)
                nk = len(ktiles)
                rk_reads = [r_qkT[kt] for kt in ktiles]
                rv_reads = [r_tm[kt] for kt in ktiles]
                for g in range(2):
                    qT = qkT[:, 4 + g, t * 128:(t + 1) * 128]
                    if nl:
                        S.op("tensor", lambda e, qT=qT, loc=loc, nl=nl: e.matmul(out=pA[:, 0:nl * 128], lhsT=qT, rhs=qkT[:, 6, loc[0] * 128:(loc[-1] + 1) * 128], start=True, stop=True),
                             reads=[r_qkT[t]] + rk_reads, writes=[r_pA])
                    S.op("tensor", lambda e, qT=qT: e.matmul(out=pB[:], lhsT=qT, rhs=qkT[:, 6, 0:256], start=True, stop=True), reads=[r_qkT[t]] + rk_reads, writes=[r_pB])
                    for li, kt in enumerate(loc):
                        rel = kt - t
                        dst = ssc[:, li * 128:(li + 1) * 128]
                        src = pA[:, li * 128:(li + 1) * 128]
                        if rel == 0:
                            S.op("vector", lambda e, dst=dst, src=src: e.tensor_copy(out=dst, in_=src), reads=[r_pA], updates=[r_ssc])
                        else:
                            mi = 4 if rel < 0 else 5
                            S.op("vector", lambda e, dst=dst, src=src, mi=mi: e.tensor_tensor(out=dst, in0=src, in1=cm[:, mi, :], op=ALU.add), reads=[r_pA, r_cm], updates=[r_ssc])
                    S.op("scalar", lambda e, nl=nl: e.copy(out=ssc[:, nl * 128:nl * 128 + 256], in_=pB[:]), reads=[r_pB], updates=[r_ssc])
                    W = nk * 128
                    S.op("vector", lambda e, W=W: e.reduce_max(out=mx[:], in_=ssc[:, 0:W], axis=AX.X), reads=[r_ssc], writes=[r_mx])
                    S.op("vector", lambda e, g=g: e.tensor_scalar(out=mx[:], in0=mx[:], scalar1=SC, scalar2=skt[:, g:g + 1], op0=ALU.mult, op1=ALU.max), reads=[r_mx, r_skt], writes=[r_mx])
                    S.op("vector", lambda e: e.tensor_scalar(out=nmx[:], in0=mx[:], scalar1=-1.0, scalar2=None, op0=ALU.mult), reads=[r_mx], writes=[r_nmx])
                    S.op("gpsimd", lambda e: e.memset(rs[:], 0.0), writes=[r_rs])
                    S.op("scalar", lambda e, W=W: e.activation(out=Pb[:, 0:W], in_=ssc[:, 0:W], func=AF.Exp, bias=nmx[:, 0:1], scale=SC, accum_out=rs[:]),
                         reads=[r_ssc, r_nmx], writes=[r_Pb], updates=[r_rs])
                    S.op("scalar", lambda e, g=g: e.activation(out=esk[:], in_=skt[:, g:g + 1], func=AF.Exp, bias=nmx[:, 0:1], scale=1.0), reads=[r_skt, r_nmx], writes=[r_esk])
                    S.op("vector", lambda e: e.tensor_tensor(out=rs[:], in0=rs[:], in1=esk[:], op=ALU.add), reads=[r_esk], updates=[r_rs])
                    S.op("vector", lambda e: e.reciprocal(out=rs[:], in_=rs[:]), reads=[], updates=[r_rs])
                    for ki in range(nk):
                        S.op("tensor", lambda e, ki=ki: e.transpose(out=pPT[:, ki, :], in_=Pb[:, ki * 128:(ki + 1) * 128], identity=idb[:]),
                             reads=[r_Pb, r_idb], updates=[r_pPT] if ki else (), writes=() if ki else [r_pPT])
                    S.op("scalar", lambda e, nk=nk: e.copy(out=PTa[:, 0:nk, :], in_=pPT[:, 0:nk, :]), reads=[r_pPT], writes=[r_PTa])
                    for ki, kt in enumerate(ktiles):
                        S.op("tensor", lambda e, ki=ki, kt=kt, nk=nk: e.matmul(out=pOa[:], lhsT=PTa[:, ki, :], rhs=tm[:, kt, 768:896], start=(ki == 0), stop=(ki == nk - 1)),
                             reads=[r_PTa] + rv_reads, updates=[r_pOa] if ki else (), writes=() if ki else [r_pOa])
                    S.op("vector", lambda e, g=g, m_t=m_t: e.tensor_scalar(out=m_t[:, 256 + g * 128:256 + (g + 1) * 128], in0=pOa[:], scalar1=rs[:, 0:1], scalar2=None, op0=ALU.mult),
                         reads=[r_pOa, r_rs], updates=[r_m])
                S.dma(mgo[t * 128:(t + 1) * 128, :], m_t[:], reads=[r_m])
        C.stack = saved


def even_consts():
    p = np.arange(128, dtype=np.float32)[:, None]
    f = np.arange(128, dtype=np.float32)[None, :]
    SC = np.float32(128 ** -0.5)
    cmat = np.stack([np.maximum(f - p, 0), np.maximum(p - f, 0), (f >= p) * SC, (p >= f) * SC,
                     np.where(f >= p, 0.0, NEG), np.where(f <= p, 0.0, NEG)], 1).astype(np.float32)
    crow = np.concatenate([f[0] + 1, 128 - f[0]])[None].astype(np.float32)
    ccol = np.concatenate([127 - p, p], 1).astype(np.float32)
    t = np.arange(L)
    row = (t // 64).astype(np.float32)
    col = (t % 64).astype(np.float32)
    inv = (10000.0 ** (-np.arange(0, 64, 2, dtype=np.float32) / 64)).astype(np.float32)
    ar = row[:, None] * inv[None]
    ac = col[:, None] * inv[None]
    cos = np.concatenate([np.cos(ar), np.cos(ar), np.cos(ac), np.cos(ac)], 1)
    sin = np.concatenate([-np.sin(ar), np.sin(ar), -np.sin(ac), np.sin(ac)], 1)
    ropec = np.concatenate([np.ones((NCTX, 128)), cos], 0).astype(np.float32)
    ropes = np.concatenate([np.zeros((NCTX, 128)), sin], 0).astype(np.float32)
    return dict(cmat=np.ascontiguousarray(cmat), crow=crow, ccol=np.ascontiguousarray(ccol), ropec=ropec, ropes=ropes)


def emit_odd(C, A, debug=False):
    nc, S = C.nc, C.S
    NT = 34
    NTOK = NT * 128
    xs, modv, nw, wc, lbl, lsel, hnw, cmat, mgo = (A[k] for k in "xs modv nw wc lbl lsel hnw cmat mgo".split())
    if debug:
        dbg = C.dout("dbg", [128, 8, 512])
        dbg2 = C.dout("dbg2", [128, 8, 512])
    with C.scope():
        idb, r_idb, idf, r_idf = make_identity(C)
        cm, r_cm = C.sb([128, 2, 128], F32, "cm")
        S.dma(cm[:], cmat, writes=[r_cm])
        ones, r_ones = C.sb([128, 128], F32, "ones")
        S.op("gpsimd", lambda e: e.memset(ones[:], 1.0), writes=[r_ones])
        hn, r_hn = C.sb([128, 128], F32, "hn")
        S.dma(hn[:], hnw.partition_broadcast(128), writes=[r_hn])
        lgt, r_lgt = C.sb([128, 4, 4], F32, "lgt")
        for l_ in range(4):
            S.dma(lgt[:, l_, :], lbl[l_, :].rearrange("(h d) -> d h", d=128), updates=[r_lgt], allow_slow_non_contiguous=True)
        sel, r_sel = C.sb([128, 4], F32, "sel")
        S.dma(sel[:], lsel.partition_broadcast(128), writes=[r_sel])
        mxl, r_mxl = C.sb([128, 4], F32, "mxl")
        S.op("vector", lambda e: e.tensor_tensor(out=mxl[:], in0=lgt[:, 0, :], in1=lgt[:, 1, :], op=ALU.max), reads=[r_lgt], writes=[r_mxl])
        S.op("vector", lambda e: e.tensor_tensor(out=mxl[:], in0=mxl[:], in1=lgt[:, 2, :], op=ALU.max), reads=[r_lgt], writes=[r_mxl])
        S.op("vector", lambda e: e.tensor_tensor(out=mxl[:], in0=mxl[:], in1=lgt[:, 3, :], op=ALU.max), reads=[r_lgt], writes=[r_mxl])
        for l_ in range(4):
            S.op("vector", lambda e, l_=l_: e.tensor_tensor(out=lgt[:, l_, :], in0=lgt[:, l_, :], in1=mxl[:], op=ALU.subtract), reads=[r_mxl], updates=[r_lgt])
        S.op("scalar", lambda e: e.activation(out=lgt[:], in_=lgt[:], func=AF.Exp), reads=[], updates=[r_lgt])
        den, r_den = C.sb([128, 4], F32, "den")
        lb, r_lb = C.sb([128, 4], F32, "lb")
        oml, r_oml = C.sb([128, 4], F32, "oml")
        tl, r_tl = C.sb([128, 4], F32, "tl")
        S.op("gpsimd", lambda e: e.memset(den[:], 0.0), writes=[r_den])
        S.op("gpsimd", lambda e: e.memset(lb[:], 0.0), writes=[r_lb])
        for l_ in range(4):
            S.op("vector", lambda e, l_=l_: e.tensor_tensor(out=den[:], in0=den[:], in1=lgt[:, l_, :], op=ALU.add), reads=[r_lgt], writes=[r_den])
            S.op("vector", lambda e, l_=l_: e.tensor_scalar(out=tl[:], in0=lgt[:, l_, :], scalar1=sel[:, l_:l_ + 1], scalar2=None, op0=ALU.mult), reads=[r_lgt, r_sel], writes=[r_tl])
            S.op("vector", lambda e: e.tensor_tensor(out=lb[:], in0=lb[:], in1=tl[:], op=ALU.add), reads=[r_tl], writes=[r_lb])
        S.op("vector", lambda e: e.reciprocal(out=den[:], in_=den[:]), reads=[], updates=[r_den])
        S.op("vector", lambda e: e.tensor_tensor(out=lb[:], in0=lb[:], in1=den[:], op=ALU.mult), reads=[r_den], writes=[r_lb])
        S.op("vector", lambda e: e.tensor_scalar(out=oml[:], in0=lb[:], scalar1=-1.0, scalar2=1.0, op0=ALU.mult, op1=ALU.add), reads=[r_lb], writes=[r_oml])

        wcb, r_wcb = C.sb([128, 8, 2560], BF16, "wcb")
        load_weight_bf16(C, wcb, r_wcb, wc, 2560)
        NI = NormIn(C, xs, modv, nw, idb, r_idb, nps=1)
        ofw, _ = C.sb([128, NT, 512], F32, "ofw")
        r_ofw = [Res("ofw%d" % t) for t in range(NT)]
        pQ, r_pQ = C.ps([128, 4, 128], F32, "pQ")
        pZ, r_pZ = C.ps([128, 4, 128], F32, "pZ")
        pV, r_pV = C.ps([128, 512], F32, "pV")
        pG, r_pG = pV, r_pV
        pS, r_pS = C.ps([128, 4, 128], F32, "pS")
        _pK, r_pK = C.ps([128, 8, 128], BF16, "pK")
        pK = _pK.rearrange("p (h c) j -> p h c j", c=2)
        pUu, r_pUu = C.ps([128, 4, 128], F32, "pUu")
        pOo, r_pOo = C.ps([128, 4, 128], F32, "pOo")
        NB = 2
        bufs = []
        for i_ in range(NB):
            d_ = {}
            for nm_ in ("sf", "ff", "kk", "cs", "E1", "E2", "qsb"):
                d_[nm_] = C.sb([128, 4, 128], F32, "%s%d" % (nm_, i_))
            d_["rr"] = C.sb([128, 4, 2], F32, "rr%d" % i_)
            d_["aa"] = C.sb([128, 4, 2, 3], F32, "aa%d" % i_)
            d_["qtP"] = C.sb([128, 4, 2, 128], BF16, "qtP%d" % i_)
            d_["ktP"] = C.sb([128, 4, 2, 128], BF16, "ktP%d" % i_)
            S.op("vector", lambda e, t_=d_["qtP"][0]: e.memset(t_[:].rearrange("p a b c -> p (a b c)"), 0.0), writes=[d_["qtP"][1]])
            S.op("vector", lambda e, t_=d_["ktP"][0]: e.memset(t_[:].rearrange("p a b c -> p (a b c)"), 0.0), writes=[d_["ktP"][1]])
            d_["vb"] = C.sb([128, 512], BF16, "vb%d" % i_)
            d_["sgg"] = C.sb([128, 512], F32, "sgg%d" % i_)
            d_["otot"] = C.sb([128, 512], F32, "hotot%d" % i_)
            d_["ssh"] = C.sb([128, 4], F32, "hssh%d" % i_)
            bufs.append(d_)
        PT_l = [C.sb([128, 4, 128], BF16, "PT%d" % i_) for i_ in range(1)] * 2
        kTM_l = [C.sb([128, 4, 2, 128], BF16, "kTM%d" % i_) for i_ in range(1)] * 2
        tU_l = [C.sb([128, 4, 128], F32, "tU%d" % i_) for i_ in range(1)] * 2
        Sall, r_Sall = C.sb([128, 4, 128], F32, "Sall")
        Spp = [C.sb([128, 2, 4, 128], BF16, "hSpp%d" % i) for i in range(2)]
        jk, r_jk = C.sb([128, 128], F32, "hjk")
        mgt = [C.sb([128, 512], BF16, "hmg%d" % i) for i in range(2)]
        kcnt = [0]

        tilek = [0]

        def do_tile(oi, t, dr, zc0):
            B_ = bufs[tilek[0] % NB]
            tilek[0] += 1
            sf, r_sf = B_["sf"]
            ff, r_ff = B_["ff"]
            kk, r_kk = B_["kk"]
            cs, r_cs = B_["cs"]
            uu, r_uu = B_["sf"]
            E1, r_E1 = B_["E1"]
            E2, r_E2 = B_["E2"]
            qsb, r_qsb = B_["qsb"]
            rr, r_rr = B_["rr"]
            aa, r_aa = B_["aa"]
            qtP, r_qtP = B_["qtP"]
            ktP, r_ktP = B_["ktP"]
            vb, r_vb = B_["vb"]
            sgg, r_sgg = B_["sgg"]
            otot, r_otot = B_["otot"]
            ssh, r_ssh = B_["ssh"]
            h_T, r_hT = NI.tile(t)
            yield 'S'
            for h in range(4):
                for c in range(8):
                    S.op("tensor", lambda e, c=c, h=h, h_T=h_T: e.matmul(out=pQ[:, h, :], lhsT=wcb[:, c, h * 128:(h + 1) * 128], rhs=h_T[:, c, :], start=(c == 0), stop=(c == 7)),
                         reads=[r_hT, r_wcb], updates=[r_pQ] if (c or h) else (), writes=() if (c or h) else [r_pQ])
            for h in range(4):
                for c in range(8):
                    S.op("tensor", lambda e, c=c, h=h, zc0=zc0, h_T=h_T: e.matmul(out=pZ[:, h, :], lhsT=wcb[:, c, zc0 + h * 128:zc0 + (h + 1) * 128], rhs=h_T[:, c, :], start=(c == 0), stop=(c == 7)),
                         reads=[r_hT, r_wcb], updates=[r_pZ] if (c or h) else (), writes=() if (c or h) else [r_pZ])
            for c in range(8):
                S.op("tensor", lambda e, c=c, h_T=h_T: e.matmul(out=pV[:], lhsT=h_T[:, c, :], rhs=wcb[:, c, 1536:2048], start=(c == 0), stop=(c == 7)),
                     reads=[r_hT, r_wcb], updates=[r_pV] if c else (), writes=() if c else [r_pV])
            S.op("scalar", lambda e: e.copy(out=vb[:], in_=pV[:]), reads=[r_pV], writes=[r_vb])
            S.op("scalar", lambda e: e.copy(out=qsb[:], in_=pQ[:]), reads=[r_pQ], writes=[r_qsb])
            if dr == 1:
                for c in range(8):
                    S.op("tensor", lambda e, c=c, h_T=h_T: e.matmul(out=pG[:], lhsT=h_T[:, c, :], rhs=wcb[:, c, 2048:2560], start=(c == 0), stop=(c == 7)),
                         reads=[r_hT, r_wcb], updates=[r_pG] if c else (), writes=() if c else [r_pG])
                S.op("scalar", lambda e: e.activation(out=sgg[:], in_=pG[:], func=AF.Silu), reads=[r_pG], writes=[r_sgg])
            yield 'S'
            S.op("scalar", lambda e: e.activation(out=sf[:], in_=pZ[:], func=AF.Sigmoid), reads=[r_pZ], writes=[r_sf])
            yield 'S'
            for h in range(4):
                S.op("vector", lambda e, h=h: e.tensor_scalar(out=ff[:, h, :], in0=sf[:, h, :], scalar1=oml[:, h:h + 1], scalar2=lb[:, h:h + 1], op0=ALU.mult, op1=ALU.add),
                     reads=[r_sf, r_oml, r_lb], updates=[r_ff] if h else (), writes=() if h else [r_ff])
            S.op("vector", lambda e: e.tensor_scalar(out=kk[:], in0=ff[:], scalar1=-1.0, scalar2=1.0, op0=ALU.mult, op1=ALU.add), reads=[r_ff], writes=[r_kk])
            yield 'S'
            S.op("scalar", lambda e: e.activation(out=ff[:], in_=ff[:], func=AF.Ln), reads=[], updates=[r_ff])
            yield 'S'
            for h in range(4):
                S.op("vector", lambda e, h=h: e.tensor_tensor_scan(out=cs[:, h, :], data0=ones[:], data1=ff[:, h, :], initial=0.0, op0=ALU.mult, op1=ALU.add),
                     reads=[r_ff, r_ones], updates=[r_cs] if h else (), writes=() if h else [r_cs])
            if dr == 0:
                u, r_u = cs, r_cs
            else:
                S.op("vector", lambda e: e.tensor_tensor(out=uu[:], in0=ff[:], in1=cs[:], op=ALU.subtract), reads=[r_ff, r_cs], writes=[r_uu])
                u, r_u = uu, r_uu
            S.op("vector", lambda e, u=u: e.tensor_copy(out=rr[:], in_=u[:].rearrange("p h (c s) -> p h c s", c=2)[:, :, :, 31]), reads=[r_u], writes=[r_rr])
            for c in range(2):
                for h in range(4):
                    a1 = aa[:, h, c, 0:1]
                    a2 = aa[:, h, c, 1:2]
                    if dr == 0:
                        if c == 0:
                            S.op("vector", lambda e, a1=a1, h=h, c=c: e.tensor_copy(out=a1, in_=rr[:, h, c:c + 1]), reads=[r_rr], updates=[r_aa])
                        else:
                            S.op("vector", lambda e, a1=a1, h=h, c=c: e.tensor_tensor(out=a1, in0=rr[:, h, c:c + 1], in1=cs[:, h, 63:64], op=ALU.subtract), reads=[r_rr, r_cs], updates=[r_aa])
                        S.op("vector", lambda e, a2=a2, h=h, c=c: e.tensor_tensor(out=a2, in0=cs[:, h, c * 64 + 63:c * 64 + 64], in1=rr[:, h, c:c + 1], op=ALU.subtract), reads=[r_rr, r_cs], updates=[r_aa])
                    else:
                        S.op("vector", lambda e, a1=a1, h=h, c=c: e.tensor_tensor(out=a1, in0=rr[:, h, c:c + 1], in1=cs[:, h, c * 64 + 63:c * 64 + 64], op=ALU.add), reads=[r_rr, r_cs], updates=[r_aa])
                        if c == 0:
                            S.op("vector", lambda e, a2=a2, h=h, c=c: e.tensor_scalar(out=a2, in0=rr[:, h, c:c + 1], scalar1=-1.0, scalar2=None, op0=ALU.mult), reads=[r_rr], updates=[r_aa])
                        else:
                            S.op("vector", lambda e, a2=a2, h=h, c=c: e.scalar_tensor_tensor(out=a2, in0=rr[:, h, c:c + 1], scalar=-1.0, in1=cs[:, h, 63:64], op0=ALU.mult, op1=ALU.subtract),
                                 reads=[r_rr, r_cs], updates=[r_aa])
            yield 'S'
            S.op("scalar", lambda e: e.activation(out=aa[:, :, :, 0:2], in_=aa[:, :, :, 0:2], func=AF.Exp), reads=[], updates=[r_aa])
            yield 'S'
            S.op("vector", lambda e: e.tensor_tensor(out=aa[:, :, :, 2], in0=aa[:, :, :, 0], in1=aa[:, :, :, 1], op=ALU.mult), reads=[], updates=[r_aa])
            for h in range(4):
                for c in range(2):
                    S.op("vector", lambda e, h=h, c=c, u=u: e.tensor_scalar(out=E1[:, h, c * 64:(c + 1) * 64], in0=u[:, h, c * 64:(c + 1) * 64], scalar1=rr[:, h, c:c + 1], scalar2=None, op0=ALU.subtract),
                         reads=[r_u, r_rr], updates=[r_E1] if (h or c) else (), writes=() if (h or c) else [r_E1])
            yield 'S'
            S.op("scalar", lambda e: e.activation(out=E2[:], in_=E1[:], func=AF.Exp, scale=-1.0), reads=[r_E1], writes=[r_E2])
            S.op("scalar", lambda e: e.activation(out=E1[:], in_=E1[:], func=AF.Exp), reads=[r_E2], updates=[r_E1])
            yield 'S'
            for c in range(2):
                cs_ = slice(c * 64, (c + 1) * 64)
                S.op("vector", lambda e, c=c, cs_=cs_: e.tensor_tensor(out=qtP[:, :, c, cs_], in0=qsb[:, :, cs_], in1=E1[:, :, cs_], op=ALU.mult), reads=[r_qsb, r_E1], updates=[r_qtP])
                S.op("vector", lambda e, c=c, cs_=cs_: e.tensor_tensor(out=ktP[:, :, c, cs_], in0=kk[:, :, cs_], in1=E2[:, :, cs_], op=ALU.mult), reads=[r_kk, r_E2], updates=[r_ktP])
            if debug and dr == 0 and t == 0:
                dtile, r_dt = C.sb([128, 8, 512], F32, "dtile")
                S.op("gpsimd", lambda e: e.memset(dtile[:], 0.0), writes=[r_dt])
                S.op("vector", lambda e: e.tensor_copy(out=dtile[:, 0, 0:4], in_=lb[:]), reads=[r_lb], updates=[r_dt])
                S.op("vector", lambda e: e.tensor_copy(out=dtile[:, 1, :], in_=ff[:].rearrange("p a b -> p (a b)")), reads=[r_ff], updates=[r_dt])
                S.op("vector", lambda e: e.tensor_copy(out=dtile[:, 2, :], in_=cs[:].rearrange("p a b -> p (a b)")), reads=[r_cs], updates=[r_dt])
                S.op("vector", lambda e: e.tensor_copy(out=dtile[:, 3, :], in_=E1[:].rearrange("p a b -> p (a b)")), reads=[r_E1], updates=[r_dt])
                S.op("vector", lambda e: e.tensor_copy(out=dtile[:, 4, :], in_=E2[:].rearrange("p a b -> p (a b)")), reads=[r_E2], updates=[r_dt])
                S.op("vector", lambda e: e.tensor_copy(out=dtile[:, 5, 0:24], in_=aa[:].rearrange("p a b c -> p (a b c)")), reads=[r_aa], updates=[r_dt])
                S.op("vector", lambda e: e.tensor_copy(out=dtile[:, 6, :], in_=qtP[:, 0:2, :, :].rearrange("p a b c -> p (a b c)")), reads=[r_qtP], updates=[r_dt])
                S.op("vector", lambda e: e.tensor_copy(out=dtile[:, 7, :], in_=pQ[:].rearrange("p a b -> p (a b)")), reads=[r_pQ], updates=[r_dt])
                S.dma(dbg, dtile[:], reads=[r_dt])
            yield 'SPLIT'
            corder = [0, 1] if dr == 0 else [1, 0]
            kx = kcnt[0]
            kcnt[0] += 1
            Sp, r_Sp = Spp[kx % 2]
            PT, r_PT = PT_l[kx % 2]
            kTM, r_kTM = kTM_l[kx % 2]
            tU, r_tU = tU_l[kx % 2]
            first = True
            for h in range(4):
                for c in range(2):
                    S.op("tensor", lambda e, c=c, h=h: e.matmul(out=pS[:, h, c * 64:(c + 1) * 64], lhsT=ktP[:, h, c, :], rhs=qtP[:, h, c, c * 64:(c + 1) * 64], start=True, stop=True),
                         reads=[r_ktP, r_qtP], updates=() if first else [r_pS], writes=[r_pS] if first else ())
                    first = False
            yield 'S'
            S.op("vector", lambda e, dr=dr: e.tensor_tensor(out=PT[:], in0=pS[:], in1=cm[:, dr:dr + 1, :].broadcast_to([128, 4, 128]), op=ALU.mult), reads=[r_pS, r_cm], writes=[r_PT])
            yield 'S'
            first = True
            for h in range(4):
                for c in range(2):
                    S.op("tensor", lambda e, c=c, h=h: e.transpose(out=pK[:, h, c, :], in_=ktP[:, h, c, :], identity=idb[:]), reads=[r_ktP, r_idb],
                         updates=() if first else [r_pK], writes=[r_pK] if first else ())
                    first = False
            yield 'S'
            S.op("scalar", lambda e: e.copy(out=kTM[:], in_=pK[:]), reads=[r_pK], writes=[r_kTM])
            yield 'S'
            for ci, c in enumerate(corder):
                for h in range(4):
                    S.op("tensor", lambda e, c=c, h=h: e.matmul(out=pUu[:, h, :], lhsT=kTM[:, h, c, :], rhs=vb[:, h * 128:(h + 1) * 128], start=True, stop=True),
                         reads=[r_kTM, r_vb], updates=[r_pUu] if h else (), writes=() if h else [r_pUu])
                yield 'S'
                a1b = aa[:, :, c, 0:1].broadcast_to([128, 4, 128])
                a2b = aa[:, :, c, 1:2].broadcast_to([128, 4, 128])
                a3b = aa[:, :, c, 2:3].broadcast_to([128, 4, 128])
                S.op("vector", lambda e, c=c, a1b=a1b: e.tensor_tensor(out=Sp[:, c, :, :], in0=Sall[:], in1=a1b, op=ALU.mult),
                     reads=[r_Sall, r_aa], updates=[r_Sp] if ci else (), writes=() if ci else [r_Sp])
                S.op("vector", lambda e, a2b=a2b: e.tensor_tensor(out=tU[:], in0=pUu[:], in1=a2b, op=ALU.mult), reads=[r_pUu, r_aa], writes=[r_tU])
                S.op("vector", lambda e, a3b=a3b: e.tensor_tensor(out=Sall[:], in0=Sall[:], in1=a3b, op=ALU.mult), reads=[r_aa], writes=[r_Sall])
                S.op("gpsimd", lambda e: e.tensor_tensor(out=Sall[:], in0=Sall[:], in1=tU[:], op=ALU.add), reads=[r_tU], writes=[r_Sall])
            yield 'S'
            for h in range(4):
                vv = vb[:, h * 128:(h + 1) * 128]
                S.op("tensor", lambda e, vv=vv, h=h: e.matmul(out=pOo[:, h, :], lhsT=PT[:, h, :], rhs=vv, start=True, stop=False), reads=[r_PT, r_vb],
                     updates=[r_pOo] if h else (), writes=() if h else [r_pOo])
                for c in range(2):
                    S.op("tensor", lambda e, c=c, h=h: e.matmul(out=pOo[:, h, :], lhsT=qtP[:, h, c, :], rhs=Sp[:, c, h, :], start=False, stop=(c == 1)), reads=[r_qtP, r_Sp], updates=[r_pOo])
            yield 'S'
            pOf = pOo[:].rearrange("p h e -> p (h e)")
            if dr == 0:
                S.op("scalar", lambda e, t=t: e.copy(out=ofw[:, t, :], in_=pOf), reads=[r_pOo], writes=[r_ofw[t]])
            else:
                S.op("vector", lambda e, t=t: e.tensor_tensor(out=otot[:], in0=pOf, in1=ofw[:, t, :], op=ALU.add), reads=[r_pOo, r_ofw[t]], writes=[r_otot])
            if debug and dr == 0 and t == 0:
                dt2, r_dt2 = C.sb([128, 8, 512], F32, "dtile2")
                S.op("gpsimd", lambda e: e.memset(dt2[:], 0.0), writes=[r_dt2])
                S.op("vector", lambda e: e.tensor_copy(out=dt2[:, 0, 0:128], in_=PT[:]), reads=[r_PT], updates=[r_dt2])
                S.op("vector", lambda e: e.tensor_copy(out=dt2[:, 1, 0:128], in_=pOo[:]), reads=[r_pOo], updates=[r_dt2])
                S.op("vector", lambda e: e.tensor_copy(out=dt2[:, 2, :], in_=ofw[:, 0, :]), reads=[r_ofw[0]], updates=[r_dt2])
                S.op("vector", lambda e: e.tensor_copy(out=dt2[:, 3, :], in_=vb[:]), reads=[r_vb], updates=[r_dt2])
                S.op("vector", lambda e: e.tensor_copy(out=dt2[:, 4, 0:128], in_=pS[:]), reads=[r_pS], updates=[r_dt2])
                S.op("vector", lambda e: e.tensor_copy(out=dt2[:, 5, 0:256], in_=kTM[:].rearrange("p a b -> p (a b)")), reads=[r_kTM], updates=[r_dt2])
                S.op("vector", lambda e: e.tensor_copy(out=dt2[:, 6, 0:128], in_=Sst[3][0][:]), reads=[Sst[3][1]], updates=[r_dt2])
                S.dma(dbg2, dt2[:], reads=[r_dt2])
            if dr == 1:
                m_t, r_m = mgt[oi % 2]
                S.op("gpsimd", lambda e: e.memset(ssh[:], 0.0), writes=[r_ssh])
                for h in range(4):
                    hs = slice(h * 128, (h + 1) * 128)
                    S.op("scalar", lambda e, hs=hs, h=h: e.activation(out=jk[:], in_=otot[:, hs], func=AF.Square, accum_out=ssh[:, h:h + 1]), reads=[r_otot], writes=[r_jk], updates=[r_ssh])
                S.op("vector", lambda e: e.tensor_scalar(out=ssh[:], in0=ssh[:], scalar1=1.0 / 128, scalar2=EPS, op0=ALU.mult, op1=ALU.add), reads=[r_ssh], writes=[r_ssh])
                S.op("scalar", lambda e: e.activation(out=ssh[:], in_=ssh[:], func=AF.Sqrt), reads=[r_ssh], writes=[r_ssh])
                S.op("vector", lambda e: e.reciprocal(out=ssh[:], in_=ssh[:]), reads=[r_ssh], writes=[r_ssh])
                for h in range(4):
                    hs = slice(h * 128, (h + 1) * 128)
                    S.op("vector", lambda e, hs=hs, h=h: e.scalar_tensor_tensor(out=otot[:, hs], in0=otot[:, hs], scalar=ssh[:, h:h + 1], in1=hn[:], op0=ALU.mult, op1=ALU.mult),
                         reads=[r_ssh, r_hn], updates=[r_otot])
                S.op("vector", lambda e, m_t=m_t: e.tensor_tensor(out=m_t[:], in0=otot[:], in1=sgg[:], op=ALU.mult), reads=[r_otot, r_sgg], writes=[r_m])
                S.dma(mgo[t * 128:(t + 1) * 128, :], m_t[:], reads=[r_m])

        for dr in range(2):
            order = list(range(NT)) if dr == 0 else [1, 0] + list(range(NT - 1, 1, -1))
            zc0 = 512 + dr * 512
            S.op("vector", lambda e: e.memset(Sall[:].rearrange("p a b -> p (a b)"), 0.0), writes=[r_Sall])
            cur = None
            for oi, t in enumerate(order):
                nxt = do_tile(oi, t, dr, zc0)
                a_done = False
                b_done = cur is None
                while not (a_done and b_done):
                    if not a_done:
                        a_done = next(nxt) == 'SPLIT'
                    if not b_done:
                        b_done = next(cur, 'END') == 'END'
                cur = nxt
            while next(cur, 'END') != 'END':
                pass


def odd_consts():
    p = np.arange(128)[:, None]
    f = np.arange(128)[None, :]
    same = (p // 64) == (f // 64)
    cmat = np.stack([(same & (p <= f)), (same & (p >= f))], 1).astype(np.float32)
    return dict(cmat=np.ascontiguousarray(cmat))


def emit_mod(C, cv, aws, abs_, modd):
    nc, S = C.nc, C.S
    with C.scope():
        ct, r_ct = C.sb([128, 8, 2], F32, "ct")
        for b_ in range(2):
            S.dma(ct[:, :, b_], cv[b_, :].rearrange("(c p) -> p c", p=128), updates=[r_ct], allow_slow_non_contiguous=True)
        sc, r_sc = C.sb([128, 8, 2], F32, "sc")
        S.op("scalar", lambda e: e.activation(out=sc[:], in_=ct[:], func=AF.Silu), reads=[r_ct], writes=[r_sc])
        wts = [C.sb([128, 8, 1536], F32, "aw%d" % i) for i in range(2)]
        bt, r_bt = C.sb([2, 6144], F32, "bt")
        ots = [C.sb([2, 6144], F32, "ot%d" % i) for i in range(2)]
        pss = [C.ps([2, 512], F32, "pm%d" % i) for i in range(2)]
        k = 0
        for l in range(DEPTH):
            ot, r_ot = ots[l % 2]
            S.dma(bt[:], abs_[l].partition_broadcast(2), writes=[r_bt])
            for cb in range(4):
                w, r_w = wts[k % 2]
                for c in range(8):
                    S.dma(w[:, c, :], aws[l][c * 128:(c + 1) * 128, cb * 1536:(cb + 1) * 1536], updates=[r_w] if c else (), writes=() if c else [r_w],
                          eng=("sync" if c % 2 == 0 else "gpsimd"))
                for j in range(3):
                    p, r_p = pss[(k * 3 + j) % 2]
                    for c in range(8):
                        S.op("tensor", lambda e, c=c, j=j, p=p, w=w: e.matmul(out=p[:], lhsT=sc[:, c, :], rhs=w[:, c, j * 512:(j + 1) * 512], start=(c == 0), stop=(c == 7)),
                             reads=[r_sc, r_w], updates=[r_p] if c else (), writes=() if c else [r_p])
                    col = cb * 1536 + j * 512
                    S.op("vector", lambda e, p=p, col=col, ot=ot: e.tensor_tensor(out=ot[:, col:col + 512], in0=p[:], in1=bt[:, col:col + 512], op=ALU.add),
                         reads=[r_p, r_bt], updates=[r_ot])
                k += 1
            S.dma(modd[l], ot[:], reads=[r_ot])


def emit_final(C, xsrc, fnw, out):
    nc, S = C.nc, C.S
    with C.scope():
        fw_t, r_fw = C.sb([128, D], F32, "fnw_sb")
        S.dma(fw_t[:], fnw.partition_broadcast(128), writes=[r_fw])
        ssf, r_ssf = C.sb([128, 1], F32, "ssf")
        jk, r_jk = C.sb([128, D], F32, "jk")
        xt = [C.sb([128, D], F32, "fx%d" % i) for i in range(2)]
        ob = [C.sb([128, D], F32, "ob%d" % i) for i in range(2)]
        for t in range(32):
            x_t, r_x = xt[t % 2]
            o_t, r_o = ob[t % 2]
            S.dma(x_t[:], xsrc[256 + t * 128:256 + (t + 1) * 128, :], reads=[C.r_xf[(t + 2) // 8]], writes=[r_x])
            S.op("gpsimd", lambda e: e.memset(ssf[:], 0.0), writes=[r_ssf])
            S.op("scalar", lambda e, x_t=x_t: e.activation(out=jk[:], in_=x_t[:], func=AF.Square, accum_out=ssf[:]), reads=[r_x], writes=[r_jk], updates=[r_ssf])
            S.op("vector", lambda e: e.tensor_scalar(out=ssf[:], in0=ssf[:], scalar1=1.0 / D, scalar2=EPS, op0=ALU.mult, op1=ALU.add), reads=[r_ssf], writes=[r_ssf])
            S.op("scalar", lambda e: e.activation(out=ssf[:], in_=ssf[:], func=AF.Sqrt), reads=[r_ssf], writes=[r_ssf])
            S.op("vector", lambda e: e.reciprocal(out=ssf[:], in_=ssf[:]), reads=[r_ssf], writes=[r_ssf])
            S.op("vector", lambda e, x_t=x_t, o_t=o_t: e.scalar_tensor_tensor(out=o_t[:], in0=x_t[:], scalar=ssf[:, 0:1], in1=fw_t[:], op0=ALU.mult, op1=ALU.mult),
                 reads=[r_x, r_ssf, r_fw], writes=[r_o])
            S.dma(out[t * 128:(t + 1) * 128, :], o_t[:], reads=[r_o], eng="gpsimd")


GROUPS = [[0, 1], [2, 3], [4, 5], [6, 7]]


def build_fused(n_layers=DEPTH, dbg_out=False):
    C = Ctx()
    nc, S = C.nc, C.S
    NTOK = 34 * 128
    xs_in = C.din("xs", [NTOK, D])
    cv = C.din("cv", [2, D])
    aws = [C.din("aw%d" % l, [D, 6144]) for l in range(DEPTH)]
    abs_ = [C.din("ab%d" % l, [1, 6144]) for l in range(DEPTH)]
    nwa = C.din("nwa", [2 * DEPTH, D])
    fnw = C.din("fnw", [1, D])
    wce = [C.din("wce%d" % p, [D, 1536]) for p in range(2)]
    draw = [C.din("draw%d" % p, [1, 4]) for p in range(2)]
    sink = [C.din("sink%d" % p, [1, 2]) for p in range(2)]
    wco = [C.din("wco%d" % p, [D, 2560]) for p in range(2)]
    lbl = C.din("lbl", [4, 512])
    lsel = [C.din("lsel%d" % p, [1, 4]) for p in range(2)]
    hnw = [C.din("hnw%d" % p, [1, 128]) for p in range(2)]
    wo = [C.din("wo%d" % l, [D, D]) for l in range(DEPTH)]
    ropec = C.din("ropec", [NTOK, 128])
    ropes = C.din("ropes", [NTOK, 128])
    cmat_e = C.din("cmat_e", [128, 6, 128])
    crow = C.din("crow", [1, 256])
    ccol = C.din("ccol", [128, 2])
    cmat_o = C.din("cmat_o", [128, 2, 128])
    wr = [C.din("wr%d" % l, [D, 36]) for l in range(DEPTH)]
    br = [C.din("br%d" % l, [1, 36]) for l in range(DEPTH)]
    w1 = [C.din("w1_%d" % l, [16, D, 512]) for l in range(DEPTH)]
    w3 = [C.din("w3_%d" % l, [16, D, 512]) for l in range(DEPTH)]
    w2 = [C.din("w2_%d" % l, [16, 512, D]) for l in range(DEPTH)]
    out = C.dout("out", [L, D])
    if dbg_out:
        xdbg = C.dout("xdbg", [NTOK, D])
    xfull = C.dram("xfull", [NTOK, D])
    ypart = C.dram("ypart", [NTOK, D])
    mgl = C.dram("mgl", [NTOK, 512], BF16)
    mgall = C.dram("mgall", [2 * NTOK, 512], BF16)
    moddt = C.dram("modd", [DEPTH, 2, 6144])
    modd = [moddt[l] for l in range(DEPTH)]
    with C.stack:
        C.prefix = "cp_"
        for t in range(34):
            S.dma(xfull[t * 128:(t + 1) * 128, :], xs_in[t * 128:(t + 1) * 128, :], updates=[C.r_xf[t // 8]], eng=("sync" if t % 2 == 0 else "gpsimd"))
        C.prefix = "mod_"
        emit_mod(C, cv, aws, abs_, modd)
        for l in range(n_layers):
            p = l // 2
            modv = modd[l].rearrange("s (k d) -> s k d", k=6)
            C.prefix = "L%dmix_" % l
            if l % 2 == 0:
                emit_even(C, dict(xs=xfull, modv=modv, nw=nwa[2 * l:2 * l + 1, :], wc=wce[p], ropec=ropec, ropes=ropes, draw=draw[p], sink=sink[p],
                                  cmat=cmat_e, crow=crow, ccol=ccol, mgo=mgl))
            else:
                emit_odd(C, dict(xs=xfull, modv=modv, nw=nwa[2 * l:2 * l + 1, :], wc=wco[p], lbl=lbl, lsel=lsel[p], hnw=hnw[p], cmat=cmat_o, mgo=mgl))
            ag = []
            for st_k in range(0, NTOK, 2048):
                R_k = min(2048, NTOK - st_k)
                ag.append(lambda g, st_k=st_k, R_k=R_k: g.collective_compute("AllGather", ALU.bypass, replica_groups=GROUPS, ins=[mgl[st_k:st_k + R_k, :].opt()],
                                                                              outs=[mgall[2 * st_k:2 * st_k + 2 * R_k, :].opt()]))
            S.collective(ag)
            def ar_chunk(k_):
                st_k = k_ * 1024
                R_k = min(1024, NTOK - st_k)
                S.cc_async(lambda g, st_k=st_k, R_k=R_k: g.collective_compute("AllReduce", ALU.add, replica_groups=GROUPS, ins=[ypart[st_k:st_k + R_k, :].opt()],
                                                                               outs=[xfull[st_k:st_k + R_k, :].opt()]),
                           reads=[C.r_yp[k_]], writes=[C.r_xf[k_]])
            for half in range(2):
                C.prefix = "L%dmoe%d_" % (l, half)
                emit_moe(C, dict(xs=xfull, mgall=mgall, wo=wo[l], modv=modv, nw=nwa[2 * l + 1:2 * l + 2, :], wr=wr[l], br=br[l], w1=w1[l], w3=w3[l], w2=w2[l], ypart=ypart),
                         tiles=list(range(half * 17, (half + 1) * 17)))
                for k_ in ([0, 1] if half == 0 else [2, 3, 4]):
                    ar_chunk(k_)
        C.prefix = "fin_"
        emit_final(C, xfull, fnw, out)
        if dbg_out:
            for t in range(34):
                S.dma(xdbg[t * 128:(t + 1) * 128, :], xfull[t * 128:(t + 1) * 128, :], reads=[C.r_xf[t // 8]])
        S.emit()
    return nc


_NC = {}


def make_in_maps(x, c, ctx, c_ctx, ada_w, ada_b, norm_w, final_norm_w, ev_w_in, ev_w_out, ret_decay_raw, att_sink,
                 od_w_in, od_w_out, hg_lb_logits, hg_norm_w, moe_wg, moe_bg, moe_we, moe_be, moe_w1, moe_w3, moe_w2):
    f32 = np.float32
    A_ = lambda v: np.ascontiguousarray(np.asarray(v, dtype=f32))
    g = {k: np.asarray(v, dtype=f32) for k, v in dict(x=x, c=c, ctx=ctx, c_ctx=c_ctx, ada_w=ada_w, ada_b=ada_b, norm_w=norm_w, final_norm_w=final_norm_w,
                                                         ev_w_in=ev_w_in, ev_w_out=ev_w_out, ret_decay_raw=ret_decay_raw, att_sink=att_sink, od_w_in=od_w_in,
                                                         od_w_out=od_w_out, hg_lb_logits=hg_lb_logits, hg_norm_w=hg_norm_w, moe_wg=moe_wg, moe_bg=moe_bg,
                                                         moe_we=moe_we, moe_be=moe_be, moe_w1=moe_w1, moe_w3=moe_w3, moe_w2=moe_w2).items()}
    ec = even_consts()
    oc = odd_consts()
    shared = dict(nwa=A_(g['norm_w'].reshape(2 * DEPTH, D)), fnw=A_(g['final_norm_w'][None]), ropec=ec['ropec'], ropes=ec['ropes'], cmat_e=ec['cmat'],
                  crow=ec['crow'], ccol=ec['ccol'], cmat_o=oc['cmat'])
    for l in range(DEPTH):
        shared["aw%d" % l] = A_(g['ada_w'][l])
        shared["ab%d" % l] = A_(g['ada_b'][l][None])
    per_s = []
    for s in range(2):
        d = {}
        for p in range(2):
            win = g['ev_w_in'][p]
            d["wce%d" % p] = A_(np.concatenate([win[:, s * 256:(s + 1) * 256], win[:, 512 + s * 256:512 + (s + 1) * 256],
                                                win[:, 2048 + s * 256:2048 + (s + 1) * 256], win[:, 2560 + s * 128:2560 + (s + 1) * 128],
                                                win[:, 1024 + s * 256:1024 + (s + 1) * 256], win[:, 1536 + s * 256:1536 + (s + 1) * 256],
                                                win[:, 2816 + s * 128:2816 + (s + 1) * 128]], 1))
            d["draw%d" % p] = A_(g['ret_decay_raw'][p][:, s * 2:(s + 1) * 2].reshape(1, 4))
            d["sink%d" % p] = A_(g['att_sink'][p][s * 2:(s + 1) * 2][None])
            wino = g['od_w_in'][p]
            d["wco%d" % p] = A_(np.concatenate([wino[:, k * 1024 + s * 512:k * 1024 + (s + 1) * 512] for k in range(5)], 1))
            lo = 2 * p + 1
            d["lsel%d" % p] = np.array([[0.0] + [1.0 if m <= lo else 0.0 for m in range(1, 4)]], f32)
            d["hnw%d" % p] = A_(g['hg_norm_w'][p][None])
        d["lbl"] = A_(g['hg_lb_logits'][:, s * 512:(s + 1) * 512])
        go = [2 * s, 2 * s + 1, 2 * (1 - s), 2 * (1 - s) + 1]
        for l in range(DEPTH):
            d["wr%d" % l] = A_(np.concatenate([g['moe_wg'][l][:, go]] + [g['moe_we'][l][gi] for gi in go], 1))
            d["br%d" % l] = A_(np.concatenate([g['moe_bg'][l][go]] + [g['moe_be'][l][gi] for gi in go])[None])
            d["w1_%d" % l] = A_(g['moe_w1'][l][s * 16:(s + 1) * 16])
            d["w3_%d" % l] = A_(g['moe_w3'][l][s * 16:(s + 1) * 16])
            d["w2_%d" % l] = A_(g['moe_w2'][l][s * 16:(s + 1) * 16])
        per_s.append(d)
    for l in range(DEPTH):
        if l % 2 == 0:
            w = g['ev_w_out'][l // 2]
            shared["wo%d" % l] = A_(np.concatenate([w[0:256], w[512:768], w[256:512], w[768:1024]], 0))
        else:
            shared["wo%d" % l] = A_(g['od_w_out'][l // 2])
    maps = []
    for i in range(8):
        b, s = i // 2, i % 2
        d = dict(shared)
        d.update(per_s[s])
        d["xs"] = A_(np.concatenate([g['ctx'][b], g['x'][b]], 0))
        d["cv"] = A_(np.stack([g['c_ctx'], g['c'][b]]))
        maps.append(d)
    return maps


def kernel(x, c, ctx, c_ctx, ada_w, ada_b, norm_w, final_norm_w, ev_w_in, ev_w_out, ret_decay_raw, att_sink,
           od_w_in, od_w_out, hg_lb_logits, hg_norm_w, moe_wg, moe_bg, moe_we, moe_be, moe_w1, moe_w3, moe_w2):
    if "fused" not in _NC:
        _NC["fused"] = build_fused()
    maps = make_in_maps(x, c, ctx, c_ctx, ada_w, ada_b, norm_w, final_norm_w, ev_w_in, ev_w_out, ret_decay_raw, att_sink,
                        od_w_in, od_w_out, hg_lb_logits, hg_norm_w, moe_wg, moe_bg, moe_we, moe_be, moe_w1, moe_w3, moe_w2)
    res = run_bass_kernel_spmd(_NC["fused"], maps, core_ids=list(range(8)))
    return np.stack([res.results[2 * b]["out"] for b in range(B)], 0).astype(np.float32)
```
